# Optimizing a Trainium2 kernel written in Bass

```python
import math
import jax
import jax.numpy as jnp
from jax import lax
import numpy as np

D_MODEL = 1024
BATCH = 8
SEQ = 8192
DEPTH = 2

CTX_LEN = 256
GRID_W = 64
N_HEADS = 8
N_KV_HEADS = 2
HEAD_DIM = 64
Q_PER_KV = N_HEADS // N_KV_HEADS
W_ATTN = N_HEADS * HEAD_DIM
W_KV = N_KV_HEADS * HEAD_DIM
WINDOW = 128
BLOCK = 128
ROPE_BASE = 10000.0
W_S5 = 256
S5_GROUP = 16
S5_GROUPS = W_S5 // S5_GROUP
S5_STATE = 64
DT_MIN = 0.001
DT_MAX = 0.1
ML_HEADS = 4
ML_HEAD_DIM = 64
W_ML = ML_HEADS * ML_HEAD_DIM
ML_CHUNK = 64
N_BRANCH = 3
D_FF = 2816
CONV_W = 3
NORM_EPS = 1e-6
IN_SPLITS = (W_ATTN, W_KV, W_KV, W_S5, W_ML, W_ML, W_ML, W_ML, 4 * ML_HEADS, N_BRANCH * D_MODEL)
D_IN = W_ATTN + 2 * W_KV + W_S5 + 4 * W_ML + 4 * ML_HEADS + N_BRANCH * D_MODEL

kernel_name = 'hybrid_prefix_swa_s5_mlstm_block'


def _rmsnorm(x, g):
    x32 = x.astype(jnp.float32)
    y = x32 * lax.rsqrt(jnp.mean(x32 * x32, axis=-1, keepdims=True) + NORM_EPS)
    return (y * g.astype(jnp.float32)).astype(x.dtype)


def _modulate(h, shift, scale):
    return h * (1 + scale) + shift


def _split_in(z):
    offsets, run = [], 0
    for size in IN_SPLITS[:-1]:
        run += size
        offsets.append(run)
    return jnp.split(z, offsets, axis=-1)


def _axial_angles(n_tok):
    rows = n_tok // GRID_W
    row = jnp.broadcast_to(jnp.arange(rows, dtype=jnp.float32)[:, None], (rows, GRID_W)).reshape(-1)
    col = jnp.broadcast_to(jnp.arange(GRID_W, dtype=jnp.float32)[None, :], (rows, GRID_W)).reshape(-1)
    n_freq = HEAD_DIM // 4
    inv_freq = ROPE_BASE ** (-jnp.arange(n_freq, dtype=jnp.float32) / n_freq)
    return row[:, None] * inv_freq[None, :], col[:, None] * inv_freq[None, :]


def _rotate(u, ang):
    half = u.shape[-1] // 2
    cos = jnp.cos(ang)[None, :, None, :].astype(u.dtype)
    sin = jnp.sin(ang)[None, :, None, :].astype(u.dtype)
    u1, u2 = u[..., :half], u[..., half:]
    return jnp.concatenate([u1 * cos - u2 * sin, u2 * cos + u1 * sin], axis=-1)


def _axial_rope(t, ang_row, ang_col):
    half = HEAD_DIM // 2
    return jnp.concatenate([_rotate(t[..., :half], ang_row), _rotate(t[..., half:], ang_col)], axis=-1)


def _attn_latent(q, k, v, kc, vc, sink):
    b, n_tok = q.shape[0], q.shape[1]
    nb = n_tok // BLOCK
    scale = HEAD_DIM ** -0.5
    pad = ((0, 0), (BLOCK, BLOCK), (0, 0), (0, 0))
    kp, vp = jnp.pad(k, pad), jnp.pad(v, pad)
    qb = jnp.moveaxis(q.reshape(b, nb, BLOCK, N_KV_HEADS, Q_PER_KV, HEAD_DIM), 1, 0)
    sink_col = jnp.broadcast_to(sink[None, :, :, None, None], (b, N_KV_HEADS, Q_PER_KV, BLOCK, 1))
    offs_q = jnp.arange(BLOCK)
    offs_k = jnp.arange(3 * BLOCK) - BLOCK

    def one_block(args):
        qj, j = args
        start = j * BLOCK
        kj = lax.dynamic_slice_in_dim(kp, start, 3 * BLOCK, axis=1)
        vj = lax.dynamic_slice_in_dim(vp, start, 3 * BLOCK, axis=1)
        qpos = start + offs_q
        kpos = start + offs_k
        valid = (jnp.abs(qpos[:, None] - kpos[None, :]) <= WINDOW) & (kpos >= 0)[None, :] & (kpos < n_tok)[None, :]
        s_loc = jnp.einsum('bqhgd,bkhd->bhgqk', qj, kj).astype(jnp.float32) * scale
        s_loc = jnp.where(valid, s_loc, -jnp.inf)
        s_ctx = jnp.einsum('bqhgd,bchd->bhgqc', qj, kc).astype(jnp.float32) * scale
        p = jax.nn.softmax(jnp.concatenate([s_loc, s_ctx, sink_col], axis=-1), axis=-1).astype(v.dtype)
        return (jnp.einsum('bhgqk,bkhd->bqhgd', p[..., :3 * BLOCK], vj)
                + jnp.einsum('bhgqc,bchd->bqhgd', p[..., 3 * BLOCK:-1], vc))

    out = lax.map(one_block, (qb, jnp.arange(nb)))
    return jnp.moveaxis(out, 0, 1).reshape(b, n_tok, W_ATTN)


def _attn_context(qc, kc, vc, sink):
    b, n_ctx = qc.shape[0], qc.shape[1]
    s = jnp.einsum('bqhgd,bchd->bhgqc', qc, kc).astype(jnp.float32) * HEAD_DIM ** -0.5
    sink_col = jnp.broadcast_to(sink[None, :, :, None, None], (b, N_KV_HEADS, Q_PER_KV, n_ctx, 1))
    p = jax.nn.softmax(jnp.concatenate([s, sink_col], axis=-1), axis=-1).astype(vc.dtype)
    return jnp.einsum('bhgqc,bchd->bqhgd', p[..., :-1], vc).reshape(b, n_ctx, W_ATTN)


def _cmul(ar, ai, br, bi):
    return ar * br - ai * bi, ar * bi + ai * br


def _s5_scan(bu_re, bu_im, lam_re, lam_im, dre, dim, init=None):
    n_t = bu_re.shape[1]
    a_re = jnp.broadcast_to(lam_re, (1, n_t) + lam_re.shape)
    a_im = jnp.broadcast_to(lam_im, (1, n_t) + lam_im.shape)

    def combine(e1, e2):
        a1r, a1i, b1r, b1i = e1
        a2r, a2i, b2r, b2i = e2
        ar, ai = _cmul(a2r, a2i, a1r, a1i)
        br, bi = _cmul(a2r, a2i, b1r, b1i)
        return ar, ai, br + b2r, bi + b2i

    _, _, s_re, s_im = lax.associative_scan(combine, (a_re, a_im, bu_re, bu_im), axis=1)
    if init is not None:
        steps = jnp.arange(1, n_t + 1, dtype=jnp.float32)[:, None, None]
        pmag = jnp.exp(steps * dre)
        p_re, p_im = pmag * jnp.cos(steps * dim), pmag * jnp.sin(steps * dim)
        i_re, i_im = _cmul(p_re[None], p_im[None], init[0][:, None], init[1][:, None])
        s_re, s_im = s_re + i_re, s_im + i_im
    return s_re, s_im


def _s5_direction(ux, uc, a_re, a_im, log_dt, b_re, b_im, reverse):
    dt = jnp.exp(log_dt)[:, None]
    dre, dim = dt * a_re, dt * a_im
    mag = jnp.exp(dre)
    lam_re, lam_im = mag * jnp.cos(dim), mag * jnp.sin(dim)
    den = a_re * a_re + a_im * a_im
    coef_re = ((lam_re - 1.0) * a_re + lam_im * a_im) / den
    coef_im = (lam_im * a_re - (lam_re - 1.0) * a_im) / den
    bb_re, bb_im = _cmul(coef_re[..., None], coef_im[..., None], b_re, b_im)

    def drive(u):
        br = jnp.einsum('btgc,gpc->btgp', u, bb_re)
        bi = jnp.einsum('btgc,gpc->btgp', u, bb_im)
        return (jnp.flip(br, 1), jnp.flip(bi, 1)) if reverse else (br, bi)

    sc_re, sc_im = _s5_scan(*drive(uc), lam_re, lam_im, dre, dim)
    sx_re, sx_im = _s5_scan(*drive(ux), lam_re, lam_im, dre, dim, init=(sc_re[:, -1], sc_im[:, -1]))
    if reverse:
        sx_re, sx_im, sc_re, sc_im = (jnp.flip(a, 1) for a in (sx_re, sx_im, sc_re, sc_im))
    return sx_re, sx_im, sc_re, sc_im


def _s5_branch(ux, uc, a_re, a_im, log_dt, b_re, b_im, c_re, c_im, d_skip, glu_w, glu_b, with_ctx):
    f32 = jnp.float32

    def grouped(u):
        b, t, _ = u.shape
        return u.astype(f32).reshape(b, t, S5_GROUPS, S5_GROUP)

    gx, gc = grouped(ux), grouped(uc)
    a_re, a_im, log_dt, b_re, b_im, c_re, c_im = (p.astype(f32) for p in (a_re, a_im, log_dt, b_re, b_im, c_re, c_im))
    fwd = _s5_direction(gx, gc, a_re[0], a_im[0], log_dt[0], b_re, b_im, False)
    bwd = _s5_direction(gx, gc, a_re[1], a_im[1], log_dt[1], b_re, b_im, True)
    dsk = d_skip.astype(f32).reshape(S5_GROUPS, S5_GROUP)
    w_glu, b_glu = glu_w.astype(f32), glu_b.astype(f32)

    def readout(s_re, s_im, u, like):
        b, t = u.shape[0], u.shape[1]
        y = (jnp.einsum('btgp,gcp->btgc', s_re, c_re) - jnp.einsum('btgp,gcp->btgc', s_im, c_im)
             + dsk * u)
        y = jax.nn.gelu(y.reshape(b, t, W_S5))
        return (y * jax.nn.sigmoid(y @ w_glu + b_glu)).astype(like.dtype)

    y_lat = readout(fwd[0] + bwd[0], fwd[1] + bwd[1], gx, ux)
    y_ctx = readout(fwd[2] + bwd[2], fwd[3] + bwd[3], gc, uc) if with_ctx else None
    return y_lat, y_ctx


def _mlstm_scan(q, k, v, ig, lf, state):
    b, h, t, _ = q.shape
    nc = t // ML_CHUNK

    def chunks(a):
        return jnp.moveaxis(a.reshape((b, h, nc, ML_CHUNK) + a.shape[3:]), 2, 0)

    tril = jnp.tril(jnp.ones((ML_CHUNK, ML_CHUNK), dtype=bool))

    def step(carry, inp):
        c_mat, n_vec, m = carry
        qc, kc, vc, ic, fc = inp
        bcum = jnp.cumsum(fc, axis=-1)
        a_inter = bcum + m[..., None]
        d_intra = jnp.where(tril, bcum[..., :, None] - bcum[..., None, :] + ic[..., None, :], -jnp.inf)
        m_t = jnp.maximum(a_inter, jnp.max(d_intra, axis=-1))
        w_inter = jnp.exp(a_inter - m_t)
        w_intra = jnp.exp(d_intra - m_t[..., None]) * jnp.einsum('bhtd,bhsd->bhts', qc, kc)
        num = (w_inter[..., None] * jnp.einsum('bhed,bhtd->bhte', c_mat, qc)
               + jnp.einsum('bhts,bhse->bhte', w_intra, vc))
        den = w_inter * jnp.einsum('bhd,bhtd->bht', n_vec, qc) + jnp.sum(w_intra, axis=-1)
        h_out = num / jnp.maximum(jnp.abs(den), jnp.exp(-m_t))[..., None]
        b_end = bcum[..., -1]
        g = b_end[..., None] - bcum + ic
        m_new = jnp.maximum(b_end + m, jnp.max(g, axis=-1))
        decay = jnp.exp(b_end + m - m_new)
        w_s = jnp.exp(g - m_new[..., None])
        c_new = decay[..., None, None] * c_mat + jnp.einsum('bhs,bhse,bhsd->bhed', w_s, vc, kc)
        n_new = decay[..., None] * n_vec + jnp.einsum('bhs,bhsd->bhd', w_s, kc)
        return (c_new, n_new, m_new), h_out

    final, hs = lax.scan(step, state, (chunks(q), chunks(k), chunks(v), chunks(ig), chunks(lf)))
    return jnp.moveaxis(hs, 0, 2).reshape(b, h, t, v.shape[-1]), final


def _mlstm_branch(qx, kx, vx, ox, gx, qc, kc, vc, oc, gc, ig_b, fg_b, norm_g, with_ctx):
    f32 = jnp.float32
    k_scale = ML_HEAD_DIM ** -0.5

    def heads(a):
        b, t, _ = a.shape
        return a.astype(f32).reshape(b, t, ML_HEADS, ML_HEAD_DIM).transpose(0, 2, 1, 3)

    def gate_preacts(g):
        b, t, _ = g.shape
        return g.astype(f32).reshape(b, t, 4, ML_HEADS).transpose(2, 0, 3, 1)

    lat = (heads(qx), heads(kx) * k_scale, heads(vx))
    ctxs = (heads(qc), heads(kc) * k_scale, heads(vc))
    g_lat, g_ctx = gate_preacts(gx), gate_preacts(gc)
    ig_b, fg_b = ig_b.astype(f32), fg_b.astype(f32)
    b = qx.shape[0]
    zero = (jnp.zeros((b, ML_HEADS, ML_HEAD_DIM, ML_HEAD_DIM), f32),
            jnp.zeros((b, ML_HEADS, ML_HEAD_DIM), f32),
            jnp.zeros((b, ML_HEADS), f32))
    h_lat, h_ctx = [], []
    for d in range(2):
        def prep(qkv, g):
            ig = g[2 * d] + ig_b[d][:, None]
            lf = jax.nn.log_sigmoid(g[2 * d + 1] + fg_b[d][:, None])
            arrs = (qkv[0], qkv[1], qkv[2], ig, lf)
            return tuple(jnp.flip(a, 2) for a in arrs) if d == 1 else arrs
        hc_d, state = _mlstm_scan(*prep(ctxs, g_ctx), zero)
        hx_d, _ = _mlstm_scan(*prep(lat, g_lat), state)
        if d == 1:
            hc_d, hx_d = jnp.flip(hc_d, 2), jnp.flip(hx_d, 2)
        h_lat.append(hx_d)
        h_ctx.append(hc_d)

    def readout(h, o):
        h = h * lax.rsqrt(jnp.mean(h * h, axis=-1, keepdims=True) + NORM_EPS)
        bb, _, t, _ = h.shape
        h = h.transpose(0, 2, 1, 3).reshape(bb, t, W_ML) * norm_g.astype(f32)
        return (h * jax.nn.sigmoid(o.astype(f32))).astype(o.dtype)

    y_lat = readout(h_lat[0] + h_lat[1], ox)
    y_ctx = readout(h_ctx[0] + h_ctx[1], oc) if with_ctx else None
    return y_lat, y_ctx


def _token_mixers(hx, hc, ang_row, ang_col, w_in, attn_sink, s5_a_re, s5_a_im, s5_log_dt, s5_b_re, s5_b_im,
                  s5_c_re, s5_c_im, s5_d, s5_glu_w, s5_glu_b, ml_igate_b, ml_fgate_b, ml_norm_g,
                  w_branch_attn, w_branch_s5, w_branch_ml, w_out, with_ctx):
    b, n_tok, _ = hx.shape
    n_ctx = hc.shape[1]
    qa_x, ka_x, va_x, us_x, qm_x, km_x, vm_x, om_x, gm_x, gb_x = _split_in(hx @ w_in)
    qa_c, ka_c, va_c, us_c, qm_c, km_c, vm_c, om_c, gm_c, gb_c = _split_in(hc @ w_in)
    q_lat = _axial_rope(qa_x.reshape(b, n_tok, N_HEADS, HEAD_DIM), ang_row, ang_col)
    q_lat = q_lat.reshape(b, n_tok, N_KV_HEADS, Q_PER_KV, HEAD_DIM)
    k_lat = _axial_rope(ka_x.reshape(b, n_tok, N_KV_HEADS, HEAD_DIM), ang_row, ang_col)
    v_lat = va_x.reshape(b, n_tok, N_KV_HEADS, HEAD_DIM)
    k_ctx = ka_c.reshape(b, n_ctx, N_KV_HEADS, HEAD_DIM)
    v_ctx = va_c.reshape(b, n_ctx, N_KV_HEADS, HEAD_DIM)
    sink = attn_sink.astype(jnp.float32).reshape(N_KV_HEADS, Q_PER_KV)
    ya_x = _attn_latent(q_lat, k_lat, v_lat, k_ctx, v_ctx, sink)
    ys_x, ys_c = _s5_branch(us_x, us_c, s5_a_re, s5_a_im, s5_log_dt, s5_b_re, s5_b_im, s5_c_re, s5_c_im,
                            s5_d, s5_glu_w, s5_glu_b, with_ctx)
    ym_x, ym_c = _mlstm_branch(qm_x, km_x, vm_x, om_x, gm_x, qm_c, km_c, vm_c, om_c, gm_c,
                               ml_igate_b, ml_fgate_b, ml_norm_g, with_ctx)

    def merge(ya, ys, ym, gates):
        g_a, g_s, g_m = jnp.split(gates, N_BRANCH, axis=-1)
        y = (jax.nn.sigmoid(g_a) * (ya @ w_branch_attn) + jax.nn.sigmoid(g_s) * (ys @ w_branch_s5)
             + jax.nn.sigmoid(g_m) * (ym @ w_branch_ml))
        return y @ w_out

    out_x = merge(ya_x, ys_x, ym_x, gb_x)
    if not with_ctx:
        return out_x, None
    q_ctx = qa_c.reshape(b, n_ctx, N_KV_HEADS, Q_PER_KV, HEAD_DIM)
    ya_c = _attn_context(q_ctx, k_ctx, v_ctx, sink)
    return out_x, merge(ya_c, ys_c, ym_c, gb_c)


def _conv_ffn(h, w_up, conv_w, conv_b, w_down):
    u = h @ w_up
    up = jnp.pad(u, ((0, 0), (1, 1), (0, 0)))
    u = up[:, :-2] * conv_w[0] + up[:, 1:-1] * conv_w[1] + up[:, 2:] * conv_w[2] + conv_b
    a, v = jnp.split(u, 2, axis=-1)
    return (jax.nn.silu(a) * v) @ w_down


def setup_inputs(seed: int = 0) -> dict:
    key = jax.random.key(seed)
    ks = iter(jax.random.split(key, 40))
    f32 = jnp.float32

    def nrm(shape, scale):
        return scale * jax.random.normal(next(ks), shape, f32)

    L, D = DEPTH, D_MODEL
    inp = {}
    inp['x'] = nrm((BATCH, SEQ, D), 1.0)
    inp['c'] = nrm((BATCH, D), 1.0)
    inp['ctx'] = nrm((BATCH, CTX_LEN, D), 1.0)
    inp['c_ctx'] = nrm((D,), 1.0)
    inp['mod_w'] = nrm((L, D, 6 * D), D ** -0.5)
    inp['mod_b'] = nrm((L, 6 * D), 0.02)
    inp['norm1_g'] = 1.0 + nrm((L, D), 0.02)
    inp['norm2_g'] = 1.0 + nrm((L, D), 0.02)
    inp['w_in'] = nrm((L, D, D_IN), D ** -0.5)
    inp['attn_sink'] = nrm((L, N_HEADS), 0.5)
    inp['s5_a_re'] = -0.5 + nrm((L, 2, S5_GROUPS, S5_STATE), 0.01)
    inp['s5_a_im'] = math.pi * jnp.arange(S5_STATE, dtype=f32) + nrm((L, 2, S5_GROUPS, S5_STATE), 0.01)
    inp['s5_log_dt'] = jax.random.uniform(next(ks), (L, 2, S5_GROUPS), f32, math.log(DT_MIN), math.log(DT_MAX))
    inp['s5_b_re'] = nrm((L, S5_GROUPS, S5_STATE, S5_GROUP), (2 * S5_GROUP) ** -0.5)
    inp['s5_b_im'] = nrm((L, S5_GROUPS, S5_STATE, S5_GROUP), (2 * S5_GROUP) ** -0.5)
    inp['s5_c_re'] = nrm((L, S5_GROUPS, S5_GROUP, S5_STATE), S5_STATE ** -0.5)
    inp['s5_c_im'] = nrm((L, S5_GROUPS, S5_GROUP, S5_STATE), S5_STATE ** -0.5)
    inp['s5_d'] = nrm((L, W_S5), 1.0)
    inp['s5_glu_w'] = nrm((L, W_S5, W_S5), W_S5 ** -0.5)
    inp['s5_glu_b'] = nrm((L, W_S5), 0.02)
    inp['ml_igate_b'] = nrm((L, 2, ML_HEADS), 0.1)
    inp['ml_fgate_b'] = jnp.linspace(3.0, 6.0, ML_HEADS, dtype=f32) + nrm((L, 2, ML_HEADS), 0.1)
    inp['ml_norm_g'] = 1.0 + nrm((L, W_ML), 0.02)
    inp['w_branch_attn'] = nrm((L, W_ATTN, D), W_ATTN ** -0.5)
    inp['w_branch_s5'] = nrm((L, W_S5, D), W_S5 ** -0.5)
    inp['w_branch_ml'] = nrm((L, W_ML, D), W_ML ** -0.5)
    inp['w_out'] = nrm((L, D, D), D ** -0.5)
    inp['ffn_w_up'] = nrm((L, D, 2 * D_FF), D ** -0.5)
    inp['ffn_conv_w'] = nrm((L, CONV_W, 2 * D_FF), CONV_W ** -0.5)
    inp['ffn_conv_b'] = nrm((L, 2 * D_FF), 0.02)
    inp['ffn_w_down'] = nrm((L, D_FF, D), D_FF ** -0.5)
    inp['final_norm_g'] = 1.0 + nrm((D,), 0.02)
    return inp


def reference(x, c, ctx, c_ctx, mod_w, mod_b, norm1_g, norm2_g, w_in, attn_sink, s5_a_re, s5_a_im, s5_log_dt,
              s5_b_re, s5_b_im, s5_c_re, s5_c_im, s5_d, s5_glu_w, s5_glu_b, ml_igate_b, ml_fgate_b, ml_norm_g,
              w_branch_attn, w_branch_s5, w_branch_ml, w_out, ffn_w_up, ffn_conv_w, ffn_conv_b, ffn_w_down,
              final_norm_g):
    ang_row, ang_col = _axial_angles(x.shape[1])
    xc = ctx
    for layer in range(DEPTH):
        with_ctx = layer < DEPTH - 1
        mod_x = (jax.nn.silu(c) @ mod_w[layer] + mod_b[layer])[:, None, :]
        mod_c = jax.nn.silu(c_ctx) @ mod_w[layer] + mod_b[layer]
        sh1x, sc1x, g1x, sh2x, sc2x, g2x = jnp.split(mod_x, 6, axis=-1)
        sh1c, sc1c, g1c, sh2c, sc2c, g2c = jnp.split(mod_c, 6, axis=-1)
        hx = _modulate(_rmsnorm(x, norm1_g[layer]), sh1x, sc1x)
        hc = _modulate(_rmsnorm(xc, norm1_g[layer]), sh1c, sc1c)
        mx, mc = _token_mixers(hx, hc, ang_row, ang_col, w_in[layer], attn_sink[layer], s5_a_re[layer],
                               s5_a_im[layer], s5_log_dt[layer], s5_b_re[layer], s5_b_im[layer], s5_c_re[layer],
                               s5_c_im[layer], s5_d[layer], s5_glu_w[layer], s5_glu_b[layer], ml_igate_b[layer],
                               ml_fgate_b[layer], ml_norm_g[layer], w_branch_attn[layer], w_branch_s5[layer],
                               w_branch_ml[layer], w_out[layer], with_ctx)
        x = x + g1x * mx
        x = x + g2x * _conv_ffn(_modulate(_rmsnorm(x, norm2_g[layer]), sh2x, sc2x), ffn_w_up[layer],
                                ffn_conv_w[layer], ffn_conv_b[layer], ffn_w_down[layer])
        if with_ctx:
            xc = xc + g1c * mc
            xc = xc + g2c * _conv_ffn(_modulate(_rmsnorm(xc, norm2_g[layer]), sh2c, sc2c), ffn_w_up[layer],
                                      ffn_conv_w[layer], ffn_conv_b[layer], ffn_w_down[layer])
    return _rmsnorm(x, final_norm_g)
```

```python
import math
from contextlib import ExitStack
import numpy as np
import ml_dtypes
import concourse.bass as bass
import concourse.mybir as mybir
from concourse.bass_utils import run_bass_kernel_spmd

F32 = mybir.dt.float32
BF16 = mybir.dt.bfloat16
I32 = mybir.dt.int32
AF = mybir.ActivationFunctionType
ALU = mybir.AluOpType
AX = mybir.AxisListType

D = 1024
CTX = 256
NH, NKV, HD = 8, 2, 64
W_S5, S5G, S5P = 256, 16, 64
MLH = 4
D_FF = 2816
EPS = 1e-6
ENG = ("pe", "act", "dve", "pool", "sp")
TWO_PI = 2.0 * math.pi


class Res:
    __slots__ = ("last_w", "readers")

    def __init__(self):
        self.last_w = None
        self.readers = []


class Op:
    __slots__ = ("eng", "fn", "dma", "deps", "sig", "idx", "sem", "val")


class _Rec:
    def __init__(self):
        self.call = None

    def __getattr__(self, name):
        def f(*a, **k):
            self.call = (name, a, k)
            return None
        return f


class Sched:
    def __init__(self, nc, ndma=8):
        self.nc, self.ops, self.ndma = nc, [], ndma
        self.phase = Res()

    def op(self, eng, fn, reads=(), writes=(), dma=False, barrier=False):
        rec = _Rec()
        fn(rec)
        name_, a_, k_ = rec.call
        fn = lambda engine, name_=name_, a_=a_, k_=k_: getattr(engine, name_)(*a_, **k_)
        o = Op()
        o.eng, o.fn, o.dma, o.deps, o.sig, o.sem, o.val = eng, fn, dma, set(), False, None, None
        o.idx = len(self.ops)
        reads = list(reads)
        writes = list(writes)
        if barrier:
            writes.append(self.phase)
        else:
            reads.append(self.phase)
        for r in reads:
            if r.last_w is not None:
                o.deps.add(r.last_w)
        for r in writes:
            if r.last_w is not None:
                o.deps.add(r.last_w)
            o.deps.update(r.readers)
        for r in reads:
            r.readers.append(o.idx)
        for r in writes:
            r.last_w = o.idx
            r.readers = []
        o.deps.discard(o.idx)
        self.ops.append(o)
        return o

    def emit(self, final_ops=()):
        nc, ops = self.nc, self.ops
        for o in ops:
            keep = set()
            for d in o.deps:
                p = ops[d]
                if p.eng == o.eng and not p.dma and o.eng in ("pe", "sp"):
                    continue
                keep.add(d)
            o.deps = keep
            for d in keep:
                ops[d].sig = True
        for f in final_ops:
            f.sig = True
        st = ExitStack()
        sems = {e: st.enter_context(nc.semaphore("s_" + e)) for e in ENG}
        dsems = [st.enter_context(nc.semaphore("d_%d" % i)) for i in range(self.ndma)]
        cnt = {e: 0 for e in ENG}
        dcnt = 0
        prev_user = {}
        for o in ops:
            if o.dma:
                n = dcnt
                dcnt += 1
                o.sem = dsems[n % self.ndma]
                o.val = 16 * (n // self.ndma + 1)
                if (n % self.ndma) in prev_user:
                    o.deps.add(prev_user[n % self.ndma])
                prev_user[n % self.ndma] = o.idx
                o.sig = True
            elif o.sig:
                cnt[o.eng] += 1
                o.sem, o.val = sems[o.eng], cnt[o.eng]
        per = {e: [o for o in ops if o.eng == e] for e in ENG}
        stats = dict(nops=len(ops), nwait=0)

        def run(e, engine):
            waited = {}
            for o in per[e]:
                need = {}
                for d in o.deps:
                    p = ops[d]
                    k = id(p.sem)
                    if k not in need or need[k][1] < p.val:
                        need[k] = (p.sem, p.val)
                for k, (sem, val) in need.items():
                    if waited.get(k, 0) >= val:
                        continue
                    engine.wait_ge(sem, val)
                    stats["nwait"] += 1
                    waited[k] = val
                ins = o.fn(engine)
                if o.dma:
                    ins.then_inc(o.sem, 16)
                elif o.sig:
                    ins.then_inc(o.sem, 1)
            if e == "sp":
                for f in final_ops:
                    engine.wait_ge(f.sem, f.val)

        with nc.Block() as block:
            @block.sync
            def _(eng):
                run("sp", eng)

            @block.tensor
            def _(eng):
                run("pe", eng)

            @block.scalar
            def _(eng):
                run("act", eng)

            @block.vector
            def _(eng):
                run("dve", eng)

            @block.gpsimd
            def _(eng):
                run("pool", eng)
        st.close()
        return stats


def _rope_perm():
    p = np.arange(64)
    out = np.empty(64, dtype=np.int64)
    for d in range(64):
        base = (d // 32) * 32
        r = d % 32
        out[d] = base + (r + 16 if r < 16 else r - 16)
    return out


def _col_layout():
    o_q, o_k, o_v = 0, 512, 640
    o_us = 768
    o_qm, o_km, o_vm, o_om = 1024, 1280, 1536, 1792
    o_gm = 2048
    o_gb = 2064
    perm = _rope_perm()
    fm = []
    for c in range(4):
        fm += [o_q + c * 64 + d for d in range(64)] + [o_q + (4 + c) * 64 + d for d in range(64)]
    for c in range(4):
        fm += [o_q + c * 64 + perm[d] for d in range(64)] + [o_q + (4 + c) * 64 + perm[d] for d in range(64)]
    fm += [o_k + d for d in range(128)]
    fm += [o_k + h * 64 + perm[d] for h in range(2) for d in range(64)]
    fm += [o_us + d for d in range(256)]
    fm += [o_qm + d for d in range(256)]
    fm += [o_km + d for d in range(256)]
    tm = [o_v + d for d in range(128)] + [o_km + d for d in range(256)] + [o_vm + d for d in range(256)] \
        + [o_om + d for d in range(256)] + [o_gm + d for d in range(16)]
    gb = [o_gb + d for d in range(3072)]
    return np.array(fm), np.array(tm), np.array(gb)


FM_Q, FM_QP, FM_K, FM_KP, FM_US, FM_QM, FM_KM = 0, 4, 8, 9, 10, 12, 14
NFM = 16
TM_V, TM_KM, TM_VM, TM_OM, TM_GM = 0, 128, 384, 640, 896
NTM = 912


def _consts(T):
    NT = CTX + T
    c = {}
    c["ident_f"] = np.eye(128, dtype=np.float32)
    k = np.arange(128)[:, None]
    q = np.arange(128)[None, :]
    c["mask_le"] = (k <= q).astype(np.float32)
    c["mask_ge"] = (k >= q).astype(np.float32)
    rows = T // 64
    row = np.repeat(np.arange(rows, dtype=np.float32), 64)
    col = np.tile(np.arange(64, dtype=np.float32), rows)
    inv = (10000.0 ** (-np.arange(16, dtype=np.float32) / 16)).astype(np.float32)
    ang_r = (row[:, None] * inv[None, :]).astype(np.float32)
    ang_c = (col[:, None] * inv[None, :]).astype(np.float32)
    cosT = np.ones((128, NT), np.float32)
    sinT = np.zeros((128, NT), np.float32)
    for p in range(128):
        d = p % 64
        a = ang_r if d < 32 else ang_c
        r = d % 32
        f = r % 16
        cosT[p, CTX:] = np.cos(a[:, f])
        s = np.sin(a[:, f])
        sinT[p, CTX:] = -s if r < 16 else s
    c["rope_cos"] = cosT
    hp = math.pi / 2
    cols = np.zeros((128, 4), np.float32)
    cols[:64, 0] = hp
    cols[64:, 1] = hp
    cols[:64, 2], cols[64:, 2] = -1.0, 1.0
    cols[:64, 3], cols[64:, 3] = 1.0, -1.0
    c["s5_cols"] = cols
    mi = np.arange(136, dtype=np.float32)
    c["s5_mphi"] = np.stack([127.0 - mi, mi - 8.0]).reshape(1, 272).astype(np.float32)
    qi = np.arange(136)
    c["s5_mpsi"] = np.stack([qi.astype(np.float32), (8 * (qi // 8) + 8 - qi % 8).astype(np.float32)]).reshape(1, 272).astype(np.float32)
    s8 = np.arange(8, dtype=np.float32)
    c["s5_mphs"] = np.stack([np.concatenate([-s8, 7.0 - s8]), np.concatenate([s8 - 8.0, s8])]).reshape(1, 32).astype(np.float32)
    rr = np.arange(128)[:, None] // 16
    cc = np.arange(128)[None, :] // 16
    c["s5_m0"] = np.stack([(cc >= rr), (rr >= cc)]).astype(np.float32)
    sw = np.zeros((128, 128), np.float32)
    sw[np.arange(128), (np.arange(128) + 64) % 128] = 1.0
    c["s5_swap"] = sw
    sel = np.zeros((128, 64, 128), np.float32)
    selT = np.zeros((128, 64, 128), np.float32)
    for gl in range(8):
        for s in range(8):
            for ch in range(16):
                sel[gl * 16 + ch, gl * 8 + s, s * 16 + ch] = 1.0
                selT[s * 16 + ch, gl * 8 + s, gl * 16 + ch] = 1.0
    c["s5_sel"] = sel.astype(ml_dtypes.bfloat16)
    c["s5_selT"] = selT.astype(ml_dtypes.bfloat16)
    c["rope_sin"] = sinT
    return c


def build(T, depth=2, debug=False, upto=None):
    NT = CTX + T
    nc = bass.Bass("TRN2", target_bir_lowering=False)
    es = ExitStack()
    S = Sched(nc)
    dbg_outs = {}

    def din(name, shape, dt=F32):
        return nc.dram_tensor(name, list(shape), dt, kind="ExternalInput").ap()

    def dscr(name, shape, dt=F32):
        kind = "ExternalOutput" if debug else "Internal"
        t = nc.dram_tensor(name, list(shape), dt, kind=kind).ap()
        if debug:
            dbg_outs[name] = t
        return t

    x_in = din("x", [T, D])
    ctx_in = din("ctx", [CTX, D])
    cvec = din("cvec", [128, 8, 2])
    mod_w = din("mod_w", [depth, D, 6 * D])
    mod_b = din("mod_b", [depth, 128, 48])
    n1g = din("norm1_g", [depth, 128, 8])
    n2g = din("norm2_g", [depth, 128, 8])
    fng = din("final_norm_g", [128, 8])
    w_fm = din("w_fm", [depth, D, NFM * 128])
    w_tm = din("w_tm", [depth, D, NTM])
    w_gb = din("w_gb", [depth, D, 3072])
    wb_a = din("w_branch_attn", [depth, 512, D])
    wb_s = din("w_branch_s5", [depth, 256, D])
    wb_m = din("w_branch_ml", [depth, 256, D])
    w_out = din("w_out", [depth, D, D])
    w_up = din("ffn_w_up", [depth, D, 2 * D_FF])
    cw = din("ffn_conv_w", [depth, 128, 3, 44])
    cb = din("ffn_conv_b", [depth, 128, 44])
    w_dn = din("ffn_w_down", [depth, D_FF, D])
    sink = din("attn_sink", [depth, 1, 8])
    ml_gb = din("ml_gate_b", [depth, 1, 16])
    ml_ng = din("ml_norm_g", [depth, 1, 256])
    s5_a = din("s5_a", [depth, 128, 64])
    s5_logdt = din("s5_logdt", [depth, 1, 32])
    s5_b = din("s5_b", [depth, 128, 512])
    s5_c = din("s5_c", [depth, 128, 512])
    s5_dcol = din("s5_dcol", [depth, 128, 16])
    s5_glu_w = din("s5_glu_w", [depth, 256, 256])
    s5_glu_b = din("s5_glu_b", [depth, 128, 2])
    s5_cols = din("s5_cols", [128, 4])
    s5_mphi = din("s5_mphi", [1, 272])
    s5_mpsi = din("s5_mpsi", [1, 272])
    s5_mphs = din("s5_mphs", [1, 32])
    s5_m0 = din("s5_m0", [2, 128, 128])
    s5_swap = din("s5_swap", [128, 128])
    s5_sel = din("s5_sel", [128, 64, 128], BF16)
    s5_selT = din("s5_selT", [128, 64, 128], BF16)
    ident_f = din("ident_f", [128, 128])
    mask_le = din("mask_le", [128, 128])
    mask_ge = din("mask_ge", [128, 128])
    rope_cos = din("rope_cos", [128, NT])
    rope_sin = din("rope_sin", [128, NT])
    y_out = nc.dram_tensor("y", [T, D], F32, kind="ExternalOutput").ap()

    xT = [dscr("xT%d" % i, [8, 128, NT]) for i in range(2)]
    qT = dscr("qT", [4, 128, NT], BF16)
    kT = dscr("kT", [128, NT], BF16)
    usT = dscr("usT", [2, 128, NT], BF16)
    qmT = dscr("qmT", [2, 128, NT], BF16)
    kmT = dscr("kmT", [2, 128, NT], BF16)
    v_tok = dscr("v_tok", [NT, 128], BF16)
    km_tok = dscr("km_tok", [NT, 256], BF16)
    vm_tok = dscr("vm_tok", [NT, 256], BF16)
    om_tok = dscr("om_tok", [NT, 256])
    gm_tok = dscr("gm_tok", [NT, 16])
    yaT = dscr("yaT", [4, 128, NT], BF16)
    ysT = dscr("ysT", [2, 128, NT], BF16)
    ymT = dscr("ymT", [2, 128, NT], BF16)
    hF = dscr("hF", [NT, 256])

    ARENA = 208896 // 2
    arena = es.enter_context(nc.sbuf_tensor("arena", [128, ARENA], BF16))
    off = [0]
    persist_off = [0]

    def tile(shape, dt, parts=128):
        n = int(np.prod(shape)) * (1 if dt == BF16 else 2)
        n = (n + 15) // 16 * 16
        assert off[0] + n <= ARENA, ("SBUF arena overflow", off[0], n)
        a = arena[0:parts, off[0]:off[0] + n]
        off[0] += n
        used = int(np.prod(shape)) * (1 if dt == BF16 else 2)
        a = a[:, 0:used]
        if dt != BF16:
            a = a.bitcast(dt)
        if len(shape) == 2:
            a = a.rearrange("p (a b) -> p a b", a=shape[0])
        elif len(shape) == 3:
            a = a.rearrange("p (a b c) -> p a b c", a=shape[0], b=shape[1])
        return a, Res()

    banks = [es.enter_context(nc.psum_tensor("bank%d" % i, [128, 512], F32)) for i in range(8)]
    bres = [Res() for _ in range(8)]
    junk, junk_r = tile((4,), F32)

    def barrier():
        S.op("pool", lambda e: e.memset(junk[:, 0:1], 0.0), writes=[junk_r], barrier=True)

    def new_phase():
        barrier()
        off[0] = persist_off[0]

    def dma(out, in_, reads=(), writes=()):
        return S.op("sp", lambda e: e.dma_start(out=out, in_=in_), reads, writes, dma=True)

    def op(eng, fn, reads=(), writes=()):
        return S.op(eng, fn, reads, writes)

    def cast_load(dst, dst_r, src_ap, shape, stage, stage_r, eng="pool"):
        dma(stage, src_ap, writes=[stage_r])
        if eng == "act":
            op("act", lambda e: e.copy(out=dst, in_=stage), [stage_r], [dst_r])
        else:
            op(eng, lambda e: e.tensor_copy(out=dst, in_=stage), [stage_r], [dst_r])

    ident, ident_r = tile((128,), F32)
    identb, identb_r = tile((128,), BF16)
    ones_f, ones_r = tile((128,), F32)
    mle, mle_r = tile((128,), F32)
    mge, mge_r = tile((128,), F32)
    mleb, mleb_r = tile((128,), BF16)
    mgeb, mgeb_r = tile((128,), BF16)
    mle8, mle8_r = tile((128,), BF16)
    mge8, mge8_r = tile((128,), BF16)
    modv, modv_r = tile((depth, 48, 2), F32)
    gs1, gs1_r = tile((depth, 8, 2), F32)
    gs2, gs2_r = tile((depth, 8, 2), F32)
    n1t, n1t_r = tile((depth, 8), F32)
    n2t, n2t_r = tile((depth, 8), F32)
    fnt, fnt_r = tile((8,), F32)
    persist_off[0] = off[0]

    dma(ident, ident_f, writes=[ident_r])
    dma(mle, mask_le, writes=[mle_r])
    dma(mge, mask_ge, writes=[mge_r])
    op("dve", lambda e: e.tensor_copy(out=identb, in_=ident), [ident_r], [identb_r])
    op("dve", lambda e: e.tensor_copy(out=mleb, in_=mle), [mle_r], [mleb_r])
    op("dve", lambda e: e.tensor_copy(out=mgeb, in_=mge), [mge_r], [mgeb_r])
    op("pool", lambda e: e.memset(ones_f, 1.0), writes=[ones_r])
    op("dve", lambda e: e.tensor_scalar_mul(out=mle8, in0=mle, scalar1=0.125), [mle_r], [mle8_r])
    op("dve", lambda e: e.tensor_scalar_mul(out=mge8, in0=mge, scalar1=0.125), [mge_r], [mge8_r])
    dma(n1t, n1g.rearrange("l p c -> p l c"), writes=[n1t_r])
    dma(n2t, n2g.rearrange("l p c -> p l c"), writes=[n2t_r])
    dma(fnt, fng, writes=[fnt_r])

    def phase_mod():
        cv, cv_r = tile((8, 2), F32)
        sc, sc_r = tile((8, 2), F32)
        dma(cv, cvec, writes=[cv_r])
        op("act", lambda e: e.activation(out=sc, in_=cv, func=AF.Silu), [cv_r], [sc_r])
        mb, mb_r = tile((depth, 48), F32)
        dma(mb, mod_b.rearrange("l p c -> p l c"), writes=[mb_r])
        wst = [tile((8, 1024), F32) for _ in range(2)]
        i = 0
        for l in range(depth):
            for piece in range(6):
                w, w_r = wst[i % 2]
                i += 1
                dma(w, mod_w[l, :, piece * 1024:(piece + 1) * 1024].rearrange("(kc p) n -> p kc n", p=128), writes=[w_r])
                bk, bk_r = banks[piece % 2], bres[piece % 2]
                for fc in range(8):
                    for kc in range(8):
                        op("pe", lambda e, w=w, fc=fc, kc=kc, bk=bk: e.matmul(
                            bk[:, fc * 2:fc * 2 + 2], lhsT=w[:, kc, fc * 128:(fc + 1) * 128], rhs=sc[:, kc, :],
                            start=(kc == 0), stop=(kc == 7)), [w_r, sc_r], [bk_r])
                dst = modv[:, l, piece * 8:(piece + 1) * 8, :]
                src = bk[:, 0:16].rearrange("p (a b) -> p a b", a=8)
                bb = mb[:, l, piece * 8:(piece + 1) * 8].unsqueeze(2).to_broadcast([128, 8, 2])
                op("dve", lambda e, dst=dst, src=src, bb=bb: e.tensor_tensor(out=dst, in0=src, in1=bb, op=ALU.add),
                   [bk_r, mb_r], [modv_r])
        for l in range(depth):
            for (gs, gs_r, nt, nt_r, pc) in ((gs1, gs1_r, n1t, n1t_r, 1), (gs2, gs2_r, n2t, n2t_r, 4)):
                src = modv[:, l, pc * 8:(pc + 1) * 8, :]
                nb = nt[:, l, :].unsqueeze(2).to_broadcast([128, 8, 2])
                dst = gs[:, l, :, :]
                op("dve", lambda e, dst=dst, src=src, nb=nb: e.scalar_tensor_tensor(
                    out=dst, in0=src, scalar=1.0, in1=nb, op0=ALU.add, op1=ALU.mult), [modv_r, nt_r], [gs_r])

    def phase_transpose_in():
        xin = [tile((1024,), F32) for _ in range(2)]
        xo = [tile((8, 128), F32) for _ in range(2)]
        nblk = NT // 128
        for b in range(nblk):
            t0 = b * 128
            xt, xt_r = xin[b % 2]
            src = ctx_in[t0:t0 + 128, :] if t0 < CTX else x_in[t0 - CTX:t0 - CTX + 128, :]
            dma(xt, src, writes=[xt_r])
            o, o_r = xo[b % 2]
            for half in range(2):
                bk, bk_r = banks[(b * 2 + half) % 4], bres[(b * 2 + half) % 4]
                for j in range(4):
                    c = half * 4 + j
                    op("pe", lambda e, bk=bk, j=j, xt=xt, c=c: e.transpose(
                        bk[:, j * 128:(j + 1) * 128], xt[:, c * 128:(c + 1) * 128], ident), [xt_r, ident_r], [bk_r])
                dst = o[:, half * 4:(half + 1) * 4, :]
                srcp = bk[:, :].rearrange("p (a b) -> p a b", a=4)
                if half == 0:
                    op("dve", lambda e, dst=dst, srcp=srcp: e.tensor_copy(out=dst, in_=srcp), [bk_r], [o_r])
                else:
                    op("act", lambda e, dst=dst, srcp=srcp: e.copy(out=dst, in_=srcp), [bk_r], [o_r])
            dma(xT[0][:, :, t0:t0 + 128].rearrange("c p t -> p c t"), o, reads=[o_r])

    def norm_mod(xt, xt_r, n, hT, hT_r, gs, gs_r, shcol, which, sq, sq_r, rs, rs_r, bk, bk_r, l, zero_cols=()):
        for c in range(8):
            if c % 2 == 0:
                op("act", lambda e, c=c: e.activation(out=sq[:, c, 0:n], in_=xt[:, c, 0:n], func=AF.Square), [xt_r], [sq_r[c]])
            else:
                op("pool", lambda e, c=c: e.tensor_tensor(out=sq[:, c, 0:n], in0=xt[:, c, 0:n], in1=xt[:, c, 0:n], op=ALU.mult),
                   [xt_r], [sq_r[c]])
        for c in range(8):
            op("pe", lambda e, c=c: e.matmul(bk[:, 0:n], lhsT=ones_f, rhs=sq[:, c, 0:n], start=(c == 0), stop=(c == 7)),
               [sq_r[c], ones_r], [bk_r])
        op("act", lambda e: e.activation(out=rs[:, 0:n], in_=bk[:, 0:n], func=AF.Sqrt, bias=EPS, scale=1.0 / D), [bk_r], [rs_r])
        op("dve", lambda e: e.reciprocal(out=rs[:, 0:n], in_=rs[:, 0:n]), [rs_r], [rs_r])
        for c in range(8):
            op("dve", lambda e, c=c: e.tensor_tensor(out=sq[:, c, 0:n], in0=xt[:, c, 0:n], in1=rs[:, 0:n], op=ALU.mult),
               [xt_r, rs_r], [sq_r[c]])
            op("act", lambda e, c=c: e.activation(out=hT[:, c, 0:n], in_=sq[:, c, 0:n], func=AF.Identity,
                                                  bias=modv[:, l, shcol * 8 + c, which:which + 1],
                                                  scale=gs[:, l, c, which:which + 1]), [sq_r[c], modv_r, gs_r], [hT_r])
        for zc in zero_cols:
            op("pool", lambda e, zc=zc: e.memset(hT[:, :, zc:zc + 1], 0.0), [], [hT_r])

    def token_tiles(with_ctx=True, size=512):
        tl = []
        if with_ctx:
            tl.append((0, CTX))
        t = CTX
        while t < NT:
            n = min(size, NT - t)
            tl.append((t, n))
            t += n
        return tl

    def phase_A(l, xsrc):
        wfm, wfm_r = tile((8, NFM * 128), BF16)
        wtm, wtm_r = tile((8, NTM), BF16)
        stg = [tile((8, 512), F32) for _ in range(2)]
        si = 0
        for cc in range(NFM * 128 // 512):
            st_, st_r = stg[si % 2]
            si += 1
            cast_load(wfm[:, :, cc * 512:(cc + 1) * 512], wfm_r,
                      w_fm[l, :, cc * 512:(cc + 1) * 512].rearrange("(kc p) n -> p kc n", p=128), None, st_, st_r,
                      eng=("pool" if cc % 2 == 0 else "dve"))
        for (c0, c1) in ((0, 512), (512, NTM)):
            st_, st_r = stg[si % 2]
            si += 1
            cast_load(wtm[:, :, c0:c1], wtm_r, w_tm[l, :, c0:c1].rearrange("(kc p) n -> p kc n", p=128), None,
                      st_[:, :, 0:c1 - c0], st_r, eng="pool")
        xts = [tile((8, 512), F32) for _ in range(2)]
        sq, _ = tile((8, 512), F32)
        sq_r = [Res() for _ in range(8)]
        rs, rs_r = tile((512,), F32)
        hT, hT_r = tile((8, 512), BF16)
        fo, fo_r = tile((NFM, 512), BF16)
        rc, rc_r = tile((512,), F32)
        rsn, rsn_r = tile((512,), F32)
        tmpa, tmpa_r = tile((512,), F32)
        tmpb, tmpb_r = tile((512,), F32)
        to_b, to_br = tile((640,), BF16)
        to_f, to_fr = tile((272,), F32)
        for ti, (t0, n) in enumerate(token_tiles()):
            which = 1 if t0 < CTX else 0
            xt, xt_r = xts[ti % 2]
            dma(xt[:, :, 0:n], xsrc[:, :, t0:t0 + n].rearrange("c p t -> p c t"), writes=[xt_r])
            norm_mod(xt, xt_r, n, hT, hT_r, gs1, gs1_r, 0, which, sq, sq_r, rs, rs_r, banks[0], bres[0], l)
            if which == 0:
                dma(rc[:, 0:n], rope_cos[:, t0:t0 + n], writes=[rc_r])
                dma(rsn[:, 0:n], rope_sin[:, t0:t0 + n], writes=[rsn_r])
            def fm_mm(m, bk, bk_r):
                for kc in range(8):
                    op("pe", lambda e, m=m, kc=kc, bk=bk: e.matmul(
                        bk[:, 0:n], lhsT=wfm[:, kc, m * 128:(m + 1) * 128], rhs=hT[:, kc, 0:n],
                        start=(kc == 0), stop=(kc == 7)), [wfm_r, hT_r], [bk_r])
            bi = 1
            for (m, mp) in [(FM_Q + c, FM_QP + c) for c in range(4)] + [(FM_K, FM_KP)]:
                b1, b1r = banks[1 + (bi % 3) * 2], bres[1 + (bi % 3) * 2]
                b2, b2r = banks[2 + (bi % 3) * 2], bres[2 + (bi % 3) * 2]
                bi += 1
                fm_mm(m, b1, b1r)
                if which == 0:
                    fm_mm(mp, b2, b2r)
                    op("dve", lambda e, b1=b1: e.tensor_tensor(out=tmpa[:, 0:n], in0=b1[:, 0:n], in1=rc[:, 0:n], op=ALU.mult),
                       [b1r, rc_r], [tmpa_r])
                    op("dve", lambda e, b2=b2: e.tensor_tensor(out=tmpb[:, 0:n], in0=b2[:, 0:n], in1=rsn[:, 0:n], op=ALU.mult),
                       [b2r, rsn_r], [tmpb_r])
                    op("pool", lambda e, m=m: e.tensor_tensor(out=fo[:, m, 0:n], in0=tmpa[:, 0:n], in1=tmpb[:, 0:n], op=ALU.add),
                       [tmpa_r, tmpb_r], [fo_r])
                else:
                    op("act", lambda e, m=m, b1=b1: e.copy(out=fo[:, m, 0:n], in_=b1[:, 0:n]), [b1r], [fo_r])
            for m in range(FM_US, NFM):
                b1, b1r = banks[1 + (m % 6)], bres[1 + (m % 6)]
                fm_mm(m, b1, b1r)
                if m % 2 == 0:
                    op("act", lambda e, m=m, b1=b1: e.copy(out=fo[:, m, 0:n], in_=b1[:, 0:n]), [b1r], [fo_r])
                else:
                    op("dve", lambda e, m=m, b1=b1: e.tensor_copy(out=fo[:, m, 0:n], in_=b1[:, 0:n]), [b1r], [fo_r])
            dma(qT[:, :, t0:t0 + n].rearrange("c p t -> p c t"), fo[:, FM_Q:FM_Q + 4, 0:n], reads=[fo_r])
            dma(kT[:, t0:t0 + n], fo[:, FM_K, 0:n], reads=[fo_r])
            dma(usT[:, :, t0:t0 + n].rearrange("c p t -> p c t"), fo[:, FM_US:FM_US + 2, 0:n], reads=[fo_r])
            dma(qmT[:, :, t0:t0 + n].rearrange("c p t -> p c t"), fo[:, FM_QM:FM_QM + 2, 0:n], reads=[fo_r])
            dma(kmT[:, :, t0:t0 + n].rearrange("c p t -> p c t"), fo[:, FM_KM:FM_KM + 2, 0:n], reads=[fo_r])
            for sb in range(n // 128):
                b1, b1r = banks[1 + (sb % 2) * 2], bres[1 + (sb % 2) * 2]
                b2, b2r = banks[2 + (sb % 2) * 2], bres[2 + (sb % 2) * 2]
                for (bk, bk_r, c0, c1) in ((b1, b1r, 0, 512), (b2, b2r, 512, NTM)):
                    for kc in range(8):
                        op("pe", lambda e, bk=bk, kc=kc, c0=c0, c1=c1, sb=sb: e.matmul(
                            bk[:, 0:c1 - c0], lhsT=hT[:, kc, sb * 128:(sb + 1) * 128], rhs=wtm[:, kc, c0:c1],
                            start=(kc == 0), stop=(kc == 7)), [hT_r, wtm_r], [bk_r])
                op("act", lambda e, b1=b1: e.copy(out=to_b[:, 0:512], in_=b1[:, 0:512]), [b1r], [to_br])
                op("dve", lambda e, b2=b2: e.tensor_copy(out=to_b[:, 512:640], in_=b2[:, 0:128]), [b2r], [to_br])
                op("dve", lambda e, b2=b2: e.tensor_copy(out=to_f, in_=b2[:, 128:400]), [b2r], [to_fr])
                tt = t0 + sb * 128
                dma(v_tok[tt:tt + 128, :], to_b[:, TM_V:TM_V + 128], reads=[to_br])
                dma(km_tok[tt:tt + 128, :], to_b[:, TM_KM:TM_KM + 256], reads=[to_br])
                dma(vm_tok[tt:tt + 128, :], to_b[:, TM_VM:TM_VM + 256], reads=[to_br])
                dma(om_tok[tt:tt + 128, :], to_f[:, 0:256], reads=[to_fr])
                dma(gm_tok[tt:tt + 128, :], to_f[:, 256:272], reads=[to_fr])

    def phase_attn(l, with_ctx):
        esk, esk_r = tile((8,), F32)
        dma(esk, sink[l].partition_broadcast(128), writes=[esk_r])
        op("act", lambda e: e.activation(out=esk, in_=esk, func=AF.Exp), [esk_r], [esk_r])
        kc_, kc_r = tile((CTX,), BF16)
        dma(kc_, kT[:, 0:CTX], writes=[kc_r])
        vc_, vc_r = tile((2, 2, 65), BF16)
        op("pool", lambda e: e.memset(vc_, 1.0), [], [vc_r])
        for j in range(2):
            dma(vc_[:, j, :, 0:64], v_tok[j * 128:(j + 1) * 128, :].rearrange("t (g d) -> t g d", g=2), writes=[vc_r])
        NS = 3
        qs = [tile((4, 128), BF16) for _ in range(NS)]
        ks = [tile((384,), BF16) for _ in range(NS)]
        vs = [tile((3, 2, 65), BF16) for _ in range(NS)]
        for (v_, v_r) in vs:
            op("pool", lambda e, v_=v_: e.memset(v_, 1.0), [], [v_r])
        pts = [tile((512,), BF16) for _ in range(10)]
        ya, ya_r = tile((512,), BF16)
        yat, yat_r = tile((4, 128), BF16)
        den, den_r = tile((4,), F32)
        nlat = T // 128
        blocks = [("lat", j) for j in range(nlat)]
        if with_ctx:
            blocks += [("ctx", 0), ("ctx", 1)]
        pi = 0
        for bi, (kind, j) in enumerate(blocks):
            q_, q_r = qs[bi % NS]
            k_, k_r = ks[bi % NS]
            v_, v_r = vs[bi % NS]
            tq = CTX + j * 128 if kind == "lat" else j * 128
            dma(q_, qT[:, :, tq:tq + 128].rearrange("c p t -> p c t"), writes=[q_r])
            keys = []
            if kind == "lat":
                lo, hi = max(0, j - 1), min(nlat - 1, j + 1)
                nk = hi - lo + 1
                dma(k_[:, 0:nk * 128], kT[:, CTX + lo * 128:CTX + (hi + 1) * 128], writes=[k_r])
                for i_ in range(nk):
                    dma(v_[:, i_, :, 0:64], v_tok[CTX + (lo + i_) * 128:CTX + (lo + i_ + 1) * 128, :].rearrange("t (g d) -> t g d", g=2),
                        writes=[v_r])
                for jj in range(lo, hi + 1):
                    m = None if jj == j else ("ge" if jj < j else "le")
                    keys.append((k_[:, (jj - lo) * 128:(jj - lo + 1) * 128], k_r, v_[:, jj - lo], v_r, m))
            for cj in range(2):
                keys.append((kc_[:, cj * 128:(cj + 1) * 128], kc_r, vc_[:, cj], vc_r, None))
            for g in range(2):
                ob, ob_r = banks[4 + g], bres[4 + g]
                plist = []
                for ki, (ka, ka_r, va, va_r, m) in enumerate(keys):
                    sbk, sbk_r = banks[pi % 4], bres[pi % 4]
                    pt, pt_r = pts[g * 5 + ki]
                    pi += 1
                    op("pe", lambda e, sbk=sbk, ka=ka, g=g, q_=q_: e.matmul(
                        sbk[:, 0:512], lhsT=ka[g * 64:(g + 1) * 64, :], rhs=q_[g * 64:(g + 1) * 64, :, :],
                        start=True, stop=True), [ka_r, q_r], [sbk_r])
                    op("act", lambda e, pt=pt, sbk=sbk: e.activation(out=pt, in_=sbk[:, 0:512], func=AF.Exp, scale=HD ** -0.5),
                       [sbk_r], [pt_r])
                    if m is not None:
                        mk, mk_r = (mgeb, mgeb_r) if m == "ge" else (mleb, mleb_r)
                        mb_ = mk.unsqueeze(1).to_broadcast([128, 4, 128])
                        ptv = pt.rearrange("p (a b) -> p a b", a=4)
                        op("dve", lambda e, ptv=ptv, mb_=mb_: e.tensor_tensor(out=ptv, in0=ptv, in1=mb_, op=ALU.mult),
                           [pt_r, mk_r], [pt_r])
                    plist.append((pt, pt_r, va, va_r))
                for hh in range(4):
                    for ki, (pt, pt_r, va, va_r) in enumerate(plist):
                        op("pe", lambda e, ob=ob, hh=hh, pt=pt, va=va, g=g, ki=ki, nk=len(plist): e.matmul(
                            ob[:, hh * 65:(hh + 1) * 65], lhsT=pt[:, hh * 128:(hh + 1) * 128], rhs=va[:, g, :],
                            start=(ki == 0), stop=(ki == nk - 1)), [pt_r, va_r], [ob_r])
                obv = ob[:, 0:260].rearrange("p (a b) -> p a b", a=4)
                op("dve", lambda e, obv=obv, g=g: e.tensor_tensor(out=den, in0=obv[:, :, 64], in1=esk[:, g * 4:(g + 1) * 4], op=ALU.add),
                   [ob_r, esk_r], [den_r])
                op("dve", lambda e: e.reciprocal(out=den, in_=den), [den_r], [den_r])
                yav = ya[:, g * 256:(g + 1) * 256].rearrange("p (a b) -> p a b", a=4)
                db = den.unsqueeze(2).to_broadcast([128, 4, 64])
                op("dve", lambda e, yav=yav, obv=obv, db=db: e.tensor_tensor(out=yav, in0=obv[:, :, 0:64], in1=db, op=ALU.mult),
                   [ob_r, den_r], [ya_r])
            tb, tb_r = banks[6], bres[6]
            tbv = tb[:, 0:256].bitcast(BF16)
            for c in range(4):
                op("pe", lambda e, c=c, tbv=tbv: e.transpose(tbv[:, c * 128:(c + 1) * 128], ya[:, c * 128:(c + 1) * 128], identb),
                   [ya_r, identb_r], [tb_r])
            op("act", lambda e, tbv=tbv: e.copy(out=yat, in_=tbv.rearrange("p (a b) -> p a b", a=4)), [tb_r], [yat_r])
            dma(yaT[:, :, tq:tq + 128].rearrange("c p t -> p c t"), yat, reads=[yat_r])

    def phase_mlstm(l, with_ctx):
        gbt, gbt_r = tile((16,), F32)
        dma(gbt, ml_gb[l].partition_broadcast(128), writes=[gbt_r])
        ngt, ngt_r = tile((256,), F32)
        dma(ngt, ml_ng[l].partition_broadcast(128), writes=[ngt_r])
        ones64b, ones64b_r = tile((64,), F32)
        op("pool", lambda e: e.memset(ones64b, 1.0), [], [ones64b_r])
        nch = NT // 128
        NS = 2
        gts = [tile((16,), F32) for _ in range(NS)]
        qTs = [tile((2, 128), BF16) for _ in range(NS)]
        kTs = [tile((2, 128), BF16) for _ in range(NS)]
        kts = [tile((256,), BF16) for _ in range(NS)]
        vts = [tile((4, 65), BF16) for _ in range(NS)]
        for (v_, v_r) in vts:
            op("pool", lambda e, v_=v_: e.memset(v_, 1.0), [], [v_r])
        sps = [tile((4,), F32) for _ in range(NS)]
        avs = [tile((4,), F32) for _ in range(NS)]
        rvs = [tile((4,), F32) for _ in range(NS)]
        dcs = [tile((2,), F32) for _ in range(NS)]
        vas = [tile((4, 65), BF16) for _ in range(NS)]
        dts = [tile((4, 128), BF16) for _ in range(NS)]
        hns = [tile((4, 65), F32) for _ in range(NS)]
        dns = [tile((4,), F32) for _ in range(NS)]
        hfs = [tile((256,), F32) for _ in range(NS)]
        oms = [tile((256,), F32) for _ in range(NS)]
        for d in range(MLDIRS):
            if d == 1:
                barrier()
            Cst, Cst_r = tile((2, 65), F32)
            Cbf, Cbf_r = tile((2, 65), BF16)
            op("pool", lambda e, Cst=Cst: e.memset(Cst, 0.0), [], [Cst_r])
            op("pool", lambda e, Cbf=Cbf: e.memset(Cbf, 0.0), [], [Cbf_r])
            order = list(range(nch)) if d == 0 else [1, 0] + list(range(nch - 1, 1, -1))
            tri, tri_r = (mle, mle_r) if d == 0 else (mge, mge_r)
            mkb, mkb_r = (mle8, mle8_r) if d == 0 else (mge8, mge8_r)
            ic0, fc0 = (0, 4) if d == 0 else (8, 12)
            for si, c in enumerate(order):
                s_ = si % NS
                t0 = c * 128
                gt, gt_r = gts[s_]
                dma(gt, gm_tok[t0:t0 + 128, :], writes=[gt_r])
                q_, q_r = qTs[s_]
                dma(q_, qmT[:, :, t0:t0 + 128].rearrange("c p t -> p c t"), writes=[q_r])
                k_, k_r = kTs[s_]
                dma(k_, kmT[:, :, t0:t0 + 128].rearrange("c p t -> p c t"), writes=[k_r])
                kt, kt_r = kts[s_]
                dma(kt, km_tok[t0:t0 + 128, :], writes=[kt_r])
                vt, vt_r = vts[s_]
                dma(vt[:, :, 0:64], vm_tok[t0:t0 + 128, :].rearrange("t (h d) -> t h d", h=4), writes=[vt_r])
                op("dve", lambda e, gt=gt: e.tensor_tensor(out=gt, in0=gt, in1=gbt, op=ALU.add), [gt_r, gbt_r], [gt_r])
                sp, sp_r = sps[s_]
                op("act", lambda e, sp=sp, gt=gt: e.activation(out=sp, in_=gt[:, fc0:fc0 + 4], func=AF.Exp, scale=-1.0), [gt_r], [sp_r])
                op("act", lambda e, sp=sp: e.activation(out=sp, in_=sp, func=AF.Ln, bias=1.0, scale=1.0), [sp_r], [sp_r])
                gb_, gb_r = banks[0], bres[0]
                op("pe", lambda e, sp=sp, gb_=gb_: e.matmul(gb_[:, 0:4], lhsT=tri, rhs=sp, start=True, stop=True), [sp_r, tri_r], [gb_r])
                op("pe", lambda e, sp=sp, gb_=gb_: e.matmul(gb_[:, 8:12], lhsT=ones_f, rhs=sp, start=True, stop=True),
                   [sp_r, ones_r], [gb_r])
                av, av_r = avs[s_]
                rv, rv_r = rvs[s_]
                dc, dc_r = dcs[s_]
                op("dve", lambda e, av=av, gt=gt, gb_=gb_: e.tensor_tensor(out=av, in0=gb_[:, 0:4], in1=gt[:, ic0:ic0 + 4], op=ALU.add),
                   [gb_r, gt_r], [av_r])
                op("act", lambda e, av=av: e.activation(out=av, in_=av, func=AF.Exp), [av_r], [av_r])
                op("act", lambda e, rv=rv, gb_=gb_: e.activation(out=rv, in_=gb_[:, 0:4], func=AF.Exp, scale=-1.0), [gb_r], [rv_r])
                totv = gb_[:, 8:12].rearrange("p (a two) -> p a two", two=2)
                op("act", lambda e, dc=dc, totv=totv: e.activation(out=dc[0:64, :], in_=totv[0:64, :, 0], func=AF.Exp, scale=-1.0), [gb_r], [dc_r])
                op("act", lambda e, dc=dc, totv=totv: e.activation(out=dc[64:128, :], in_=totv[64:128, :, 1], func=AF.Exp, scale=-1.0), [gb_r], [dc_r])
                if MLCUT == 1:
                    continue
                va, va_r = vas[s_]
                ab = av.unsqueeze(2).to_broadcast([128, 4, 65])
                op("dve", lambda e, va=va, vt=vt, ab=ab: e.tensor_tensor(out=va, in0=vt, in1=ab, op=ALU.mult), [vt_r, av_r], [va_r])
                dt, dt_r = dts[s_]
                dtv = dt.rearrange("p (a two) t -> p a two t", two=2)
                mb_ = mkb.unsqueeze(1).to_broadcast([128, 2, 128])
                for par in range(2):
                    sb_, sb_r = banks[1 + par], bres[1 + par]
                    for a in range(2):
                        h = 2 * a + par
                        hp = par * 64
                        op("pe", lambda e, sb_=sb_, a=a, hp=hp, k_=k_, q_=q_: e.matmul(
                            sb_[:, a * 128:(a + 1) * 128], lhsT=k_[hp:hp + 64, a, :], rhs=q_[hp:hp + 64, a, :],
                            start=True, stop=True), [k_r, q_r], [sb_r])
                    sbv = sb_[:, 0:256].rearrange("p (a b) -> p a b", a=2)
                    op("dve", lambda e, dtv=dtv, sbv=sbv, mb_=mb_, par=par: e.tensor_tensor(out=dtv[:, :, par, :], in0=sbv, in1=mb_, op=ALU.mult),
                       [sb_r, mkb_r], [dt_r])
                if MLCUT == 2:
                    continue
                kvb, kvb_r = banks[3 + si % 2], bres[3 + si % 2]
                for h in range(4):
                    op("pe", lambda e, kvb=kvb, h=h, kt=kt, va=va: e.matmul(
                        kvb[:, h * 65:(h + 1) * 65], lhsT=kt[:, (h // 2) * 128:(h // 2 + 1) * 128], rhs=va[:, h, :], start=True, stop=True),
                       [kt_r, va_r], [kvb_r])
                if MLCUT == 3:
                    continue
                hn, hn_r = hns[s_]
                hnv4 = hn.rearrange("p (a two) b -> p a two b", two=2)
                rv4 = rv.rearrange("p (a two) -> p a two", two=2)
                for par in range(2):
                    ob, ob_r = banks[5 + par], bres[5 + par]
                    hp = par * 64
                    for a in range(2):
                        h = 2 * a + par
                        op("pe", lambda e, ob=ob, a=a, h=h, dt=dt, va=va: e.matmul(
                            ob[:, a * 65:(a + 1) * 65], lhsT=dt[:, h, :], rhs=va[:, h, :], start=True, stop=False), [dt_r, va_r], [ob_r])
                        op("pe", lambda e, ob=ob, a=a, hp=hp, q_=q_, Cbf=Cbf: e.matmul(
                            ob[:, a * 65:(a + 1) * 65], lhsT=q_[hp:hp + 64, a, :], rhs=Cbf[hp:hp + 64, a, :],
                            start=False, stop=True), [q_r, Cbf_r], [ob_r])
                    obv = ob[:, 0:130].rearrange("p (a b) -> p a b", a=2)
                    rb = rv4[:, :, par].unsqueeze(2).to_broadcast([128, 2, 65])
                    op("dve", lambda e, hnv4=hnv4, obv=obv, rb=rb, par=par: e.tensor_tensor(out=hnv4[:, :, par, :], in0=obv, in1=rb, op=ALU.mult),
                       [ob_r, rv_r], [hn_r])
                if MLCUT == 4:
                    continue
                kvv = kvb[:, 0:260].rearrange("p (a two b) -> p a two b", a=2, two=2)
                for hf_ in range(2):
                    ps_ = slice(hf_ * 64, hf_ * 64 + 64)
                    op("dve", lambda e, Cst=Cst, kvv=kvv, ps_=ps_, hf_=hf_: e.scalar_tensor_tensor(
                        out=Cst[ps_, :, :], in0=kvv[ps_, :, hf_, :], scalar=0.125, in1=Cst[ps_, :, :], op0=ALU.mult, op1=ALU.add),
                       [kvb_r, Cst_r], [Cst_r])
                dcb = dc.unsqueeze(2).to_broadcast([128, 2, 65])
                op("dve", lambda e, Cst=Cst, dcb=dcb: e.tensor_tensor(out=Cst, in0=Cst, in1=dcb, op=ALU.mult), [Cst_r, dc_r], [Cst_r])
                op("act", lambda e, Cst=Cst, Cbf=Cbf: e.copy(out=Cbf, in_=Cst), [Cst_r], [Cbf_r])
                if MLCUT == 5:
                    continue
                dn, dn_r = dns[s_]
                op("act", lambda e, dn=dn, hn=hn: e.activation(out=dn, in_=hn[:, :, 64], func=AF.Abs), [hn_r], [dn_r])
                op("dve", lambda e, dn=dn: e.tensor_scalar_max(out=dn, in0=dn, scalar1=1.0), [dn_r], [dn_r])
                op("dve", lambda e, dn=dn: e.reciprocal(out=dn, in_=dn), [dn_r], [dn_r])
                hf, hf_r = hfs[s_]
                hfv = hf.rearrange("p (a b) -> p a b", a=4)
                dnb = dn.unsqueeze(2).to_broadcast([128, 4, 64])
                if d == 0:
                    op("dve", lambda e, hfv=hfv, hn=hn, dnb=dnb: e.tensor_tensor(out=hfv, in0=hn[:, :, 0:64], in1=dnb, op=ALU.mult),
                       [hn_r, dn_r], [hf_r])
                    dma(hF[t0:t0 + 128, :], hf, reads=[hf_r])
                    continue
                if c < 2 and not with_ctx:
                    continue
                dma(hf, hF[t0:t0 + 128, :], writes=[hf_r])
                om, om_r = oms[s_]
                dma(om, om_tok[t0:t0 + 128, :], writes=[om_r])
                hnv = hn[:, :, 0:64]
                op("dve", lambda e, hnv=hnv, dnb=dnb: e.tensor_tensor(out=hnv, in0=hnv, in1=dnb, op=ALU.mult), [hn_r, dn_r], [hn_r])
                op("dve", lambda e, hfv=hfv, hnv=hnv: e.tensor_tensor(out=hfv, in0=hfv, in1=hnv, op=ALU.add), [hf_r, hn_r], [hf_r])
                op("pool", lambda e, hnv=hnv, hfv=hfv: e.tensor_tensor(out=hnv, in0=hfv, in1=hfv, op=ALU.mult), [hf_r], [hn_r])
                op("dve", lambda e, dn=dn, hnv=hnv: e.tensor_reduce(out=dn, in_=hnv, axis=AX.X, op=ALU.add), [hn_r], [dn_r])
                op("act", lambda e, dn=dn: e.activation(out=dn, in_=dn, func=AF.Sqrt, bias=EPS, scale=1.0 / 64), [dn_r], [dn_r])
                op("dve", lambda e, dn=dn: e.reciprocal(out=dn, in_=dn), [dn_r], [dn_r])
                op("act", lambda e, om=om: e.activation(out=om, in_=om, func=AF.Sigmoid), [om_r], [om_r])
                op("pool", lambda e, om=om: e.tensor_tensor(out=om, in0=om, in1=ngt, op=ALU.mult), [om_r, ngt_r], [om_r])
                op("dve", lambda e, hfv=hfv, dnb=dnb: e.tensor_tensor(out=hfv, in0=hfv, in1=dnb, op=ALU.mult), [hf_r, dn_r], [hf_r])
                ymb = kt
                op("dve", lambda e, ymb=ymb, hf=hf, om=om: e.tensor_tensor(out=ymb, in0=hf, in1=om, op=ALU.mult), [hf_r, om_r], [kt_r])
                tb, tb_r = banks[7], bres[7]
                tbv = tb[:, 0:128].bitcast(BF16)
                for cc in range(2):
                    op("pe", lambda e, cc=cc, tbv=tbv, ymb=ymb: e.transpose(tbv[:, cc * 128:(cc + 1) * 128], ymb[:, cc * 128:(cc + 1) * 128], identb),
                       [kt_r, identb_r], [tb_r])
                yo = k_
                op("act", lambda e, yo=yo, tbv=tbv: e.copy(out=yo, in_=tbv.rearrange("p (a b) -> p a b", a=2)), [tb_r], [k_r])
                dma(ymT[:, :, t0:t0 + 128].rearrange("c p t -> p c t"), yo, reads=[k_r])

    def phase_C(l, xsrc, with_ctx):
        wgb, wgb_r = tile((8, 3072), BF16)
        wba, wba_r = tile((4, 1024), BF16)
        wbs, wbs_r = tile((2, 1024), BF16)
        wbm, wbm_r = tile((2, 1024), BF16)
        wo, wo_r = tile((8, 1024), BF16)
        stg = [tile((8, 512), F32)]
        si = 0
        def wload(dst, dst_r, src, nk, ncols):
            nonlocal si
            for c0 in range(0, ncols, 512):
                st_, st_r = stg[0]
                cast_load(dst[:, :, c0:c0 + 512], dst_r, src[:, c0:c0 + 512].rearrange("(kc p) n -> p kc n", p=128), None,
                          st_[:, 0:nk, :], st_r, eng=("pool" if si % 2 == 0 else "dve"))
                si += 1
        wload(wgb, wgb_r, w_gb[l], 8, 3072)
        wload(wba, wba_r, wb_a[l], 4, 1024)
        wload(wbs, wbs_r, wb_s[l], 2, 1024)
        wload(wbm, wbm_r, wb_m[l], 2, 1024)
        wload(wo, wo_r, w_out[l], 8, 1024)
        xt, xt_r = tile((8, 512), F32)
        sq, _ = tile((8, 512), F32)
        sq_r = [Res() for _ in range(8)]
        rs, rs_r = tile((512,), F32)
        hT, hT_r = tile((8, 512), BF16)
        yat, yat_r = tile((4, 512), BF16)
        yst, yst_r = tile((2, 512), BF16)
        ymt, ymt_r = tile((2, 512), BF16)
        sig, _ = tile((3, 512), F32)
        sig_r = [Res() for _ in range(3)]
        t1, t1_r = tile((512,), F32)
        t2, t2_r = tile((512,), F32)
        yT, yT_r = tile((8, 512), BF16)
        for ti, (t0, n) in enumerate(token_tiles(with_ctx)):
            which = 1 if t0 < CTX else 0
            dma(xt[:, :, 0:n], xsrc[:, :, t0:t0 + n].rearrange("c p t -> p c t"), writes=[xt_r])
            norm_mod(xt, xt_r, n, hT, hT_r, gs1, gs1_r, 0, which, sq, sq_r, rs, rs_r, banks[0], bres[0], l)
            dma(yat[:, :, 0:n], yaT[:, :, t0:t0 + n].rearrange("c p t -> p c t"), writes=[yat_r])
            dma(yst[:, :, 0:n], ysT[:, :, t0:t0 + n].rearrange("c p t -> p c t"), writes=[yst_r])
            dma(ymt[:, :, 0:n], ymT[:, :, t0:t0 + n].rearrange("c p t -> p c t"), writes=[ymt_r])
            for fc in range(8):
                for b in range(3):
                    bk, bk_r = banks[1 + b], bres[1 + b]
                    for kc in range(8):
                        op("pe", lambda e, bk=bk, kc=kc, b=b, fc=fc: e.matmul(
                            bk[:, 0:n], lhsT=wgb[:, kc, b * 1024 + fc * 128:b * 1024 + (fc + 1) * 128], rhs=hT[:, kc, 0:n],
                            start=(kc == 0), stop=(kc == 7)), [wgb_r, hT_r], [bk_r])
                    op("act", lambda e, bk=bk, b=b: e.activation(out=sig[:, b, 0:n], in_=bk[:, 0:n], func=AF.Sigmoid), [bk_r], [sig_r[b]])
                for b, (w_, w_r, y_, y_r, nk) in enumerate(((wba, wba_r, yat, yat_r, 4), (wbs, wbs_r, yst, yst_r, 2), (wbm, wbm_r, ymt, ymt_r, 2))):
                    bk, bk_r = banks[4 + b], bres[4 + b]
                    for kc in range(nk):
                        op("pe", lambda e, bk=bk, kc=kc, w_=w_, y_=y_, fc=fc, nk=nk: e.matmul(
                            bk[:, 0:n], lhsT=w_[:, kc, fc * 128:(fc + 1) * 128], rhs=y_[:, kc, 0:n],
                            start=(kc == 0), stop=(kc == nk - 1)), [w_r, y_r], [bk_r])
                op("dve", lambda e: e.tensor_tensor(out=t1[:, 0:n], in0=banks[4][:, 0:n], in1=sig[:, 0, 0:n], op=ALU.mult), [bres[4], sig_r[0]], [t1_r])
                op("dve", lambda e: e.tensor_tensor(out=t2[:, 0:n], in0=banks[5][:, 0:n], in1=sig[:, 1, 0:n], op=ALU.mult), [bres[5], sig_r[1]], [t2_r])
                op("pool", lambda e: e.tensor_tensor(out=t1[:, 0:n], in0=t1[:, 0:n], in1=t2[:, 0:n], op=ALU.add), [t1_r, t2_r], [t1_r])
                op("dve", lambda e: e.tensor_tensor(out=t2[:, 0:n], in0=banks[6][:, 0:n], in1=sig[:, 2, 0:n], op=ALU.mult), [bres[6], sig_r[2]], [t2_r])
                op("pool", lambda e, fc=fc: e.tensor_tensor(out=yT[:, fc, 0:n], in0=t1[:, 0:n], in1=t2[:, 0:n], op=ALU.add), [t1_r, t2_r], [yT_r])
            for fo in range(8):
                bk, bk_r = banks[1 + fo % 3], bres[1 + fo % 3]
                for kc in range(8):
                    op("pe", lambda e, bk=bk, kc=kc, fo=fo: e.matmul(
                        bk[:, 0:n], lhsT=wo[:, kc, fo * 128:(fo + 1) * 128], rhs=yT[:, kc, 0:n], start=(kc == 0), stop=(kc == 7)),
                       [wo_r, yT_r], [bk_r])
                op("dve", lambda e, bk=bk, fo=fo: e.scalar_tensor_tensor(
                    out=xt[:, fo, 0:n], in0=bk[:, 0:n], scalar=modv[:, l, 16 + fo, which:which + 1], in1=xt[:, fo, 0:n],
                    op0=ALU.mult, op1=ALU.add), [bk_r, modv_r, xt_r], [xt_r])
            dma(xsrc[:, :, t0:t0 + n].rearrange("c p t -> p c t"), xt[:, :, 0:n], reads=[xt_r])

    def phase_D(l, xsrc, xdst, with_ctx):
        wu, wu_r = tile((8, 2 * D_FF), BF16)
        wd, wd_r = tile((22, 1024), BF16)
        stg, stg_r = tile((8, 256), F32)
        for c0 in range(0, 2 * D_FF, 256):
            cast_load(wu[:, :, c0:c0 + 256], wu_r, w_up[l, :, c0:c0 + 256].rearrange("(kc p) n -> p kc n", p=128), None,
                      stg, stg_r, eng=("pool" if (c0 // 256) % 2 == 0 else "dve"))
        for k0 in range(0, 22, 2):
            for c0 in range(0, 1024, 1024):
                cast_load(wd[:, k0:k0 + 2, :], wd_r, w_dn[l, k0 * 128:(k0 + 2) * 128, :].rearrange("(kc p) n -> p kc n", p=128), None,
                          stg.rearrange("p a b -> p (a b)")[:, 0:2048].rearrange("p (a b) -> p a b", a=2), stg_r,
                          eng=("pool" if (k0 // 2) % 2 == 0 else "dve"))
        cwt, cwt_r = tile((3, 44), F32)
        cbt, cbt_r = tile((44,), F32)
        dma(cwt, cw[l], writes=[cwt_r])
        dma(cbt, cb[l], writes=[cbt_r])
        xt, xt_r = tile((8, 258), F32)
        sq, _ = tile((8, 258), F32)
        sq_r = [Res() for _ in range(8)]
        rs, rs_r = tile((258,), F32)
        hT, hT_r = tile((8, 258), BF16)
        gb_, gb_r = tile((22, 256), BF16)
        tas = [tile((256,), F32) for _ in range(2)]
        tvs = [tile((256,), F32) for _ in range(2)]
        sas = [tile((256,), F32) for _ in range(2)]
        tl = []
        if with_ctx:
            tl.append((0, 0, CTX))
        for t0 in range(CTX, NT, 256):
            tl.append((t0, CTX, NT))
        for (t0, seg0, seg1) in tl:
            which = 1 if t0 < CTX else 0
            lo, hi = t0 - 1, t0 + 257
            zc = []
            clo, chi = max(lo, seg0), min(hi, seg1)
            if lo < seg0:
                op("pool", lambda e: e.memset(xt[:, :, 0:1], 1.0), [], [xt_r])
                zc.append(0)
            if hi > seg1:
                op("pool", lambda e: e.memset(xt[:, :, 257:258], 1.0), [], [xt_r])
                zc.append(257)
            dma(xt[:, :, clo - lo:chi - lo], xsrc[:, :, clo:chi].rearrange("c p t -> p c t"), writes=[xt_r])
            norm_mod(xt, xt_r, 258, hT, hT_r, gs2, gs2_r, 3, which, sq, sq_r, rs, rs_r, banks[0], bres[0], l, zero_cols=zc)
            for j in range(22):
                ta, ta_r = tas[j % 2]
                tv, tv_r = tvs[j % 2]
                sa, sa_r = sas[j % 2]
                for (ch, bk, bk_r, tt, tt_r) in ((j, banks[1 + (j % 3) * 2], bres[1 + (j % 3) * 2], ta, ta_r),
                                                 (22 + j, banks[2 + (j % 3) * 2], bres[2 + (j % 3) * 2], tv, tv_r)):
                    for kc in range(8):
                        op("pe", lambda e, bk=bk, kc=kc, ch=ch: e.matmul(
                            bk[:, 0:258], lhsT=wu[:, kc, ch * 128:(ch + 1) * 128], rhs=hT[:, kc, :], start=(kc == 0), stop=(kc == 7)),
                           [wu_r, hT_r], [bk_r])
                    op("act", lambda e, bk=bk, ch=ch, tt=tt: e.activation(out=tt, in_=bk[:, 0:256], func=AF.Identity,
                                                                          bias=cbt[:, ch:ch + 1], scale=cwt[:, 0, ch:ch + 1]),
                       [bk_r, cbt_r, cwt_r], [tt_r])
                    op("dve", lambda e, bk=bk, ch=ch, tt=tt: e.scalar_tensor_tensor(
                        out=tt, in0=bk[:, 1:257], scalar=cwt[:, 1, ch:ch + 1], in1=tt, op0=ALU.mult, op1=ALU.add), [bk_r, cwt_r, tt_r], [tt_r])
                    op("dve", lambda e, bk=bk, ch=ch, tt=tt: e.scalar_tensor_tensor(
                        out=tt, in0=bk[:, 2:258], scalar=cwt[:, 2, ch:ch + 1], in1=tt, op0=ALU.mult, op1=ALU.add), [bk_r, cwt_r, tt_r], [tt_r])
                op("act", lambda e, sa=sa, ta=ta: e.activation(out=sa, in_=ta, func=AF.Silu), [ta_r], [sa_r])
                op("pool", lambda e, j=j, sa=sa, tv=tv: e.tensor_tensor(out=gb_[:, j, :], in0=sa, in1=tv, op=ALU.mult), [sa_r, tv_r], [gb_r])
            for fo in range(8):
                bk, bk_r = banks[7 if fo % 2 == 0 else 0], bres[7 if fo % 2 == 0 else 0]
                for kc in range(22):
                    op("pe", lambda e, bk=bk, kc=kc, fo=fo: e.matmul(
                        bk[:, 0:256], lhsT=wd[:, kc, fo * 128:(fo + 1) * 128], rhs=gb_[:, kc, :], start=(kc == 0), stop=(kc == 21)),
                       [wd_r, gb_r], [bk_r])
                op("dve", lambda e, bk=bk, fo=fo: e.scalar_tensor_tensor(
                    out=xt[:, fo, 1:257], in0=bk[:, 0:256], scalar=modv[:, l, 40 + fo, which:which + 1], in1=xt[:, fo, 1:257],
                    op0=ALU.mult, op1=ALU.add), [bk_r, modv_r, xt_r], [xt_r])
            dma(xdst[:, :, t0:t0 + 256].rearrange("c p t -> p c t"), xt[:, :, 1:257], reads=[xt_r])

    def phase_E(xsrc):
        xt, xt_r = tile((8, 512), F32)
        sq, _ = tile((8, 512), F32)
        sq_r = [Res() for _ in range(8)]
        rs, rs_r = tile((512,), F32)
        ots = [tile((1024,), F32) for _ in range(2)]
        fins = []
        bi = 0
        for (t0, n) in token_tiles(False):
            dma(xt[:, :, 0:n], xsrc[:, :, t0:t0 + n].rearrange("c p t -> p c t"), writes=[xt_r])
            for c in range(8):
                if c % 2 == 0:
                    op("act", lambda e, c=c: e.activation(out=sq[:, c, 0:n], in_=xt[:, c, 0:n], func=AF.Square), [xt_r], [sq_r[c]])
                else:
                    op("pool", lambda e, c=c: e.tensor_tensor(out=sq[:, c, 0:n], in0=xt[:, c, 0:n], in1=xt[:, c, 0:n], op=ALU.mult), [xt_r], [sq_r[c]])
            for c in range(8):
                op("pe", lambda e, c=c: e.matmul(banks[0][:, 0:n], lhsT=ones_f, rhs=sq[:, c, 0:n], start=(c == 0), stop=(c == 7)),
                   [sq_r[c], ones_r], [bres[0]])
            op("act", lambda e: e.activation(out=rs[:, 0:n], in_=banks[0][:, 0:n], func=AF.Sqrt, bias=EPS, scale=1.0 / D), [bres[0]], [rs_r])
            op("dve", lambda e: e.reciprocal(out=rs[:, 0:n], in_=rs[:, 0:n]), [rs_r], [rs_r])
            for c in range(8):
                op("dve", lambda e, c=c: e.scalar_tensor_tensor(out=sq[:, c, 0:n], in0=xt[:, c, 0:n], scalar=fnt[:, c:c + 1], in1=rs[:, 0:n],
                                                               op0=ALU.mult, op1=ALU.mult), [xt_r, rs_r, fnt_r], [sq_r[c]])
            for sb in range(n // 128):
                ot, ot_r = ots[bi % 2]
                for half in range(2):
                    bk, bk_r = banks[1 + (bi * 2 + half) % 4], bres[1 + (bi * 2 + half) % 4]
                    for j in range(4):
                        c = half * 4 + j
                        op("pe", lambda e, bk=bk, j=j, c=c, sb=sb: e.transpose(bk[:, j * 128:(j + 1) * 128], sq[:, c, sb * 128:(sb + 1) * 128], ident),
                           [sq_r[c], ident_r], [bk_r])
                    if half == 0:
                        op("act", lambda e, bk=bk, ot=ot: e.copy(out=ot[:, 0:512], in_=bk[:, 0:512]), [bk_r], [ot_r])
                    else:
                        op("dve", lambda e, bk=bk, ot=ot: e.tensor_copy(out=ot[:, 512:1024], in_=bk[:, 0:512]), [bk_r], [ot_r])
                bi += 1
                tt = t0 - CTX + sb * 128
                fins.append(dma(y_out[tt:tt + 128, :], ot, reads=[ot_r]))
        return fins

    def phase_s5(l, with_ctx):
        NB, NCH, G = NT // 8, NT // 128, 16
        INV2PI = 1.0 / TWO_PI
        cols, cols_r = tile((4,), F32)
        dma(cols, s5_cols, writes=[cols_r])
        mphi, mphi_r = tile((2, 136), F32)
        dma(mphi.rearrange("p a b -> p (a b)"), s5_mphi.partition_broadcast(128), writes=[mphi_r])
        mpsi, mpsi_r = tile((2, 136), F32)
        dma(mpsi.rearrange("p a b -> p (a b)"), s5_mpsi.partition_broadcast(128), writes=[mpsi_r])
        mphs, mphs_r = tile((2, 16), F32)
        dma(mphs.rearrange("p a b -> p (a b)"), s5_mphs.partition_broadcast(128), writes=[mphs_r])
        at, at_r = tile((2, 16, 2), F32)
        dma(at.rearrange("p a b c -> p (a b c)"), s5_a[l], writes=[at_r])
        ldt, ldt_r = tile((2, 16), F32)
        dma(ldt.rearrange("p a b -> p (a b)"), s5_logdt[l].partition_broadcast(128), writes=[ldt_r])
        bt, bt_r = tile((16, 16, 2), F32)
        dma(bt.rearrange("p a b c -> p (a b c)"), s5_b[l], writes=[bt_r])
        ct, ct_r = tile((16, 16, 2), F32)
        dma(ct.rearrange("p a b c -> p (a b c)"), s5_c[l], writes=[ct_r])
        dcol, dcol_r = tile((16,), F32)
        dma(dcol, s5_dcol[l], writes=[dcol_r])
        m0, m0_r = tile((2, 128), F32)
        dma(m0, s5_m0.rearrange("d k m -> k d m"), writes=[m0_r])
        swp, swp_r = tile((128,), F32)
        dma(swp, s5_swap, writes=[swp_r])
        gst, gst_r = tile((2, 256), F32)
        gw, gw_r = tile((2, 256), BF16)
        cast_load(gw, gw_r, s5_glu_w[l].rearrange("(kc p) n -> p kc n", p=128), None, gst, gst_r, eng="dve")
        glb, glb_r = tile((2,), F32)
        dma(glb, s5_glu_b[l], writes=[glb_r])
        sel, sel_r = tile((64, 128), BF16)
        U, _ = tile((G, NB), BF16)
        NBG = (NCH + 31) // 32
        bgs = [(b * 32, min(32, NCH - b * 32)) for b in range(NBG)]
        U_r = [[Res() for _ in bgs] for _ in range(G)]

        small = lambda: tile((2, 16), F32)
        dts, dts_r = small()
        op("act", lambda e: e.activation(out=dts, in_=ldt, func=AF.Exp), [ldt_r], [dts_r])
        dre, dre_r = small()
        dim, dim_r = small()
        op("dve", lambda e: e.tensor_tensor(out=dre, in0=dts, in1=at[:, :, :, 0], op=ALU.mult), [dts_r, at_r], [dre_r])
        op("dve", lambda e: e.tensor_tensor(out=dim, in0=dts, in1=at[:, :, :, 1], op=ALU.mult), [dts_r, at_r], [dim_r])
        tI, tI_r = tile((2, 16), I32)
        tF, tF_r = small()

        def sin_red(dst, dst_r, ang, ang_r, tI, tI_r, tF, tF_r):
            op("dve", lambda e: e.tensor_scalar(out=tI, in0=ang, scalar1=INV2PI, scalar2=None, op0=ALU.mult), [ang_r], [tI_r])
            op("dve", lambda e: e.tensor_copy(out=tF, in_=tI), [tI_r], [tF_r])
            op("dve", lambda e: e.scalar_tensor_tensor(out=tF, in0=tF, scalar=-TWO_PI, in1=ang, op0=ALU.mult, op1=ALU.add),
               [tF_r, ang_r], [tF_r])
            op("act", lambda e: e.activation(out=dst, in_=tF, func=AF.Sin), [tF_r], [dst_r])

        def lam_pow(kpow):
            a1, a1_r = small()
            a2, a2_r = small()
            mg, mg_r = small()
            sr, sr_r = small()
            ci_, ci_r = small()
            op("dve", lambda e: e.tensor_scalar(out=a1, in0=dim, scalar1=float(kpow), scalar2=None, op0=ALU.mult), [dim_r], [a1_r])
            op("dve", lambda e: e.tensor_scalar(out=a2, in0=dim, scalar1=float(kpow), scalar2=math.pi / 2, op0=ALU.mult, op1=ALU.add),
               [dim_r], [a2_r])
            sin_red(sr, sr_r, a1, a1_r, tI, tI_r, tF, tF_r)
            sin_red(ci_, ci_r, a2, a2_r, tI, tI_r, tF, tF_r)
            op("act", lambda e: e.activation(out=mg, in_=dre, func=AF.Exp, scale=float(kpow)), [dre_r], [mg_r])
            op("dve", lambda e: e.tensor_tensor(out=sr, in0=sr, in1=mg, op=ALU.mult), [sr_r, mg_r], [sr_r])
            op("dve", lambda e: e.tensor_tensor(out=ci_, in0=ci_, in1=mg, op=ALU.mult), [ci_r, mg_r], [ci_r])
            return (ci_, ci_r), (sr, sr_r)

        (lr, lr_r), (li, li_r) = lam_pow(1)
        (l128r, l128r_r), (l128i, l128i_r) = lam_pow(128)
        ar, ai = at[:, :, :, 0], at[:, :, :, 1]
        den, den_r = small()
        t1, t1_r = small()
        t2, t2_r = small()
        cr, cr_r = small()
        ci, ci_r = small()
        op("dve", lambda e: e.tensor_tensor(out=den, in0=ar, in1=ar, op=ALU.mult), [at_r], [den_r])
        op("dve", lambda e: e.tensor_tensor(out=t1, in0=ai, in1=ai, op=ALU.mult), [at_r], [t1_r])
        op("dve", lambda e: e.tensor_tensor(out=den, in0=den, in1=t1, op=ALU.add), [den_r, t1_r], [den_r])
        op("dve", lambda e: e.reciprocal(out=den, in_=den), [den_r], [den_r])
        op("dve", lambda e: e.tensor_scalar_add(out=lr, in0=lr, scalar1=-1.0), [lr_r], [lr_r])
        op("dve", lambda e: e.tensor_tensor(out=t1, in0=lr, in1=ar, op=ALU.mult), [lr_r, at_r], [t1_r])
        op("dve", lambda e: e.tensor_tensor(out=t2, in0=li, in1=ai, op=ALU.mult), [li_r, at_r], [t2_r])
        op("dve", lambda e: e.tensor_tensor(out=t1, in0=t1, in1=t2, op=ALU.add), [t1_r, t2_r], [t1_r])
        op("dve", lambda e: e.tensor_tensor(out=cr, in0=t1, in1=den, op=ALU.mult), [t1_r, den_r], [cr_r])
        op("dve", lambda e: e.tensor_tensor(out=t1, in0=li, in1=ar, op=ALU.mult), [li_r, at_r], [t1_r])
        op("dve", lambda e: e.tensor_tensor(out=t2, in0=lr, in1=ai, op=ALU.mult), [lr_r, at_r], [t2_r])
        op("dve", lambda e: e.tensor_tensor(out=t1, in0=t1, in1=t2, op=ALU.subtract), [t1_r, t2_r], [t1_r])
        op("dve", lambda e: e.tensor_tensor(out=ci, in0=t1, in1=den, op=ALU.mult), [t1_r, den_r], [ci_r])
        br, bi = bt[:, :, :, 0], bt[:, :, :, 1]
        BA, BA_r = tile((2, 16, 16), F32)
        BB, BB_r = tile((2, 16, 16), F32)
        w1, w1_r = tile((16, 16), F32)
        w2, w2_r = tile((16, 16), F32)
        for d in range(2):
            crb = cr[:, d, :].unsqueeze(2).to_broadcast([128, 16, 16])
            cib = ci[:, d, :].unsqueeze(2).to_broadcast([128, 16, 16])
            op("dve", lambda e: e.tensor_tensor(out=w1, in0=br, in1=crb, op=ALU.mult), [bt_r, cr_r], [w1_r])
            op("dve", lambda e: e.tensor_tensor(out=w2, in0=bi, in1=cib, op=ALU.mult), [bt_r, ci_r], [w2_r])
            op("dve", lambda e: e.tensor_tensor(out=BA[:, d], in0=w1, in1=w2, op=ALU.subtract), [w1_r, w2_r], [BA_r])
            op("dve", lambda e: e.tensor_tensor(out=w1, in0=bi, in1=crb, op=ALU.mult), [bt_r, cr_r], [w1_r])
            op("dve", lambda e: e.tensor_tensor(out=w2, in0=br, in1=cib, op=ALU.mult), [bt_r, ci_r], [w2_r])
            op("dve", lambda e: e.tensor_tensor(out=w1, in0=w1, in1=w2, op=ALU.add), [w1_r, w2_r], [w1_r])
            op("dve", lambda e: e.tensor_scalar(out=BB[:, d], in0=w1, scalar1=cols[:, 2:3], scalar2=None, op0=ALU.mult), [w1_r, cols_r], [BB_r])
        CA, CA_r = tile((16, 16), F32)
        CB, CB_r = tile((16, 16), F32)
        op("dve", lambda e: e.tensor_scalar(out=CA, in0=ct[:, :, :, 0], scalar1=cols[:, 3:4], scalar2=None, op0=ALU.mult), [ct_r, cols_r], [CA_r])
        op("dve", lambda e: e.tensor_scalar(out=CB, in0=ct[:, :, :, 1], scalar1=-1.0, scalar2=None, op0=ALU.mult), [ct_r], [CB_r])
        ais, ais_r = small()
        op("dve", lambda e: e.tensor_scalar(out=ais, in0=l128i, scalar1=cols[:, 3:4], scalar2=None, op0=ALU.mult), [l128i_r, cols_r], [ais_r])

        HG = 8
        pa, pa_r = tile((HG, 136), F32)
        pI, pI_r = tile((HG, 136), I32)
        pF, pF_r = tile((HG, 136), F32)
        pm, pm_r = tile((HG, 136), F32)

        def pw_tables(d, g0, mt, mt_r, n, outA, outA_r, outB, outB_r):
            mtb = mt.unsqueeze(1).to_broadcast([128, HG, n])
            dimb = dim[:, d, g0:g0 + HG].unsqueeze(2).to_broadcast([128, HG, n])
            dreb = dre[:, d, g0:g0 + HG].unsqueeze(2).to_broadcast([128, HG, n])
            pa_, pI_, pF_, pm_ = pa[:, :, 0:n], pI[:, :, 0:n], pF[:, :, 0:n], pm[:, :, 0:n]
            op("dve", lambda e: e.tensor_tensor(out=pm_, in0=mtb, in1=dreb, op=ALU.mult), [mt_r, dre_r], [pm_r])
            op("act", lambda e: e.activation(out=pm_, in_=pm_, func=AF.Exp), [pm_r], [pm_r])
            for (o_, o_r, phc) in ((outA, outA_r, 0), (outB, outB_r, 1)):
                op("dve", lambda e: e.tensor_tensor(out=pa_, in0=mtb, in1=dimb, op=ALU.mult), [mt_r, dim_r], [pa_r])
                op("dve", lambda e: e.tensor_scalar(out=pa_, in0=pa_, scalar1=cols[:, phc:phc + 1], scalar2=None, op0=ALU.add), [pa_r, cols_r], [pa_r])
                sin_red(o_, o_r, pa_, pa_r, pI_, pI_r, pF_, pF_r)
                op("pool", lambda e: e.tensor_tensor(out=o_, in0=o_, in1=pm_, op=ALU.mult), [o_r, pm_r], [o_r])

        oa, oa_r = tile((136, 16), F32)
        aw, aw_r = oa.rearrange("p a b -> p (a b)")[:, 0:G * 128].rearrange("p (a b) -> p a b", a=G), oa_r
        ob_, ob_r = tile((136, 16), F32)

        def outer(dst, dst_r, PA, PA_r, PB, PB_r, gi, XA, XA_r, XB, XB_r, n):
            pab = PA[:, gi, :].unsqueeze(2).to_broadcast([128, n, 16])
            pbb = PB[:, gi, :].unsqueeze(2).to_broadcast([128, n, 16])
            xab = XA.unsqueeze(1).to_broadcast([128, n, 16])
            xbb = XB.unsqueeze(1).to_broadcast([128, n, 16])
            op("dve", lambda e: e.tensor_tensor(out=oa[:, 0:n, :], in0=pab, in1=xab, op=ALU.mult), [PA_r, XA_r], [oa_r])
            op("pool", lambda e: e.tensor_tensor(out=ob_[:, 0:n, :], in0=pbb, in1=xbb, op=ALU.mult), [PB_r, XB_r], [ob_r])
            op("dve", lambda e: e.tensor_tensor(out=dst, in0=oa[:, 0:n, :], in1=ob_[:, 0:n, :], op=ALU.add), [oa_r, ob_r], [dst_r])

        dma(sel, s5_sel, writes=[sel_r])
        ust, ust_r = tile((2, 4096), BF16)
        cgs = [(c0, min(512, NB - c0)) for c0 in range(0, NB, 512)]
        bg_of_col = lambda c0: c0 // 512
        for (c0, nb) in cgs:
            dma(ust[:, :, 0:nb * 8], usT[:, :, c0 * 8:(c0 + nb) * 8].rearrange("c p t -> p c t"), writes=[ust_r])
            for g in range(G):
                hf, gl = g // 8, g % 8
                bk, bk_r = banks[g % 2], bres[g % 2]
                uv = ust[:, hf, 0:nb * 8].rearrange("p (tb s) -> p tb s", s=8)
                for s8 in range(8):
                    op("pe", lambda e: e.matmul(bk[:, 0:nb], lhsT=sel[:, gl * 8 + s8, :], rhs=uv[:, :, s8], start=(s8 == 0), stop=(s8 == 7)),
                       [sel_r, ust_r], [bk_r])
                if g % 2 == 0:
                    op("act", lambda e: e.copy(out=U[:, g, c0:c0 + nb], in_=bk[:, 0:nb]), [bk_r], [U_r[g][bg_of_col(c0)]])
                else:
                    op("dve", lambda e: e.tensor_copy(out=U[:, g, c0:c0 + nb], in_=bk[:, 0:nb]), [bk_r], [U_r[g][bg_of_col(c0)]])
        dma(sel, s5_selT, writes=[sel_r])

        Eall, Eall_r = tile((2, G, NCH), F32)
        Zst, Zst_r = tile((G, NCH), F32)
        Zin, Zin_r = tile((2, G, NCH), BF16)
        op("pool", lambda e: e.memset(Zin, 0.0), [], [Zin_r])
        A128, A128_r = tile((G, 128), F32)
        PhA, PhA_r = tile((HG, 136), F32)
        PhB, PhB_r = tile((HG, 136), F32)
        Phi, Phi_r = tile((136, 16), BF16)
        Wt, Wt_r = tile((16, 128), BF16)
        for d in range(2):
            for g0 in range(0, G, HG):
                pw_tables(d, g0, mphi[:, d, :], mphi_r, 136, PhA, PhA_r, PhB, PhB_r)
                for gi in range(HG):
                    g = g0 + gi
                    outer(Phi, Phi_r, PhA, PhA_r, PhB, PhB_r, gi, BA[:, d, g], BA_r, BB[:, d, g], BB_r, 136)
                    woff = 0 if d == 0 else 8
                    for hb in range(2):
                        bk, bk_r = banks[1 + hb], bres[1 + hb]
                        bkv = bk[:, 0:512].bitcast(BF16)
                        for jj in range(8):
                            j = hb * 8 + jj
                            src = Phi[:, woff + 8 * j:woff + 8 * j + 8, :].rearrange("p a b -> p (a b)")
                            op("pe", lambda e: e.transpose(bkv[:, jj * 128:(jj + 1) * 128], src, identb), [Phi_r, identb_r], [bk_r])
                        dstw = Wt[:, hb * 8:(hb + 1) * 8, :]
                        srcw = bkv.rearrange("p (a b) -> p a b", a=8)
                        if hb == 0:
                            op("act", lambda e: e.copy(out=dstw, in_=srcw), [bk_r], [Wt_r])
                        else:
                            op("dve", lambda e: e.tensor_copy(out=dstw, in_=srcw), [bk_r], [Wt_r])
                    eb, eb_r = banks[3], bres[3]
                    ucv = U[:, g, :].rearrange("p (ch j) -> p ch j", j=16)
                    for j in range(16):
                        op("pe", lambda e: e.matmul(eb[:, 0:NCH], lhsT=Wt[:, j, :], rhs=ucv[:, :, j], start=(j == 0), stop=(j == 15)),
                           [Wt_r] + U_r[g], [eb_r])
                    op("act", lambda e: e.copy(out=Eall[:, d, g, :], in_=eb[:, 0:NCH]), [eb_r], [Eall_r])
            idb = ident.unsqueeze(1).to_broadcast([128, G, 128])
            swb = swp.unsqueeze(1).to_broadcast([128, G, 128])
            arb = l128r[:, d, :].unsqueeze(2).to_broadcast([128, G, 128])
            aib = ais[:, d, :].unsqueeze(2).to_broadcast([128, G, 128])
            op("dve", lambda e: e.tensor_tensor(out=A128, in0=idb, in1=arb, op=ALU.mult), [ident_r, l128r_r], [A128_r])
            op("pool", lambda e: e.tensor_tensor(out=aw, in0=swb, in1=aib, op=ALU.mult), [swp_r, ais_r], [aw_r])
            op("dve", lambda e: e.tensor_tensor(out=A128, in0=A128, in1=aw, op=ALU.add), [A128_r, aw_r], [A128_r])
            order = list(range(NCH)) if d == 0 else [1, 0] + list(range(NCH - 1, 1, -1))
            prev = None
            zb, zb_r = banks[4], bres[4]
            for ch in order:
                if prev is None:
                    op("dve", lambda e: e.tensor_copy(out=Zst[:, :, ch], in_=Eall[:, d, :, ch]), [Eall_r], [Zst_r])
                else:
                    for g in range(G):
                        op("pe", lambda e: e.matmul(zb[:, g:g + 1], lhsT=A128[:, g, :], rhs=Zst[:, g, prev:prev + 1], start=True, stop=True),
                           [A128_r, Zst_r], [zb_r])
                    op("dve", lambda e: e.tensor_tensor(out=Zst[:, :, ch], in0=zb[:, 0:G], in1=Eall[:, d, :, ch], op=ALU.add),
                       [zb_r, Eall_r], [Zst_r])
                    op("act", lambda e: e.copy(out=Zin[:, d, :, ch], in_=Zst[:, :, prev]), [Zst_r], [Zin_r])
                prev = ch

        PsA = [(PhA, PhA_r), tile((HG, 136), F32)]
        PsB = [(PhB, PhB_r), tile((HG, 136), F32)]
        PsmA = [tile((HG, 16), F32) for _ in range(2)]
        PsmB = [tile((HG, 16), F32) for _ in range(2)]
        Rt = [(Phi, Phi_r), tile((136, 16), BF16)]
        Lt = [tile((16, 16), BF16) for _ in range(2)]
        Tt = [(Wt, Wt_r), tile((16, 128), BF16)]
        yv, yv_r = tile((512,), F32)
        y2, y2_r = tile((512,), F32)
        y3, y3_r = tile((512,), F32)
        for g0 in range(0, G, HG):
            for d in range(2):
                pw_tables(d, g0, mpsi[:, d, :], mpsi_r, 136, PsA[d][0], PsA[d][1], PsB[d][0], PsB[d][1])
                pw_tables(d, g0, mphs[:, d, :], mphs_r, 16, PsmA[d][0], PsmA[d][1], PsmB[d][0], PsmB[d][1])
            for gi in range(HG):
                g = g0 + gi
                for d in range(2):
                    R, R_r = Rt[d]
                    L, L_r = Lt[d]
                    Tt_, Tt_r = Tt[d]
                    outer(R, R_r, PsA[d][0], PsA[d][1], PsB[d][0], PsB[d][1], gi, CA[:, g], CA_r, CB[:, g], CB_r, 136)
                    outer(L, L_r, PsmA[d][0], PsmA[d][1], PsmB[d][0], PsmB[d][1], gi, BA[:, d, g], BA_r, BB[:, d, g], BB_r, 16)
                    for q4 in range(4):
                        tb_, tb_r = banks[4], bres[4]
                        for dd in range(4):
                            Dl = q4 * 4 + dd
                            lh = L[:, 0:8, :] if Dl == 0 else L[:, 8:16, :]
                            if d == 0:
                                r0 = 0 if Dl == 0 else 8 * (Dl - 1) + 1
                            else:
                                r0 = 0 if Dl == 0 else 8 * (Dl - 1)
                            op("pe", lambda e: e.matmul(tb_[:, dd * 128:(dd + 1) * 128], lhsT=lh.rearrange("p a b -> p (a b)"),
                                                        rhs=R[:, r0:r0 + 8, :].rearrange("p a b -> p (a b)"), start=True, stop=True),
                               [L_r, R_r], [tb_r])
                        if q4 == 0:
                            op("dve", lambda e: e.tensor_tensor(out=y3[:, 0:128], in0=tb_[:, 0:128], in1=m0[:, d, :], op=ALU.mult), [tb_r, m0_r], [y3_r])
                            if d == 0:
                                op("dve", lambda e: e.scalar_tensor_tensor(out=Tt_[:, 0, :], in0=ident, scalar=dcol[:, g:g + 1], in1=y3[:, 0:128],
                                                                          op0=ALU.mult, op1=ALU.add), [y3_r, ident_r, dcol_r], [Tt_r])
                            else:
                                op("dve", lambda e: e.tensor_copy(out=Tt_[:, 0, :], in_=y3[:, 0:128]), [y3_r], [Tt_r])
                            op("act", lambda e: e.copy(out=Tt_[:, 1:4, :], in_=tb_[:, 128:512].rearrange("p (a b) -> p a b", a=3)), [tb_r], [Tt_r])
                        else:
                            op("act", lambda e: e.copy(out=Tt_[:, q4 * 4:q4 * 4 + 4, :], in_=tb_[:, 0:512].rearrange("p (a b) -> p a b", a=4)), [tb_r], [Tt_r])
                for bgi, (ch0, nch) in enumerate(bgs):
                    yb, yb_r = banks[5 + (g * NBG + bgi) % 2], bres[5 + (g * NBG + bgi) % 2]
                    ncol = nch * 16
                    ybv = yb[:, 0:ncol].rearrange("p (ch j) -> p ch j", j=16)
                    ucv = U[:, g, ch0 * 16:ch0 * 16 + ncol].rearrange("p (ch j) -> p ch j", j=16)
                    first = True
                    for d in range(2):
                        R, R_r = Rt[d]
                        Tt_, Tt_r = Tt[d]
                        for Dl in range(16):
                            if d == 0:
                                o_, r_ = ybv[:, :, Dl:16], ucv[:, :, 0:16 - Dl]
                            else:
                                o_, r_ = ybv[:, :, 0:16 - Dl], ucv[:, :, Dl:16]
                            op("pe", lambda e: e.matmul(o_, lhsT=Tt_[:, Dl, :], rhs=r_, start=first, stop=False), [Tt_r, U_r[g][bgi]], [yb_r])
                            first = False
                        for j in range(16):
                            r0 = 8 * j + 1 if d == 0 else 8 * (15 - j)
                            last = (d == 1 and j == 15)
                            op("pe", lambda e: e.matmul(ybv[:, :, j], lhsT=R[:, r0:r0 + 8, :].rearrange("p a b -> p (a b)"),
                                                        rhs=Zin[:, d, g, ch0:ch0 + nch], start=False, stop=last), [R_r, Zin_r], [yb_r])
                    n_ = ncol
                    op("act", lambda e: e.copy(out=yv[:, 0:n_], in_=yb[:, 0:n_]), [yb_r], [yv_r])
                    op("pool", lambda e: e.tensor_tensor(out=y2[:, 0:n_], in0=yv[:, 0:n_], in1=yv[:, 0:n_], op=ALU.mult), [yv_r], [y2_r])
                    op("dve", lambda e: e.tensor_scalar(out=y2[:, 0:n_], in0=y2[:, 0:n_], scalar1=0.044715, scalar2=1.0, op0=ALU.mult, op1=ALU.add),
                       [y2_r], [y2_r])
                    op("pool", lambda e: e.tensor_tensor(out=y2[:, 0:n_], in0=y2[:, 0:n_], in1=yv[:, 0:n_], op=ALU.mult), [y2_r, yv_r], [y2_r])
                    op("act", lambda e: e.activation(out=y2[:, 0:n_], in_=y2[:, 0:n_], func=AF.Sigmoid, scale=1.5957691216057308), [y2_r], [y2_r])
                    op("dve", lambda e: e.tensor_tensor(out=U[:, g, ch0 * 16:ch0 * 16 + n_], in0=yv[:, 0:n_], in1=y2[:, 0:n_], op=ALU.mult),
                       [yv_r, y2_r], [U_r[g][bgi]])

        yfm, yfm_r = ust, ust_r
        yo, yo_r = tile((2, 512), BF16)
        sg, sg_r = tile((512,), F32)
        for (c0, nb) in cgs:
            bgi = bg_of_col(c0)
            for hf in range(2):
                yfv = yfm[:, hf, 0:nb * 8].rearrange("p (tb s) -> p tb s", s=8)
                for t8 in range(8):
                    bk, bk_r = banks[t8 % 2], bres[t8 % 2]
                    for gl in range(8):
                        g = hf * 8 + gl
                        op("pe", lambda e: e.matmul(bk[:, 0:nb], lhsT=sel[:, gl * 8 + t8, :], rhs=U[:, g, c0:c0 + nb], start=(gl == 0), stop=(gl == 7)),
                           [sel_r, U_r[g][bgi]], [bk_r])
                    if t8 % 2 == 0:
                        op("act", lambda e: e.copy(out=yfv[:, :, t8], in_=bk[:, 0:nb]), [bk_r], [yfm_r])
                    else:
                        op("dve", lambda e: e.tensor_copy(out=yfv[:, :, t8], in_=bk[:, 0:nb]), [bk_r], [yfm_r])
            ntok = nb * 8
            for tt in range(0, ntok, 512):
                n_ = min(512, ntok - tt)
                tglob = c0 * 8 + tt
                for fc in range(2):
                    bk, bk_r = banks[7], bres[7]
                    for kc in range(2):
                        op("pe", lambda e: e.matmul(bk[:, 0:n_], lhsT=gw[:, kc, fc * 128:(fc + 1) * 128], rhs=yfm[:, kc, tt:tt + n_],
                                                    start=(kc == 0), stop=(kc == 1)), [gw_r, yfm_r], [bk_r])
                    op("act", lambda e: e.activation(out=sg[:, 0:n_], in_=bk[:, 0:n_], func=AF.Sigmoid, bias=glb[:, fc:fc + 1], scale=1.0),
                       [bk_r, glb_r], [sg_r])
                    op("dve", lambda e: e.tensor_tensor(out=yo[:, fc, 0:n_], in0=yfm[:, fc, tt:tt + n_], in1=sg[:, 0:n_], op=ALU.mult),
                       [yfm_r, sg_r], [yo_r])
                dma(ysT[:, :, tglob:tglob + n_].rearrange("c p t -> p c t"), yo[:, :, 0:n_], reads=[yo_r])

    def phase_s5_stub(l, with_ctx):
        z, z_r = tile((2, 512), BF16)
        op("pool", lambda e: e.memset(z, 0.0), [], [z_r])
        for (t0, n) in token_tiles(True):
            dma(ysT[:, :, t0:t0 + n].rearrange("c p t -> p c t"), z[:, :, 0:n], reads=[z_r])

    fins = []
    steps = [("mod", lambda: phase_mod()), ("tin", lambda: phase_transpose_in())]
    cur = 0
    for l in range(depth):
        with_ctx = l < depth - 1
        steps.append(("A%d" % l, lambda l=l, cur=cur: phase_A(l, xT[cur])))
        steps.append(("attn%d" % l, lambda l=l, w=with_ctx: phase_attn(l, w)))
        steps.append(("ml%d" % l, lambda l=l, w=with_ctx: phase_mlstm(l, w)))
        steps.append(("s5%d" % l, lambda l=l, w=with_ctx: (phase_s5 if HAVE_S5 else phase_s5_stub)(l, w)))
        steps.append(("C%d" % l, lambda l=l, cur=cur, w=with_ctx: phase_C(l, xT[cur], w)))
        steps.append(("D%d" % l, lambda l=l, cur=cur, w=with_ctx: phase_D(l, xT[cur], xT[1 - cur], w)))
        cur = 1 - cur
    steps.append(("E", lambda cur=cur: fins.extend(phase_E(xT[cur]))))
    for i, (nm, fn) in enumerate(steps):
        if upto is not None and nm == upto:
            break
        if i > 0:
            new_phase()
        fn()
    stats = S.emit(final_ops=fins)
    es.close()
    return nc, dbg_outs, stats


HAVE_S5 = True
import os
MLCUT = int(os.environ.get('MLCUT', '0'))
MLDIRS = int(os.environ.get('MLDIRS', '2'))


def make_in_maps(inp, T, depth=2, ncores=8):
    fm, tm, gb = _col_layout()
    cst = _consts(T)
    f = lambda a: np.ascontiguousarray(a, dtype=np.float32)
    shared = {}
    shared["mod_w"] = f(inp["mod_w"])
    shared["mod_b"] = f(inp["mod_b"].reshape(depth, 48, 128).transpose(0, 2, 1))
    shared["norm1_g"] = f(inp["norm1_g"].reshape(depth, 8, 128).transpose(0, 2, 1))
    shared["norm2_g"] = f(inp["norm2_g"].reshape(depth, 8, 128).transpose(0, 2, 1))
    shared["final_norm_g"] = f(inp["final_norm_g"].reshape(8, 128).T)
    shared["w_fm"] = f(inp["w_in"][:, :, fm])
    shared["w_tm"] = f(inp["w_in"][:, :, tm])
    shared["w_gb"] = f(inp["w_in"][:, :, gb])
    for k in ("w_branch_attn", "w_branch_s5", "w_branch_ml", "w_out", "ffn_w_up", "ffn_w_down"):
        shared[k] = f(inp[k])
    shared["ffn_conv_w"] = f(inp["ffn_conv_w"].reshape(depth, 3, 44, 128).transpose(0, 3, 1, 2))
    shared["ffn_conv_b"] = f(inp["ffn_conv_b"].reshape(depth, 44, 128).transpose(0, 2, 1))
    shared["attn_sink"] = f(inp["attn_sink"].reshape(depth, 1, 8))
    gbias = np.stack([np.concatenate([inp["ml_igate_b"][l, 0], inp["ml_fgate_b"][l, 0], inp["ml_igate_b"][l, 1], inp["ml_fgate_b"][l, 1]])
                      for l in range(depth)])
    shared["ml_gate_b"] = f(gbias.reshape(depth, 1, 16))
    shared["ml_norm_g"] = f(inp["ml_norm_g"].reshape(depth, 1, 256))
    a_st = np.stack([inp["s5_a_re"], inp["s5_a_im"]], axis=-1)
    a_st = a_st.transpose(0, 3, 1, 2, 4)
    shared["s5_a"] = f(np.concatenate([a_st, a_st], axis=1).reshape(depth, 128, 64))
    shared["s5_logdt"] = f(inp["s5_log_dt"].reshape(depth, 1, 32))
    b_st = np.stack([inp["s5_b_re"], inp["s5_b_im"]], axis=-1).transpose(0, 2, 1, 3, 4)
    shared["s5_b"] = f(np.concatenate([b_st, b_st], axis=1).reshape(depth, 128, 512))
    c_st = np.stack([inp["s5_c_re"], inp["s5_c_im"]], axis=-1).transpose(0, 3, 1, 2, 4)
    shared["s5_c"] = f(np.concatenate([c_st, c_st], axis=1).reshape(depth, 128, 512))
    dd = inp["s5_d"].reshape(depth, 16, 16).transpose(0, 2, 1)
    shared["s5_dcol"] = f(np.tile(dd, (1, 8, 1)))
    shared["s5_glu_w"] = f(inp["s5_glu_w"])
    shared["s5_glu_b"] = f(inp["s5_glu_b"].reshape(depth, 2, 128).transpose(0, 2, 1))
    shared.update(cst)
    maps = []
    for b in range(ncores):
        m = dict(shared)
        m["x"] = f(inp["x"][b, :T])
        m["ctx"] = f(inp["ctx"][b])
        cv = np.stack([inp["c"][b], inp["c_ctx"]], axis=-1)
        m["cvec"] = f(cv.reshape(8, 128, 2).transpose(1, 0, 2))
        maps.append(m)
    return maps


_CACHE = {}


def kernel(**inputs):
    inp = {k: np.asarray(v) for k, v in inputs.items()}
    B, T = inp["x"].shape[0], inp["x"].shape[1]
    if T not in _CACHE:
        _CACHE[T] = build(T)
    nc = _CACHE[T][0]
    maps = make_in_maps(inp, T, ncores=B)
    res = run_bass_kernel_spmd(nc, maps, core_ids=list(range(B)))
    return np.stack([r["y"] for r in res.results], axis=0).astype(np.float32)
```

```python
import math
from contextlib import ExitStack
import numpy as np
import ml_dtypes
import concourse.bass as bass
import concourse.mybir as mybir
from concourse.bass_utils import run_bass_kernel_spmd

F32 = mybir.dt.float32
BF16 = mybir.dt.bfloat16
I32 = mybir.dt.int32
AF = mybir.ActivationFunctionType
ALU = mybir.AluOpType
AX = mybir.AxisListType

D = 1024
CTX = 256
NH, NKV, HD = 8, 2, 64
W_S5, S5G, S5P = 256, 16, 64
MLH = 4
D_FF = 2816
EPS = 1e-6
ENG = ("pe", "act", "dve", "pool", "sp")
TWO_PI = 2.0 * math.pi


class Res:
    __slots__ = ("last_w", "readers")

    def __init__(self):
        self.last_w = None
        self.readers = []


class Op:
    __slots__ = ("eng", "fn", "dma", "deps", "sig", "idx", "sem", "val")


class _Rec:
    def __init__(self):
        self.call = None

    def __getattr__(self, name):
        def f(*a, **k):
            self.call = (name, a, k)
            return None
        return f


class Sched:
    def __init__(self, nc, ndma=8):
        self.nc, self.ops, self.ndma = nc, [], ndma
        self.phase = Res()

    def op(self, eng, fn, reads=(), writes=(), dma=False, barrier=False):
        rec = _Rec()
        fn(rec)
        name_, a_, k_ = rec.call
        fn = lambda engine, name_=name_, a_=a_, k_=k_: getattr(engine, name_)(*a_, **k_)
        o = Op()
        o.eng, o.fn, o.dma, o.deps, o.sig, o.sem, o.val = eng, fn, dma, set(), False, None, None
        o.idx = len(self.ops)
        reads = list(reads)
        writes = list(writes)
        if barrier:
            writes.append(self.phase)
        else:
            reads.append(self.phase)
        for r in reads:
            if r.last_w is not None:
                o.deps.add(r.last_w)
        for r in writes:
            if r.last_w is not None:
                o.deps.add(r.last_w)
            o.deps.update(r.readers)
        for r in reads:
            r.readers.append(o.idx)
        for r in writes:
            r.last_w = o.idx
            r.readers = []
        o.deps.discard(o.idx)
        self.ops.append(o)
        return o

    def emit(self, final_ops=()):
        nc, ops = self.nc, self.ops
        for o in ops:
            keep = set()
            for d in o.deps:
                p = ops[d]
                if p.eng == o.eng and not p.dma and o.eng in ("pe", "sp"):
                    continue
                keep.add(d)
            o.deps = keep
            for d in keep:
                ops[d].sig = True
        for f in final_ops:
            f.sig = True
        st = ExitStack()
        sems = {e: st.enter_context(nc.semaphore("s_" + e)) for e in ENG}
        dsems = [st.enter_context(nc.semaphore("d_%d" % i)) for i in range(self.ndma)]
        cnt = {e: 0 for e in ENG}
        dcnt = 0
        prev_user = {}
        for o in ops:
            if o.dma:
                n = dcnt
                dcnt += 1
                o.sem = dsems[n % self.ndma]
                o.val = 16 * (n // self.ndma + 1)
                if (n % self.ndma) in prev_user:
                    o.deps.add(prev_user[n % self.ndma])
                prev_user[n % self.ndma] = o.idx
                o.sig = True
            elif o.sig:
                cnt[o.eng] += 1
                o.sem, o.val = sems[o.eng], cnt[o.eng]
        per = {e: [o for o in ops if o.eng == e] for e in ENG}
        stats = dict(nops=len(ops), nwait=0)

        def run(e, engine):
            waited = {}
            for o in per[e]:
                need = {}
                for d in o.deps:
                    p = ops[d]
                    k = id(p.sem)
                    if k not in need or need[k][1] < p.val:
                        need[k] = (p.sem, p.val)
                for k, (sem, val) in need.items():
                    if waited.get(k, 0) >= val:
                        continue
                    engine.wait_ge(sem, val)
                    stats["nwait"] += 1
                    waited[k] = val
                ins = o.fn(engine)
                if o.dma:
                    ins.then_inc(o.sem, 16)
                elif o.sig:
                    ins.then_inc(o.sem, 1)
            if e == "sp":
                for f in final_ops:
                    engine.wait_ge(f.sem, f.val)

        with nc.Block() as block:
            @block.sync
            def _(eng):
                run("sp", eng)

            @block.tensor
            def _(eng):
                run("pe", eng)

            @block.scalar
            def _(eng):
                run("act", eng)

            @block.vector
            def _(eng):
                run("dve", eng)

            @block.gpsimd
            def _(eng):
                run("pool", eng)
        st.close()
        return stats


def _rope_perm():
    p = np.arange(64)
    out = np.empty(64, dtype=np.int64)
    for d in range(64):
        base = (d // 32) * 32
        r = d % 32
        out[d] = base + (r + 16 if r < 16 else r - 16)
    return out


def _col_layout():
    o_q, o_k, o_v = 0, 512, 640
    o_us = 768
    o_qm, o_km, o_vm, o_om = 1024, 1280, 1536, 1792
    o_gm = 2048
    o_gb = 2064
    perm = _rope_perm()
    fm = []
    for c in range(4):
        fm += [o_q + c * 64 + d for d in range(64)] + [o_q + (4 + c) * 64 + d for d in range(64)]
    for c in range(4):
        fm += [o_q + c * 64 + perm[d] for d in range(64)] + [o_q + (4 + c) * 64 + perm[d] for d in range(64)]
    fm += [o_k + d for d in range(128)]
    fm += [o_k + h * 64 + perm[d] for h in range(2) for d in range(64)]
    fm += [o_us + d for d in range(256)]
    fm += [o_qm + d for d in range(256)]
    fm += [o_km + d for d in range(256)]
    tm = [o_v + d for d in range(128)] + [o_km + d for d in range(256)] + [o_vm + d for d in range(256)] \
        + [o_om + d for d in range(256)] + [o_gm + d for d in range(16)]
    gb = [o_gb + d for d in range(3072)]
    return np.array(fm), np.array(tm), np.array(gb)


FM_Q, FM_QP, FM_K, FM_KP, FM_US, FM_QM, FM_KM = 0, 4, 8, 9, 10, 12, 14
NFM = 16
TM_V, TM_KM, TM_VM, TM_OM, TM_GM = 0, 128, 384, 640, 896
NTM = 912


def _consts(T):
    NT = CTX + T
    c = {}
    c["ident_f"] = np.eye(128, dtype=np.float32)
    k = np.arange(128)[:, None]
    q = np.arange(128)[None, :]
    c["mask_le"] = (k <= q).astype(np.float32)
    c["mask_ge"] = (k >= q).astype(np.float32)
    rows = T // 64
    row = np.repeat(np.arange(rows, dtype=np.float32), 64)
    col = np.tile(np.arange(64, dtype=np.float32), rows)
    inv = (10000.0 ** (-np.arange(16, dtype=np.float32) / 16)).astype(np.float32)
    ang_r = (row[:, None] * inv[None, :]).astype(np.float32)
    ang_c = (col[:, None] * inv[None, :]).astype(np.float32)
    cosT = np.ones((128, NT), np.float32)
    sinT = np.zeros((128, NT), np.float32)
    for p in range(128):
        d = p % 64
        a = ang_r if d < 32 else ang_c
        r = d % 32
        f = r % 16
        cosT[p, CTX:] = np.cos(a[:, f])
        s = np.sin(a[:, f])
        sinT[p, CTX:] = -s if r < 16 else s
    c["rope_cos"] = cosT
    hp = math.pi / 2
    cols = np.zeros((128, 4), np.float32)
    cols[:64, 0] = hp
    cols[64:, 1] = hp
    cols[:64, 2], cols[64:, 2] = -1.0, 1.0
    cols[:64, 3], cols[64:, 3] = 1.0, -1.0
    c["s5_cols"] = cols
    mi = np.arange(136, dtype=np.float32)
    c["s5_mphi"] = np.stack([127.0 - mi, mi - 8.0]).reshape(1, 272).astype(np.float32)
    qi = np.arange(136)
    c["s5_mpsi"] = np.stack([qi.astype(np.float32), (8 * (qi // 8) + 8 - qi % 8).astype(np.float32)]).reshape(1, 272).astype(np.float32)
    s8 = np.arange(8, dtype=np.float32)
    c["s5_mphs"] = np.stack([np.concatenate([-s8, 7.0 - s8]), np.concatenate([s8 - 8.0, s8])]).reshape(1, 32).astype(np.float32)
    rr = np.arange(128)[:, None] // 16
    cc = np.arange(128)[None, :] // 16
    c["s5_m0"] = np.stack([(cc >= rr), (rr >= cc)]).astype(np.float32)
    sw = np.zeros((128, 128), np.float32)
    sw[np.arange(128), (np.arange(128) + 64) % 128] = 1.0
    c["s5_swap"] = sw
    sel = np.zeros((128, 64, 128), np.float32)
    selT = np.zeros((128, 64, 128), np.float32)
    for gl in range(8):
        for s in range(8):
            for ch in range(16):
                sel[gl * 16 + ch, gl * 8 + s, s * 16 + ch] = 1.0
                selT[s * 16 + ch, gl * 8 + s, gl * 16 + ch] = 1.0
    c["s5_sel"] = sel.astype(ml_dtypes.bfloat16)
    c["s5_selT"] = selT.astype(ml_dtypes.bfloat16)
    c["rope_sin"] = sinT
    return c


def build(T, depth=2, debug=False, upto=None):
    NT = CTX + T
    nc = bass.Bass("TRN2", target_bir_lowering=False)
    es = ExitStack()
    S = Sched(nc)
    dbg_outs = {}

    def din(name, shape, dt=F32):
        return nc.dram_tensor(name, list(shape), dt, kind="ExternalInput").ap()

    def dscr(name, shape, dt=F32):
        kind = "ExternalOutput" if debug else "Internal"
        t = nc.dram_tensor(name, list(shape), dt, kind=kind).ap()
        if debug:
            dbg_outs[name] = t
        return t

    x_in = din("x", [T, D])
    ctx_in = din("ctx", [CTX, D])
    cvec = din("cvec", [128, 8, 2])
    mod_w = din("mod_w", [depth, D, 6 * D])
    mod_b = din("mod_b", [depth, 128, 48])
    n1g = din("norm1_g", [depth, 128, 8])
    n2g = din("norm2_g", [depth, 128, 8])
    fng = din("final_norm_g", [128, 8])
    w_fm = din("w_fm", [depth, D, NFM * 128])
    w_tm = din("w_tm", [depth, D, NTM])
    w_gb = din("w_gb", [depth, D, 3072])
    wb_a = din("w_branch_attn", [depth, 512, D])
    wb_s = din("w_branch_s5", [depth, 256, D])
    wb_m = din("w_branch_ml", [depth, 256, D])
    w_out = din("w_out", [depth, D, D])
    w_up = din("ffn_w_up", [depth, D, 2 * D_FF])
    cw = din("ffn_conv_w", [depth, 128, 3, 44])
    cb = din("ffn_conv_b", [depth, 128, 44])
    w_dn = din("ffn_w_down", [depth, D_FF, D])
    sink = din("attn_sink", [depth, 1, 8])
    ml_gb = din("ml_gate_b", [depth, 1, 16])
    ml_ng = din("ml_norm_g", [depth, 1, 256])
    s5_a = din("s5_a", [depth, 128, 64])
    s5_logdt = din("s5_logdt", [depth, 1, 32])
    s5_b = din("s5_b", [depth, 128, 512])
    s5_c = din("s5_c", [depth, 128, 512])
    s5_dcol = din("s5_dcol", [depth, 128, 16])
    s5_glu_w = din("s5_glu_w", [depth, 256, 256])
    s5_glu_b = din("s5_glu_b", [depth, 128, 2])
    s5_cols = din("s5_cols", [128, 4])
    s5_mphi = din("s5_mphi", [1, 272])
    s5_mpsi = din("s5_mpsi", [1, 272])
    s5_mphs = din("s5_mphs", [1, 32])
    s5_m0 = din("s5_m0", [2, 128, 128])
    s5_swap = din("s5_swap", [128, 128])
    s5_sel = din("s5_sel", [128, 64, 128], BF16)
    s5_selT = din("s5_selT", [128, 64, 128], BF16)
    ident_f = din("ident_f", [128, 128])
    mask_le = din("mask_le", [128, 128])
    mask_ge = din("mask_ge", [128, 128])
    rope_cos = din("rope_cos", [128, NT])
    rope_sin = din("rope_sin", [128, NT])
    y_out = nc.dram_tensor("y", [T, D], F32, kind="ExternalOutput").ap()

    xT = [dscr("xT%d" % i, [8, 128, NT]) for i in range(2)]
    qT = dscr("qT", [4, 128, NT], BF16)
    kT = dscr("kT", [128, NT], BF16)
    usT = dscr("usT", [2, 128, NT], BF16)
    qmT = dscr("qmT", [2, 128, NT], BF16)
    kmT = dscr("kmT", [2, 128, NT], BF16)
    v_tok = dscr("v_tok", [NT, 128], BF16)
    km_tok = dscr("km_tok", [NT, 256], BF16)
    vm_tok = dscr("vm_tok", [NT, 256], BF16)
    om_tok = dscr("om_tok", [NT, 256])
    gm_tok = dscr("gm_tok", [NT, 16])
    yaT = dscr("yaT", [4, 128, NT], BF16)
    ysT = dscr("ysT", [2, 128, NT], BF16)
    ymT = dscr("ymT", [2, 128, NT], BF16)
    hF = dscr("hF", [NT, 256])

    ARENA = 208896 // 2
    arena = es.enter_context(nc.sbuf_tensor("arena", [128, ARENA], BF16))
    off = [0]
    persist_off = [0]

    def tile(shape, dt, parts=128):
        n = int(np.prod(shape)) * (1 if dt == BF16 else 2)
        n = (n + 15) // 16 * 16
        assert off[0] + n <= ARENA, ("SBUF arena overflow", off[0], n)
        a = arena[0:parts, off[0]:off[0] + n]
        off[0] += n
        used = int(np.prod(shape)) * (1 if dt == BF16 else 2)
        a = a[:, 0:used]
        if dt != BF16:
            a = a.bitcast(dt)
        if len(shape) == 2:
            a = a.rearrange("p (a b) -> p a b", a=shape[0])
        elif len(shape) == 3:
            a = a.rearrange("p (a b c) -> p a b c", a=shape[0], b=shape[1])
        return a, Res()

    banks = [es.enter_context(nc.psum_tensor("bank%d" % i, [128, 512], F32)) for i in range(8)]
    bres = [Res() for _ in range(8)]
    junk, junk_r = tile((4,), F32)

    def barrier():
        S.op("pool", lambda e: e.memset(junk[:, 0:1], 0.0), writes=[junk_r], barrier=True)

    def new_phase():
        barrier()
        off[0] = persist_off[0]

    def dma(out, in_, reads=(), writes=()):
        return S.op("sp", lambda e: e.dma_start(out=out, in_=in_), reads, writes, dma=True)

    def op(eng, fn, reads=(), writes=()):
        return S.op(eng, fn, reads, writes)

    def cast_load(dst, dst_r, src_ap, shape, stage, stage_r, eng="pool"):
        dma(stage, src_ap, writes=[stage_r])
        if eng == "act":
            op("act", lambda e: e.copy(out=dst, in_=stage), [stage_r], [dst_r])
        else:
            op(eng, lambda e: e.tensor_copy(out=dst, in_=stage), [stage_r], [dst_r])

    ident, ident_r = tile((128,), F32)
    identb, identb_r = tile((128,), BF16)
    ones_f, ones_r = tile((128,), F32)
    mle, mle_r = tile((128,), F32)
    mge, mge_r = tile((128,), F32)
    mleb, mleb_r = tile((128,), BF16)
    mgeb, mgeb_r = tile((128,), BF16)
    mle8, mle8_r = tile((128,), BF16)
    mge8, mge8_r = tile((128,), BF16)
    modv, modv_r = tile((depth, 48, 2), F32)
    gs1, gs1_r = tile((depth, 8, 2), F32)
    gs2, gs2_r = tile((depth, 8, 2), F32)
    n1t, n1t_r = tile((depth, 8), F32)
    n2t, n2t_r = tile((depth, 8), F32)
    fnt, fnt_r = tile((8,), F32)
    persist_off[0] = off[0]

    dma(ident, ident_f, writes=[ident_r])
    dma(mle, mask_le, writes=[mle_r])
    dma(mge, mask_ge, writes=[mge_r])
    op("dve", lambda e: e.tensor_copy(out=identb, in_=ident), [ident_r], [identb_r])
    op("dve", lambda e: e.tensor_copy(out=mleb, in_=mle), [mle_r], [mleb_r])
    op("dve", lambda e: e.tensor_copy(out=mgeb, in_=mge), [mge_r], [mgeb_r])
    op("pool", lambda e: e.memset(ones_f, 1.0), writes=[ones_r])
    op("dve", lambda e: e.tensor_scalar_mul(out=mle8, in0=mle, scalar1=0.125), [mle_r], [mle8_r])
    op("dve", lambda e: e.tensor_scalar_mul(out=mge8, in0=mge, scalar1=0.125), [mge_r], [mge8_r])
    dma(n1t, n1g.rearrange("l p c -> p l c"), writes=[n1t_r])
    dma(n2t, n2g.rearrange("l p c -> p l c"), writes=[n2t_r])
    dma(fnt, fng, writes=[fnt_r])

    def phase_mod():
        cv, cv_r = tile((8, 2), F32)
        sc, sc_r = tile((8, 2), F32)
        dma(cv, cvec, writes=[cv_r])
        op("act", lambda e: e.activation(out=sc, in_=cv, func=AF.Silu), [cv_r], [sc_r])
        mb, mb_r = tile((depth, 48), F32)
        dma(mb, mod_b.rearrange("l p c -> p l c"), writes=[mb_r])
        wst = [tile((8, 1024), F32) for _ in range(2)]
        i = 0
        for l in range(depth):
            for piece in range(6):
                w, w_r = wst[i % 2]
                i += 1
                dma(w, mod_w[l, :, piece * 1024:(piece + 1) * 1024].rearrange("(kc p) n -> p kc n", p=128), writes=[w_r])
                bk, bk_r = banks[piece % 2], bres[piece % 2]
                for fc in range(8):
                    for kc in range(8):
                        op("pe", lambda e, w=w, fc=fc, kc=kc, bk=bk: e.matmul(
                            bk[:, fc * 2:fc * 2 + 2], lhsT=w[:, kc, fc * 128:(fc + 1) * 128], rhs=sc[:, kc, :],
                            start=(kc == 0), stop=(kc == 7)), [w_r, sc_r], [bk_r])
                dst = modv[:, l, piece * 8:(piece + 1) * 8, :]
                src = bk[:, 0:16].rearrange("p (a b) -> p a b", a=8)
                bb = mb[:, l, piece * 8:(piece + 1) * 8].unsqueeze(2).to_broadcast([128, 8, 2])
                op("dve", lambda e, dst=dst, src=src, bb=bb: e.tensor_tensor(out=dst, in0=src, in1=bb, op=ALU.add),
                   [bk_r, mb_r], [modv_r])
        for l in range(depth):
            for (gs, gs_r, nt, nt_r, pc) in ((gs1, gs1_r, n1t, n1t_r, 1), (gs2, gs2_r, n2t, n2t_r, 4)):
                src = modv[:, l, pc * 8:(pc + 1) * 8, :]
                nb = nt[:, l, :].unsqueeze(2).to_broadcast([128, 8, 2])
                dst = gs[:, l, :, :]
                op("dve", lambda e, dst=dst, src=src, nb=nb: e.scalar_tensor_tensor(
                    out=dst, in0=src, scalar=1.0, in1=nb, op0=ALU.add, op1=ALU.mult), [modv_r, nt_r], [gs_r])

    def phase_transpose_in():
        xin = [tile((1024,), F32) for _ in range(2)]
        xo = [tile((8, 128), F32) for _ in range(2)]
        nblk = NT // 128
        for b in range(nblk):
            t0 = b * 128
            xt, xt_r = xin[b % 2]
            src = ctx_in[t0:t0 + 128, :] if t0 < CTX else x_in[t0 - CTX:t0 - CTX + 128, :]
            dma(xt, src, writes=[xt_r])
            o, o_r = xo[b % 2]
            for half in range(2):
                bk, bk_r = banks[(b * 2 + half) % 4], bres[(b * 2 + half) % 4]
                for j in range(4):
                    c = half * 4 + j
                    op("pe", lambda e, bk=bk, j=j, xt=xt, c=c: e.transpose(
                        bk[:, j * 128:(j + 1) * 128], xt[:, c * 128:(c + 1) * 128], ident), [xt_r, ident_r], [bk_r])
                dst = o[:, half * 4:(half + 1) * 4, :]
                srcp = bk[:, :].rearrange("p (a b) -> p a b", a=4)
                if half == 0:
                    op("dve", lambda e, dst=dst, srcp=srcp: e.tensor_copy(out=dst, in_=srcp), [bk_r], [o_r])
                else:
                    op("act", lambda e, dst=dst, srcp=srcp: e.copy(out=dst, in_=srcp), [bk_r], [o_r])
            dma(xT[0][:, :, t0:t0 + 128].rearrange("c p t -> p c t"), o, reads=[o_r])

    def norm_mod(xt, xt_r, n, hT, hT_r, gs, gs_r, shcol, which, sq, sq_r, rs, rs_r, bk, bk_r, l, zero_cols=()):
        for c in range(8):
            if c % 2 == 0:
                op("act", lambda e, c=c: e.activation(out=sq[:, c, 0:n], in_=xt[:, c, 0:n], func=AF.Square), [xt_r], [sq_r[c]])
            else:
                op("pool", lambda e, c=c: e.tensor_tensor(out=sq[:, c, 0:n], in0=xt[:, c, 0:n], in1=xt[:, c, 0:n], op=ALU.mult),
                   [xt_r], [sq_r[c]])
        for c in range(8):
            op("pe", lambda e, c=c: e.matmul(bk[:, 0:n], lhsT=ones_f, rhs=sq[:, c, 0:n], start=(c == 0), stop=(c == 7)),
               [sq_r[c], ones_r], [bk_r])
        op("act", lambda e: e.activation(out=rs[:, 0:n], in_=bk[:, 0:n], func=AF.Sqrt, bias=EPS, scale=1.0 / D), [bk_r], [rs_r])
        op("dve", lambda e: e.reciprocal(out=rs[:, 0:n], in_=rs[:, 0:n]), [rs_r], [rs_r])
        for c in range(8):
            op("dve", lambda e, c=c: e.tensor_tensor(out=sq[:, c, 0:n], in0=xt[:, c, 0:n], in1=rs[:, 0:n], op=ALU.mult),
               [xt_r, rs_r], [sq_r[c]])
            op("act", lambda e, c=c: e.activation(out=hT[:, c, 0:n], in_=sq[:, c, 0:n], func=AF.Identity,
                                                  bias=modv[:, l, shcol * 8 + c, which:which + 1],
                                                  scale=gs[:, l, c, which:which + 1]), [sq_r[c], modv_r, gs_r], [hT_r])
        for zc in zero_cols:
            op("pool", lambda e, zc=zc: e.memset(hT[:, :, zc:zc + 1], 0.0), [], [hT_r])

    def token_tiles(with_ctx=True, size=512):
        tl = []
        if with_ctx:
            tl.append((0, CTX))
        t = CTX
        while t < NT:
            n = min(size, NT - t)
            tl.append((t, n))
            t += n
        return tl

    def phase_A(l, xsrc):
        wfm, wfm_r = tile((8, NFM * 128), BF16)
        wtm, wtm_r = tile((8, NTM), BF16)
        stg = [tile((8, 512), F32) for _ in range(2)]
        si = 0
        for cc in range(NFM * 128 // 512):
            st_, st_r = stg[si % 2]
            si += 1
            cast_load(wfm[:, :, cc * 512:(cc + 1) * 512], wfm_r,
                      w_fm[l, :, cc * 512:(cc + 1) * 512].rearrange("(kc p) n -> p kc n", p=128), None, st_, st_r,
                      eng=("pool" if cc % 2 == 0 else "dve"))
        for (c0, c1) in ((0, 512), (512, NTM)):
            st_, st_r = stg[si % 2]
            si += 1
            cast_load(wtm[:, :, c0:c1], wtm_r, w_tm[l, :, c0:c1].rearrange("(kc p) n -> p kc n", p=128), None,
                      st_[:, :, 0:c1 - c0], st_r, eng="pool")
        xts = [tile((8, 512), F32) for _ in range(2)]
        sq, _ = tile((8, 512), F32)
        sq_r = [Res() for _ in range(8)]
        rs, rs_r = tile((512,), F32)
        hT, hT_r = tile((8, 512), BF16)
        fo, fo_r = tile((NFM, 512), BF16)
        rc, rc_r = tile((512,), F32)
        rsn, rsn_r = tile((512,), F32)
        tmpa, tmpa_r = tile((512,), F32)
        tmpb, tmpb_r = tile((512,), F32)
        to_b, to_br = tile((640,), BF16)
        to_f, to_fr = tile((272,), F32)
        hTs = [(hT, hT_r), tile((8, 512), BF16)]
        tiles_ = token_tiles()

        def ln_A(ti):
            t0, n = tiles_[ti]
            xt, xt_r = xts[ti % 2]
            dma(xt[:, :, 0:n], xsrc[:, :, t0:t0 + n].rearrange("c p t -> p c t"), writes=[xt_r])
            norm_mod(xt, xt_r, n, hTs[ti % 2][0], hTs[ti % 2][1], gs1, gs1_r, 0, 1 if t0 < CTX else 0, sq, sq_r, rs, rs_r,
                     banks[0], bres[0], l)

        ln_A(0)
        for ti, (t0, n) in enumerate(tiles_):
            if ti + 1 < len(tiles_):
                ln_A(ti + 1)
            which = 1 if t0 < CTX else 0
            hT, hT_r = hTs[ti % 2]
            if which == 0:
                dma(rc[:, 0:n], rope_cos[:, t0:t0 + n], writes=[rc_r])
                dma(rsn[:, 0:n], rope_sin[:, t0:t0 + n], writes=[rsn_r])
            def fm_mm(m, bk, bk_r):
                for kc in range(8):
                    op("pe", lambda e, m=m, kc=kc, bk=bk: e.matmul(
                        bk[:, 0:n], lhsT=wfm[:, kc, m * 128:(m + 1) * 128], rhs=hT[:, kc, 0:n],
                        start=(kc == 0), stop=(kc == 7)), [wfm_r, hT_r], [bk_r])
            bi = 1
            for (m, mp) in [(FM_Q + c, FM_QP + c) for c in range(4)] + [(FM_K, FM_KP)]:
                b1, b1r = banks[1 + (bi % 3) * 2], bres[1 + (bi % 3) * 2]
                b2, b2r = banks[2 + (bi % 3) * 2], bres[2 + (bi % 3) * 2]
                bi += 1
                fm_mm(m, b1, b1r)
                if which == 0:
                    fm_mm(mp, b2, b2r)
                    op("dve", lambda e, b1=b1: e.tensor_tensor(out=tmpa[:, 0:n], in0=b1[:, 0:n], in1=rc[:, 0:n], op=ALU.mult),
                       [b1r, rc_r], [tmpa_r])
                    op("dve", lambda e, b2=b2: e.tensor_tensor(out=tmpb[:, 0:n], in0=b2[:, 0:n], in1=rsn[:, 0:n], op=ALU.mult),
                       [b2r, rsn_r], [tmpb_r])
                    op("pool", lambda e, m=m: e.tensor_tensor(out=fo[:, m, 0:n], in0=tmpa[:, 0:n], in1=tmpb[:, 0:n], op=ALU.add),
                       [tmpa_r, tmpb_r], [fo_r])
                else:
                    op("act", lambda e, m=m, b1=b1: e.copy(out=fo[:, m, 0:n], in_=b1[:, 0:n]), [b1r], [fo_r])
            for m in range(FM_US, NFM):
                b1, b1r = banks[1 + (m % 6)], bres[1 + (m % 6)]
                fm_mm(m, b1, b1r)
                if m % 2 == 0:
                    op("act", lambda e, m=m, b1=b1: e.copy(out=fo[:, m, 0:n], in_=b1[:, 0:n]), [b1r], [fo_r])
                else:
                    op("dve", lambda e, m=m, b1=b1: e.tensor_copy(out=fo[:, m, 0:n], in_=b1[:, 0:n]), [b1r], [fo_r])
            dma(qT[:, :, t0:t0 + n].rearrange("c p t -> p c t"), fo[:, FM_Q:FM_Q + 4, 0:n], reads=[fo_r])
            dma(kT[:, t0:t0 + n], fo[:, FM_K, 0:n], reads=[fo_r])
            dma(usT[:, :, t0:t0 + n].rearrange("c p t -> p c t"), fo[:, FM_US:FM_US + 2, 0:n], reads=[fo_r])
            dma(qmT[:, :, t0:t0 + n].rearrange("c p t -> p c t"), fo[:, FM_QM:FM_QM + 2, 0:n], reads=[fo_r])
            dma(kmT[:, :, t0:t0 + n].rearrange("c p t -> p c t"), fo[:, FM_KM:FM_KM + 2, 0:n], reads=[fo_r])
            for sb in range(n // 128):
                b1, b1r = banks[1 + (sb % 2) * 2], bres[1 + (sb % 2) * 2]
                b2, b2r = banks[2 + (sb % 2) * 2], bres[2 + (sb % 2) * 2]
                for (bk, bk_r, c0, c1) in ((b1, b1r, 0, 512), (b2, b2r, 512, NTM)):
                    for kc in range(8):
                        op("pe", lambda e, bk=bk, kc=kc, c0=c0, c1=c1, sb=sb: e.matmul(
                            bk[:, 0:c1 - c0], lhsT=hT[:, kc, sb * 128:(sb + 1) * 128], rhs=wtm[:, kc, c0:c1],
                            start=(kc == 0), stop=(kc == 7)), [hT_r, wtm_r], [bk_r])
                op("act", lambda e, b1=b1: e.copy(out=to_b[:, 0:512], in_=b1[:, 0:512]), [b1r], [to_br])
                op("dve", lambda e, b2=b2: e.tensor_copy(out=to_b[:, 512:640], in_=b2[:, 0:128]), [b2r], [to_br])
                op("dve", lambda e, b2=b2: e.tensor_copy(out=to_f, in_=b2[:, 128:400]), [b2r], [to_fr])
                tt = t0 + sb * 128
                dma(v_tok[tt:tt + 128, :], to_b[:, TM_V:TM_V + 128], reads=[to_br])
                dma(km_tok[tt:tt + 128, :], to_b[:, TM_KM:TM_KM + 256], reads=[to_br])
                dma(vm_tok[tt:tt + 128, :], to_b[:, TM_VM:TM_VM + 256], reads=[to_br])
                dma(om_tok[tt:tt + 128, :], to_f[:, 0:256], reads=[to_fr])
                dma(gm_tok[tt:tt + 128, :], to_f[:, 256:272], reads=[to_fr])

    def phase_attn(l, with_ctx):
        esk, esk_r = tile((8,), F32)
        dma(esk, sink[l].partition_broadcast(128), writes=[esk_r])
        op("act", lambda e: e.activation(out=esk, in_=esk, func=AF.Exp), [esk_r], [esk_r])
        kc_, kc_r = tile((CTX,), BF16)
        dma(kc_, kT[:, 0:CTX], writes=[kc_r])
        vc_, vc_r = tile((2, 2, 65), BF16)
        op("pool", lambda e: e.memset(vc_, 1.0), [], [vc_r])
        for j in range(2):
            dma(vc_[:, j, :, 0:64], v_tok[j * 128:(j + 1) * 128, :].rearrange("t (g d) -> t g d", g=2), writes=[vc_r])
        NS = 3
        qs = [tile((4, 128), BF16) for _ in range(NS)]
        ks = [tile((384,), BF16) for _ in range(NS)]
        vs = [tile((3, 2, 65), BF16) for _ in range(NS)]
        for (v_, v_r) in vs:
            op("pool", lambda e, v_=v_: e.memset(v_, 1.0), [], [v_r])
        pts = [tile((512,), BF16) for _ in range(10)]
        ya, ya_r = tile((512,), BF16)
        yat, yat_r = tile((4, 128), BF16)
        den, den_r = tile((4,), F32)
        nlat = T // 128
        blocks = [("lat", j) for j in range(nlat)]
        if with_ctx:
            blocks += [("ctx", 0), ("ctx", 1)]
        pi = 0
        for bi, (kind, j) in enumerate(blocks):
            q_, q_r = qs[bi % NS]
            k_, k_r = ks[bi % NS]
            v_, v_r = vs[bi % NS]
            tq = CTX + j * 128 if kind == "lat" else j * 128
            dma(q_, qT[:, :, tq:tq + 128].rearrange("c p t -> p c t"), writes=[q_r])
            keys = []
            if kind == "lat":
                lo, hi = max(0, j - 1), min(nlat - 1, j + 1)
                nk = hi - lo + 1
                dma(k_[:, 0:nk * 128], kT[:, CTX + lo * 128:CTX + (hi + 1) * 128], writes=[k_r])
                for i_ in range(nk):
                    dma(v_[:, i_, :, 0:64], v_tok[CTX + (lo + i_) * 128:CTX + (lo + i_ + 1) * 128, :].rearrange("t (g d) -> t g d", g=2),
                        writes=[v_r])
                for jj in range(lo, hi + 1):
                    m = None if jj == j else ("ge" if jj < j else "le")
                    keys.append((k_[:, (jj - lo) * 128:(jj - lo + 1) * 128], k_r, v_[:, jj - lo], v_r, m))
            for cj in range(2):
                keys.append((kc_[:, cj * 128:(cj + 1) * 128], kc_r, vc_[:, cj], vc_r, None))
            for g in range(2):
                ob, ob_r = banks[4 + g], bres[4 + g]
                plist = []
                for ki, (ka, ka_r, va, va_r, m) in enumerate(keys):
                    sbk, sbk_r = banks[pi % 4], bres[pi % 4]
                    pt, pt_r = pts[g * 5 + ki]
                    pi += 1
                    op("pe", lambda e, sbk=sbk, ka=ka, g=g, q_=q_: e.matmul(
                        sbk[:, 0:512], lhsT=ka[g * 64:(g + 1) * 64, :], rhs=q_[g * 64:(g + 1) * 64, :, :],
                        start=True, stop=True), [ka_r, q_r], [sbk_r])
                    op("act", lambda e, pt=pt, sbk=sbk: e.activation(out=pt, in_=sbk[:, 0:512], func=AF.Exp, scale=HD ** -0.5),
                       [sbk_r], [pt_r])
                    if m is not None:
                        mk, mk_r = (mgeb, mgeb_r) if m == "ge" else (mleb, mleb_r)
                        mb_ = mk.unsqueeze(1).to_broadcast([128, 4, 128])
                        ptv = pt.rearrange("p (a b) -> p a b", a=4)
                        op("dve", lambda e, ptv=ptv, mb_=mb_: e.tensor_tensor(out=ptv, in0=ptv, in1=mb_, op=ALU.mult),
                           [pt_r, mk_r], [pt_r])
                    plist.append((pt, pt_r, va, va_r))
                for hh in range(4):
                    for ki, (pt, pt_r, va, va_r) in enumerate(plist):
                        op("pe", lambda e, ob=ob, hh=hh, pt=pt, va=va, g=g, ki=ki, nk=len(plist): e.matmul(
                            ob[:, hh * 65:(hh + 1) * 65], lhsT=pt[:, hh * 128:(hh + 1) * 128], rhs=va[:, g, :],
                            start=(ki == 0), stop=(ki == nk - 1)), [pt_r, va_r], [ob_r])
                obv = ob[:, 0:260].rearrange("p (a b) -> p a b", a=4)
                op("dve", lambda e, obv=obv, g=g: e.tensor_tensor(out=den, in0=obv[:, :, 64], in1=esk[:, g * 4:(g + 1) * 4], op=ALU.add),
                   [ob_r, esk_r], [den_r])
                op("dve", lambda e: e.reciprocal(out=den, in_=den), [den_r], [den_r])
                yav = ya[:, g * 256:(g + 1) * 256].rearrange("p (a b) -> p a b", a=4)
                db = den.unsqueeze(2).to_broadcast([128, 4, 64])
                op("dve", lambda e, yav=yav, obv=obv, db=db: e.tensor_tensor(out=yav, in0=obv[:, :, 0:64], in1=db, op=ALU.mult),
                   [ob_r, den_r], [ya_r])
            tb, tb_r = banks[6], bres[6]
            tbv = tb[:, 0:256].bitcast(BF16)
            for c in range(4):
                op("pe", lambda e, c=c, tbv=tbv: e.transpose(tbv[:, c * 128:(c + 1) * 128], ya[:, c * 128:(c + 1) * 128], identb),
                   [ya_r, identb_r], [tb_r])
            op("act", lambda e, tbv=tbv: e.copy(out=yat, in_=tbv.rearrange("p (a b) -> p a b", a=4)), [tb_r], [yat_r])
            dma(yaT[:, :, tq:tq + 128].rearrange("c p t -> p c t"), yat, reads=[yat_r])

    def phase_mlstm(l, with_ctx):
        gbt, gbt_r = tile((16,), F32)
        dma(gbt, ml_gb[l].partition_broadcast(128), writes=[gbt_r])
        ngt, ngt_r = tile((256,), F32)
        dma(ngt, ml_ng[l].partition_broadcast(128), writes=[ngt_r])
        ones64b, ones64b_r = tile((64,), F32)
        op("pool", lambda e: e.memset(ones64b, 1.0), [], [ones64b_r])
        nch = NT // 128
        NS = 3
        gts = [tile((16,), F32) for _ in range(NS)]
        qTs = [tile((2, 128), BF16) for _ in range(NS)]
        kTs = [tile((2, 128), BF16) for _ in range(NS)]
        kts = [tile((256,), BF16) for _ in range(NS)]
        vts = [tile((4, 65), BF16) for _ in range(NS)]
        for (v_, v_r) in vts:
            op("pool", lambda e, v_=v_: e.memset(v_, 1.0), [], [v_r])
        sps = [tile((4,), F32) for _ in range(NS)]
        avs = [tile((4,), F32) for _ in range(NS)]
        rvs = [tile((4,), F32) for _ in range(NS)]
        dcs = [tile((2,), F32) for _ in range(NS)]
        vas = [tile((4, 65), BF16) for _ in range(NS)]
        dts = [tile((4, 128), BF16) for _ in range(NS)]
        hns = [tile((4, 65), F32) for _ in range(NS)]
        dns = [tile((4,), F32) for _ in range(NS)]
        hfs = [tile((256,), F32) for _ in range(NS)]
        oms = [tile((256,), F32) for _ in range(NS)]
        for d in range(MLDIRS):
            if d == 1:
                barrier()
            Cst, Cst_r = tile((2, 65), F32)
            Cbf, Cbf_r = tile((2, 65), BF16)
            op("pool", lambda e, Cst=Cst: e.memset(Cst, 0.0), [], [Cst_r])
            op("pool", lambda e, Cbf=Cbf: e.memset(Cbf, 0.0), [], [Cbf_r])
            order = list(range(nch)) if d == 0 else [1, 0] + list(range(nch - 1, 1, -1))
            tri, tri_r = (mle, mle_r) if d == 0 else (mge, mge_r)
            mkb, mkb_r = (mle8, mle8_r) if d == 0 else (mge8, mge8_r)
            ic0, fc0 = (0, 4) if d == 0 else (8, 12)
            def prep(si, c):
                s_ = si % NS
                t0 = c * 128
                gt, gt_r = gts[s_]
                dma(gt, gm_tok[t0:t0 + 128, :], writes=[gt_r])
                q_, q_r = qTs[s_]
                dma(q_, qmT[:, :, t0:t0 + 128].rearrange("c p t -> p c t"), writes=[q_r])
                k_, k_r = kTs[s_]
                dma(k_, kmT[:, :, t0:t0 + 128].rearrange("c p t -> p c t"), writes=[k_r])
                kt, kt_r = kts[s_]
                dma(kt, km_tok[t0:t0 + 128, :], writes=[kt_r])
                vt, vt_r = vts[s_]
                dma(vt[:, :, 0:64], vm_tok[t0:t0 + 128, :].rearrange("t (h d) -> t h d", h=4), writes=[vt_r])
                op("dve", lambda e, gt=gt: e.tensor_tensor(out=gt, in0=gt, in1=gbt, op=ALU.add), [gt_r, gbt_r], [gt_r])
                sp, sp_r = sps[s_]
                op("act", lambda e, sp=sp, gt=gt: e.activation(out=sp, in_=gt[:, fc0:fc0 + 4], func=AF.Exp, scale=-1.0), [gt_r], [sp_r])
                op("act", lambda e, sp=sp: e.activation(out=sp, in_=sp, func=AF.Ln, bias=1.0, scale=1.0), [sp_r], [sp_r])
                gb_, gb_r = banks[0], bres[0]
                op("pe", lambda e, sp=sp, gb_=gb_: e.matmul(gb_[:, 0:4], lhsT=tri, rhs=sp, start=True, stop=True), [sp_r, tri_r], [gb_r])
                op("pe", lambda e, sp=sp, gb_=gb_: e.matmul(gb_[:, 8:12], lhsT=ones_f, rhs=sp, start=True, stop=True),
                   [sp_r, ones_r], [gb_r])
                av, av_r = avs[s_]
                rv, rv_r = rvs[s_]
                dc, dc_r = dcs[s_]
                op("dve", lambda e, av=av, gt=gt, gb_=gb_: e.tensor_tensor(out=av, in0=gb_[:, 0:4], in1=gt[:, ic0:ic0 + 4], op=ALU.add),
                   [gb_r, gt_r], [av_r])
                op("act", lambda e, av=av: e.activation(out=av, in_=av, func=AF.Exp), [av_r], [av_r])
                op("act", lambda e, rv=rv, gb_=gb_: e.activation(out=rv, in_=gb_[:, 0:4], func=AF.Exp, scale=-1.0), [gb_r], [rv_r])
                totv = gb_[:, 8:12].rearrange("p (a two) -> p a two", two=2)
                op("act", lambda e, dc=dc, totv=totv: e.activation(out=dc[0:64, :], in_=totv[0:64, :, 0], func=AF.Exp, scale=-1.0), [gb_r], [dc_r])
                op("act", lambda e, dc=dc, totv=totv: e.activation(out=dc[64:128, :], in_=totv[64:128, :, 1], func=AF.Exp, scale=-1.0), [gb_r], [dc_r])
                va, va_r = vas[s_]
                ab = av.unsqueeze(2).to_broadcast([128, 4, 65])
                op("dve", lambda e, va=va, vt=vt, ab=ab: e.tensor_tensor(out=va, in0=vt, in1=ab, op=ALU.mult), [vt_r, av_r], [va_r])
                dt, dt_r = dts[s_]
                dtv = dt.rearrange("p (a two) t -> p a two t", two=2)
                mb_ = mkb.unsqueeze(1).to_broadcast([128, 2, 128])
                for par in range(2):
                    sb_, sb_r = banks[1 + par], bres[1 + par]
                    for a in range(2):
                        h = 2 * a + par
                        hp = par * 64
                        op("pe", lambda e, sb_=sb_, a=a, hp=hp, k_=k_, q_=q_: e.matmul(
                            sb_[:, a * 128:(a + 1) * 128], lhsT=k_[hp:hp + 64, a, :], rhs=q_[hp:hp + 64, a, :],
                            start=True, stop=True), [k_r, q_r], [sb_r])
                    sbv = sb_[:, 0:256].rearrange("p (a b) -> p a b", a=2)
                    op("dve", lambda e, dtv=dtv, sbv=sbv, mb_=mb_, par=par: e.tensor_tensor(out=dtv[:, :, par, :], in0=sbv, in1=mb_, op=ALU.mult),
                       [sb_r, mkb_r], [dt_r])
                kvb, kvb_r = banks[3 + si % 2], bres[3 + si % 2]
                for h in range(4):
                    op("pe", lambda e, kvb=kvb, h=h, kt=kt, va=va: e.matmul(
                        kvb[:, h * 65:(h + 1) * 65], lhsT=kt[:, (h // 2) * 128:(h // 2 + 1) * 128], rhs=va[:, h, :], start=True, stop=True),
                       [kt_r, va_r], [kvb_r])

            def seq(si, c):
                s_ = si % NS
                t0 = c * 128
                q_, q_r = qTs[s_]
                k_, k_r = kTs[s_]
                kt, kt_r = kts[s_]
                va, va_r = vas[s_]
                dt, dt_r = dts[s_]
                rv, rv_r = rvs[s_]
                dc, dc_r = dcs[s_]
                kvb, kvb_r = banks[3 + si % 2], bres[3 + si % 2]
                hn, hn_r = hns[s_]
                hnv4 = hn.rearrange("p (a two) b -> p a two b", two=2)
                rv4 = rv.rearrange("p (a two) -> p a two", two=2)
                for par in range(2):
                    ob, ob_r = banks[5 + par], bres[5 + par]
                    hp = par * 64
                    for a in range(2):
                        h = 2 * a + par
                        op("pe", lambda e, ob=ob, a=a, h=h, dt=dt, va=va: e.matmul(
                            ob[:, a * 65:(a + 1) * 65], lhsT=dt[:, h, :], rhs=va[:, h, :], start=True, stop=False), [dt_r, va_r], [ob_r])
                        op("pe", lambda e, ob=ob, a=a, hp=hp, q_=q_, Cbf=Cbf: e.matmul(
                            ob[:, a * 65:(a + 1) * 65], lhsT=q_[hp:hp + 64, a, :], rhs=Cbf[hp:hp + 64, a, :],
                            start=False, stop=True), [q_r, Cbf_r], [ob_r])
                    obv = ob[:, 0:130].rearrange("p (a b) -> p a b", a=2)
                    rb = rv4[:, :, par].unsqueeze(2).to_broadcast([128, 2, 65])
                    op("dve", lambda e, hnv4=hnv4, obv=obv, rb=rb, par=par: e.tensor_tensor(out=hnv4[:, :, par, :], in0=obv, in1=rb, op=ALU.mult),
                       [ob_r, rv_r], [hn_r])
                kvv = kvb[:, 0:260].rearrange("p (a two b) -> p a two b", a=2, two=2)
                for hf_ in range(2):
                    ps_ = slice(hf_ * 64, hf_ * 64 + 64)
                    op("dve", lambda e, Cst=Cst, kvv=kvv, ps_=ps_, hf_=hf_: e.scalar_tensor_tensor(
                        out=Cst[ps_, :, :], in0=kvv[ps_, :, hf_, :], scalar=0.125, in1=Cst[ps_, :, :], op0=ALU.mult, op1=ALU.add),
                       [kvb_r, Cst_r], [Cst_r])
                dcb = dc.unsqueeze(2).to_broadcast([128, 2, 65])
                op("dve", lambda e, Cst=Cst, dcb=dcb: e.tensor_tensor(out=Cst, in0=Cst, in1=dcb, op=ALU.mult), [Cst_r, dc_r], [Cst_r])
                op("act", lambda e, Cst=Cst, Cbf=Cbf: e.copy(out=Cbf, in_=Cst), [Cst_r], [Cbf_r])
                dn, dn_r = dns[s_]
                op("act", lambda e, dn=dn, hn=hn: e.activation(out=dn, in_=hn[:, :, 64], func=AF.Abs), [hn_r], [dn_r])
                op("dve", lambda e, dn=dn: e.tensor_scalar_max(out=dn, in0=dn, scalar1=1.0), [dn_r], [dn_r])
                op("dve", lambda e, dn=dn: e.reciprocal(out=dn, in_=dn), [dn_r], [dn_r])
                hf, hf_r = hfs[s_]
                hfv = hf.rearrange("p (a b) -> p a b", a=4)
                dnb = dn.unsqueeze(2).to_broadcast([128, 4, 64])
                if d == 0:
                    op("dve", lambda e, hfv=hfv, hn=hn, dnb=dnb: e.tensor_tensor(out=hfv, in0=hn[:, :, 0:64], in1=dnb, op=ALU.mult),
                       [hn_r, dn_r], [hf_r])
                    dma(hF[t0:t0 + 128, :], hf, reads=[hf_r])
                    return
                if c < 2 and not with_ctx:
                    return
                dma(hf, hF[t0:t0 + 128, :], writes=[hf_r])
                om, om_r = oms[s_]
                dma(om, om_tok[t0:t0 + 128, :], writes=[om_r])
                hnv = hn[:, :, 0:64]
                op("dve", lambda e, hnv=hnv, dnb=dnb: e.tensor_tensor(out=hnv, in0=hnv, in1=dnb, op=ALU.mult), [hn_r, dn_r], [hn_r])
                op("dve", lambda e, hfv=hfv, hnv=hnv: e.tensor_tensor(out=hfv, in0=hfv, in1=hnv, op=ALU.add), [hf_r, hn_r], [hf_r])
                op("pool", lambda e, hnv=hnv, hfv=hfv: e.tensor_tensor(out=hnv, in0=hfv, in1=hfv, op=ALU.mult), [hf_r], [hn_r])
                op("dve", lambda e, dn=dn, hnv=hnv: e.tensor_reduce(out=dn, in_=hnv, axis=AX.X, op=ALU.add), [hn_r], [dn_r])
                op("act", lambda e, dn=dn: e.activation(out=dn, in_=dn, func=AF.Sqrt, bias=EPS, scale=1.0 / 64), [dn_r], [dn_r])
                op("dve", lambda e, dn=dn: e.reciprocal(out=dn, in_=dn), [dn_r], [dn_r])
                op("act", lambda e, om=om: e.activation(out=om, in_=om, func=AF.Sigmoid), [om_r], [om_r])
                op("pool", lambda e, om=om: e.tensor_tensor(out=om, in0=om, in1=ngt, op=ALU.mult), [om_r, ngt_r], [om_r])
                op("dve", lambda e, hfv=hfv, dnb=dnb: e.tensor_tensor(out=hfv, in0=hfv, in1=dnb, op=ALU.mult), [hf_r, dn_r], [hf_r])
                ymb = kt
                op("dve", lambda e, ymb=ymb, hf=hf, om=om: e.tensor_tensor(out=ymb, in0=hf, in1=om, op=ALU.mult), [hf_r, om_r], [kt_r])
                tb, tb_r = banks[7], bres[7]
                tbv = tb[:, 0:128].bitcast(BF16)
                for cc in range(2):
                    op("pe", lambda e, cc=cc, tbv=tbv, ymb=ymb: e.transpose(tbv[:, cc * 128:(cc + 1) * 128], ymb[:, cc * 128:(cc + 1) * 128], identb),
                       [kt_r, identb_r], [tb_r])
                yo = k_
                op("act", lambda e, yo=yo, tbv=tbv: e.copy(out=yo, in_=tbv.rearrange("p (a b) -> p a b", a=2)), [tb_r], [k_r])
                dma(ymT[:, :, t0:t0 + 128].rearrange("c p t -> p c t"), yo, reads=[k_r])

            prep(0, order[0])
            for si, c in enumerate(order):
                if si + 1 < len(order):
                    prep(si + 1, order[si + 1])
                seq(si, c)


    def phase_C(l, xsrc, with_ctx):
        wgb, wgb_r = tile((8, 3072), BF16)
        wba, wba_r = tile((4, 1024), BF16)
        wbs, wbs_r = tile((2, 1024), BF16)
        wbm, wbm_r = tile((2, 1024), BF16)
        wo, wo_r = tile((8, 1024), BF16)
        stg = [tile((8, 512), F32)]
        si = 0
        def wload(dst, dst_r, src, nk, ncols):
            nonlocal si
            for c0 in range(0, ncols, 512):
                st_, st_r = stg[0]
                cast_load(dst[:, :, c0:c0 + 512], dst_r, src[:, c0:c0 + 512].rearrange("(kc p) n -> p kc n", p=128), None,
                          st_[:, 0:nk, :], st_r, eng=("pool" if si % 2 == 0 else "dve"))
                si += 1
        wload(wgb, wgb_r, w_gb[l], 8, 3072)
        wload(wba, wba_r, wb_a[l], 4, 1024)
        wload(wbs, wbs_r, wb_s[l], 2, 1024)
        wload(wbm, wbm_r, wb_m[l], 2, 1024)
        wload(wo, wo_r, w_out[l], 8, 1024)
        xt, xt_r = tile((8, 512), F32)
        sq, _ = tile((8, 512), F32)
        sq_r = [Res() for _ in range(8)]
        rs, rs_r = tile((512,), F32)
        hT, hT_r = tile((8, 512), BF16)
        yat, yat_r = tile((4, 512), BF16)
        yst, yst_r = tile((2, 512), BF16)
        ymt, ymt_r = tile((2, 512), BF16)
        sig, _ = tile((3, 512), F32)
        sig_r = [Res() for _ in range(3)]
        t1, t1_r = tile((512,), F32)
        t2, t2_r = tile((512,), F32)
        yT, yT_r = tile((8, 512), BF16)
        hTs = [(hT, hT_r), tile((8, 512), BF16)]
        xts = [(xt, xt_r), stg[0]]
        tiles_ = token_tiles(with_ctx)

        def ln_C(ti):
            t0, n = tiles_[ti]
            x_, x_r = xts[ti % 2]
            dma(x_[:, :, 0:n], xsrc[:, :, t0:t0 + n].rearrange("c p t -> p c t"), writes=[x_r])
            norm_mod(x_, x_r, n, hTs[ti % 2][0], hTs[ti % 2][1], gs1, gs1_r, 0, 1 if t0 < CTX else 0, sq, sq_r, rs, rs_r,
                     banks[0], bres[0], l)

        ln_C(0)
        for ti, (t0, n) in enumerate(tiles_):
            if ti + 1 < len(tiles_):
                ln_C(ti + 1)
            which = 1 if t0 < CTX else 0
            xt, xt_r = xts[ti % 2]
            hT, hT_r = hTs[ti % 2]
            dma(yat[:, :, 0:n], yaT[:, :, t0:t0 + n].rearrange("c p t -> p c t"), writes=[yat_r])
            dma(yst[:, :, 0:n], ysT[:, :, t0:t0 + n].rearrange("c p t -> p c t"), writes=[yst_r])
            dma(ymt[:, :, 0:n], ymT[:, :, t0:t0 + n].rearrange("c p t -> p c t"), writes=[ymt_r])
            for fc in range(8):
                for b in range(3):
                    bk, bk_r = banks[1 + b], bres[1 + b]
                    for kc in range(8):
                        op("pe", lambda e, bk=bk, kc=kc, b=b, fc=fc: e.matmul(
                            bk[:, 0:n], lhsT=wgb[:, kc, b * 1024 + fc * 128:b * 1024 + (fc + 1) * 128], rhs=hT[:, kc, 0:n],
                            start=(kc == 0), stop=(kc == 7)), [wgb_r, hT_r], [bk_r])
                    op("act", lambda e, bk=bk, b=b: e.activation(out=sig[:, b, 0:n], in_=bk[:, 0:n], func=AF.Sigmoid), [bk_r], [sig_r[b]])
                for b, (w_, w_r, y_, y_r, nk) in enumerate(((wba, wba_r, yat, yat_r, 4), (wbs, wbs_r, yst, yst_r, 2), (wbm, wbm_r, ymt, ymt_r, 2))):
                    bk, bk_r = banks[4 + b], bres[4 + b]
                    for kc in range(nk):
                        op("pe", lambda e, bk=bk, kc=kc, w_=w_, y_=y_, fc=fc, nk=nk: e.matmul(
                            bk[:, 0:n], lhsT=w_[:, kc, fc * 128:(fc + 1) * 128], rhs=y_[:, kc, 0:n],
                            start=(kc == 0), stop=(kc == nk - 1)), [w_r, y_r], [bk_r])
                op("dve", lambda e: e.tensor_tensor(out=t1[:, 0:n], in0=banks[4][:, 0:n], in1=sig[:, 0, 0:n], op=ALU.mult), [bres[4], sig_r[0]], [t1_r])
                op("dve", lambda e: e.tensor_tensor(out=t2[:, 0:n], in0=banks[5][:, 0:n], in1=sig[:, 1, 0:n], op=ALU.mult), [bres[5], sig_r[1]], [t2_r])
                op("pool", lambda e: e.tensor_tensor(out=t1[:, 0:n], in0=t1[:, 0:n], in1=t2[:, 0:n], op=ALU.add), [t1_r, t2_r], [t1_r])
                op("dve", lambda e: e.tensor_tensor(out=t2[:, 0:n], in0=banks[6][:, 0:n], in1=sig[:, 2, 0:n], op=ALU.mult), [bres[6], sig_r[2]], [t2_r])
                op("pool", lambda e, fc=fc: e.tensor_tensor(out=yT[:, fc, 0:n], in0=t1[:, 0:n], in1=t2[:, 0:n], op=ALU.add), [t1_r, t2_r], [yT_r])
            for fo in range(8):
                bk, bk_r = banks[1 + fo % 3], bres[1 + fo % 3]
                for kc in range(8):
                    op("pe", lambda e, bk=bk, kc=kc, fo=fo: e.matmul(
                        bk[:, 0:n], lhsT=wo[:, kc, fo * 128:(fo + 1) * 128], rhs=yT[:, kc, 0:n], start=(kc == 0), stop=(kc == 7)),
                       [wo_r, yT_r], [bk_r])
                op("dve", lambda e, bk=bk, fo=fo: e.scalar_tensor_tensor(
                    out=xt[:, fo, 0:n], in0=bk[:, 0:n], scalar=modv[:, l, 16 + fo, which:which + 1], in1=xt[:, fo, 0:n],
                    op0=ALU.mult, op1=ALU.add), [bk_r, modv_r, xt_r], [xt_r])
            dma(xsrc[:, :, t0:t0 + n].rearrange("c p t -> p c t"), xt[:, :, 0:n], reads=[xt_r])

    def phase_D(l, xsrc, xdst, with_ctx):
        wu, wu_r = tile((8, 2 * D_FF), BF16)
        wd, wd_r = tile((22, 1024), BF16)
        stg, stg_r = tile((8, 256), F32)
        for c0 in range(0, 2 * D_FF, 256):
            cast_load(wu[:, :, c0:c0 + 256], wu_r, w_up[l, :, c0:c0 + 256].rearrange("(kc p) n -> p kc n", p=128), None,
                      stg, stg_r, eng=("pool" if (c0 // 256) % 2 == 0 else "dve"))
        for k0 in range(0, 22, 2):
            for c0 in range(0, 1024, 1024):
                cast_load(wd[:, k0:k0 + 2, :], wd_r, w_dn[l, k0 * 128:(k0 + 2) * 128, :].rearrange("(kc p) n -> p kc n", p=128), None,
                          stg.rearrange("p a b -> p (a b)")[:, 0:2048].rearrange("p (a b) -> p a b", a=2), stg_r,
                          eng=("pool" if (k0 // 2) % 2 == 0 else "dve"))
        cwt, cwt_r = tile((3, 44), F32)
        cbt, cbt_r = tile((44,), F32)
        dma(cwt, cw[l], writes=[cwt_r])
        dma(cbt, cb[l], writes=[cbt_r])
        xt, xt_r = tile((8, 258), F32)
        sq, _ = tile((8, 258), F32)
        sq_r = [Res() for _ in range(8)]
        rs, rs_r = tile((258,), F32)
        hT, hT_r = tile((8, 258), BF16)
        gb_, gb_r = tile((22, 256), BF16)
        tas = [tile((256,), F32) for _ in range(2)]
        tvs = [tile((256,), F32) for _ in range(2)]
        sas = [tile((256,), F32) for _ in range(2)]
        tl = []
        if with_ctx:
            tl.append((0, 0, CTX))
        for t0 in range(CTX, NT, 256):
            tl.append((t0, CTX, NT))
        hTs = [(hT, hT_r), tile((8, 258), BF16)]
        xts = [(xt, xt_r), tile((8, 258), F32)]

        def ln_D(ti):
            t0, seg0, seg1 = tl[ti]
            x_, x_r = xts[ti % 2]
            lo, hi = t0 - 1, t0 + 257
            zc = []
            clo, chi = max(lo, seg0), min(hi, seg1)
            if lo < seg0:
                op("pool", lambda e: e.memset(x_[:, :, 0:1], 1.0), [], [x_r])
                zc.append(0)
            if hi > seg1:
                op("pool", lambda e: e.memset(x_[:, :, 257:258], 1.0), [], [x_r])
                zc.append(257)
            dma(x_[:, :, clo - lo:chi - lo], xsrc[:, :, clo:chi].rearrange("c p t -> p c t"), writes=[x_r])
            norm_mod(x_, x_r, 258, hTs[ti % 2][0], hTs[ti % 2][1], gs2, gs2_r, 3, 1 if t0 < CTX else 0, sq, sq_r, rs, rs_r,
                     banks[0], bres[0], l, zero_cols=zc)

        ln_D(0)
        for ti, (t0, seg0, seg1) in enumerate(tl):
            if ti + 1 < len(tl):
                ln_D(ti + 1)
            which = 1 if t0 < CTX else 0
            xt, xt_r = xts[ti % 2]
            hT, hT_r = hTs[ti % 2]
            for j in range(22):
                ta, ta_r = tas[j % 2]
                tv, tv_r = tvs[j % 2]
                sa, sa_r = sas[j % 2]
                for (ch, bk, bk_r, tt, tt_r) in ((j, banks[1 + (j % 3) * 2], bres[1 + (j % 3) * 2], ta, ta_r),
                                                 (22 + j, banks[2 + (j % 3) * 2], bres[2 + (j % 3) * 2], tv, tv_r)):
                    for kc in range(8):
                        op("pe", lambda e, bk=bk, kc=kc, ch=ch: e.matmul(
                            bk[:, 0:258], lhsT=wu[:, kc, ch * 128:(ch + 1) * 128], rhs=hT[:, kc, :], start=(kc == 0), stop=(kc == 7)),
                           [wu_r, hT_r], [bk_r])
                    op("act", lambda e, bk=bk, ch=ch, tt=tt: e.activation(out=tt, in_=bk[:, 0:256], func=AF.Identity,
                                                                          bias=cbt[:, ch:ch + 1], scale=cwt[:, 0, ch:ch + 1]),
                       [bk_r, cbt_r, cwt_r], [tt_r])
                    op("dve", lambda e, bk=bk, ch=ch, tt=tt: e.scalar_tensor_tensor(
                        out=tt, in0=bk[:, 1:257], scalar=cwt[:, 1, ch:ch + 1], in1=tt, op0=ALU.mult, op1=ALU.add), [bk_r, cwt_r, tt_r], [tt_r])
                    op("dve", lambda e, bk=bk, ch=ch, tt=tt: e.scalar_tensor_tensor(
                        out=tt, in0=bk[:, 2:258], scalar=cwt[:, 2, ch:ch + 1], in1=tt, op0=ALU.mult, op1=ALU.add), [bk_r, cwt_r, tt_r], [tt_r])
                op("act", lambda e, sa=sa, ta=ta: e.activation(out=sa, in_=ta, func=AF.Silu), [ta_r], [sa_r])
                op("pool", lambda e, j=j, sa=sa, tv=tv: e.tensor_tensor(out=gb_[:, j, :], in0=sa, in1=tv, op=ALU.mult), [sa_r, tv_r], [gb_r])
            for fo in range(8):
                bk, bk_r = banks[7 if fo % 2 == 0 else 0], bres[7 if fo % 2 == 0 else 0]
                for kc in range(22):
                    op("pe", lambda e, bk=bk, kc=kc, fo=fo: e.matmul(
                        bk[:, 0:256], lhsT=wd[:, kc, fo * 128:(fo + 1) * 128], rhs=gb_[:, kc, :], start=(kc == 0), stop=(kc == 21)),
                       [wd_r, gb_r], [bk_r])
                op("dve", lambda e, bk=bk, fo=fo: e.scalar_tensor_tensor(
                    out=xt[:, fo, 1:257], in0=bk[:, 0:256], scalar=modv[:, l, 40 + fo, which:which + 1], in1=xt[:, fo, 1:257],
                    op0=ALU.mult, op1=ALU.add), [bk_r, modv_r, xt_r], [xt_r])
            dma(xdst[:, :, t0:t0 + 256].rearrange("c p t -> p c t"), xt[:, :, 1:257], reads=[xt_r])

    def phase_E(xsrc):
        xt, xt_r = tile((8, 512), F32)
        sq, _ = tile((8, 512), F32)
        sq_r = [Res() for _ in range(8)]
        rs, rs_r = tile((512,), F32)
        ots = [tile((1024,), F32) for _ in range(2)]
        fins = []
        bi = 0
        for (t0, n) in token_tiles(False):
            dma(xt[:, :, 0:n], xsrc[:, :, t0:t0 + n].rearrange("c p t -> p c t"), writes=[xt_r])
            for c in range(8):
                if c % 2 == 0:
                    op("act", lambda e, c=c: e.activation(out=sq[:, c, 0:n], in_=xt[:, c, 0:n], func=AF.Square), [xt_r], [sq_r[c]])
                else:
                    op("pool", lambda e, c=c: e.tensor_tensor(out=sq[:, c, 0:n], in0=xt[:, c, 0:n], in1=xt[:, c, 0:n], op=ALU.mult), [xt_r], [sq_r[c]])
            for c in range(8):
                op("pe", lambda e, c=c: e.matmul(banks[0][:, 0:n], lhsT=ones_f, rhs=sq[:, c, 0:n], start=(c == 0), stop=(c == 7)),
                   [sq_r[c], ones_r], [bres[0]])
            op("act", lambda e: e.activation(out=rs[:, 0:n], in_=banks[0][:, 0:n], func=AF.Sqrt, bias=EPS, scale=1.0 / D), [bres[0]], [rs_r])
            op("dve", lambda e: e.reciprocal(out=rs[:, 0:n], in_=rs[:, 0:n]), [rs_r], [rs_r])
            for c in range(8):
                op("dve", lambda e, c=c: e.scalar_tensor_tensor(out=sq[:, c, 0:n], in0=xt[:, c, 0:n], scalar=fnt[:, c:c + 1], in1=rs[:, 0:n],
                                                               op0=ALU.mult, op1=ALU.mult), [xt_r, rs_r, fnt_r], [sq_r[c]])
            for sb in range(n // 128):
                ot, ot_r = ots[bi % 2]
                for half in range(2):
                    bk, bk_r = banks[1 + (bi * 2 + half) % 4], bres[1 + (bi * 2 + half) % 4]
                    for j in range(4):
                        c = half * 4 + j
                        op("pe", lambda e, bk=bk, j=j, c=c, sb=sb: e.transpose(bk[:, j * 128:(j + 1) * 128], sq[:, c, sb * 128:(sb + 1) * 128], ident),
                           [sq_r[c], ident_r], [bk_r])
                    if half == 0:
                        op("act", lambda e, bk=bk, ot=ot: e.copy(out=ot[:, 0:512], in_=bk[:, 0:512]), [bk_r], [ot_r])
                    else:
                        op("dve", lambda e, bk=bk, ot=ot: e.tensor_copy(out=ot[:, 512:1024], in_=bk[:, 0:512]), [bk_r], [ot_r])
                bi += 1
                tt = t0 - CTX + sb * 128
                fins.append(dma(y_out[tt:tt + 128, :], ot, reads=[ot_r]))
        return fins

    def phase_s5(l, with_ctx):
        NB, NCH, G = NT // 8, NT // 128, 16
        INV2PI = 1.0 / TWO_PI
        cols, cols_r = tile((4,), F32)
        dma(cols, s5_cols, writes=[cols_r])
        mphi, mphi_r = tile((2, 136), F32)
        dma(mphi.rearrange("p a b -> p (a b)"), s5_mphi.partition_broadcast(128), writes=[mphi_r])
        mpsi, mpsi_r = tile((2, 136), F32)
        dma(mpsi.rearrange("p a b -> p (a b)"), s5_mpsi.partition_broadcast(128), writes=[mpsi_r])
        mphs, mphs_r = tile((2, 16), F32)
        dma(mphs.rearrange("p a b -> p (a b)"), s5_mphs.partition_broadcast(128), writes=[mphs_r])
        at, at_r = tile((2, 16, 2), F32)
        dma(at.rearrange("p a b c -> p (a b c)"), s5_a[l], writes=[at_r])
        ldt, ldt_r = tile((2, 16), F32)
        dma(ldt.rearrange("p a b -> p (a b)"), s5_logdt[l].partition_broadcast(128), writes=[ldt_r])
        bt, bt_r = tile((16, 16, 2), F32)
        dma(bt.rearrange("p a b c -> p (a b c)"), s5_b[l], writes=[bt_r])
        ct, ct_r = tile((16, 16, 2), F32)
        dma(ct.rearrange("p a b c -> p (a b c)"), s5_c[l], writes=[ct_r])
        dcol, dcol_r = tile((16,), F32)
        dma(dcol, s5_dcol[l], writes=[dcol_r])
        m0, m0_r = tile((2, 128), F32)
        dma(m0, s5_m0.rearrange("d k m -> k d m"), writes=[m0_r])
        swp, swp_r = tile((128,), F32)
        dma(swp, s5_swap, writes=[swp_r])
        gst, gst_r = tile((2, 256), F32)
        gw, gw_r = tile((2, 256), BF16)
        cast_load(gw, gw_r, s5_glu_w[l].rearrange("(kc p) n -> p kc n", p=128), None, gst, gst_r, eng="dve")
        glb, glb_r = tile((2,), F32)
        dma(glb, s5_glu_b[l], writes=[glb_r])
        sel, sel_r = tile((64, 128), BF16)
        U, _ = tile((G, NB), BF16)
        NBG = (NCH + 31) // 32
        bgs = [(b * 32, min(32, NCH - b * 32)) for b in range(NBG)]
        U_r = [[Res() for _ in bgs] for _ in range(G)]

        small = lambda: tile((2, 16), F32)
        dts, dts_r = small()
        op("act", lambda e: e.activation(out=dts, in_=ldt, func=AF.Exp), [ldt_r], [dts_r])
        dre, dre_r = small()
        dim, dim_r = small()
        op("dve", lambda e: e.tensor_tensor(out=dre, in0=dts, in1=at[:, :, :, 0], op=ALU.mult), [dts_r, at_r], [dre_r])
        op("dve", lambda e: e.tensor_tensor(out=dim, in0=dts, in1=at[:, :, :, 1], op=ALU.mult), [dts_r, at_r], [dim_r])
        tI, tI_r = tile((2, 16), I32)
        tF, tF_r = small()

        def sin_red(dst, dst_r, ang, ang_r, tI, tI_r, tF, tF_r):
            op("dve", lambda e: e.tensor_scalar(out=tI, in0=ang, scalar1=INV2PI, scalar2=None, op0=ALU.mult), [ang_r], [tI_r])
            op("dve", lambda e: e.tensor_copy(out=tF, in_=tI), [tI_r], [tF_r])
            op("dve", lambda e: e.scalar_tensor_tensor(out=tF, in0=tF, scalar=-TWO_PI, in1=ang, op0=ALU.mult, op1=ALU.add),
               [tF_r, ang_r], [tF_r])
            op("act", lambda e: e.activation(out=dst, in_=tF, func=AF.Sin), [tF_r], [dst_r])

        def lam_pow(kpow):
            a1, a1_r = small()
            a2, a2_r = small()
            mg, mg_r = small()
            sr, sr_r = small()
            ci_, ci_r = small()
            op("dve", lambda e: e.tensor_scalar(out=a1, in0=dim, scalar1=float(kpow), scalar2=None, op0=ALU.mult), [dim_r], [a1_r])
            op("dve", lambda e: e.tensor_scalar(out=a2, in0=dim, scalar1=float(kpow), scalar2=math.pi / 2, op0=ALU.mult, op1=ALU.add),
               [dim_r], [a2_r])
            sin_red(sr, sr_r, a1, a1_r, tI, tI_r, tF, tF_r)
            sin_red(ci_, ci_r, a2, a2_r, tI, tI_r, tF, tF_r)
            op("act", lambda e: e.activation(out=mg, in_=dre, func=AF.Exp, scale=float(kpow)), [dre_r], [mg_r])
            op("dve", lambda e: e.tensor_tensor(out=sr, in0=sr, in1=mg, op=ALU.mult), [sr_r, mg_r], [sr_r])
            op("dve", lambda e: e.tensor_tensor(out=ci_, in0=ci_, in1=mg, op=ALU.mult), [ci_r, mg_r], [ci_r])
            return (ci_, ci_r), (sr, sr_r)

        (lr, lr_r), (li, li_r) = lam_pow(1)
        (l128r, l128r_r), (l128i, l128i_r) = lam_pow(128)
        ar, ai = at[:, :, :, 0], at[:, :, :, 1]
        den, den_r = small()
        t1, t1_r = small()
        t2, t2_r = small()
        cr, cr_r = small()
        ci, ci_r = small()
        op("dve", lambda e: e.tensor_tensor(out=den, in0=ar, in1=ar, op=ALU.mult), [at_r], [den_r])
        op("dve", lambda e: e.tensor_tensor(out=t1, in0=ai, in1=ai, op=ALU.mult), [at_r], [t1_r])
        op("dve", lambda e: e.tensor_tensor(out=den, in0=den, in1=t1, op=ALU.add), [den_r, t1_r], [den_r])
        op("dve", lambda e: e.reciprocal(out=den, in_=den), [den_r], [den_r])
        op("dve", lambda e: e.tensor_scalar_add(out=lr, in0=lr, scalar1=-1.0), [lr_r], [lr_r])
        op("dve", lambda e: e.tensor_tensor(out=t1, in0=lr, in1=ar, op=ALU.mult), [lr_r, at_r], [t1_r])
        op("dve", lambda e: e.tensor_tensor(out=t2, in0=li, in1=ai, op=ALU.mult), [li_r, at_r], [t2_r])
        op("dve", lambda e: e.tensor_tensor(out=t1, in0=t1, in1=t2, op=ALU.add), [t1_r, t2_r], [t1_r])
        op("dve", lambda e: e.tensor_tensor(out=cr, in0=t1, in1=den, op=ALU.mult), [t1_r, den_r], [cr_r])
        op("dve", lambda e: e.tensor_tensor(out=t1, in0=li, in1=ar, op=ALU.mult), [li_r, at_r], [t1_r])
        op("dve", lambda e: e.tensor_tensor(out=t2, in0=lr, in1=ai, op=ALU.mult), [lr_r, at_r], [t2_r])
        op("dve", lambda e: e.tensor_tensor(out=t1, in0=t1, in1=t2, op=ALU.subtract), [t1_r, t2_r], [t1_r])
        op("dve", lambda e: e.tensor_tensor(out=ci, in0=t1, in1=den, op=ALU.mult), [t1_r, den_r], [ci_r])
        br, bi = bt[:, :, :, 0], bt[:, :, :, 1]
        BA, BA_r = tile((2, 16, 16), F32)
        BB, BB_r = tile((2, 16, 16), F32)
        w1, w1_r = tile((16, 16), F32)
        w2, w2_r = tile((16, 16), F32)
        for d in range(2):
            crb = cr[:, d, :].unsqueeze(2).to_broadcast([128, 16, 16])
            cib = ci[:, d, :].unsqueeze(2).to_broadcast([128, 16, 16])
            op("dve", lambda e: e.tensor_tensor(out=w1, in0=br, in1=crb, op=ALU.mult), [bt_r, cr_r], [w1_r])
            op("dve", lambda e: e.tensor_tensor(out=w2, in0=bi, in1=cib, op=ALU.mult), [bt_r, ci_r], [w2_r])
            op("dve", lambda e: e.tensor_tensor(out=BA[:, d], in0=w1, in1=w2, op=ALU.subtract), [w1_r, w2_r], [BA_r])
            op("dve", lambda e: e.tensor_tensor(out=w1, in0=bi, in1=crb, op=ALU.mult), [bt_r, cr_r], [w1_r])
            op("dve", lambda e: e.tensor_tensor(out=w2, in0=br, in1=cib, op=ALU.mult), [bt_r, ci_r], [w2_r])
            op("dve", lambda e: e.tensor_tensor(out=w1, in0=w1, in1=w2, op=ALU.add), [w1_r, w2_r], [w1_r])
            op("dve", lambda e: e.tensor_scalar(out=BB[:, d], in0=w1, scalar1=cols[:, 2:3], scalar2=None, op0=ALU.mult), [w1_r, cols_r], [BB_r])
        CA, CA_r = tile((16, 16), F32)
        CB, CB_r = tile((16, 16), F32)
        op("dve", lambda e: e.tensor_scalar(out=CA, in0=ct[:, :, :, 0], scalar1=cols[:, 3:4], scalar2=None, op0=ALU.mult), [ct_r, cols_r], [CA_r])
        op("dve", lambda e: e.tensor_scalar(out=CB, in0=ct[:, :, :, 1], scalar1=-1.0, scalar2=None, op0=ALU.mult), [ct_r], [CB_r])
        ais, ais_r = small()
        op("dve", lambda e: e.tensor_scalar(out=ais, in0=l128i, scalar1=cols[:, 3:4], scalar2=None, op0=ALU.mult), [l128i_r, cols_r], [ais_r])

        HG = 8
        pa, pa_r = tile((HG, 136), F32)
        pI, pI_r = tile((HG, 136), I32)
        pF, pF_r = tile((HG, 136), F32)
        pm, pm_r = tile((HG, 136), F32)

        def pw_tables(d, g0, mt, mt_r, n, outA, outA_r, outB, outB_r):
            mtb = mt.unsqueeze(1).to_broadcast([128, HG, n])
            dimb = dim[:, d, g0:g0 + HG].unsqueeze(2).to_broadcast([128, HG, n])
            dreb = dre[:, d, g0:g0 + HG].unsqueeze(2).to_broadcast([128, HG, n])
            pa_, pI_, pF_, pm_ = pa[:, :, 0:n], pI[:, :, 0:n], pF[:, :, 0:n], pm[:, :, 0:n]
            op("dve", lambda e: e.tensor_tensor(out=pm_, in0=mtb, in1=dreb, op=ALU.mult), [mt_r, dre_r], [pm_r])
            op("act", lambda e: e.activation(out=pm_, in_=pm_, func=AF.Exp), [pm_r], [pm_r])
            for (o_, o_r, phc) in ((outA, outA_r, 0), (outB, outB_r, 1)):
                op("dve", lambda e: e.tensor_tensor(out=pa_, in0=mtb, in1=dimb, op=ALU.mult), [mt_r, dim_r], [pa_r])
                op("dve", lambda e: e.tensor_scalar(out=pa_, in0=pa_, scalar1=cols[:, phc:phc + 1], scalar2=None, op0=ALU.add), [pa_r, cols_r], [pa_r])
                sin_red(o_, o_r, pa_, pa_r, pI_, pI_r, pF_, pF_r)
                op("pool", lambda e: e.tensor_tensor(out=o_, in0=o_, in1=pm_, op=ALU.mult), [o_r, pm_r], [o_r])

        oa, oa_r = tile((136, 16), F32)
        aw, aw_r = oa.rearrange("p a b -> p (a b)")[:, 0:G * 128].rearrange("p (a b) -> p a b", a=G), oa_r
        ob_, ob_r = tile((136, 16), F32)

        def outer(dst, dst_r, PA, PA_r, PB, PB_r, gi, XA, XA_r, XB, XB_r, n):
            pab = PA[:, gi, :].unsqueeze(2).to_broadcast([128, n, 16])
            pbb = PB[:, gi, :].unsqueeze(2).to_broadcast([128, n, 16])
            xab = XA.unsqueeze(1).to_broadcast([128, n, 16])
            xbb = XB.unsqueeze(1).to_broadcast([128, n, 16])
            op("dve", lambda e: e.tensor_tensor(out=oa[:, 0:n, :], in0=pab, in1=xab, op=ALU.mult), [PA_r, XA_r], [oa_r])
            op("pool", lambda e: e.tensor_tensor(out=ob_[:, 0:n, :], in0=pbb, in1=xbb, op=ALU.mult), [PB_r, XB_r], [ob_r])
            op("dve", lambda e: e.tensor_tensor(out=dst, in0=oa[:, 0:n, :], in1=ob_[:, 0:n, :], op=ALU.add), [oa_r, ob_r], [dst_r])

        dma(sel, s5_sel, writes=[sel_r])
        ust, ust_r = tile((2, 4096), BF16)
        cgs = [(c0, min(512, NB - c0)) for c0 in range(0, NB, 512)]
        bg_of_col = lambda c0: c0 // 512
        for (c0, nb) in cgs:
            dma(ust[:, :, 0:nb * 8], usT[:, :, c0 * 8:(c0 + nb) * 8].rearrange("c p t -> p c t"), writes=[ust_r])
            for g in range(G):
                hf, gl = g // 8, g % 8
                bk, bk_r = banks[g % 2], bres[g % 2]
                uv = ust[:, hf, 0:nb * 8].rearrange("p (tb s) -> p tb s", s=8)
                for s8 in range(8):
                    op("pe", lambda e: e.matmul(bk[:, 0:nb], lhsT=sel[:, gl * 8 + s8, :], rhs=uv[:, :, s8], start=(s8 == 0), stop=(s8 == 7)),
                       [sel_r, ust_r], [bk_r])
                if g % 2 == 0:
                    op("act", lambda e: e.copy(out=U[:, g, c0:c0 + nb], in_=bk[:, 0:nb]), [bk_r], [U_r[g][bg_of_col(c0)]])
                else:
                    op("dve", lambda e: e.tensor_copy(out=U[:, g, c0:c0 + nb], in_=bk[:, 0:nb]), [bk_r], [U_r[g][bg_of_col(c0)]])
        dma(sel, s5_selT, writes=[sel_r])

        Eall, Eall_r = tile((2, G, NCH), F32)
        Zst, Zst_r = tile((G, NCH), F32)
        Zin, Zin_r = tile((2, G, NCH), BF16)
        op("pool", lambda e: e.memset(Zin, 0.0), [], [Zin_r])
        A128, A128_r = tile((G, 128), F32)
        PhA, PhA_r = tile((HG, 136), F32)
        PhB, PhB_r = tile((HG, 136), F32)
        Phi, Phi_r = tile((136, 16), BF16)
        Wt, Wt_r = tile((16, 128), BF16)
        for d in range(2):
            for g0 in range(0, G, HG):
                pw_tables(d, g0, mphi[:, d, :], mphi_r, 136, PhA, PhA_r, PhB, PhB_r)
                for gi in range(HG):
                    g = g0 + gi
                    outer(Phi, Phi_r, PhA, PhA_r, PhB, PhB_r, gi, BA[:, d, g], BA_r, BB[:, d, g], BB_r, 136)
                    woff = 0 if d == 0 else 8
                    for hb in range(2):
                        bk, bk_r = banks[1 + hb], bres[1 + hb]
                        bkv = bk[:, 0:512].bitcast(BF16)
                        for jj in range(8):
                            j = hb * 8 + jj
                            src = Phi[:, woff + 8 * j:woff + 8 * j + 8, :].rearrange("p a b -> p (a b)")
                            op("pe", lambda e: e.transpose(bkv[:, jj * 128:(jj + 1) * 128], src, identb), [Phi_r, identb_r], [bk_r])
                        dstw = Wt[:, hb * 8:(hb + 1) * 8, :]
                        srcw = bkv.rearrange("p (a b) -> p a b", a=8)
                        if hb == 0:
                            op("act", lambda e: e.copy(out=dstw, in_=srcw), [bk_r], [Wt_r])
                        else:
                            op("dve", lambda e: e.tensor_copy(out=dstw, in_=srcw), [bk_r], [Wt_r])
                    eb, eb_r = banks[3], bres[3]
                    ucv = U[:, g, :].rearrange("p (ch j) -> p ch j", j=16)
                    for j in range(16):
                        op("pe", lambda e: e.matmul(eb[:, 0:NCH], lhsT=Wt[:, j, :], rhs=ucv[:, :, j], start=(j == 0), stop=(j == 15)),
                           [Wt_r] + U_r[g], [eb_r])
                    op("act", lambda e: e.copy(out=Eall[:, d, g, :], in_=eb[:, 0:NCH]), [eb_r], [Eall_r])
            idb = ident.unsqueeze(1).to_broadcast([128, G, 128])
            swb = swp.unsqueeze(1).to_broadcast([128, G, 128])
            arb = l128r[:, d, :].unsqueeze(2).to_broadcast([128, G, 128])
            aib = ais[:, d, :].unsqueeze(2).to_broadcast([128, G, 128])
            op("dve", lambda e: e.tensor_tensor(out=A128, in0=idb, in1=arb, op=ALU.mult), [ident_r, l128r_r], [A128_r])
            op("pool", lambda e: e.tensor_tensor(out=aw, in0=swb, in1=aib, op=ALU.mult), [swp_r, ais_r], [aw_r])
            op("dve", lambda e: e.tensor_tensor(out=A128, in0=A128, in1=aw, op=ALU.add), [A128_r, aw_r], [A128_r])
            order = list(range(NCH)) if d == 0 else [1, 0] + list(range(NCH - 1, 1, -1))
            prev = None
            zb, zb_r = banks[4], bres[4]
            for ch in order:
                if prev is None:
                    op("dve", lambda e: e.tensor_copy(out=Zst[:, :, ch], in_=Eall[:, d, :, ch]), [Eall_r], [Zst_r])
                else:
                    for g in range(G):
                        op("pe", lambda e: e.matmul(zb[:, g:g + 1], lhsT=A128[:, g, :], rhs=Zst[:, g, prev:prev + 1], start=True, stop=True),
                           [A128_r, Zst_r], [zb_r])
                    op("dve", lambda e: e.tensor_tensor(out=Zst[:, :, ch], in0=zb[:, 0:G], in1=Eall[:, d, :, ch], op=ALU.add),
                       [zb_r, Eall_r], [Zst_r])
                    op("act", lambda e: e.copy(out=Zin[:, d, :, ch], in_=Zst[:, :, prev]), [Zst_r], [Zin_r])
                prev = ch

        PsA = [(PhA, PhA_r), tile((HG, 136), F32)]
        PsB = [(PhB, PhB_r), tile((HG, 136), F32)]
        PsmA = [tile((HG, 16), F32) for _ in range(2)]
        PsmB = [tile((HG, 16), F32) for _ in range(2)]
        Rt = [(Phi, Phi_r), tile((136, 16), BF16)]
        Lt = [tile((16, 16), BF16) for _ in range(2)]
        Tt = [(Wt, Wt_r), tile((16, 128), BF16)]
        yv, yv_r = tile((512,), F32)
        y2, y2_r = tile((512,), F32)
        y3, y3_r = tile((512,), F32)
        for g0 in range(0, G, HG):
            for d in range(2):
                pw_tables(d, g0, mpsi[:, d, :], mpsi_r, 136, PsA[d][0], PsA[d][1], PsB[d][0], PsB[d][1])
                pw_tables(d, g0, mphs[:, d, :], mphs_r, 16, PsmA[d][0], PsmA[d][1], PsmB[d][0], PsmB[d][1])
            for gi in range(HG):
                g = g0 + gi
                for d in range(2):
                    R, R_r = Rt[d]
                    L, L_r = Lt[d]
                    Tt_, Tt_r = Tt[d]
                    outer(R, R_r, PsA[d][0], PsA[d][1], PsB[d][0], PsB[d][1], gi, CA[:, g], CA_r, CB[:, g], CB_r, 136)
                    outer(L, L_r, PsmA[d][0], PsmA[d][1], PsmB[d][0], PsmB[d][1], gi, BA[:, d, g], BA_r, BB[:, d, g], BB_r, 16)
                    for q4 in range(4):
                        tb_, tb_r = banks[4], bres[4]
                        for dd in range(4):
                            Dl = q4 * 4 + dd
                            lh = L[:, 0:8, :] if Dl == 0 else L[:, 8:16, :]
                            if d == 0:
                                r0 = 0 if Dl == 0 else 8 * (Dl - 1) + 1
                            else:
                                r0 = 0 if Dl == 0 else 8 * (Dl - 1)
                            op("pe", lambda e: e.matmul(tb_[:, dd * 128:(dd + 1) * 128], lhsT=lh.rearrange("p a b -> p (a b)"),
                                                        rhs=R[:, r0:r0 + 8, :].rearrange("p a b -> p (a b)"), start=True, stop=True),
                               [L_r, R_r], [tb_r])
                        if q4 == 0:
                            op("dve", lambda e: e.tensor_tensor(out=y3[:, 0:128], in0=tb_[:, 0:128], in1=m0[:, d, :], op=ALU.mult), [tb_r, m0_r], [y3_r])
                            if d == 0:
                                op("dve", lambda e: e.scalar_tensor_tensor(out=Tt_[:, 0, :], in0=ident, scalar=dcol[:, g:g + 1], in1=y3[:, 0:128],
                                                                          op0=ALU.mult, op1=ALU.add), [y3_r, ident_r, dcol_r], [Tt_r])
                            else:
                                op("dve", lambda e: e.tensor_copy(out=Tt_[:, 0, :], in_=y3[:, 0:128]), [y3_r], [Tt_r])
                            op("act", lambda e: e.copy(out=Tt_[:, 1:4, :], in_=tb_[:, 128:512].rearrange("p (a b) -> p a b", a=3)), [tb_r], [Tt_r])
                        else:
                            op("act", lambda e: e.copy(out=Tt_[:, q4 * 4:q4 * 4 + 4, :], in_=tb_[:, 0:512].rearrange("p (a b) -> p a b", a=4)), [tb_r], [Tt_r])
                for bgi, (ch0, nch) in enumerate(bgs):
                    yb, yb_r = banks[5 + (g * NBG + bgi) % 2], bres[5 + (g * NBG + bgi) % 2]
                    ncol = nch * 16
                    ybv = yb[:, 0:ncol].rearrange("p (ch j) -> p ch j", j=16)
                    ucv = U[:, g, ch0 * 16:ch0 * 16 + ncol].rearrange("p (ch j) -> p ch j", j=16)
                    first = True
                    for d in range(2):
                        R, R_r = Rt[d]
                        Tt_, Tt_r = Tt[d]
                        for Dl in range(16):
                            if d == 0:
                                o_, r_ = ybv[:, :, Dl:16], ucv[:, :, 0:16 - Dl]
                            else:
                                o_, r_ = ybv[:, :, 0:16 - Dl], ucv[:, :, Dl:16]
                            op("pe", lambda e: e.matmul(o_, lhsT=Tt_[:, Dl, :], rhs=r_, start=first, stop=False), [Tt_r, U_r[g][bgi]], [yb_r])
                            first = False
                        for j in range(16):
                            r0 = 8 * j + 1 if d == 0 else 8 * (15 - j)
                            last = (d == 1 and j == 15)
                            op("pe", lambda e: e.matmul(ybv[:, :, j], lhsT=R[:, r0:r0 + 8, :].rearrange("p a b -> p (a b)"),
                                                        rhs=Zin[:, d, g, ch0:ch0 + nch], start=False, stop=last), [R_r, Zin_r], [yb_r])
                    n_ = ncol
                    op("act", lambda e: e.copy(out=yv[:, 0:n_], in_=yb[:, 0:n_]), [yb_r], [yv_r])
                    op("pool", lambda e: e.tensor_tensor(out=y2[:, 0:n_], in0=yv[:, 0:n_], in1=yv[:, 0:n_], op=ALU.mult), [yv_r], [y2_r])
                    op("dve", lambda e: e.tensor_scalar(out=y2[:, 0:n_], in0=y2[:, 0:n_], scalar1=0.044715, scalar2=1.0, op0=ALU.mult, op1=ALU.add),
                       [y2_r], [y2_r])
                    op("pool", lambda e: e.tensor_tensor(out=y2[:, 0:n_], in0=y2[:, 0:n_], in1=yv[:, 0:n_], op=ALU.mult), [y2_r, yv_r], [y2_r])
                    op("act", lambda e: e.activation(out=y2[:, 0:n_], in_=y2[:, 0:n_], func=AF.Sigmoid, scale=1.5957691216057308), [y2_r], [y2_r])
                    op("dve", lambda e: e.tensor_tensor(out=U[:, g, ch0 * 16:ch0 * 16 + n_], in0=yv[:, 0:n_], in1=y2[:, 0:n_], op=ALU.mult),
                       [yv_r, y2_r], [U_r[g][bgi]])

        yfm, yfm_r = ust, ust_r
        yo, yo_r = tile((2, 512), BF16)
        sg, sg_r = tile((512,), F32)
        for (c0, nb) in cgs:
            bgi = bg_of_col(c0)
            for hf in range(2):
                yfv = yfm[:, hf, 0:nb * 8].rearrange("p (tb s) -> p tb s", s=8)
                for t8 in range(8):
                    bk, bk_r = banks[t8 % 2], bres[t8 % 2]
                    for gl in range(8):
                        g = hf * 8 + gl
                        op("pe", lambda e: e.matmul(bk[:, 0:nb], lhsT=sel[:, gl * 8 + t8, :], rhs=U[:, g, c0:c0 + nb], start=(gl == 0), stop=(gl == 7)),
                           [sel_r, U_r[g][bgi]], [bk_r])
                    if t8 % 2 == 0:
                        op("act", lambda e: e.copy(out=yfv[:, :, t8], in_=bk[:, 0:nb]), [bk_r], [yfm_r])
                    else:
                        op("dve", lambda e: e.tensor_copy(out=yfv[:, :, t8], in_=bk[:, 0:nb]), [bk_r], [yfm_r])
            ntok = nb * 8
            for tt in range(0, ntok, 512):
                n_ = min(512, ntok - tt)
                tglob = c0 * 8 + tt
                for fc in range(2):
                    bk, bk_r = banks[7], bres[7]
                    for kc in range(2):
                        op("pe", lambda e: e.matmul(bk[:, 0:n_], lhsT=gw[:, kc, fc * 128:(fc + 1) * 128], rhs=yfm[:, kc, tt:tt + n_],
                                                    start=(kc == 0), stop=(kc == 1)), [gw_r, yfm_r], [bk_r])
                    op("act", lambda e: e.activation(out=sg[:, 0:n_], in_=bk[:, 0:n_], func=AF.Sigmoid, bias=glb[:, fc:fc + 1], scale=1.0),
                       [bk_r, glb_r], [sg_r])
                    op("dve", lambda e: e.tensor_tensor(out=yo[:, fc, 0:n_], in0=yfm[:, fc, tt:tt + n_], in1=sg[:, 0:n_], op=ALU.mult),
                       [yfm_r, sg_r], [yo_r])
                dma(ysT[:, :, tglob:tglob + n_].rearrange("c p t -> p c t"), yo[:, :, 0:n_], reads=[yo_r])

    def phase_s5_stub(l, with_ctx):
        z, z_r = tile((2, 512), BF16)
        op("pool", lambda e: e.memset(z, 0.0), [], [z_r])
        for (t0, n) in token_tiles(True):
            dma(ysT[:, :, t0:t0 + n].rearrange("c p t -> p c t"), z[:, :, 0:n], reads=[z_r])

    fins = []
    steps = [("mod", lambda: phase_mod()), ("tin", lambda: phase_transpose_in())]
    cur = 0
    for l in range(depth):
        with_ctx = l < depth - 1
        steps.append(("A%d" % l, lambda l=l, cur=cur: phase_A(l, xT[cur])))
        steps.append(("attn%d" % l, lambda l=l, w=with_ctx: phase_attn(l, w)))
        steps.append(("ml%d" % l, lambda l=l, w=with_ctx: phase_mlstm(l, w)))
        steps.append(("s5%d" % l, lambda l=l, w=with_ctx: (phase_s5 if HAVE_S5 else phase_s5_stub)(l, w)))
        steps.append(("C%d" % l, lambda l=l, cur=cur, w=with_ctx: phase_C(l, xT[cur], w)))
        steps.append(("D%d" % l, lambda l=l, cur=cur, w=with_ctx: phase_D(l, xT[cur], xT[1 - cur], w)))
        cur = 1 - cur
    steps.append(("E", lambda cur=cur: fins.extend(phase_E(xT[cur]))))
    for i, (nm, fn) in enumerate(steps):
        if upto is not None and nm == upto:
            break
        if i > 0:
            new_phase()
        fn()
    stats = S.emit(final_ops=fins)
    es.close()
    return nc, dbg_outs, stats


HAVE_S5 = True
import os
MLCUT = int(os.environ.get('MLCUT', '0'))
MLDIRS = int(os.environ.get('MLDIRS', '2'))


def make_in_maps(inp, T, depth=2, ncores=8):
    fm, tm, gb = _col_layout()
    cst = _consts(T)
    f = lambda a: np.ascontiguousarray(a, dtype=np.float32)
    shared = {}
    shared["mod_w"] = f(inp["mod_w"])
    shared["mod_b"] = f(inp["mod_b"].reshape(depth, 48, 128).transpose(0, 2, 1))
    shared["norm1_g"] = f(inp["norm1_g"].reshape(depth, 8, 128).transpose(0, 2, 1))
    shared["norm2_g"] = f(inp["norm2_g"].reshape(depth, 8, 128).transpose(0, 2, 1))
    shared["final_norm_g"] = f(inp["final_norm_g"].reshape(8, 128).T)
    shared["w_fm"] = f(inp["w_in"][:, :, fm])
    shared["w_tm"] = f(inp["w_in"][:, :, tm])
    shared["w_gb"] = f(inp["w_in"][:, :, gb])
    for k in ("w_branch_attn", "w_branch_s5", "w_branch_ml", "w_out", "ffn_w_up", "ffn_w_down"):
        shared[k] = f(inp[k])
    shared["ffn_conv_w"] = f(inp["ffn_conv_w"].reshape(depth, 3, 44, 128).transpose(0, 3, 1, 2))
    shared["ffn_conv_b"] = f(inp["ffn_conv_b"].reshape(depth, 44, 128).transpose(0, 2, 1))
    shared["attn_sink"] = f(inp["attn_sink"].reshape(depth, 1, 8))
    gbias = np.stack([np.concatenate([inp["ml_igate_b"][l, 0], inp["ml_fgate_b"][l, 0], inp["ml_igate_b"][l, 1], inp["ml_fgate_b"][l, 1]])
                      for l in range(depth)])
    shared["ml_gate_b"] = f(gbias.reshape(depth, 1, 16))
    shared["ml_norm_g"] = f(inp["ml_norm_g"].reshape(depth, 1, 256))
    a_st = np.stack([inp["s5_a_re"], inp["s5_a_im"]], axis=-1)
    a_st = a_st.transpose(0, 3, 1, 2, 4)
    shared["s5_a"] = f(np.concatenate([a_st, a_st], axis=1).reshape(depth, 128, 64))
    shared["s5_logdt"] = f(inp["s5_log_dt"].reshape(depth, 1, 32))
    b_st = np.stack([inp["s5_b_re"], inp["s5_b_im"]], axis=-1).transpose(0, 2, 1, 3, 4)
    shared["s5_b"] = f(np.concatenate([b_st, b_st], axis=1).reshape(depth, 128, 512))
    c_st = np.stack([inp["s5_c_re"], inp["s5_c_im"]], axis=-1).transpose(0, 3, 1, 2, 4)
    shared["s5_c"] = f(np.concatenate([c_st, c_st], axis=1).reshape(depth, 128, 512))
    dd = inp["s5_d"].reshape(depth, 16, 16).transpose(0, 2, 1)
    shared["s5_dcol"] = f(np.tile(dd, (1, 8, 1)))
    shared["s5_glu_w"] = f(inp["s5_glu_w"])
    shared["s5_glu_b"] = f(inp["s5_glu_b"].reshape(depth, 2, 128).transpose(0, 2, 1))
    shared.update(cst)
    maps = []
    for b in range(ncores):
        m = dict(shared)
        m["x"] = f(inp["x"][b, :T])
        m["ctx"] = f(inp["ctx"][b])
        cv = np.stack([inp["c"][b], inp["c_ctx"]], axis=-1)
        m["cvec"] = f(cv.reshape(8, 128, 2).transpose(1, 0, 2))
        maps.append(m)
    return maps


_CACHE = {}


def kernel(**inputs):
    inp = {k: np.asarray(v) for k, v in inputs.items()}
    B, T = inp["x"].shape[0], inp["x"].shape[1]
    if T not in _CACHE:
        _CACHE[T] = build(T)
    nc = _CACHE[T][0]
    maps = make_in_maps(inp, T, ncores=B)
    res = run_bass_kernel_spmd(nc, maps, core_ids=list(range(B)))
    return np.stack([r["y"] for r in res.results], axis=0).astype(np.float32)
```

```python
import math
from contextlib import ExitStack
import numpy as np
import ml_dtypes
import concourse.bass as bass
import concourse.mybir as mybir
from concourse.bass_utils import run_bass_kernel_spmd

F32 = mybir.dt.float32
BF16 = mybir.dt.bfloat16
I32 = mybir.dt.int32
AF = mybir.ActivationFunctionType
ALU = mybir.AluOpType
AX = mybir.AxisListType

D = 1024
CTX = 256
NH, NKV, HD = 8, 2, 64
W_S5, S5G, S5P = 256, 16, 64
MLH = 4
D_FF = 2816
EPS = 1e-6
ENG = ("pe", "act", "dve", "pool", "sp")
TWO_PI = 2.0 * math.pi


class Res:
    __slots__ = ("last_w", "readers")

    def __init__(self):
        self.last_w = None
        self.readers = []


class Op:
    __slots__ = ("eng", "fn", "dma", "deps", "sig", "idx", "sem", "val")


class _Rec:
    def __init__(self):
        self.call = None

    def __getattr__(self, name):
        def f(*a, **k):
            self.call = (name, a, k)
            return None
        return f


class Sched:
    def __init__(self, nc, ndma=8):
        self.nc, self.ops, self.ndma = nc, [], ndma
        self.phase = Res()

    def op(self, eng, fn, reads=(), writes=(), dma=False, barrier=False):
        rec = _Rec()
        fn(rec)
        name_, a_, k_ = rec.call
        fn = lambda engine, name_=name_, a_=a_, k_=k_: getattr(engine, name_)(*a_, **k_)
        o = Op()
        o.eng, o.fn, o.dma, o.deps, o.sig, o.sem, o.val = eng, fn, dma, set(), False, None, None
        o.idx = len(self.ops)
        reads = list(reads)
        writes = list(writes)
        if barrier:
            writes.append(self.phase)
        else:
            reads.append(self.phase)
        for r in reads:
            if r.last_w is not None:
                o.deps.add(r.last_w)
        for r in writes:
            if r.last_w is not None:
                o.deps.add(r.last_w)
            o.deps.update(r.readers)
        for r in reads:
            r.readers.append(o.idx)
        for r in writes:
            r.last_w = o.idx
            r.readers = []
        o.deps.discard(o.idx)
        self.ops.append(o)
        return o

    def emit(self, final_ops=()):
        nc, ops = self.nc, self.ops
        for o in ops:
            keep = set()
            for d in o.deps:
                p = ops[d]
                if p.eng == o.eng and not p.dma and o.eng in ("pe", "sp"):
                    continue
                keep.add(d)
            o.deps = keep
            for d in keep:
                ops[d].sig = True
        for f in final_ops:
            f.sig = True
        st = ExitStack()
        sems = {e: st.enter_context(nc.semaphore("s_" + e)) for e in ENG}
        dsems = [st.enter_context(nc.semaphore("d_%d" % i)) for i in range(self.ndma)]
        cnt = {e: 0 for e in ENG}
        dcnt = 0
        prev_user = {}
        for o in ops:
            if o.dma:
                n = dcnt
                dcnt += 1
                o.sem = dsems[n % self.ndma]
                o.val = 16 * (n // self.ndma + 1)
                if (n % self.ndma) in prev_user:
                    o.deps.add(prev_user[n % self.ndma])
                prev_user[n % self.ndma] = o.idx
                o.sig = True
            elif o.sig:
                cnt[o.eng] += 1
                o.sem, o.val = sems[o.eng], cnt[o.eng]
        per = {e: [o for o in ops if o.eng == e] for e in ENG}
        stats = dict(nops=len(ops), nwait=0)

        def run(e, engine):
            waited = {}
            for o in per[e]:
                need = {}
                for d in o.deps:
                    p = ops[d]
                    k = id(p.sem)
                    if k not in need or need[k][1] < p.val:
                        need[k] = (p.sem, p.val)
                for k, (sem, val) in need.items():
                    if waited.get(k, 0) >= val:
                        continue
                    engine.wait_ge(sem, val)
                    stats["nwait"] += 1
                    waited[k] = val
                ins = o.fn(engine)
                if o.dma:
                    ins.then_inc(o.sem, 16)
                elif o.sig:
                    ins.then_inc(o.sem, 1)
            if e == "sp":
                for f in final_ops:
                    engine.wait_ge(f.sem, f.val)

        with nc.Block() as block:
            @block.sync
            def _(eng):
                run("sp", eng)

            @block.tensor
            def _(eng):
                run("pe", eng)

            @block.scalar
            def _(eng):
                run("act", eng)

            @block.vector
            def _(eng):
                run("dve", eng)

            @block.gpsimd
            def _(eng):
                run("pool", eng)
        st.close()
        return stats


def _rope_perm():
    p = np.arange(64)
    out = np.empty(64, dtype=np.int64)
    for d in range(64):
        base = (d // 32) * 32
        r = d % 32
        out[d] = base + (r + 16 if r < 16 else r - 16)
    return out


def _col_layout():
    o_q, o_k, o_v = 0, 512, 640
    o_us = 768
    o_qm, o_km, o_vm, o_om = 1024, 1280, 1536, 1792
    o_gm = 2048
    o_gb = 2064
    perm = _rope_perm()
    fm = []
    for c in range(4):
        fm += [o_q + c * 64 + d for d in range(64)] + [o_q + (4 + c) * 64 + d for d in range(64)]
    for c in range(4):
        fm += [o_q + c * 64 + perm[d] for d in range(64)] + [o_q + (4 + c) * 64 + perm[d] for d in range(64)]
    fm += [o_k + d for d in range(128)]
    fm += [o_k + h * 64 + perm[d] for h in range(2) for d in range(64)]
    fm += [o_us + d for d in range(256)]
    fm += [o_qm + d for d in range(256)]
    fm += [o_km + d for d in range(256)]
    tm = [o_v + d for d in range(128)] + [o_km + d for d in range(256)] + [o_vm + d for d in range(256)] \
        + [o_om + d for d in range(256)] + [o_gm + d for d in range(16)]
    gb = [o_gb + d for d in range(3072)]
    return np.array(fm), np.array(tm), np.array(gb)


FM_Q, FM_QP, FM_K, FM_KP, FM_US, FM_QM, FM_KM = 0, 4, 8, 9, 10, 12, 14
NFM = 16
TM_V, TM_KM, TM_VM, TM_OM, TM_GM = 0, 128, 384, 640, 896
NTM = 912


def _consts(T):
    NT = CTX + T
    c = {}
    c["ident_f"] = np.eye(128, dtype=np.float32)
    k = np.arange(128)[:, None]
    q = np.arange(128)[None, :]
    c["mask_le"] = (k <= q).astype(np.float32)
    c["mask_ge"] = (k >= q).astype(np.float32)
    rows = T // 64
    row = np.repeat(np.arange(rows, dtype=np.float32), 64)
    col = np.tile(np.arange(64, dtype=np.float32), rows)
    inv = (10000.0 ** (-np.arange(16, dtype=np.float32) / 16)).astype(np.float32)
    ang_r = (row[:, None] * inv[None, :]).astype(np.float32)
    ang_c = (col[:, None] * inv[None, :]).astype(np.float32)
    cosT = np.ones((128, NT), np.float32)
    sinT = np.zeros((128, NT), np.float32)
    for p in range(128):
        d = p % 64
        a = ang_r if d < 32 else ang_c
        r = d % 32
        f = r % 16
        cosT[p, CTX:] = np.cos(a[:, f])
        s = np.sin(a[:, f])
        sinT[p, CTX:] = -s if r < 16 else s
    c["rope_cos"] = cosT
    hp = math.pi / 2
    cols = np.zeros((128, 4), np.float32)
    cols[:64, 0] = hp
    cols[64:, 1] = hp
    cols[:64, 2], cols[64:, 2] = -1.0, 1.0
    cols[:64, 3], cols[64:, 3] = 1.0, -1.0
    c["s5_cols"] = cols
    mi = np.arange(136, dtype=np.float32)
    c["s5_mphi"] = np.stack([127.0 - mi, mi - 8.0]).reshape(1, 272).astype(np.float32)
    qi = np.arange(136)
    c["s5_mpsi"] = np.stack([qi.astype(np.float32), (8 * (qi // 8) + 8 - qi % 8).astype(np.float32)]).reshape(1, 272).astype(np.float32)
    s8 = np.arange(8, dtype=np.float32)
    c["s5_mphs"] = np.stack([np.concatenate([-s8, 7.0 - s8]), np.concatenate([s8 - 8.0, s8])]).reshape(1, 32).astype(np.float32)
    rr = np.arange(128)[:, None] // 16
    cc = np.arange(128)[None, :] // 16
    c["s5_m0"] = np.stack([(cc >= rr), (rr >= cc)]).astype(np.float32)
    sw = np.zeros((128, 128), np.float32)
    sw[np.arange(128), (np.arange(128) + 64) % 128] = 1.0
    c["s5_swap"] = sw
    sel = np.zeros((128, 64, 128), np.float32)
    selT = np.zeros((128, 64, 128), np.float32)
    for gl in range(8):
        for s in range(8):
            for ch in range(16):
                sel[gl * 16 + ch, gl * 8 + s, s * 16 + ch] = 1.0
                selT[s * 16 + ch, gl * 8 + s, gl * 16 + ch] = 1.0
    c["s5_sel"] = sel.astype(ml_dtypes.bfloat16)
    c["s5_selT"] = selT.astype(ml_dtypes.bfloat16)
    c["rope_sin"] = sinT
    return c


def build(T, depth=2, debug=False, upto=None):
    NT = CTX + T
    nc = bass.Bass("TRN2", target_bir_lowering=False)
    es = ExitStack()
    S = Sched(nc)
    dbg_outs = {}

    def din(name, shape, dt=F32):
        return nc.dram_tensor(name, list(shape), dt, kind="ExternalInput").ap()

    def dscr(name, shape, dt=F32):
        kind = "ExternalOutput" if debug else "Internal"
        t = nc.dram_tensor(name, list(shape), dt, kind=kind).ap()
        if debug:
            dbg_outs[name] = t
        return t

    x_in = din("x", [T, D])
    ctx_in = din("ctx", [CTX, D])
    cvec = din("cvec", [128, 8, 2])
    mod_w = din("mod_w", [depth, D, 6 * D])
    mod_b = din("mod_b", [depth, 128, 48])
    n1g = din("norm1_g", [depth, 128, 8])
    n2g = din("norm2_g", [depth, 128, 8])
    fng = din("final_norm_g", [128, 8])
    w_fm = din("w_fm", [depth, D, NFM * 128])
    w_tm = din("w_tm", [depth, D, NTM])
    w_gb = din("w_gb", [depth, D, 3072])
    wb_a = din("w_branch_attn", [depth, 512, D])
    wb_s = din("w_branch_s5", [depth, 256, D])
    wb_m = din("w_branch_ml", [depth, 256, D])
    w_out = din("w_out", [depth, D, D])
    w_up = din("ffn_w_up", [depth, D, 2 * D_FF])
    cw = din("ffn_conv_w", [depth, 128, 3, 44])
    cb = din("ffn_conv_b", [depth, 128, 44])
    w_dn = din("ffn_w_down", [depth, D_FF, D])
    sink = din("attn_sink", [depth, 1, 8])
    ml_gb = din("ml_gate_b", [depth, 1, 16])
    ml_ng = din("ml_norm_g", [depth, 1, 256])
    s5_a = din("s5_a", [depth, 128, 64])
    s5_logdt = din("s5_logdt", [depth, 1, 32])
    s5_b = din("s5_b", [depth, 128, 512])
    s5_c = din("s5_c", [depth, 128, 512])
    s5_dcol = din("s5_dcol", [depth, 128, 16])
    s5_glu_w = din("s5_glu_w", [depth, 256, 256])
    s5_glu_b = din("s5_glu_b", [depth, 128, 2])
    s5_cols = din("s5_cols", [128, 4])
    s5_mphi = din("s5_mphi", [1, 272])
    s5_mpsi = din("s5_mpsi", [1, 272])
    s5_mphs = din("s5_mphs", [1, 32])
    s5_m0 = din("s5_m0", [2, 128, 128])
    s5_swap = din("s5_swap", [128, 128])
    s5_sel = din("s5_sel", [128, 64, 128], BF16)
    s5_selT = din("s5_selT", [128, 64, 128], BF16)
    ident_f = din("ident_f", [128, 128])
    mask_le = din("mask_le", [128, 128])
    mask_ge = din("mask_ge", [128, 128])
    rope_cos = din("rope_cos", [128, NT])
    rope_sin = din("rope_sin", [128, NT])
    y_out = nc.dram_tensor("y", [T, D], F32, kind="ExternalOutput").ap()

    xT = [dscr("xT%d" % i, [8, 128, NT]) for i in range(2)]
    qT = dscr("qT", [4, 128, NT], BF16)
    kT = dscr("kT", [128, NT], BF16)
    usT = dscr("usT", [2, 128, NT], BF16)
    qmT = dscr("qmT", [2, 128, NT], BF16)
    kmT = dscr("kmT", [2, 128, NT], BF16)
    v_tok = dscr("v_tok", [NT, 128], BF16)
    km_tok = dscr("km_tok", [NT, 256], BF16)
    vm_tok = dscr("vm_tok", [NT, 256], BF16)
    om_tok = dscr("om_tok", [NT, 256])
    gm_tok = dscr("gm_tok", [NT, 16])
    yaT = dscr("yaT", [4, 128, NT], BF16)
    ysT = dscr("ysT", [2, 128, NT], BF16)
    ymT = dscr("ymT", [2, 128, NT], BF16)
    hF = dscr("hF", [NT, 256])

    ARENA = 208896 // 2
    arena = es.enter_context(nc.sbuf_tensor("arena", [128, ARENA], BF16))
    off = [0]
    persist_off = [0]

    def tile(shape, dt, parts=128):
        n = int(np.prod(shape)) * (1 if dt == BF16 else 2)
        n = (n + 15) // 16 * 16
        assert off[0] + n <= ARENA, ("SBUF arena overflow", off[0], n)
        a = arena[0:parts, off[0]:off[0] + n]
        off[0] += n
        used = int(np.prod(shape)) * (1 if dt == BF16 else 2)
        a = a[:, 0:used]
        if dt != BF16:
            a = a.bitcast(dt)
        if len(shape) == 2:
            a = a.rearrange("p (a b) -> p a b", a=shape[0])
        elif len(shape) == 3:
            a = a.rearrange("p (a b c) -> p a b c", a=shape[0], b=shape[1])
        return a, Res()

    banks = [es.enter_context(nc.psum_tensor("bank%d" % i, [128, 512], F32)) for i in range(8)]
    bres = [Res() for _ in range(8)]
    junk, junk_r = tile((4,), F32)

    def barrier():
        S.op("pool", lambda e: e.memset(junk[:, 0:1], 0.0), writes=[junk_r], barrier=True)

    def new_phase():
        barrier()
        off[0] = persist_off[0]

    def dma(out, in_, reads=(), writes=()):
        return S.op("sp", lambda e: e.dma_start(out=out, in_=in_), reads, writes, dma=True)

    def op(eng, fn, reads=(), writes=()):
        return S.op(eng, fn, reads, writes)

    def cast_load(dst, dst_r, src_ap, shape, stage, stage_r, eng="pool"):
        dma(stage, src_ap, writes=[stage_r])
        if eng == "act":
            op("act", lambda e: e.copy(out=dst, in_=stage), [stage_r], [dst_r])
        else:
            op(eng, lambda e: e.tensor_copy(out=dst, in_=stage), [stage_r], [dst_r])

    ident, ident_r = tile((128,), F32)
    identb, identb_r = tile((128,), BF16)
    ones_f, ones_r = tile((128,), F32)
    mle, mle_r = tile((128,), F32)
    mge, mge_r = tile((128,), F32)
    mleb, mleb_r = tile((128,), BF16)
    mgeb, mgeb_r = tile((128,), BF16)
    mle8, mle8_r = tile((128,), BF16)
    mge8, mge8_r = tile((128,), BF16)
    modv, modv_r = tile((depth, 48, 2), F32)
    gs1, gs1_r = tile((depth, 8, 2), F32)
    gs2, gs2_r = tile((depth, 8, 2), F32)
    n1t, n1t_r = tile((depth, 8), F32)
    n2t, n2t_r = tile((depth, 8), F32)
    fnt, fnt_r = tile((8,), F32)
    persist_off[0] = off[0]

    dma(ident, ident_f, writes=[ident_r])
    dma(mle, mask_le, writes=[mle_r])
    dma(mge, mask_ge, writes=[mge_r])
    op("dve", lambda e: e.tensor_copy(out=identb, in_=ident), [ident_r], [identb_r])
    op("dve", lambda e: e.tensor_copy(out=mleb, in_=mle), [mle_r], [mleb_r])
    op("dve", lambda e: e.tensor_copy(out=mgeb, in_=mge), [mge_r], [mgeb_r])
    op("pool", lambda e: e.memset(ones_f, 1.0), writes=[ones_r])
    op("dve", lambda e: e.tensor_scalar_mul(out=mle8, in0=mle, scalar1=0.125), [mle_r], [mle8_r])
    op("dve", lambda e: e.tensor_scalar_mul(out=mge8, in0=mge, scalar1=0.125), [mge_r], [mge8_r])
    dma(n1t, n1g.rearrange("l p c -> p l c"), writes=[n1t_r])
    dma(n2t, n2g.rearrange("l p c -> p l c"), writes=[n2t_r])
    dma(fnt, fng, writes=[fnt_r])

    def phase_mod():
        cv, cv_r = tile((8, 2), F32)
        sc, sc_r = tile((8, 2), F32)
        dma(cv, cvec, writes=[cv_r])
        op("act", lambda e: e.activation(out=sc, in_=cv, func=AF.Silu), [cv_r], [sc_r])
        mb, mb_r = tile((depth, 48), F32)
        dma(mb, mod_b.rearrange("l p c -> p l c"), writes=[mb_r])
        wst = [tile((8, 1024), F32) for _ in range(2)]
        i = 0
        for l in range(depth):
            for piece in range(6):
                w, w_r = wst[i % 2]
                i += 1
                dma(w, mod_w[l, :, piece * 1024:(piece + 1) * 1024].rearrange("(kc p) n -> p kc n", p=128), writes=[w_r])
                bk, bk_r = banks[piece % 2], bres[piece % 2]
                for fc in range(8):
                    for kc in range(8):
                        op("pe", lambda e, w=w, fc=fc, kc=kc, bk=bk: e.matmul(
                            bk[:, fc * 2:fc * 2 + 2], lhsT=w[:, kc, fc * 128:(fc + 1) * 128], rhs=sc[:, kc, :],
                            start=(kc == 0), stop=(kc == 7)), [w_r, sc_r], [bk_r])
                dst = modv[:, l, piece * 8:(piece + 1) * 8, :]
                src = bk[:, 0:16].rearrange("p (a b) -> p a b", a=8)
                bb = mb[:, l, piece * 8:(piece + 1) * 8].unsqueeze(2).to_broadcast([128, 8, 2])
                op("dve", lambda e, dst=dst, src=src, bb=bb: e.tensor_tensor(out=dst, in0=src, in1=bb, op=ALU.add),
                   [bk_r, mb_r], [modv_r])
        for l in range(depth):
            for (gs, gs_r, nt, nt_r, pc) in ((gs1, gs1_r, n1t, n1t_r, 1), (gs2, gs2_r, n2t, n2t_r, 4)):
                src = modv[:, l, pc * 8:(pc + 1) * 8, :]
                nb = nt[:, l, :].unsqueeze(2).to_broadcast([128, 8, 2])
                dst = gs[:, l, :, :]
                op("dve", lambda e, dst=dst, src=src, nb=nb: e.scalar_tensor_tensor(
                    out=dst, in0=src, scalar=1.0, in1=nb, op0=ALU.add, op1=ALU.mult), [modv_r, nt_r], [gs_r])

    def phase_transpose_in():
        xin = [tile((1024,), F32) for _ in range(2)]
        xo = [tile((8, 128), F32) for _ in range(2)]
        nblk = NT // 128
        for b in range(nblk):
            t0 = b * 128
            xt, xt_r = xin[b % 2]
            src = ctx_in[t0:t0 + 128, :] if t0 < CTX else x_in[t0 - CTX:t0 - CTX + 128, :]
            dma(xt, src, writes=[xt_r])
            o, o_r = xo[b % 2]
            for half in range(2):
                bk, bk_r = banks[(b * 2 + half) % 4], bres[(b * 2 + half) % 4]
                for j in range(4):
                    c = half * 4 + j
                    op("pe", lambda e, bk=bk, j=j, xt=xt, c=c: e.transpose(
                        bk[:, j * 128:(j + 1) * 128], xt[:, c * 128:(c + 1) * 128], ident), [xt_r, ident_r], [bk_r])
                dst = o[:, half * 4:(half + 1) * 4, :]
                srcp = bk[:, :].rearrange("p (a b) -> p a b", a=4)
                if half == 0:
                    op("dve", lambda e, dst=dst, srcp=srcp: e.tensor_copy(out=dst, in_=srcp), [bk_r], [o_r])
                else:
                    op("act", lambda e, dst=dst, srcp=srcp: e.copy(out=dst, in_=srcp), [bk_r], [o_r])
            dma(xT[0][:, :, t0:t0 + 128].rearrange("c p t -> p c t"), o, reads=[o_r])

    def norm_mod(xt, xt_r, n, hT, hT_r, gs, gs_r, shcol, which, sq, sq_r, rs, rs_r, bk, bk_r, l, zero_cols=()):
        for c in range(8):
            if c % 2 == 0:
                op("act", lambda e, c=c: e.activation(out=sq[:, c, 0:n], in_=xt[:, c, 0:n], func=AF.Square), [xt_r], [sq_r[c]])
            else:
                op("pool", lambda e, c=c: e.tensor_tensor(out=sq[:, c, 0:n], in0=xt[:, c, 0:n], in1=xt[:, c, 0:n], op=ALU.mult),
                   [xt_r], [sq_r[c]])
        for c in range(8):
            op("pe", lambda e, c=c: e.matmul(bk[:, 0:n], lhsT=ones_f, rhs=sq[:, c, 0:n], start=(c == 0), stop=(c == 7)),
               [sq_r[c], ones_r], [bk_r])
        op("act", lambda e: e.activation(out=rs[:, 0:n], in_=bk[:, 0:n], func=AF.Sqrt, bias=EPS, scale=1.0 / D), [bk_r], [rs_r])
        op("dve", lambda e: e.reciprocal(out=rs[:, 0:n], in_=rs[:, 0:n]), [rs_r], [rs_r])
        for c in range(8):
            op("dve", lambda e, c=c: e.tensor_tensor(out=sq[:, c, 0:n], in0=xt[:, c, 0:n], in1=rs[:, 0:n], op=ALU.mult),
               [xt_r, rs_r], [sq_r[c]])
            op("act", lambda e, c=c: e.activation(out=hT[:, c, 0:n], in_=sq[:, c, 0:n], func=AF.Identity,
                                                  bias=modv[:, l, shcol * 8 + c, which:which + 1],
                                                  scale=gs[:, l, c, which:which + 1]), [sq_r[c], modv_r, gs_r], [hT_r])
        for zc in zero_cols:
            op("pool", lambda e, zc=zc: e.memset(hT[:, :, zc:zc + 1], 0.0), [], [hT_r])

    def token_tiles(with_ctx=True, size=512):
        tl = []
        if with_ctx:
            tl.append((0, CTX))
        t = CTX
        while t < NT:
            n = min(size, NT - t)
            tl.append((t, n))
            t += n
        return tl

    def phase_A(l, xsrc):
        wfm, wfm_r = tile((8, NFM * 128), BF16)
        wtm, wtm_r = tile((8, NTM), BF16)
        stg = [tile((8, 512), F32) for _ in range(2)]
        si = 0
        for cc in range(NFM * 128 // 512):
            st_, st_r = stg[si % 2]
            si += 1
            cast_load(wfm[:, :, cc * 512:(cc + 1) * 512], wfm_r,
                      w_fm[l, :, cc * 512:(cc + 1) * 512].rearrange("(kc p) n -> p kc n", p=128), None, st_, st_r,
                      eng=("pool" if cc % 2 == 0 else "dve"))
        for (c0, c1) in ((0, 512), (512, NTM)):
            st_, st_r = stg[si % 2]
            si += 1
            cast_load(wtm[:, :, c0:c1], wtm_r, w_tm[l, :, c0:c1].rearrange("(kc p) n -> p kc n", p=128), None,
                      st_[:, :, 0:c1 - c0], st_r, eng="pool")
        xts = [tile((8, 512), F32) for _ in range(2)]
        sq, _ = tile((8, 512), F32)
        sq_r = [Res() for _ in range(8)]
        rs, rs_r = tile((512,), F32)
        hT, hT_r = tile((8, 512), BF16)
        fo, fo_r = tile((NFM, 512), BF16)
        rc, rc_r = tile((512,), F32)
        rsn, rsn_r = tile((512,), F32)
        tmpa, tmpa_r = tile((512,), F32)
        tmpb, tmpb_r = tile((512,), F32)
        to_b, to_br = tile((640,), BF16)
        to_f, to_fr = tile((272,), F32)
        hTs = [(hT, hT_r), tile((8, 512), BF16)]
        tiles_ = token_tiles()

        def ln_A(ti):
            t0, n = tiles_[ti]
            xt, xt_r = xts[ti % 2]
            dma(xt[:, :, 0:n], xsrc[:, :, t0:t0 + n].rearrange("c p t -> p c t"), writes=[xt_r])
            norm_mod(xt, xt_r, n, hTs[ti % 2][0], hTs[ti % 2][1], gs1, gs1_r, 0, 1 if t0 < CTX else 0, sq, sq_r, rs, rs_r,
                     banks[0], bres[0], l)

        ln_A(0)
        for ti, (t0, n) in enumerate(tiles_):
            if ti + 1 < len(tiles_):
                ln_A(ti + 1)
            which = 1 if t0 < CTX else 0
            hT, hT_r = hTs[ti % 2]
            if which == 0:
                dma(rc[:, 0:n], rope_cos[:, t0:t0 + n], writes=[rc_r])
                dma(rsn[:, 0:n], rope_sin[:, t0:t0 + n], writes=[rsn_r])
            def fm_mm(m, bk, bk_r):
                for kc in range(8):
                    op("pe", lambda e, m=m, kc=kc, bk=bk: e.matmul(
                        bk[:, 0:n], lhsT=wfm[:, kc, m * 128:(m + 1) * 128], rhs=hT[:, kc, 0:n],
                        start=(kc == 0), stop=(kc == 7)), [wfm_r, hT_r], [bk_r])
            bi = 1
            for (m, mp) in [(FM_Q + c, FM_QP + c) for c in range(4)] + [(FM_K, FM_KP)]:
                b1, b1r = banks[1 + (bi % 3) * 2], bres[1 + (bi % 3) * 2]
                b2, b2r = banks[2 + (bi % 3) * 2], bres[2 + (bi % 3) * 2]
                bi += 1
                fm_mm(m, b1, b1r)
                if which == 0:
                    fm_mm(mp, b2, b2r)
                    op("dve", lambda e, b1=b1: e.tensor_tensor(out=tmpa[:, 0:n], in0=b1[:, 0:n], in1=rc[:, 0:n], op=ALU.mult),
                       [b1r, rc_r], [tmpa_r])
                    op("dve", lambda e, b2=b2: e.tensor_tensor(out=tmpb[:, 0:n], in0=b2[:, 0:n], in1=rsn[:, 0:n], op=ALU.mult),
                       [b2r, rsn_r], [tmpb_r])
                    op("pool", lambda e, m=m: e.tensor_tensor(out=fo[:, m, 0:n], in0=tmpa[:, 0:n], in1=tmpb[:, 0:n], op=ALU.add),
                       [tmpa_r, tmpb_r], [fo_r])
                else:
                    op("act", lambda e, m=m, b1=b1: e.copy(out=fo[:, m, 0:n], in_=b1[:, 0:n]), [b1r], [fo_r])
            for m in range(FM_US, NFM):
                b1, b1r = banks[1 + (m % 6)], bres[1 + (m % 6)]
                fm_mm(m, b1, b1r)
                if m % 2 == 0:
                    op("act", lambda e, m=m, b1=b1: e.copy(out=fo[:, m, 0:n], in_=b1[:, 0:n]), [b1r], [fo_r])
                else:
                    op("dve", lambda e, m=m, b1=b1: e.tensor_copy(out=fo[:, m, 0:n], in_=b1[:, 0:n]), [b1r], [fo_r])
            dma(qT[:, :, t0:t0 + n].rearrange("c p t -> p c t"), fo[:, FM_Q:FM_Q + 4, 0:n], reads=[fo_r])
            dma(kT[:, t0:t0 + n], fo[:, FM_K, 0:n], reads=[fo_r])
            dma(usT[:, :, t0:t0 + n].rearrange("c p t -> p c t"), fo[:, FM_US:FM_US + 2, 0:n], reads=[fo_r])
            dma(qmT[:, :, t0:t0 + n].rearrange("c p t -> p c t"), fo[:, FM_QM:FM_QM + 2, 0:n], reads=[fo_r])
            dma(kmT[:, :, t0:t0 + n].rearrange("c p t -> p c t"), fo[:, FM_KM:FM_KM + 2, 0:n], reads=[fo_r])
            for sb in range(n // 128):
                b1, b1r = banks[1 + (sb % 2) * 2], bres[1 + (sb % 2) * 2]
                b2, b2r = banks[2 + (sb % 2) * 2], bres[2 + (sb % 2) * 2]
                for (bk, bk_r, c0, c1) in ((b1, b1r, 0, 512), (b2, b2r, 512, NTM)):
                    for kc in range(8):
                        op("pe", lambda e, bk=bk, kc=kc, c0=c0, c1=c1, sb=sb: e.matmul(
                            bk[:, 0:c1 - c0], lhsT=hT[:, kc, sb * 128:(sb + 1) * 128], rhs=wtm[:, kc, c0:c1],
                            start=(kc == 0), stop=(kc == 7)), [hT_r, wtm_r], [bk_r])
                op("act", lambda e, b1=b1: e.copy(out=to_b[:, 0:512], in_=b1[:, 0:512]), [b1r], [to_br])
                op("dve", lambda e, b2=b2: e.tensor_copy(out=to_b[:, 512:640], in_=b2[:, 0:128]), [b2r], [to_br])
                op("dve", lambda e, b2=b2: e.tensor_copy(out=to_f, in_=b2[:, 128:400]), [b2r], [to_fr])
                tt = t0 + sb * 128
                dma(v_tok[tt:tt + 128, :], to_b[:, TM_V:TM_V + 128], reads=[to_br])
                dma(km_tok[tt:tt + 128, :], to_b[:, TM_KM:TM_KM + 256], reads=[to_br])
                dma(vm_tok[tt:tt + 128, :], to_b[:, TM_VM:TM_VM + 256], reads=[to_br])
                dma(om_tok[tt:tt + 128, :], to_f[:, 0:256], reads=[to_fr])
                dma(gm_tok[tt:tt + 128, :], to_f[:, 256:272], reads=[to_fr])

    def phase_attn(l, with_ctx):
        esk, esk_r = tile((8,), F32)
        dma(esk, sink[l].partition_broadcast(128), writes=[esk_r])
        op("act", lambda e: e.activation(out=esk, in_=esk, func=AF.Exp), [esk_r], [esk_r])
        kc_, kc_r = tile((CTX,), BF16)
        dma(kc_, kT[:, 0:CTX], writes=[kc_r])
        vc_, vc_r = tile((2, 2, 65), BF16)
        op("pool", lambda e: e.memset(vc_, 1.0), [], [vc_r])
        for j in range(2):
            dma(vc_[:, j, :, 0:64], v_tok[j * 128:(j + 1) * 128, :].rearrange("t (g d) -> t g d", g=2), writes=[vc_r])
        NS = 3
        qs = [tile((4, 128), BF16) for _ in range(NS)]
        ks = [tile((384,), BF16) for _ in range(NS)]
        vs = [tile((3, 2, 65), BF16) for _ in range(NS)]
        for (v_, v_r) in vs:
            op("pool", lambda e, v_=v_: e.memset(v_, 1.0), [], [v_r])
        pts = [tile((512,), BF16) for _ in range(10)]
        ya, ya_r = tile((512,), BF16)
        yat, yat_r = tile((4, 128), BF16)
        den, den_r = tile((4,), F32)
        nlat = T // 128
        blocks = [("lat", j) for j in range(nlat)]
        if with_ctx:
            blocks += [("ctx", 0), ("ctx", 1)]
        pi = 0
        for bi, (kind, j) in enumerate(blocks):
            q_, q_r = qs[bi % NS]
            k_, k_r = ks[bi % NS]
            v_, v_r = vs[bi % NS]
            tq = CTX + j * 128 if kind == "lat" else j * 128
            dma(q_, qT[:, :, tq:tq + 128].rearrange("c p t -> p c t"), writes=[q_r])
            keys = []
            if kind == "lat":
                lo, hi = max(0, j - 1), min(nlat - 1, j + 1)
                nk = hi - lo + 1
                dma(k_[:, 0:nk * 128], kT[:, CTX + lo * 128:CTX + (hi + 1) * 128], writes=[k_r])
                for i_ in range(nk):
                    dma(v_[:, i_, :, 0:64], v_tok[CTX + (lo + i_) * 128:CTX + (lo + i_ + 1) * 128, :].rearrange("t (g d) -> t g d", g=2),
                        writes=[v_r])
                for jj in range(lo, hi + 1):
                    m = None if jj == j else ("ge" if jj < j else "le")
                    keys.append((k_[:, (jj - lo) * 128:(jj - lo + 1) * 128], k_r, v_[:, jj - lo], v_r, m))
            for cj in range(2):
                keys.append((kc_[:, cj * 128:(cj + 1) * 128], kc_r, vc_[:, cj], vc_r, None))
            for g in range(2):
                ob, ob_r = banks[4 + g], bres[4 + g]
                plist = []
                for ki, (ka, ka_r, va, va_r, m) in enumerate(keys):
                    sbk, sbk_r = banks[pi % 4], bres[pi % 4]
                    pt, pt_r = pts[g * 5 + ki]
                    pi += 1
                    op("pe", lambda e, sbk=sbk, ka=ka, g=g, q_=q_: e.matmul(
                        sbk[:, 0:512], lhsT=ka[g * 64:(g + 1) * 64, :], rhs=q_[g * 64:(g + 1) * 64, :, :],
                        start=True, stop=True), [ka_r, q_r], [sbk_r])
                    op("act", lambda e, pt=pt, sbk=sbk: e.activation(out=pt, in_=sbk[:, 0:512], func=AF.Exp, scale=HD ** -0.5),
                       [sbk_r], [pt_r])
                    if m is not None:
                        mk, mk_r = (mgeb, mgeb_r) if m == "ge" else (mleb, mleb_r)
                        mb_ = mk.unsqueeze(1).to_broadcast([128, 4, 128])
                        ptv = pt.rearrange("p (a b) -> p a b", a=4)
                        op("dve", lambda e, ptv=ptv, mb_=mb_: e.tensor_tensor(out=ptv, in0=ptv, in1=mb_, op=ALU.mult),
                           [pt_r, mk_r], [pt_r])
                    plist.append((pt, pt_r, va, va_r))
                for hh in range(4):
                    for ki, (pt, pt_r, va, va_r) in enumerate(plist):
                        op("pe", lambda e, ob=ob, hh=hh, pt=pt, va=va, g=g, ki=ki, nk=len(plist): e.matmul(
                            ob[:, hh * 65:(hh + 1) * 65], lhsT=pt[:, hh * 128:(hh + 1) * 128], rhs=va[:, g, :],
                            start=(ki == 0), stop=(ki == nk - 1)), [pt_r, va_r], [ob_r])
                obv = ob[:, 0:260].rearrange("p (a b) -> p a b", a=4)
                op("dve", lambda e, obv=obv, g=g: e.tensor_tensor(out=den, in0=obv[:, :, 64], in1=esk[:, g * 4:(g + 1) * 4], op=ALU.add),
                   [ob_r, esk_r], [den_r])
                op("dve", lambda e: e.reciprocal(out=den, in_=den), [den_r], [den_r])
                yav = ya[:, g * 256:(g + 1) * 256].rearrange("p (a b) -> p a b", a=4)
                db = den.unsqueeze(2).to_broadcast([128, 4, 64])
                op("dve", lambda e, yav=yav, obv=obv, db=db: e.tensor_tensor(out=yav, in0=obv[:, :, 0:64], in1=db, op=ALU.mult),
                   [ob_r, den_r], [ya_r])
            tb, tb_r = banks[6], bres[6]
            tbv = tb[:, 0:256].bitcast(BF16)
            for c in range(4):
                op("pe", lambda e, c=c, tbv=tbv: e.transpose(tbv[:, c * 128:(c + 1) * 128], ya[:, c * 128:(c + 1) * 128], identb),
                   [ya_r, identb_r], [tb_r])
            op("act", lambda e, tbv=tbv: e.copy(out=yat, in_=tbv.rearrange("p (a b) -> p a b", a=4)), [tb_r], [yat_r])
            dma(yaT[:, :, tq:tq + 128].rearrange("c p t -> p c t"), yat, reads=[yat_r])

    def phase_mlstm(l, with_ctx):
        gbt, gbt_r = tile((16,), F32)
        dma(gbt, ml_gb[l].partition_broadcast(128), writes=[gbt_r])
        ngt, ngt_r = tile((256,), F32)
        dma(ngt, ml_ng[l].partition_broadcast(128), writes=[ngt_r])
        ones64b, ones64b_r = tile((64,), F32)
        op("pool", lambda e: e.memset(ones64b, 1.0), [], [ones64b_r])
        nch = NT // 128
        NS = 6
        gts = [tile((16,), F32) for _ in range(NS)]
        qTs = [tile((2, 128), BF16) for _ in range(NS)]
        kTs = [tile((2, 128), BF16) for _ in range(NS)]
        kts = [tile((256,), BF16) for _ in range(NS)]
        vts = [tile((4, 65), BF16) for _ in range(NS)]
        for (v_, v_r) in vts:
            op("pool", lambda e, v_=v_: e.memset(v_, 1.0), [], [v_r])
        sps = [tile((4,), F32) for _ in range(NS)]
        avs = [tile((4,), F32) for _ in range(NS)]
        rvs = [tile((4,), F32) for _ in range(NS)]
        dcs = [tile((2,), F32) for _ in range(NS)]
        vas = [tile((4, 65), BF16) for _ in range(NS)]
        dts = [tile((4, 128), BF16) for _ in range(NS)]
        hns = [tile((4, 65), F32) for _ in range(NS)]
        dns = [tile((4,), F32) for _ in range(NS)]
        hfs = [tile((256,), F32) for _ in range(NS)]
        oms = [tile((256,), F32) for _ in range(NS)]
        for d in range(MLDIRS):
            if d == 1:
                barrier()
            Cst, Cst_r = tile((2, 65), F32)
            Cbf, Cbf_r = tile((2, 65), BF16)
            op("pool", lambda e, Cst=Cst: e.memset(Cst, 0.0), [], [Cst_r])
            op("pool", lambda e, Cbf=Cbf: e.memset(Cbf, 0.0), [], [Cbf_r])
            order = list(range(nch)) if d == 0 else [1, 0] + list(range(nch - 1, 1, -1))
            tri, tri_r = (mle, mle_r) if d == 0 else (mge, mge_r)
            mkb, mkb_r = (mle8, mle8_r) if d == 0 else (mge8, mge8_r)
            ic0, fc0 = (0, 4) if d == 0 else (8, 12)
            gb_res = [Res(), Res()]
            sc_res = [[Res(), Res()], [Res(), Res()]]

            def stA(si, c):
                s_ = si % NS
                t0 = c * 128
                gt, gt_r = gts[s_]
                dma(gt, gm_tok[t0:t0 + 128, :], writes=[gt_r])
                q_, q_r = qTs[s_]
                dma(q_, qmT[:, :, t0:t0 + 128].rearrange("c p t -> p c t"), writes=[q_r])
                k_, k_r = kTs[s_]
                dma(k_, kmT[:, :, t0:t0 + 128].rearrange("c p t -> p c t"), writes=[k_r])
                kt, kt_r = kts[s_]
                dma(kt, km_tok[t0:t0 + 128, :], writes=[kt_r])
                vt, vt_r = vts[s_]
                dma(vt[:, :, 0:64], vm_tok[t0:t0 + 128, :].rearrange("t (h d) -> t h d", h=4), writes=[vt_r])
                op("dve", lambda e: e.tensor_tensor(out=gt, in0=gt, in1=gbt, op=ALU.add), [gt_r, gbt_r], [gt_r])
                sp, sp_r = sps[s_]
                op("act", lambda e: e.activation(out=sp, in_=gt[:, fc0:fc0 + 4], func=AF.Exp, scale=-1.0), [gt_r], [sp_r])
                op("act", lambda e: e.activation(out=sp, in_=sp, func=AF.Ln, bias=1.0, scale=1.0), [sp_r], [sp_r])
                for par in range(2):
                    sb_ = banks[par * 2 + si % 2]
                    for a in range(2):
                        hp = par * 64
                        op("pe", lambda e: e.matmul(
                            sb_[:, a * 128:(a + 1) * 128], lhsT=k_[hp:hp + 64, a, :], rhs=q_[hp:hp + 64, a, :],
                            start=True, stop=True), [k_r, q_r], [bres[par * 2 + si % 2]])

            def stB(si, c):
                s_ = si % NS
                sp, sp_r = sps[s_]
                gb_ = banks[si % 2]
                g0 = 256
                op("pe", lambda e: e.matmul(gb_[:, g0:g0 + 4], lhsT=tri, rhs=sp, start=True, stop=True), [sp_r, tri_r], [bres[si % 2]])
                op("pe", lambda e: e.matmul(gb_[:, g0 + 8:g0 + 12], lhsT=ones_f, rhs=sp, start=True, stop=True),
                   [sp_r, ones_r], [bres[si % 2]])
                dt, dt_r = dts[s_]
                dtv = dt.rearrange("p (a two) t -> p a two t", two=2)
                mb_ = mkb.unsqueeze(1).to_broadcast([128, 2, 128])
                for par in range(2):
                    sbv = banks[par * 2 + si % 2][:, 0:256].rearrange("p (a b) -> p a b", a=2)
                    op("dve", lambda e: e.tensor_tensor(out=dtv[:, :, par, :], in0=sbv, in1=mb_, op=ALU.mult),
                       [bres[par * 2 + si % 2], mkb_r], [dt_r])

            def stC(si, c):
                s_ = si % NS
                gt, gt_r = gts[s_]
                gb_ = banks[si % 2]
                g0 = 256
                gb_r = bres[si % 2]
                av, av_r = avs[s_]
                rv, rv_r = rvs[s_]
                dc, dc_r = dcs[s_]
                op("dve", lambda e: e.tensor_tensor(out=av, in0=gb_[:, g0:g0 + 4], in1=gt[:, ic0:ic0 + 4], op=ALU.add),
                   [gb_r, gt_r], [av_r])
                op("act", lambda e: e.activation(out=av, in_=av, func=AF.Exp), [av_r], [av_r])
                op("act", lambda e: e.activation(out=rv, in_=gb_[:, g0:g0 + 4], func=AF.Exp, scale=-1.0), [gb_r], [rv_r])
                totv = gb_[:, g0 + 8:g0 + 12].rearrange("p (a two) -> p a two", two=2)
                op("act", lambda e: e.activation(out=dc[0:64, :], in_=totv[0:64, :, 0], func=AF.Exp, scale=-1.0), [gb_r], [dc_r])
                op("act", lambda e: e.activation(out=dc[64:128, :], in_=totv[64:128, :, 1], func=AF.Exp, scale=-1.0), [gb_r], [dc_r])

            def stD(si, c):
                s_ = si % NS
                av, av_r = avs[s_]
                vt, vt_r = vts[s_]
                kt, kt_r = kts[s_]
                va, va_r = vas[s_]
                ab = av.unsqueeze(2).to_broadcast([128, 4, 65])
                op("dve", lambda e: e.tensor_tensor(out=va, in0=vt, in1=ab, op=ALU.mult), [vt_r, av_r], [va_r])
                kvb, kvb_r = banks[4 + si % 2], bres[4 + si % 2]
                for h in range(4):
                    op("pe", lambda e: e.matmul(
                        kvb[:, h * 65:(h + 1) * 65], lhsT=kt[:, (h // 2) * 128:(h // 2 + 1) * 128], rhs=va[:, h, :], start=True, stop=True),
                       [kt_r, va_r], [kvb_r])

            def seq(si, c):
                s_ = si % NS
                t0 = c * 128
                q_, q_r = qTs[s_]
                k_, k_r = kTs[s_]
                kt, kt_r = kts[s_]
                va, va_r = vas[s_]
                dt, dt_r = dts[s_]
                rv, rv_r = rvs[s_]
                dc, dc_r = dcs[s_]
                kvb, kvb_r = banks[4 + si % 2], bres[4 + si % 2]
                hn, hn_r = hns[s_]
                hnv4 = hn.rearrange("p (a two) b -> p a two b", two=2)
                rv4 = rv.rearrange("p (a two) -> p a two", two=2)
                for par in range(2):
                    ob, ob_r = banks[6 + par], bres[6 + par]
                    hp = par * 64
                    for a in range(2):
                        h = 2 * a + par
                        op("pe", lambda e, ob=ob, a=a, h=h, dt=dt, va=va: e.matmul(
                            ob[:, a * 65:(a + 1) * 65], lhsT=dt[:, h, :], rhs=va[:, h, :], start=True, stop=False), [dt_r, va_r], [ob_r])
                        op("pe", lambda e, ob=ob, a=a, hp=hp, q_=q_, Cbf=Cbf: e.matmul(
                            ob[:, a * 65:(a + 1) * 65], lhsT=q_[hp:hp + 64, a, :], rhs=Cbf[hp:hp + 64, a, :],
                            start=False, stop=True), [q_r, Cbf_r], [ob_r])
                    obv = ob[:, 0:130].rearrange("p (a b) -> p a b", a=2)
                    rb = rv4[:, :, par].unsqueeze(2).to_broadcast([128, 2, 65])
                    op("dve", lambda e, hnv4=hnv4, obv=obv, rb=rb, par=par: e.tensor_tensor(out=hnv4[:, :, par, :], in0=obv, in1=rb, op=ALU.mult),
                       [ob_r, rv_r], [hn_r])
                kvv = kvb[:, 0:260].rearrange("p (a two b) -> p a two b", a=2, two=2)
                for hf_ in range(2):
                    ps_ = slice(hf_ * 64, hf_ * 64 + 64)
                    op("dve", lambda e, Cst=Cst, kvv=kvv, ps_=ps_, hf_=hf_: e.scalar_tensor_tensor(
                        out=Cst[ps_, :, :], in0=kvv[ps_, :, hf_, :], scalar=0.125, in1=Cst[ps_, :, :], op0=ALU.mult, op1=ALU.add),
                       [kvb_r, Cst_r], [Cst_r])
                dcb = dc.unsqueeze(2).to_broadcast([128, 2, 65])
                op("dve", lambda e, Cst=Cst, dcb=dcb: e.tensor_tensor(out=Cst, in0=Cst, in1=dcb, op=ALU.mult), [Cst_r, dc_r], [Cst_r])
                op("act", lambda e, Cst=Cst, Cbf=Cbf: e.copy(out=Cbf, in_=Cst), [Cst_r], [Cbf_r])
                dn, dn_r = dns[s_]
                op("act", lambda e, dn=dn, hn=hn: e.activation(out=dn, in_=hn[:, :, 64], func=AF.Abs), [hn_r], [dn_r])
                op("dve", lambda e, dn=dn: e.tensor_scalar_max(out=dn, in0=dn, scalar1=1.0), [dn_r], [dn_r])
                op("dve", lambda e, dn=dn: e.reciprocal(out=dn, in_=dn), [dn_r], [dn_r])
                hf, hf_r = hfs[s_]
                hfv = hf.rearrange("p (a b) -> p a b", a=4)
                dnb = dn.unsqueeze(2).to_broadcast([128, 4, 64])
                if d == 0:
                    op("dve", lambda e, hfv=hfv, hn=hn, dnb=dnb: e.tensor_tensor(out=hfv, in0=hn[:, :, 0:64], in1=dnb, op=ALU.mult),
                       [hn_r, dn_r], [hf_r])
                    dma(hF[t0:t0 + 128, :], hf, reads=[hf_r])
                    return
                if c < 2 and not with_ctx:
                    return
                dma(hf, hF[t0:t0 + 128, :], writes=[hf_r])
                om, om_r = oms[s_]
                dma(om, om_tok[t0:t0 + 128, :], writes=[om_r])
                hnv = hn[:, :, 0:64]
                op("dve", lambda e, hnv=hnv, dnb=dnb: e.tensor_tensor(out=hnv, in0=hnv, in1=dnb, op=ALU.mult), [hn_r, dn_r], [hn_r])
                op("dve", lambda e, hfv=hfv, hnv=hnv: e.tensor_tensor(out=hfv, in0=hfv, in1=hnv, op=ALU.add), [hf_r, hn_r], [hf_r])
                op("pool", lambda e, hnv=hnv, hfv=hfv: e.tensor_tensor(out=hnv, in0=hfv, in1=hfv, op=ALU.mult), [hf_r], [hn_r])
                op("dve", lambda e, dn=dn, hnv=hnv: e.tensor_reduce(out=dn, in_=hnv, axis=AX.X, op=ALU.add), [hn_r], [dn_r])
                op("act", lambda e, dn=dn: e.activation(out=dn, in_=dn, func=AF.Sqrt, bias=EPS, scale=1.0 / 64), [dn_r], [dn_r])
                op("dve", lambda e, dn=dn: e.reciprocal(out=dn, in_=dn), [dn_r], [dn_r])
                op("act", lambda e, om=om: e.activation(out=om, in_=om, func=AF.Sigmoid), [om_r], [om_r])
                op("pool", lambda e, om=om: e.tensor_tensor(out=om, in0=om, in1=ngt, op=ALU.mult), [om_r, ngt_r], [om_r])
                op("dve", lambda e, hfv=hfv, dnb=dnb: e.tensor_tensor(out=hfv, in0=hfv, in1=dnb, op=ALU.mult), [hf_r, dn_r], [hf_r])
                ymb = kt
                op("dve", lambda e, ymb=ymb, hf=hf, om=om: e.tensor_tensor(out=ymb, in0=hf, in1=om, op=ALU.mult), [hf_r, om_r], [kt_r])
                tb, tb_r = banks[6], bres[6]
                tbv = tb[:, 256:384].bitcast(BF16)
                for cc in range(2):
                    op("pe", lambda e, cc=cc, tbv=tbv, ymb=ymb: e.transpose(tbv[:, cc * 128:(cc + 1) * 128], ymb[:, cc * 128:(cc + 1) * 128], identb),
                       [kt_r, identb_r], [tb_r])
                yo = k_
                op("act", lambda e, yo=yo, tbv=tbv: e.copy(out=yo, in_=tbv.rearrange("p (a b) -> p a b", a=2)), [tb_r], [k_r])
                dma(ymT[:, :, t0:t0 + 128].rearrange("c p t -> p c t"), yo, reads=[k_r])

            stages = [stA, stB, stC, stD, seq]
            n_ = len(order)
            for it in range(n_ + len(stages) - 1):
                for k, st_ in enumerate(stages):
                    si = it - k
                    if 0 <= si < n_:
                        st_(si, order[si])

    def phase_C(l, xsrc, with_ctx):
        wgb, wgb_r = tile((8, 3072), BF16)
        wba, wba_r = tile((4, 1024), BF16)
        wbs, wbs_r = tile((2, 1024), BF16)
        wbm, wbm_r = tile((2, 1024), BF16)
        wo, wo_r = tile((8, 1024), BF16)
        stg = [tile((8, 512), F32)]
        si = 0
        def wload(dst, dst_r, src, nk, ncols):
            nonlocal si
            for c0 in range(0, ncols, 512):
                st_, st_r = stg[0]
                cast_load(dst[:, :, c0:c0 + 512], dst_r, src[:, c0:c0 + 512].rearrange("(kc p) n -> p kc n", p=128), None,
                          st_[:, 0:nk, :], st_r, eng=("pool" if si % 2 == 0 else "dve"))
                si += 1
        wload(wgb, wgb_r, w_gb[l], 8, 3072)
        wload(wba, wba_r, wb_a[l], 4, 1024)
        wload(wbs, wbs_r, wb_s[l], 2, 1024)
        wload(wbm, wbm_r, wb_m[l], 2, 1024)
        wload(wo, wo_r, w_out[l], 8, 1024)
        xt, xt_r = tile((8, 512), F32)
        sq, _ = tile((8, 512), F32)
        sq_r = [Res() for _ in range(8)]
        rs, rs_r = tile((512,), F32)
        hT, hT_r = tile((8, 512), BF16)
        yat, yat_r = tile((4, 512), BF16)
        yst, yst_r = tile((2, 512), BF16)
        ymt, ymt_r = tile((2, 512), BF16)
        sig, _ = tile((3, 512), F32)
        sig_r = [Res() for _ in range(3)]
        t1, t1_r = tile((512,), F32)
        t2, t2_r = tile((512,), F32)
        yT, yT_r = tile((8, 512), BF16)
        hTs = [(hT, hT_r), tile((8, 512), BF16)]
        xts = [(xt, xt_r), stg[0]]
        tiles_ = token_tiles(with_ctx)

        def ln_C(ti):
            t0, n = tiles_[ti]
            x_, x_r = xts[ti % 2]
            dma(x_[:, :, 0:n], xsrc[:, :, t0:t0 + n].rearrange("c p t -> p c t"), writes=[x_r])
            norm_mod(x_, x_r, n, hTs[ti % 2][0], hTs[ti % 2][1], gs1, gs1_r, 0, 1 if t0 < CTX else 0, sq, sq_r, rs, rs_r,
                     banks[0], bres[0], l)

        ln_C(0)
        for ti, (t0, n) in enumerate(tiles_):
            if ti + 1 < len(tiles_):
                ln_C(ti + 1)
            which = 1 if t0 < CTX else 0
            xt, xt_r = xts[ti % 2]
            hT, hT_r = hTs[ti % 2]
            dma(yat[:, :, 0:n], yaT[:, :, t0:t0 + n].rearrange("c p t -> p c t"), writes=[yat_r])
            dma(yst[:, :, 0:n], ysT[:, :, t0:t0 + n].rearrange("c p t -> p c t"), writes=[yst_r])
            dma(ymt[:, :, 0:n], ymT[:, :, t0:t0 + n].rearrange("c p t -> p c t"), writes=[ymt_r])
            for fc in range(8):
                for b in range(3):
                    bk, bk_r = banks[1 + b], bres[1 + b]
                    for kc in range(8):
                        op("pe", lambda e, bk=bk, kc=kc, b=b, fc=fc: e.matmul(
                            bk[:, 0:n], lhsT=wgb[:, kc, b * 1024 + fc * 128:b * 1024 + (fc + 1) * 128], rhs=hT[:, kc, 0:n],
                            start=(kc == 0), stop=(kc == 7)), [wgb_r, hT_r], [bk_r])
                    op("act", lambda e, bk=bk, b=b: e.activation(out=sig[:, b, 0:n], in_=bk[:, 0:n], func=AF.Sigmoid), [bk_r], [sig_r[b]])
                for b, (w_, w_r, y_, y_r, nk) in enumerate(((wba, wba_r, yat, yat_r, 4), (wbs, wbs_r, yst, yst_r, 2), (wbm, wbm_r, ymt, ymt_r, 2))):
                    bk, bk_r = banks[4 + b], bres[4 + b]
                    for kc in range(nk):
                        op("pe", lambda e, bk=bk, kc=kc, w_=w_, y_=y_, fc=fc, nk=nk: e.matmul(
                            bk[:, 0:n], lhsT=w_[:, kc, fc * 128:(fc + 1) * 128], rhs=y_[:, kc, 0:n],
                            start=(kc == 0), stop=(kc == nk - 1)), [w_r, y_r], [bk_r])
                op("dve", lambda e: e.tensor_tensor(out=t1[:, 0:n], in0=banks[4][:, 0:n], in1=sig[:, 0, 0:n], op=ALU.mult), [bres[4], sig_r[0]], [t1_r])
                op("dve", lambda e: e.tensor_tensor(out=t2[:, 0:n], in0=banks[5][:, 0:n], in1=sig[:, 1, 0:n], op=ALU.mult), [bres[5], sig_r[1]], [t2_r])
                op("pool", lambda e: e.tensor_tensor(out=t1[:, 0:n], in0=t1[:, 0:n], in1=t2[:, 0:n], op=ALU.add), [t1_r, t2_r], [t1_r])
                op("dve", lambda e: e.tensor_tensor(out=t2[:, 0:n], in0=banks[6][:, 0:n], in1=sig[:, 2, 0:n], op=ALU.mult), [bres[6], sig_r[2]], [t2_r])
                op("pool", lambda e, fc=fc: e.tensor_tensor(out=yT[:, fc, 0:n], in0=t1[:, 0:n], in1=t2[:, 0:n], op=ALU.add), [t1_r, t2_r], [yT_r])
            for fo in range(8):
                bk, bk_r = banks[1 + fo % 3], bres[1 + fo % 3]
                for kc in range(8):
                    op("pe", lambda e, bk=bk, kc=kc, fo=fo: e.matmul(
                        bk[:, 0:n], lhsT=wo[:, kc, fo * 128:(fo + 1) * 128], rhs=yT[:, kc, 0:n], start=(kc == 0), stop=(kc == 7)),
                       [wo_r, yT_r], [bk_r])
                op("dve", lambda e, bk=bk, fo=fo: e.scalar_tensor_tensor(
                    out=xt[:, fo, 0:n], in0=bk[:, 0:n], scalar=modv[:, l, 16 + fo, which:which + 1], in1=xt[:, fo, 0:n],
                    op0=ALU.mult, op1=ALU.add), [bk_r, modv_r, xt_r], [xt_r])
            dma(xsrc[:, :, t0:t0 + n].rearrange("c p t -> p c t"), xt[:, :, 0:n], reads=[xt_r])

    def phase_D(l, xsrc, xdst, with_ctx):
        wu, wu_r = tile((8, 2 * D_FF), BF16)
        wd, wd_r = tile((22, 1024), BF16)
        stg, stg_r = tile((8, 256), F32)
        for c0 in range(0, 2 * D_FF, 256):
            cast_load(wu[:, :, c0:c0 + 256], wu_r, w_up[l, :, c0:c0 + 256].rearrange("(kc p) n -> p kc n", p=128), None,
                      stg, stg_r, eng=("pool" if (c0 // 256) % 2 == 0 else "dve"))
        for k0 in range(0, 22, 2):
            for c0 in range(0, 1024, 1024):
                cast_load(wd[:, k0:k0 + 2, :], wd_r, w_dn[l, k0 * 128:(k0 + 2) * 128, :].rearrange("(kc p) n -> p kc n", p=128), None,
                          stg.rearrange("p a b -> p (a b)")[:, 0:2048].rearrange("p (a b) -> p a b", a=2), stg_r,
                          eng=("pool" if (k0 // 2) % 2 == 0 else "dve"))
        cwt, cwt_r = tile((3, 44), F32)
        cbt, cbt_r = tile((44,), F32)
        dma(cwt, cw[l], writes=[cwt_r])
        dma(cbt, cb[l], writes=[cbt_r])
        xt, xt_r = tile((8, 258), F32)
        sq, _ = tile((8, 258), F32)
        sq_r = [Res() for _ in range(8)]
        rs, rs_r = tile((258,), F32)
        hT, hT_r = tile((8, 258), BF16)
        gb_, gb_r = tile((22, 256), BF16)
        tas = [tile((256,), F32) for _ in range(2)]
        tvs = [tile((256,), F32) for _ in range(2)]
        sas = [tile((256,), F32) for _ in range(2)]
        tl = []
        if with_ctx:
            tl.append((0, 0, CTX))
        for t0 in range(CTX, NT, 256):
            tl.append((t0, CTX, NT))
        hTs = [(hT, hT_r), tile((8, 258), BF16)]
        xts = [(xt, xt_r), tile((8, 258), F32)]

        def ln_D(ti):
            t0, seg0, seg1 = tl[ti]
            x_, x_r = xts[ti % 2]
            lo, hi = t0 - 1, t0 + 257
            zc = []
            clo, chi = max(lo, seg0), min(hi, seg1)
            if lo < seg0:
                op("pool", lambda e: e.memset(x_[:, :, 0:1], 1.0), [], [x_r])
                zc.append(0)
            if hi > seg1:
                op("pool", lambda e: e.memset(x_[:, :, 257:258], 1.0), [], [x_r])
                zc.append(257)
            dma(x_[:, :, clo - lo:chi - lo], xsrc[:, :, clo:chi].rearrange("c p t -> p c t"), writes=[x_r])
            norm_mod(x_, x_r, 258, hTs[ti % 2][0], hTs[ti % 2][1], gs2, gs2_r, 3, 1 if t0 < CTX else 0, sq, sq_r, rs, rs_r,
                     banks[0], bres[0], l, zero_cols=zc)

        ln_D(0)
        for ti, (t0, seg0, seg1) in enumerate(tl):
            if ti + 1 < len(tl):
                ln_D(ti + 1)
            which = 1 if t0 < CTX else 0
            xt, xt_r = xts[ti % 2]
            hT, hT_r = hTs[ti % 2]
            for j in range(22):
                ta, ta_r = tas[j % 2]
                tv, tv_r = tvs[j % 2]
                sa, sa_r = sas[j % 2]
                for (ch, bk, bk_r, tt, tt_r) in ((j, banks[1 + (j % 3) * 2], bres[1 + (j % 3) * 2], ta, ta_r),
                                                 (22 + j, banks[2 + (j % 3) * 2], bres[2 + (j % 3) * 2], tv, tv_r)):
                    for kc in range(8):
                        op("pe", lambda e, bk=bk, kc=kc, ch=ch: e.matmul(
                            bk[:, 0:258], lhsT=wu[:, kc, ch * 128:(ch + 1) * 128], rhs=hT[:, kc, :], start=(kc == 0), stop=(kc == 7)),
                           [wu_r, hT_r], [bk_r])
                    op("act", lambda e, bk=bk, ch=ch, tt=tt: e.activation(out=tt, in_=bk[:, 0:256], func=AF.Identity,
                                                                          bias=cbt[:, ch:ch + 1], scale=cwt[:, 0, ch:ch + 1]),
                       [bk_r, cbt_r, cwt_r], [tt_r])
                    op("dve", lambda e, bk=bk, ch=ch, tt=tt: e.scalar_tensor_tensor(
                        out=tt, in0=bk[:, 1:257], scalar=cwt[:, 1, ch:ch + 1], in1=tt, op0=ALU.mult, op1=ALU.add), [bk_r, cwt_r, tt_r], [tt_r])
                    op("dve", lambda e, bk=bk, ch=ch, tt=tt: e.scalar_tensor_tensor(
                        out=tt, in0=bk[:, 2:258], scalar=cwt[:, 2, ch:ch + 1], in1=tt, op0=ALU.mult, op1=ALU.add), [bk_r, cwt_r, tt_r], [tt_r])
                op("act", lambda e, sa=sa, ta=ta: e.activation(out=sa, in_=ta, func=AF.Silu), [ta_r], [sa_r])
                op("pool", lambda e, j=j, sa=sa, tv=tv: e.tensor_tensor(out=gb_[:, j, :], in0=sa, in1=tv, op=ALU.mult), [sa_r, tv_r], [gb_r])
            for fo in range(8):
                bk, bk_r = banks[7 if fo % 2 == 0 else 0], bres[7 if fo % 2 == 0 else 0]
                for kc in range(22):
                    op("pe", lambda e, bk=bk, kc=kc, fo=fo: e.matmul(
                        bk[:, 0:256], lhsT=wd[:, kc, fo * 128:(fo + 1) * 128], rhs=gb_[:, kc, :], start=(kc == 0), stop=(kc == 21)),
                       [wd_r, gb_r], [bk_r])
                op("dve", lambda e, bk=bk, fo=fo: e.scalar_tensor_tensor(
                    out=xt[:, fo, 1:257], in0=bk[:, 0:256], scalar=modv[:, l, 40 + fo, which:which + 1], in1=xt[:, fo, 1:257],
                    op0=ALU.mult, op1=ALU.add), [bk_r, modv_r, xt_r], [xt_r])
            dma(xdst[:, :, t0:t0 + 256].rearrange("c p t -> p c t"), xt[:, :, 1:257], reads=[xt_r])

    def phase_E(xsrc):
        xt, xt_r = tile((8, 512), F32)
        sq, _ = tile((8, 512), F32)
        sq_r = [Res() for _ in range(8)]
        rs, rs_r = tile((512,), F32)
        ots = [tile((1024,), F32) for _ in range(2)]
        fins = []
        bi = 0
        for (t0, n) in token_tiles(False):
            dma(xt[:, :, 0:n], xsrc[:, :, t0:t0 + n].rearrange("c p t -> p c t"), writes=[xt_r])
            for c in range(8):
                if c % 2 == 0:
                    op("act", lambda e, c=c: e.activation(out=sq[:, c, 0:n], in_=xt[:, c, 0:n], func=AF.Square), [xt_r], [sq_r[c]])
                else:
                    op("pool", lambda e, c=c: e.tensor_tensor(out=sq[:, c, 0:n], in0=xt[:, c, 0:n], in1=xt[:, c, 0:n], op=ALU.mult), [xt_r], [sq_r[c]])
            for c in range(8):
                op("pe", lambda e, c=c: e.matmul(banks[0][:, 0:n], lhsT=ones_f, rhs=sq[:, c, 0:n], start=(c == 0), stop=(c == 7)),
                   [sq_r[c], ones_r], [bres[0]])
            op("act", lambda e: e.activation(out=rs[:, 0:n], in_=banks[0][:, 0:n], func=AF.Sqrt, bias=EPS, scale=1.0 / D), [bres[0]], [rs_r])
            op("dve", lambda e: e.reciprocal(out=rs[:, 0:n], in_=rs[:, 0:n]), [rs_r], [rs_r])
            for c in range(8):
                op("dve", lambda e, c=c: e.scalar_tensor_tensor(out=sq[:, c, 0:n], in0=xt[:, c, 0:n], scalar=fnt[:, c:c + 1], in1=rs[:, 0:n],
                                                               op0=ALU.mult, op1=ALU.mult), [xt_r, rs_r, fnt_r], [sq_r[c]])
            for sb in range(n // 128):
                ot, ot_r = ots[bi % 2]
                for half in range(2):
                    bk, bk_r = banks[1 + (bi * 2 + half) % 4], bres[1 + (bi * 2 + half) % 4]
                    for j in range(4):
                        c = half * 4 + j
                        op("pe", lambda e, bk=bk, j=j, c=c, sb=sb: e.transpose(bk[:, j * 128:(j + 1) * 128], sq[:, c, sb * 128:(sb + 1) * 128], ident),
                           [sq_r[c], ident_r], [bk_r])
                    if half == 0:
                        op("act", lambda e, bk=bk, ot=ot: e.copy(out=ot[:, 0:512], in_=bk[:, 0:512]), [bk_r], [ot_r])
                    else:
                        op("dve", lambda e, bk=bk, ot=ot: e.tensor_copy(out=ot[:, 512:1024], in_=bk[:, 0:512]), [bk_r], [ot_r])
                bi += 1
                tt = t0 - CTX + sb * 128
                fins.append(dma(y_out[tt:tt + 128, :], ot, reads=[ot_r]))
        return fins

    def phase_s5(l, with_ctx):
        NB, NCH, G = NT // 8, NT // 128, 16
        INV2PI = 1.0 / TWO_PI
        cols, cols_r = tile((4,), F32)
        dma(cols, s5_cols, writes=[cols_r])
        mphi, mphi_r = tile((2, 136), F32)
        dma(mphi.rearrange("p a b -> p (a b)"), s5_mphi.partition_broadcast(128), writes=[mphi_r])
        mpsi, mpsi_r = tile((2, 136), F32)
        dma(mpsi.rearrange("p a b -> p (a b)"), s5_mpsi.partition_broadcast(128), writes=[mpsi_r])
        mphs, mphs_r = tile((2, 16), F32)
        dma(mphs.rearrange("p a b -> p (a b)"), s5_mphs.partition_broadcast(128), writes=[mphs_r])
        at, at_r = tile((2, 16, 2), F32)
        dma(at.rearrange("p a b c -> p (a b c)"), s5_a[l], writes=[at_r])
        ldt, ldt_r = tile((2, 16), F32)
        dma(ldt.rearrange("p a b -> p (a b)"), s5_logdt[l].partition_broadcast(128), writes=[ldt_r])
        bt, bt_r = tile((16, 16, 2), F32)
        dma(bt.rearrange("p a b c -> p (a b c)"), s5_b[l], writes=[bt_r])
        ct, ct_r = tile((16, 16, 2), F32)
        dma(ct.rearrange("p a b c -> p (a b c)"), s5_c[l], writes=[ct_r])
        dcol, dcol_r = tile((16,), F32)
        dma(dcol, s5_dcol[l], writes=[dcol_r])
        m0, m0_r = tile((2, 128), F32)
        dma(m0, s5_m0.rearrange("d k m -> k d m"), writes=[m0_r])
        swp, swp_r = tile((128,), F32)
        dma(swp, s5_swap, writes=[swp_r])
        gst, gst_r = tile((2, 256), F32)
        gw, gw_r = tile((2, 256), BF16)
        cast_load(gw, gw_r, s5_glu_w[l].rearrange("(kc p) n -> p kc n", p=128), None, gst, gst_r, eng="dve")
        glb, glb_r = tile((2,), F32)
        dma(glb, s5_glu_b[l], writes=[glb_r])
        sel, sel_r = tile((64, 128), BF16)
        U, _ = tile((G, NB), BF16)
        NBG = (NCH + 31) // 32
        bgs = [(b * 32, min(32, NCH - b * 32)) for b in range(NBG)]
        U_r = [[Res() for _ in bgs] for _ in range(G)]

        small = lambda: tile((2, 16), F32)
        dts, dts_r = small()
        op("act", lambda e: e.activation(out=dts, in_=ldt, func=AF.Exp), [ldt_r], [dts_r])
        dre, dre_r = small()
        dim, dim_r = small()
        op("dve", lambda e: e.tensor_tensor(out=dre, in0=dts, in1=at[:, :, :, 0], op=ALU.mult), [dts_r, at_r], [dre_r])
        op("dve", lambda e: e.tensor_tensor(out=dim, in0=dts, in1=at[:, :, :, 1], op=ALU.mult), [dts_r, at_r], [dim_r])
        tI, tI_r = tile((2, 16), I32)
        tF, tF_r = small()

        def sin_red(dst, dst_r, ang, ang_r, tI, tI_r, tF, tF_r):
            op("dve", lambda e: e.tensor_scalar(out=tI, in0=ang, scalar1=INV2PI, scalar2=None, op0=ALU.mult), [ang_r], [tI_r])
            op("dve", lambda e: e.tensor_copy(out=tF, in_=tI), [tI_r], [tF_r])
            op("dve", lambda e: e.scalar_tensor_tensor(out=tF, in0=tF, scalar=-TWO_PI, in1=ang, op0=ALU.mult, op1=ALU.add),
               [tF_r, ang_r], [tF_r])
            op("act", lambda e: e.activation(out=dst, in_=tF, func=AF.Sin), [tF_r], [dst_r])

        def lam_pow(kpow):
            a1, a1_r = small()
            a2, a2_r = small()
            mg, mg_r = small()
            sr, sr_r = small()
            ci_, ci_r = small()
            op("dve", lambda e: e.tensor_scalar(out=a1, in0=dim, scalar1=float(kpow), scalar2=None, op0=ALU.mult), [dim_r], [a1_r])
            op("dve", lambda e: e.tensor_scalar(out=a2, in0=dim, scalar1=float(kpow), scalar2=math.pi / 2, op0=ALU.mult, op1=ALU.add),
               [dim_r], [a2_r])
            sin_red(sr, sr_r, a1, a1_r, tI, tI_r, tF, tF_r)
            sin_red(ci_, ci_r, a2, a2_r, tI, tI_r, tF, tF_r)
            op("act", lambda e: e.activation(out=mg, in_=dre, func=AF.Exp, scale=float(kpow)), [dre_r], [mg_r])
            op("dve", lambda e: e.tensor_tensor(out=sr, in0=sr, in1=mg, op=ALU.mult), [sr_r, mg_r], [sr_r])
            op("dve", lambda e: e.tensor_tensor(out=ci_, in0=ci_, in1=mg, op=ALU.mult), [ci_r, mg_r], [ci_r])
            return (ci_, ci_r), (sr, sr_r)

        (lr, lr_r), (li, li_r) = lam_pow(1)
        (l128r, l128r_r), (l128i, l128i_r) = lam_pow(128)
        ar, ai = at[:, :, :, 0], at[:, :, :, 1]
        den, den_r = small()
        t1, t1_r = small()
        t2, t2_r = small()
        cr, cr_r = small()
        ci, ci_r = small()
        op("dve", lambda e: e.tensor_tensor(out=den, in0=ar, in1=ar, op=ALU.mult), [at_r], [den_r])
        op("dve", lambda e: e.tensor_tensor(out=t1, in0=ai, in1=ai, op=ALU.mult), [at_r], [t1_r])
        op("dve", lambda e: e.tensor_tensor(out=den, in0=den, in1=t1, op=ALU.add), [den_r, t1_r], [den_r])
        op("dve", lambda e: e.reciprocal(out=den, in_=den), [den_r], [den_r])
        op("dve", lambda e: e.tensor_scalar_add(out=lr, in0=lr, scalar1=-1.0), [lr_r], [lr_r])
        op("dve", lambda e: e.tensor_tensor(out=t1, in0=lr, in1=ar, op=ALU.mult), [lr_r, at_r], [t1_r])
        op("dve", lambda e: e.tensor_tensor(out=t2, in0=li, in1=ai, op=ALU.mult), [li_r, at_r], [t2_r])
        op("dve", lambda e: e.tensor_tensor(out=t1, in0=t1, in1=t2, op=ALU.add), [t1_r, t2_r], [t1_r])
        op("dve", lambda e: e.tensor_tensor(out=cr, in0=t1, in1=den, op=ALU.mult), [t1_r, den_r], [cr_r])
        op("dve", lambda e: e.tensor_tensor(out=t1, in0=li, in1=ar, op=ALU.mult), [li_r, at_r], [t1_r])
        op("dve", lambda e: e.tensor_tensor(out=t2, in0=lr, in1=ai, op=ALU.mult), [lr_r, at_r], [t2_r])
        op("dve", lambda e: e.tensor_tensor(out=t1, in0=t1, in1=t2, op=ALU.subtract), [t1_r, t2_r], [t1_r])
        op("dve", lambda e: e.tensor_tensor(out=ci, in0=t1, in1=den, op=ALU.mult), [t1_r, den_r], [ci_r])
        br, bi = bt[:, :, :, 0], bt[:, :, :, 1]
        BA, BA_r = tile((2, 16, 16), F32)
        BB, BB_r = tile((2, 16, 16), F32)
        w1, w1_r = tile((16, 16), F32)
        w2, w2_r = tile((16, 16), F32)
        for d in range(2):
            crb = cr[:, d, :].unsqueeze(2).to_broadcast([128, 16, 16])
            cib = ci[:, d, :].unsqueeze(2).to_broadcast([128, 16, 16])
            op("dve", lambda e: e.tensor_tensor(out=w1, in0=br, in1=crb, op=ALU.mult), [bt_r, cr_r], [w1_r])
            op("dve", lambda e: e.tensor_tensor(out=w2, in0=bi, in1=cib, op=ALU.mult), [bt_r, ci_r], [w2_r])
            op("dve", lambda e: e.tensor_tensor(out=BA[:, d], in0=w1, in1=w2, op=ALU.subtract), [w1_r, w2_r], [BA_r])
            op("dve", lambda e: e.tensor_tensor(out=w1, in0=bi, in1=crb, op=ALU.mult), [bt_r, cr_r], [w1_r])
            op("dve", lambda e: e.tensor_tensor(out=w2, in0=br, in1=cib, op=ALU.mult), [bt_r, ci_r], [w2_r])
            op("dve", lambda e: e.tensor_tensor(out=w1, in0=w1, in1=w2, op=ALU.add), [w1_r, w2_r], [w1_r])
            op("dve", lambda e: e.tensor_scalar(out=BB[:, d], in0=w1, scalar1=cols[:, 2:3], scalar2=None, op0=ALU.mult), [w1_r, cols_r], [BB_r])
        CA, CA_r = tile((16, 16), F32)
        CB, CB_r = tile((16, 16), F32)
        op("dve", lambda e: e.tensor_scalar(out=CA, in0=ct[:, :, :, 0], scalar1=cols[:, 3:4], scalar2=None, op0=ALU.mult), [ct_r, cols_r], [CA_r])
        op("dve", lambda e: e.tensor_scalar(out=CB, in0=ct[:, :, :, 1], scalar1=-1.0, scalar2=None, op0=ALU.mult), [ct_r], [CB_r])
        ais, ais_r = small()
        op("dve", lambda e: e.tensor_scalar(out=ais, in0=l128i, scalar1=cols[:, 3:4], scalar2=None, op0=ALU.mult), [l128i_r, cols_r], [ais_r])

        HG = 8
        pa, pa_r = tile((HG, 136), F32)
        pI, pI_r = tile((HG, 136), I32)
        pF, pF_r = tile((HG, 136), F32)
        pm, pm_r = tile((HG, 136), F32)

        def pw_tables(d, g0, mt, mt_r, n, outA, outA_r, outB, outB_r):
            mtb = mt.unsqueeze(1).to_broadcast([128, HG, n])
            dimb = dim[:, d, g0:g0 + HG].unsqueeze(2).to_broadcast([128, HG, n])
            dreb = dre[:, d, g0:g0 + HG].unsqueeze(2).to_broadcast([128, HG, n])
            pa_, pI_, pF_, pm_ = pa[:, :, 0:n], pI[:, :, 0:n], pF[:, :, 0:n], pm[:, :, 0:n]
            op("dve", lambda e: e.tensor_tensor(out=pm_, in0=mtb, in1=dreb, op=ALU.mult), [mt_r, dre_r], [pm_r])
            op("act", lambda e: e.activation(out=pm_, in_=pm_, func=AF.Exp), [pm_r], [pm_r])
            for (o_, o_r, phc) in ((outA, outA_r, 0), (outB, outB_r, 1)):
                op("dve", lambda e: e.tensor_tensor(out=pa_, in0=mtb, in1=dimb, op=ALU.mult), [mt_r, dim_r], [pa_r])
                op("dve", lambda e: e.tensor_scalar(out=pa_, in0=pa_, scalar1=cols[:, phc:phc + 1], scalar2=None, op0=ALU.add), [pa_r, cols_r], [pa_r])
                sin_red(o_, o_r, pa_, pa_r, pI_, pI_r, pF_, pF_r)
                op("pool", lambda e: e.tensor_tensor(out=o_, in0=o_, in1=pm_, op=ALU.mult), [o_r, pm_r], [o_r])

        oa, oa_r = tile((136, 16), F32)
        aw, aw_r = oa.rearrange("p a b -> p (a b)")[:, 0:G * 128].rearrange("p (a b) -> p a b", a=G), oa_r
        ob_, ob_r = tile((136, 16), F32)

        def outer(dst, dst_r, PA, PA_r, PB, PB_r, gi, XA, XA_r, XB, XB_r, n):
            pab = PA[:, gi, :].unsqueeze(2).to_broadcast([128, n, 16])
            pbb = PB[:, gi, :].unsqueeze(2).to_broadcast([128, n, 16])
            xab = XA.unsqueeze(1).to_broadcast([128, n, 16])
            xbb = XB.unsqueeze(1).to_broadcast([128, n, 16])
            op("dve", lambda e: e.tensor_tensor(out=oa[:, 0:n, :], in0=pab, in1=xab, op=ALU.mult), [PA_r, XA_r], [oa_r])
            op("pool", lambda e: e.tensor_tensor(out=ob_[:, 0:n, :], in0=pbb, in1=xbb, op=ALU.mult), [PB_r, XB_r], [ob_r])
            op("dve", lambda e: e.tensor_tensor(out=dst, in0=oa[:, 0:n, :], in1=ob_[:, 0:n, :], op=ALU.add), [oa_r, ob_r], [dst_r])

        dma(sel, s5_sel, writes=[sel_r])
        ust, ust_r = tile((2, 4096), BF16)
        cgs = [(c0, min(512, NB - c0)) for c0 in range(0, NB, 512)]
        bg_of_col = lambda c0: c0 // 512
        for (c0, nb) in cgs:
            dma(ust[:, :, 0:nb * 8], usT[:, :, c0 * 8:(c0 + nb) * 8].rearrange("c p t -> p c t"), writes=[ust_r])
            for g in range(G):
                hf, gl = g // 8, g % 8
                bk, bk_r = banks[g % 2], bres[g % 2]
                uv = ust[:, hf, 0:nb * 8].rearrange("p (tb s) -> p tb s", s=8)
                for s8 in range(8):
                    op("pe", lambda e: e.matmul(bk[:, 0:nb], lhsT=sel[:, gl * 8 + s8, :], rhs=uv[:, :, s8], start=(s8 == 0), stop=(s8 == 7)),
                       [sel_r, ust_r], [bk_r])
                if g % 2 == 0:
                    op("act", lambda e: e.copy(out=U[:, g, c0:c0 + nb], in_=bk[:, 0:nb]), [bk_r], [U_r[g][bg_of_col(c0)]])
                else:
                    op("dve", lambda e: e.tensor_copy(out=U[:, g, c0:c0 + nb], in_=bk[:, 0:nb]), [bk_r], [U_r[g][bg_of_col(c0)]])
        dma(sel, s5_selT, writes=[sel_r])

        Eall, Eall_r = tile((2, G, NCH), F32)
        Zst, Zst_r = tile((G, NCH), F32)
        Zin, Zin_r = tile((2, G, NCH), BF16)
        op("pool", lambda e: e.memset(Zin, 0.0), [], [Zin_r])
        A128, A128_r = tile((G, 128), F32)
        PhA, PhA_r = tile((HG, 136), F32)
        PhB, PhB_r = tile((HG, 136), F32)
        Phi, Phi_r = tile((136, 16), BF16)
        Wt, Wt_r = tile((16, 128), BF16)
        for d in range(2):
            for g0 in range(0, G, HG):
                pw_tables(d, g0, mphi[:, d, :], mphi_r, 136, PhA, PhA_r, PhB, PhB_r)
                for gi in range(HG):
                    g = g0 + gi
                    outer(Phi, Phi_r, PhA, PhA_r, PhB, PhB_r, gi, BA[:, d, g], BA_r, BB[:, d, g], BB_r, 136)
                    woff = 0 if d == 0 else 8
                    for hb in range(2):
                        bk, bk_r = banks[1 + hb], bres[1 + hb]
                        bkv = bk[:, 0:512].bitcast(BF16)
                        for jj in range(8):
                            j = hb * 8 + jj
                            src = Phi[:, woff + 8 * j:woff + 8 * j + 8, :].rearrange("p a b -> p (a b)")
                            op("pe", lambda e: e.transpose(bkv[:, jj * 128:(jj + 1) * 128], src, identb), [Phi_r, identb_r], [bk_r])
                        dstw = Wt[:, hb * 8:(hb + 1) * 8, :]
                        srcw = bkv.rearrange("p (a b) -> p a b", a=8)
                        if hb == 0:
                            op("act", lambda e: e.copy(out=dstw, in_=srcw), [bk_r], [Wt_r])
                        else:
                            op("dve", lambda e: e.tensor_copy(out=dstw, in_=srcw), [bk_r], [Wt_r])
                    eb, eb_r = banks[3], bres[3]
                    ucv = U[:, g, :].rearrange("p (ch j) -> p ch j", j=16)
                    for j in range(16):
                        op("pe", lambda e: e.matmul(eb[:, 0:NCH], lhsT=Wt[:, j, :], rhs=ucv[:, :, j], start=(j == 0), stop=(j == 15)),
                           [Wt_r] + U_r[g], [eb_r])
                    op("act", lambda e: e.copy(out=Eall[:, d, g, :], in_=eb[:, 0:NCH]), [eb_r], [Eall_r])
            idb = ident.unsqueeze(1).to_broadcast([128, G, 128])
            swb = swp.unsqueeze(1).to_broadcast([128, G, 128])
            arb = l128r[:, d, :].unsqueeze(2).to_broadcast([128, G, 128])
            aib = ais[:, d, :].unsqueeze(2).to_broadcast([128, G, 128])
            op("dve", lambda e: e.tensor_tensor(out=A128, in0=idb, in1=arb, op=ALU.mult), [ident_r, l128r_r], [A128_r])
            op("pool", lambda e: e.tensor_tensor(out=aw, in0=swb, in1=aib, op=ALU.mult), [swp_r, ais_r], [aw_r])
            op("dve", lambda e: e.tensor_tensor(out=A128, in0=A128, in1=aw, op=ALU.add), [A128_r, aw_r], [A128_r])
            order = list(range(NCH)) if d == 0 else [1, 0] + list(range(NCH - 1, 1, -1))
            prev = None
            zb, zb_r = banks[4], bres[4]
            for ch in order:
                if prev is None:
                    op("dve", lambda e: e.tensor_copy(out=Zst[:, :, ch], in_=Eall[:, d, :, ch]), [Eall_r], [Zst_r])
                else:
                    for g in range(G):
                        op("pe", lambda e: e.matmul(zb[:, g:g + 1], lhsT=A128[:, g, :], rhs=Zst[:, g, prev:prev + 1], start=True, stop=True),
                           [A128_r, Zst_r], [zb_r])
                    op("dve", lambda e: e.tensor_tensor(out=Zst[:, :, ch], in0=zb[:, 0:G], in1=Eall[:, d, :, ch], op=ALU.add),
                       [zb_r, Eall_r], [Zst_r])
                    op("act", lambda e: e.copy(out=Zin[:, d, :, ch], in_=Zst[:, :, prev]), [Zst_r], [Zin_r])
                prev = ch

        PsA = [(PhA, PhA_r), tile((HG, 136), F32)]
        PsB = [(PhB, PhB_r), tile((HG, 136), F32)]
        PsmA = [tile((HG, 16), F32) for _ in range(2)]
        PsmB = [tile((HG, 16), F32) for _ in range(2)]
        Rt = [(Phi, Phi_r), tile((136, 16), BF16)]
        Lt = [tile((16, 16), BF16) for _ in range(2)]
        Tt = [(Wt, Wt_r), tile((16, 128), BF16)]
        yv, yv_r = tile((512,), F32)
        y2, y2_r = tile((512,), F32)
        y3, y3_r = tile((512,), F32)
        for g0 in range(0, G, HG):
            for d in range(2):
                pw_tables(d, g0, mpsi[:, d, :], mpsi_r, 136, PsA[d][0], PsA[d][1], PsB[d][0], PsB[d][1])
                pw_tables(d, g0, mphs[:, d, :], mphs_r, 16, PsmA[d][0], PsmA[d][1], PsmB[d][0], PsmB[d][1])
            for gi in range(HG):
                g = g0 + gi
                for d in range(2):
                    R, R_r = Rt[d]
                    L, L_r = Lt[d]
                    Tt_, Tt_r = Tt[d]
                    outer(R, R_r, PsA[d][0], PsA[d][1], PsB[d][0], PsB[d][1], gi, CA[:, g], CA_r, CB[:, g], CB_r, 136)
                    outer(L, L_r, PsmA[d][0], PsmA[d][1], PsmB[d][0], PsmB[d][1], gi, BA[:, d, g], BA_r, BB[:, d, g], BB_r, 16)
                    for q4 in range(4):
                        tb_, tb_r = banks[4], bres[4]
                        for dd in range(4):
                            Dl = q4 * 4 + dd
                            lh = L[:, 0:8, :] if Dl == 0 else L[:, 8:16, :]
                            if d == 0:
                                r0 = 0 if Dl == 0 else 8 * (Dl - 1) + 1
                            else:
                                r0 = 0 if Dl == 0 else 8 * (Dl - 1)
                            op("pe", lambda e: e.matmul(tb_[:, dd * 128:(dd + 1) * 128], lhsT=lh.rearrange("p a b -> p (a b)"),
                                                        rhs=R[:, r0:r0 + 8, :].rearrange("p a b -> p (a b)"), start=True, stop=True),
                               [L_r, R_r], [tb_r])
                        if q4 == 0:
                            op("dve", lambda e: e.tensor_tensor(out=y3[:, 0:128], in0=tb_[:, 0:128], in1=m0[:, d, :], op=ALU.mult), [tb_r, m0_r], [y3_r])
                            if d == 0:
                                op("dve", lambda e: e.scalar_tensor_tensor(out=Tt_[:, 0, :], in0=ident, scalar=dcol[:, g:g + 1], in1=y3[:, 0:128],
                                                                          op0=ALU.mult, op1=ALU.add), [y3_r, ident_r, dcol_r], [Tt_r])
                            else:
                                op("dve", lambda e: e.tensor_copy(out=Tt_[:, 0, :], in_=y3[:, 0:128]), [y3_r], [Tt_r])
                            op("act", lambda e: e.copy(out=Tt_[:, 1:4, :], in_=tb_[:, 128:512].rearrange("p (a b) -> p a b", a=3)), [tb_r], [Tt_r])
                        else:
                            op("act", lambda e: e.copy(out=Tt_[:, q4 * 4:q4 * 4 + 4, :], in_=tb_[:, 0:512].rearrange("p (a b) -> p a b", a=4)), [tb_r], [Tt_r])
                for bgi, (ch0, nch) in enumerate(bgs):
                    yb, yb_r = banks[5 + (g * NBG + bgi) % 2], bres[5 + (g * NBG + bgi) % 2]
                    ncol = nch * 16
                    ybv = yb[:, 0:ncol].rearrange("p (ch j) -> p ch j", j=16)
                    ucv = U[:, g, ch0 * 16:ch0 * 16 + ncol].rearrange("p (ch j) -> p ch j", j=16)
                    first = True
                    for d in range(2):
                        R, R_r = Rt[d]
                        Tt_, Tt_r = Tt[d]
                        for Dl in range(16):
                            if d == 0:
                                o_, r_ = ybv[:, :, Dl:16], ucv[:, :, 0:16 - Dl]
                            else:
                                o_, r_ = ybv[:, :, 0:16 - Dl], ucv[:, :, Dl:16]
                            op("pe", lambda e: e.matmul(o_, lhsT=Tt_[:, Dl, :], rhs=r_, start=first, stop=False), [Tt_r, U_r[g][bgi]], [yb_r])
                            first = False
                        for j in range(16):
                            r0 = 8 * j + 1 if d == 0 else 8 * (15 - j)
                            last = (d == 1 and j == 15)
                            op("pe", lambda e: e.matmul(ybv[:, :, j], lhsT=R[:, r0:r0 + 8, :].rearrange("p a b -> p (a b)"),
                                                        rhs=Zin[:, d, g, ch0:ch0 + nch], start=False, stop=last), [R_r, Zin_r], [yb_r])
                    n_ = ncol
                    op("act", lambda e: e.copy(out=yv[:, 0:n_], in_=yb[:, 0:n_]), [yb_r], [yv_r])
                    op("pool", lambda e: e.tensor_tensor(out=y2[:, 0:n_], in0=yv[:, 0:n_], in1=yv[:, 0:n_], op=ALU.mult), [yv_r], [y2_r])
                    op("dve", lambda e: e.tensor_scalar(out=y2[:, 0:n_], in0=y2[:, 0:n_], scalar1=0.044715, scalar2=1.0, op0=ALU.mult, op1=ALU.add),
                       [y2_r], [y2_r])
                    op("pool", lambda e: e.tensor_tensor(out=y2[:, 0:n_], in0=y2[:, 0:n_], in1=yv[:, 0:n_], op=ALU.mult), [y2_r, yv_r], [y2_r])
                    op("act", lambda e: e.activation(out=y2[:, 0:n_], in_=y2[:, 0:n_], func=AF.Sigmoid, scale=1.5957691216057308), [y2_r], [y2_r])
                    op("dve", lambda e: e.tensor_tensor(out=U[:, g, ch0 * 16:ch0 * 16 + n_], in0=yv[:, 0:n_], in1=y2[:, 0:n_], op=ALU.mult),
                       [yv_r, y2_r], [U_r[g][bgi]])

        yfm, yfm_r = ust, ust_r
        yo, yo_r = tile((2, 512), BF16)
        sg, sg_r = tile((512,), F32)
        for (c0, nb) in cgs:
            bgi = bg_of_col(c0)
            for hf in range(2):
                yfv = yfm[:, hf, 0:nb * 8].rearrange("p (tb s) -> p tb s", s=8)
                for t8 in range(8):
                    bk, bk_r = banks[t8 % 2], bres[t8 % 2]
                    for gl in range(8):
                        g = hf * 8 + gl
                        op("pe", lambda e: e.matmul(bk[:, 0:nb], lhsT=sel[:, gl * 8 + t8, :], rhs=U[:, g, c0:c0 + nb], start=(gl == 0), stop=(gl == 7)),
                           [sel_r, U_r[g][bgi]], [bk_r])
                    if t8 % 2 == 0:
                        op("act", lambda e: e.copy(out=yfv[:, :, t8], in_=bk[:, 0:nb]), [bk_r], [yfm_r])
                    else:
                        op("dve", lambda e: e.tensor_copy(out=yfv[:, :, t8], in_=bk[:, 0:nb]), [bk_r], [yfm_r])
            ntok = nb * 8
            for tt in range(0, ntok, 512):
                n_ = min(512, ntok - tt)
                tglob = c0 * 8 + tt
                for fc in range(2):
                    bk, bk_r = banks[7], bres[7]
                    for kc in range(2):
                        op("pe", lambda e: e.matmul(bk[:, 0:n_], lhsT=gw[:, kc, fc * 128:(fc + 1) * 128], rhs=yfm[:, kc, tt:tt + n_],
                                                    start=(kc == 0), stop=(kc == 1)), [gw_r, yfm_r], [bk_r])
                    op("act", lambda e: e.activation(out=sg[:, 0:n_], in_=bk[:, 0:n_], func=AF.Sigmoid, bias=glb[:, fc:fc + 1], scale=1.0),
                       [bk_r, glb_r], [sg_r])
                    op("dve", lambda e: e.tensor_tensor(out=yo[:, fc, 0:n_], in0=yfm[:, fc, tt:tt + n_], in1=sg[:, 0:n_], op=ALU.mult),
                       [yfm_r, sg_r], [yo_r])
                dma(ysT[:, :, tglob:tglob + n_].rearrange("c p t -> p c t"), yo[:, :, 0:n_], reads=[yo_r])

    def phase_s5_stub(l, with_ctx):
        z, z_r = tile((2, 512), BF16)
        op("pool", lambda e: e.memset(z, 0.0), [], [z_r])
        for (t0, n) in token_tiles(True):
            dma(ysT[:, :, t0:t0 + n].rearrange("c p t -> p c t"), z[:, :, 0:n], reads=[z_r])

    fins = []
    steps = [("mod", lambda: phase_mod()), ("tin", lambda: phase_transpose_in())]
    cur = 0
    for l in range(depth):
        with_ctx = l < depth - 1
        steps.append(("A%d" % l, lambda l=l, cur=cur: phase_A(l, xT[cur])))
        steps.append(("attn%d" % l, lambda l=l, w=with_ctx: phase_attn(l, w)))
        steps.append(("ml%d" % l, lambda l=l, w=with_ctx: phase_mlstm(l, w)))
        steps.append(("s5%d" % l, lambda l=l, w=with_ctx: (phase_s5 if HAVE_S5 else phase_s5_stub)(l, w)))
        steps.append(("C%d" % l, lambda l=l, cur=cur, w=with_ctx: phase_C(l, xT[cur], w)))
        steps.append(("D%d" % l, lambda l=l, cur=cur, w=with_ctx: phase_D(l, xT[cur], xT[1 - cur], w)))
        cur = 1 - cur
    steps.append(("E", lambda cur=cur: fins.extend(phase_E(xT[cur]))))
    for i, (nm, fn) in enumerate(steps):
        if upto is not None and nm == upto:
            break
        if i > 0:
            new_phase()
        fn()
    stats = S.emit(final_ops=fins)
    es.close()
    return nc, dbg_outs, stats


HAVE_S5 = True
import os
MLCUT = int(os.environ.get('MLCUT', '0'))
MLDIRS = int(os.environ.get('MLDIRS', '2'))


def make_in_maps(inp, T, depth=2, ncores=8):
    fm, tm, gb = _col_layout()
    cst = _consts(T)
    f = lambda a: np.ascontiguousarray(a, dtype=np.float32)
    shared = {}
    shared["mod_w"] = f(inp["mod_w"])
    shared["mod_b"] = f(inp["mod_b"].reshape(depth, 48, 128).transpose(0, 2, 1))
    shared["norm1_g"] = f(inp["norm1_g"].reshape(depth, 8, 128).transpose(0, 2, 1))
    shared["norm2_g"] = f(inp["norm2_g"].reshape(depth, 8, 128).transpose(0, 2, 1))
    shared["final_norm_g"] = f(inp["final_norm_g"].reshape(8, 128).T)
    shared["w_fm"] = f(inp["w_in"][:, :, fm])
    shared["w_tm"] = f(inp["w_in"][:, :, tm])
    shared["w_gb"] = f(inp["w_in"][:, :, gb])
    for k in ("w_branch_attn", "w_branch_s5", "w_branch_ml", "w_out", "ffn_w_up", "ffn_w_down"):
        shared[k] = f(inp[k])
    shared["ffn_conv_w"] = f(inp["ffn_conv_w"].reshape(depth, 3, 44, 128).transpose(0, 3, 1, 2))
    shared["ffn_conv_b"] = f(inp["ffn_conv_b"].reshape(depth, 44, 128).transpose(0, 2, 1))
    shared["attn_sink"] = f(inp["attn_sink"].reshape(depth, 1, 8))
    gbias = np.stack([np.concatenate([inp["ml_igate_b"][l, 0], inp["ml_fgate_b"][l, 0], inp["ml_igate_b"][l, 1], inp["ml_fgate_b"][l, 1]])
                      for l in range(depth)])
    shared["ml_gate_b"] = f(gbias.reshape(depth, 1, 16))
    shared["ml_norm_g"] = f(inp["ml_norm_g"].reshape(depth, 1, 256))
    a_st = np.stack([inp["s5_a_re"], inp["s5_a_im"]], axis=-1)
    a_st = a_st.transpose(0, 3, 1, 2, 4)
    shared["s5_a"] = f(np.concatenate([a_st, a_st], axis=1).reshape(depth, 128, 64))
    shared["s5_logdt"] = f(inp["s5_log_dt"].reshape(depth, 1, 32))
    b_st = np.stack([inp["s5_b_re"], inp["s5_b_im"]], axis=-1).transpose(0, 2, 1, 3, 4)
    shared["s5_b"] = f(np.concatenate([b_st, b_st], axis=1).reshape(depth, 128, 512))
    c_st = np.stack([inp["s5_c_re"], inp["s5_c_im"]], axis=-1).transpose(0, 3, 1, 2, 4)
    shared["s5_c"] = f(np.concatenate([c_st, c_st], axis=1).reshape(depth, 128, 512))
    dd = inp["s5_d"].reshape(depth, 16, 16).transpose(0, 2, 1)
    shared["s5_dcol"] = f(np.tile(dd, (1, 8, 1)))
    shared["s5_glu_w"] = f(inp["s5_glu_w"])
    shared["s5_glu_b"] = f(inp["s5_glu_b"].reshape(depth, 2, 128).transpose(0, 2, 1))
    shared.update(cst)
    maps = []
    for b in range(ncores):
        m = dict(shared)
        m["x"] = f(inp["x"][b, :T])
        m["ctx"] = f(inp["ctx"][b])
        cv = np.stack([inp["c"][b], inp["c_ctx"]], axis=-1)
        m["cvec"] = f(cv.reshape(8, 128, 2).transpose(1, 0, 2))
        maps.append(m)
    return maps


_CACHE = {}


def kernel(**inputs):
    inp = {k: np.asarray(v) for k, v in inputs.items()}
    B, T = inp["x"].shape[0], inp["x"].shape[1]
    if T not in _CACHE:
        _CACHE[T] = build(T)
    nc = _CACHE[T][0]
    maps = make_in_maps(inp, T, ncores=B)
    res = run_bass_kernel_spmd(nc, maps, core_ids=list(range(B)))
    return np.stack([r["y"] for r in res.results], axis=0).astype(np.float32)
```

```python
import math
import os
from contextlib import ExitStack
import numpy as np
import ml_dtypes
import concourse.bass as bass
import concourse.mybir as mybir
from concourse.bass_utils import run_bass_kernel_spmd

F32 = mybir.dt.float32
BF16 = mybir.dt.bfloat16
I32 = mybir.dt.int32
AF = mybir.ActivationFunctionType
ALU = mybir.AluOpType
AX = mybir.AxisListType

D = 1024
CTX = 256
NH, NKV, HD = 8, 2, 64
W_S5, S5G, S5P = 256, 16, 64
MLH = 4
D_FF = 2816
EPS = 1e-6
ENG = ("pe", "act", "dve", "pool", "sp")
TWO_PI = 2.0 * math.pi


class Res:
    __slots__ = ("last_w", "readers")

    def __init__(self):
        self.last_w = None
        self.readers = []


class Op:
    __slots__ = ("eng", "fn", "dma", "deps", "sig", "idx", "sem", "val")


class _Rec:
    def __init__(self):
        self.call = None

    def __getattr__(self, name):
        def f(*a, **k):
            self.call = (name, a, k)
            return None
        return f


class Sched:
    def __init__(self, nc, ndma=8):
        self.nc, self.ops, self.ndma = nc, [], ndma
        self.phase = Res()

    def op(self, eng, fn, reads=(), writes=(), dma=False, barrier=False):
        rec = _Rec()
        fn(rec)
        name_, a_, k_ = rec.call
        fn = lambda engine, name_=name_, a_=a_, k_=k_: getattr(engine, name_)(*a_, **k_)
        o = Op()
        o.eng, o.fn, o.dma, o.deps, o.sig, o.sem, o.val = eng, fn, dma, set(), False, None, None
        o.idx = len(self.ops)
        reads = list(reads)
        writes = list(writes)
        if barrier:
            writes.append(self.phase)
        else:
            reads.append(self.phase)
        for r in reads:
            if r.last_w is not None:
                o.deps.add(r.last_w)
        for r in writes:
            if r.last_w is not None:
                o.deps.add(r.last_w)
            o.deps.update(r.readers)
        for r in reads:
            r.readers.append(o.idx)
        for r in writes:
            r.last_w = o.idx
            r.readers = []
        o.deps.discard(o.idx)
        self.ops.append(o)
        return o

    def emit(self, final_ops=()):
        nc, ops = self.nc, self.ops
        for o in ops:
            keep = set()
            for d in o.deps:
                p = ops[d]
                if p.eng == o.eng and not p.dma and o.eng in ("pe", "sp"):
                    continue
                keep.add(d)
            o.deps = keep
            for d in keep:
                ops[d].sig = True
        for f in final_ops:
            f.sig = True
        st = ExitStack()
        sems = {e: st.enter_context(nc.semaphore("s_" + e)) for e in ENG}
        dsems = [st.enter_context(nc.semaphore("d_%d" % i)) for i in range(self.ndma)]
        cnt = {e: 0 for e in ENG}
        dcnt = 0
        prev_user = {}
        for o in ops:
            if o.dma:
                n = dcnt
                dcnt += 1
                o.sem = dsems[n % self.ndma]
                o.val = 16 * (n // self.ndma + 1)
                if (n % self.ndma) in prev_user:
                    o.deps.add(prev_user[n % self.ndma])
                prev_user[n % self.ndma] = o.idx
                o.sig = True
            elif o.sig:
                cnt[o.eng] += 1
                o.sem, o.val = sems[o.eng], cnt[o.eng]
        per = {e: [o for o in ops if o.eng == e] for e in ENG}
        stats = dict(nops=len(ops), nwait=0)

        def run(e, engine):
            waited = {}
            for o in per[e]:
                need = {}
                for d in o.deps:
                    p = ops[d]
                    k = id(p.sem)
                    if k not in need or need[k][1] < p.val:
                        need[k] = (p.sem, p.val)
                for k, (sem, val) in need.items():
                    if waited.get(k, 0) >= val:
                        continue
                    engine.wait_ge(sem, val)
                    stats["nwait"] += 1
                    waited[k] = val
                ins = o.fn(engine)
                if o.dma:
                    ins.then_inc(o.sem, 16)
                elif o.sig:
                    ins.then_inc(o.sem, 1)
            if e == "sp":
                for f in final_ops:
                    engine.wait_ge(f.sem, f.val)

        with nc.Block() as block:
            @block.sync
            def _(eng):
                run("sp", eng)

            @block.tensor
            def _(eng):
                run("pe", eng)

            @block.scalar
            def _(eng):
                run("act", eng)

            @block.vector
            def _(eng):
                run("dve", eng)

            @block.gpsimd
            def _(eng):
                run("pool", eng)
        st.close()
        return stats


def _rope_perm():
    p = np.arange(64)
    out = np.empty(64, dtype=np.int64)
    for d in range(64):
        base = (d // 32) * 32
        r = d % 32
        out[d] = base + (r + 16 if r < 16 else r - 16)
    return out


def _col_layout():
    o_q, o_k, o_v = 0, 512, 640
    o_us = 768
    o_qm, o_km, o_vm, o_om = 1024, 1280, 1536, 1792
    o_gm = 2048
    o_gb = 2064
    perm = _rope_perm()
    fm = []
    for c in range(4):
        fm += [o_q + c * 64 + d for d in range(64)] + [o_q + (4 + c) * 64 + d for d in range(64)]
    for c in range(4):
        fm += [o_q + c * 64 + perm[d] for d in range(64)] + [o_q + (4 + c) * 64 + perm[d] for d in range(64)]
    fm += [o_k + d for d in range(128)]
    fm += [o_k + h * 64 + perm[d] for h in range(2) for d in range(64)]
    fm += [o_us + d for d in range(256)]
    fm += [o_qm + d for d in range(256)]
    fm += [o_km + d for d in range(256)]
    tm = [o_v + d for d in range(128)] + [o_km + d for d in range(256)] + [o_vm + d for d in range(256)] \
        + [o_om + d for d in range(256)] + [o_gm + d for d in range(16)]
    gb = [o_gb + d for d in range(3072)]
    return np.array(fm), np.array(tm), np.array(gb)


FM_Q, FM_QP, FM_K, FM_KP, FM_US, FM_QM, FM_KM = 0, 4, 8, 9, 10, 12, 14
NFM = 16
TM_V, TM_KM, TM_VM, TM_OM, TM_GM = 0, 128, 384, 640, 896
NTM = 912


def _consts(T):
    NT = CTX + T
    c = {}
    c["ident_f"] = np.eye(128, dtype=np.float32)
    k = np.arange(128)[:, None]
    q = np.arange(128)[None, :]
    c["mask_le"] = (k <= q).astype(np.float32)
    c["mask_ge"] = (k >= q).astype(np.float32)
    rows = T // 64
    row = np.repeat(np.arange(rows, dtype=np.float32), 64)
    col = np.tile(np.arange(64, dtype=np.float32), rows)
    inv = (10000.0 ** (-np.arange(16, dtype=np.float32) / 16)).astype(np.float32)
    ang_r = (row[:, None] * inv[None, :]).astype(np.float32)
    ang_c = (col[:, None] * inv[None, :]).astype(np.float32)
    cosT = np.ones((128, NT), np.float32)
    sinT = np.zeros((128, NT), np.float32)
    for p in range(128):
        d = p % 64
        a = ang_r if d < 32 else ang_c
        r = d % 32
        f = r % 16
        cosT[p, CTX:] = np.cos(a[:, f])
        s = np.sin(a[:, f])
        sinT[p, CTX:] = -s if r < 16 else s
    c["rope_cos"] = cosT
    hp = math.pi / 2
    cols = np.zeros((128, 4), np.float32)
    cols[:64, 0] = hp
    cols[64:, 1] = hp
    cols[:64, 2], cols[64:, 2] = -1.0, 1.0
    cols[:64, 3], cols[64:, 3] = 1.0, -1.0
    c["s5_cols"] = cols
    mi = np.arange(136, dtype=np.float32)
    c["s5_mphi"] = np.stack([127.0 - mi, mi - 8.0]).reshape(1, 272).astype(np.float32)
    qi = np.arange(136)
    c["s5_mpsi"] = np.stack([qi.astype(np.float32), (8 * (qi // 8) + 8 - qi % 8).astype(np.float32)]).reshape(1, 272).astype(np.float32)
    s8 = np.arange(8, dtype=np.float32)
    c["s5_mphs"] = np.stack([np.concatenate([-s8, 7.0 - s8]), np.concatenate([s8 - 8.0, s8])]).reshape(1, 32).astype(np.float32)
    rr = np.arange(128)[:, None] // 16
    cc = np.arange(128)[None, :] // 16
    c["s5_m0"] = np.stack([(cc >= rr), (rr >= cc)]).astype(np.float32)
    sw = np.zeros((128, 128), np.float32)
    sw[np.arange(128), (np.arange(128) + 64) % 128] = 1.0
    c["s5_swap"] = sw
    sel = np.zeros((128, 64, 128), np.float32)
    selT = np.zeros((128, 64, 128), np.float32)
    for gl in range(8):
        for s in range(8):
            for ch in range(16):
                sel[gl * 16 + ch, gl * 8 + s, s * 16 + ch] = 1.0
                selT[s * 16 + ch, gl * 8 + s, gl * 16 + ch] = 1.0
    c["s5_sel"] = sel.astype(ml_dtypes.bfloat16)
    c["s5_selT"] = selT.astype(ml_dtypes.bfloat16)
    c["rope_sin"] = sinT
    return c


def build(T, depth=2, debug=False, upto=None):
    NT = CTX + T
    nc = bass.Bass("TRN2", target_bir_lowering=False)
    es = ExitStack()
    S = Sched(nc)
    dbg_outs = {}

    def din(name, shape, dt=F32):
        return nc.dram_tensor(name, list(shape), dt, kind="ExternalInput").ap()

    def dscr(name, shape, dt=F32):
        kind = "ExternalOutput" if debug else "Internal"
        t = nc.dram_tensor(name, list(shape), dt, kind=kind).ap()
        if debug:
            dbg_outs[name] = t
        return t

    x_in = din("x", [T, D])
    ctx_in = din("ctx", [CTX, D])
    cvec = din("cvec", [128, 8, 2])
    mod_w = din("mod_w", [depth, D, 6 * D])
    mod_b = din("mod_b", [depth, 128, 48])
    n1g = din("norm1_g", [depth, 128, 8])
    n2g = din("norm2_g", [depth, 128, 8])
    fng = din("final_norm_g", [128, 8])
    w_fm = din("w_fm", [depth, D, NFM * 128])
    w_tm = din("w_tm", [depth, D, NTM])
    w_gb = din("w_gb", [depth, D, 3072])
    wb_a = din("w_branch_attn", [depth, 512, D])
    wb_s = din("w_branch_s5", [depth, 256, D])
    wb_m = din("w_branch_ml", [depth, 256, D])
    w_out = din("w_out", [depth, D, D])
    w_up = din("ffn_w_up", [depth, D, 2 * D_FF])
    cw = din("ffn_conv_w", [depth, 128, 3, 44])
    cb = din("ffn_conv_b", [depth, 128, 44])
    w_dn = din("ffn_w_down", [depth, D_FF, D])
    sink = din("attn_sink", [depth, 1, 8])
    ml_gb = din("ml_gate_b", [depth, 1, 16])
    ml_ng = din("ml_norm_g", [depth, 1, 256])
    s5_a = din("s5_a", [depth, 128, 64])
    s5_logdt = din("s5_logdt", [depth, 1, 32])
    s5_b = din("s5_b", [depth, 128, 512])
    s5_c = din("s5_c", [depth, 128, 512])
    s5_dcol = din("s5_dcol", [depth, 128, 16])
    s5_glu_w = din("s5_glu_w", [depth, 256, 256])
    s5_glu_b = din("s5_glu_b", [depth, 128, 2])
    s5_cols = din("s5_cols", [128, 4])
    s5_mphi = din("s5_mphi", [1, 272])
    s5_mpsi = din("s5_mpsi", [1, 272])
    s5_mphs = din("s5_mphs", [1, 32])
    s5_m0 = din("s5_m0", [2, 128, 128])
    s5_swap = din("s5_swap", [128, 128])
    s5_sel = din("s5_sel", [128, 64, 128], BF16)
    s5_selT = din("s5_selT", [128, 64, 128], BF16)
    ident_f = din("ident_f", [128, 128])
    mask_le = din("mask_le", [128, 128])
    mask_ge = din("mask_ge", [128, 128])
    rope_cos = din("rope_cos", [128, NT])
    rope_sin = din("rope_sin", [128, NT])
    y_out = nc.dram_tensor("y", [T, D], F32, kind="ExternalOutput").ap()

    xT = [dscr("xT%d" % i, [8, 128, NT]) for i in range(2)]
    kT = dscr("kT", [128, NT], BF16)
    usT = dscr("usT", [2, 128, NT], BF16)
    km_tok = dscr("km_tok", [NT, 256], BF16)
    yaT = dscr("yaT", [4, 128, NT], BF16)
    ysT = dscr("ysT", [2, 128, NT], BF16)
    ymT = dscr("ymT", [2, 128, NT], BF16)
    hF = dscr("hF", [NT, 256])
    qa_c = dscr("qa_c", [NT // 128, 128, 512], BF16)
    qkm_c = dscr("qkm_c", [NT // 128, 128, 512], BF16)
    vaug_tok = dscr("vaug_tok", [NT, 132], BF16)
    vmaug_tok = dscr("vmaug_tok", [NT, 264], BF16)
    gmom_tok = dscr("gmom_tok", [NT, 272])

    ARENA = 208896 // 2
    arena = es.enter_context(nc.sbuf_tensor("arena", [128, ARENA], BF16))
    off = [0]
    persist_off = [0]

    def tile(shape, dt, parts=128):
        n = int(np.prod(shape)) * (1 if dt == BF16 else 2)
        n = (n + 15) // 16 * 16
        assert off[0] + n <= ARENA, ("SBUF arena overflow", off[0], n)
        a = arena[0:parts, off[0]:off[0] + n]
        off[0] += n
        used = int(np.prod(shape)) * (1 if dt == BF16 else 2)
        a = a[:, 0:used]
        if dt != BF16:
            a = a.bitcast(dt)
        if len(shape) == 2:
            a = a.rearrange("p (a b) -> p a b", a=shape[0])
        elif len(shape) == 3:
            a = a.rearrange("p (a b c) -> p a b c", a=shape[0], b=shape[1])
        return a, Res()

    banks = [es.enter_context(nc.psum_tensor("bank%d" % i, [128, 512], F32)) for i in range(8)]
    bres = [Res() for _ in range(8)]
    junk, junk_r = tile((4,), F32)

    def barrier():
        S.op("pool", lambda e: e.memset(junk[:, 0:1], 0.0), writes=[junk_r], barrier=True)

    def new_phase():
        barrier()
        off[0] = persist_off[0]

    def dma(out, in_, reads=(), writes=()):
        return S.op("sp", lambda e: e.dma_start(out=out, in_=in_), reads, writes, dma=True)

    def op(eng, fn, reads=(), writes=()):
        return S.op(eng, fn, reads, writes)

    def cast_load(dst, dst_r, src_ap, shape, stage, stage_r, eng="pool"):
        dma(stage, src_ap, writes=[stage_r])
        if eng == "act":
            op("act", lambda e: e.copy(out=dst, in_=stage), [stage_r], [dst_r])
        else:
            op(eng, lambda e: e.tensor_copy(out=dst, in_=stage), [stage_r], [dst_r])

    ident, ident_r = tile((128,), F32)
    identb, identb_r = tile((128,), BF16)
    ones_f, ones_r = tile((128,), F32)
    mle, mle_r = tile((128,), F32)
    mge, mge_r = tile((128,), F32)
    mleb, mleb_r = tile((128,), BF16)
    mgeb, mgeb_r = tile((128,), BF16)
    mle8, mle8_r = tile((128,), BF16)
    mge8, mge8_r = tile((128,), BF16)
    modv, modv_r = tile((depth, 48, 2), F32)
    gs1, gs1_r = tile((depth, 8, 2), F32)
    gs2, gs2_r = tile((depth, 8, 2), F32)
    n1t, n1t_r = tile((depth, 8), F32)
    n2t, n2t_r = tile((depth, 8), F32)
    fnt, fnt_r = tile((8,), F32)
    persist_off[0] = off[0]

    dma(ident, ident_f, writes=[ident_r])
    dma(mle, mask_le, writes=[mle_r])
    dma(mge, mask_ge, writes=[mge_r])
    op("dve", lambda e: e.tensor_copy(out=identb, in_=ident), [ident_r], [identb_r])
    op("dve", lambda e: e.tensor_copy(out=mleb, in_=mle), [mle_r], [mleb_r])
    op("dve", lambda e: e.tensor_copy(out=mgeb, in_=mge), [mge_r], [mgeb_r])
    op("pool", lambda e: e.memset(ones_f, 1.0), writes=[ones_r])
    op("dve", lambda e: e.tensor_scalar_mul(out=mle8, in0=mle, scalar1=0.125), [mle_r], [mle8_r])
    op("dve", lambda e: e.tensor_scalar_mul(out=mge8, in0=mge, scalar1=0.125), [mge_r], [mge8_r])
    dma(n1t, n1g.rearrange("l p c -> p l c"), writes=[n1t_r])
    dma(n2t, n2g.rearrange("l p c -> p l c"), writes=[n2t_r])
    dma(fnt, fng, writes=[fnt_r])

    def phase_mod():
        cv, cv_r = tile((8, 2), F32)
        sc, sc_r = tile((8, 2), F32)
        dma(cv, cvec, writes=[cv_r])
        op("act", lambda e: e.activation(out=sc, in_=cv, func=AF.Silu), [cv_r], [sc_r])
        mb, mb_r = tile((depth, 48), F32)
        dma(mb, mod_b.rearrange("l p c -> p l c"), writes=[mb_r])
        wst = [tile((8, 1024), F32) for _ in range(2)]
        i = 0
        for l in range(depth):
            for piece in range(6):
                w, w_r = wst[i % 2]
                i += 1
                dma(w, mod_w[l, :, piece * 1024:(piece + 1) * 1024].rearrange("(kc p) n -> p kc n", p=128), writes=[w_r])
                bk, bk_r = banks[piece % 2], bres[piece % 2]
                for fc in range(8):
                    for kc in range(8):
                        op("pe", lambda e, w=w, fc=fc, kc=kc, bk=bk: e.matmul(
                            bk[:, fc * 2:fc * 2 + 2], lhsT=w[:, kc, fc * 128:(fc + 1) * 128], rhs=sc[:, kc, :],
                            start=(kc == 0), stop=(kc == 7)), [w_r, sc_r], [bk_r])
                dst = modv[:, l, piece * 8:(piece + 1) * 8, :]
                src = bk[:, 0:16].rearrange("p (a b) -> p a b", a=8)
                bb = mb[:, l, piece * 8:(piece + 1) * 8].unsqueeze(2).to_broadcast([128, 8, 2])
                op("dve", lambda e, dst=dst, src=src, bb=bb: e.tensor_tensor(out=dst, in0=src, in1=bb, op=ALU.add),
                   [bk_r, mb_r], [modv_r])
        for l in range(depth):
            for (gs, gs_r, nt, nt_r, pc) in ((gs1, gs1_r, n1t, n1t_r, 1), (gs2, gs2_r, n2t, n2t_r, 4)):
                src = modv[:, l, pc * 8:(pc + 1) * 8, :]
                nb = nt[:, l, :].unsqueeze(2).to_broadcast([128, 8, 2])
                dst = gs[:, l, :, :]
                op("dve", lambda e, dst=dst, src=src, nb=nb: e.scalar_tensor_tensor(
                    out=dst, in0=src, scalar=1.0, in1=nb, op0=ALU.add, op1=ALU.mult), [modv_r, nt_r], [gs_r])

    def phase_transpose_in():
        xin = [tile((1024,), F32) for _ in range(2)]
        xo = [tile((8, 128), F32) for _ in range(2)]
        nblk = NT // 128
        for b in range(nblk):
            t0 = b * 128
            xt, xt_r = xin[b % 2]
            src = ctx_in[t0:t0 + 128, :] if t0 < CTX else x_in[t0 - CTX:t0 - CTX + 128, :]
            dma(xt, src, writes=[xt_r])
            o, o_r = xo[b % 2]
            for half in range(2):
                bk, bk_r = banks[(b * 2 + half) % 4], bres[(b * 2 + half) % 4]
                for j in range(4):
                    c = half * 4 + j
                    op("pe", lambda e, bk=bk, j=j, xt=xt, c=c: e.transpose(
                        bk[:, j * 128:(j + 1) * 128], xt[:, c * 128:(c + 1) * 128], ident), [xt_r, ident_r], [bk_r])
                dst = o[:, half * 4:(half + 1) * 4, :]
                srcp = bk[:, :].rearrange("p (a b) -> p a b", a=4)
                if half == 0:
                    op("dve", lambda e, dst=dst, srcp=srcp: e.tensor_copy(out=dst, in_=srcp), [bk_r], [o_r])
                else:
                    op("act", lambda e, dst=dst, srcp=srcp: e.copy(out=dst, in_=srcp), [bk_r], [o_r])
            dma(xT[0][:, :, t0:t0 + 128].rearrange("c p t -> p c t"), o, reads=[o_r])

    def norm_mod(xt, xt_r, n, hT, hT_r, gs, gs_r, shcol, which, sq, sq_r, rs, rs_r, bk, bk_r, l, zero_cols=()):
        for c in range(8):
            if c % 2 == 0:
                op("act", lambda e, c=c: e.activation(out=sq[:, c, 0:n], in_=xt[:, c, 0:n], func=AF.Square), [xt_r], [sq_r[c]])
            else:
                op("pool", lambda e, c=c: e.tensor_tensor(out=sq[:, c, 0:n], in0=xt[:, c, 0:n], in1=xt[:, c, 0:n], op=ALU.mult),
                   [xt_r], [sq_r[c]])
        for c in range(8):
            op("pe", lambda e, c=c: e.matmul(bk[:, 0:n], lhsT=ones_f, rhs=sq[:, c, 0:n], start=(c == 0), stop=(c == 7)),
               [sq_r[c], ones_r], [bk_r])
        op("act", lambda e: e.activation(out=rs[:, 0:n], in_=bk[:, 0:n], func=AF.Sqrt, bias=EPS, scale=1.0 / D), [bk_r], [rs_r])
        op("dve", lambda e: e.reciprocal(out=rs[:, 0:n], in_=rs[:, 0:n]), [rs_r], [rs_r])
        for c in range(8):
            op("dve", lambda e, c=c: e.tensor_tensor(out=sq[:, c, 0:n], in0=xt[:, c, 0:n], in1=rs[:, 0:n], op=ALU.mult),
               [xt_r, rs_r], [sq_r[c]])
            op("act", lambda e, c=c: e.activation(out=hT[:, c, 0:n], in_=sq[:, c, 0:n], func=AF.Identity,
                                                  bias=modv[:, l, shcol * 8 + c, which:which + 1],
                                                  scale=gs[:, l, c, which:which + 1]), [sq_r[c], modv_r, gs_r], [hT_r])
        for zc in zero_cols:
            op("pool", lambda e, zc=zc: e.memset(hT[:, :, zc:zc + 1], 0.0), [], [hT_r])

    def token_tiles(with_ctx=True, size=512):
        tl = []
        if with_ctx:
            tl.append((0, CTX))
        t = CTX
        while t < NT:
            n = min(size, NT - t)
            tl.append((t, n))
            t += n
        return tl

    def phase_A(l, xsrc):
        wfm, wfm_r = tile((8, NFM * 128), BF16)
        wtm, wtm_r = tile((8, NTM), BF16)
        stg = [tile((8, 512), F32) for _ in range(2)]
        si = 0
        for cc in range(NFM * 128 // 512):
            st_, st_r = stg[si % 2]
            si += 1
            cast_load(wfm[:, :, cc * 512:(cc + 1) * 512], wfm_r,
                      w_fm[l, :, cc * 512:(cc + 1) * 512].rearrange("(kc p) n -> p kc n", p=128), None, st_, st_r,
                      eng=("pool" if cc % 2 == 0 else "dve"))
        for (c0, c1) in ((0, 512), (512, NTM)):
            st_, st_r = stg[si % 2]
            si += 1
            cast_load(wtm[:, :, c0:c1], wtm_r, w_tm[l, :, c0:c1].rearrange("(kc p) n -> p kc n", p=128), None,
                      st_[:, :, 0:c1 - c0], st_r, eng="pool")
        xts = [tile((8, 512), F32) for _ in range(2)]
        sq, _ = tile((8, 512), F32)
        sq_r = [Res() for _ in range(8)]
        rs, rs_r = tile((512,), F32)
        hT, hT_r = tile((8, 512), BF16)
        fo, fo_r = tile((NFM, 512), BF16)
        rc, rc_r = tile((512,), F32)
        rsn, rsn_r = tile((512,), F32)
        tmpa, tmpa_r = tile((512,), F32)
        tmpb, tmpb_r = tile((512,), F32)
        to_f, to_fr = tile((272,), F32)
        to_va, to_va_r = tile((2, 66), BF16)
        to_vm, to_vm_r = tile((4, 66), BF16)
        to_km, to_km_r = tile((256,), BF16)
        op("pool", lambda e: e.memset(to_va, 1.0), [], [to_va_r])
        op("pool", lambda e: e.memset(to_vm, 1.0), [], [to_vm_r])
        foq, foq_r = tile((4, 4, 128), BF16)
        fom, fom_r = tile((4, 4, 128), BF16)
        hTs = [(hT, hT_r), tile((8, 512), BF16)]
        tiles_ = token_tiles()

        def ln_A(ti):
            t0, n = tiles_[ti]
            xt, xt_r = xts[ti % 2]
            dma(xt[:, :, 0:n], xsrc[:, :, t0:t0 + n].rearrange("c p t -> p c t"), writes=[xt_r])
            norm_mod(xt, xt_r, n, hTs[ti % 2][0], hTs[ti % 2][1], gs1, gs1_r, 0, 1 if t0 < CTX else 0, sq, sq_r, rs, rs_r,
                     banks[0], bres[0], l)

        ln_A(0)
        for ti, (t0, n) in enumerate(tiles_):
            if ti + 1 < len(tiles_):
                ln_A(ti + 1)
            which = 1 if t0 < CTX else 0
            hT, hT_r = hTs[ti % 2]
            if which == 0:
                dma(rc[:, 0:n], rope_cos[:, t0:t0 + n], writes=[rc_r])
                dma(rsn[:, 0:n], rope_sin[:, t0:t0 + n], writes=[rsn_r])
            def fm_mm(m, bk, bk_r):
                for kc in range(8):
                    op("pe", lambda e, m=m, kc=kc, bk=bk: e.matmul(
                        bk[:, 0:n], lhsT=wfm[:, kc, m * 128:(m + 1) * 128], rhs=hT[:, kc, 0:n],
                        start=(kc == 0), stop=(kc == 7)), [wfm_r, hT_r], [bk_r])
            bi = 1
            for (m, mp) in [(FM_Q + c, FM_QP + c) for c in range(4)] + [(FM_K, FM_KP)]:
                b1, b1r = banks[1 + (bi % 3) * 2], bres[1 + (bi % 3) * 2]
                b2, b2r = banks[2 + (bi % 3) * 2], bres[2 + (bi % 3) * 2]
                bi += 1
                fm_mm(m, b1, b1r)
                if which == 0:
                    fm_mm(mp, b2, b2r)
                    op("dve", lambda e, b1=b1: e.tensor_tensor(out=tmpa[:, 0:n], in0=b1[:, 0:n], in1=rc[:, 0:n], op=ALU.mult),
                       [b1r, rc_r], [tmpa_r])
                    op("dve", lambda e, b2=b2: e.tensor_tensor(out=tmpb[:, 0:n], in0=b2[:, 0:n], in1=rsn[:, 0:n], op=ALU.mult),
                       [b2r, rsn_r], [tmpb_r])
                    cm = lambda a: a[:, 0:n].rearrange("p (ch t) -> p ch t", t=128)
                    if m < FM_Q + 4:
                        op("pool", lambda e: e.tensor_tensor(out=foq[:, 0:n // 128, m - FM_Q, :], in0=cm(tmpa), in1=cm(tmpb), op=ALU.add),
                           [tmpa_r, tmpb_r], [foq_r])
                    else:
                        op("pool", lambda e: e.tensor_tensor(out=fo[:, m, 0:n], in0=tmpa[:, 0:n], in1=tmpb[:, 0:n], op=ALU.add),
                           [tmpa_r, tmpb_r], [fo_r])
                else:
                    if m < FM_Q + 4:
                        op("act", lambda e: e.copy(out=foq[:, 0:n // 128, m - FM_Q, :], in_=b1[:, 0:n].rearrange("p (ch t) -> p ch t", t=128)),
                           [b1r], [foq_r])
                    else:
                        op("act", lambda e: e.copy(out=fo[:, m, 0:n], in_=b1[:, 0:n]), [b1r], [fo_r])
            for m in range(FM_US, NFM):
                b1, b1r = banks[1 + (m % 6)], bres[1 + (m % 6)]
                fm_mm(m, b1, b1r)
                if m >= FM_QM:
                    dst_, dst_r = fom[:, 0:n // 128, m - FM_QM, :], fom_r
                    src_ = b1[:, 0:n].rearrange("p (ch t) -> p ch t", t=128)
                else:
                    dst_, dst_r, src_ = fo[:, m, 0:n], fo_r, b1[:, 0:n]
                if m % 2 == 0:
                    op("act", lambda e: e.copy(out=dst_, in_=src_), [b1r], [dst_r])
                else:
                    op("dve", lambda e: e.tensor_copy(out=dst_, in_=src_), [b1r], [dst_r])
            c0_, nc_ = t0 // 128, n // 128
            if os.environ.get("R4SKIP") != "dmaq":
                dma(qa_c[c0_:c0_ + nc_].rearrange("ch p x -> p ch x"), foq[:, 0:nc_].rearrange("p ch m t -> p ch (m t)"), reads=[foq_r])
                dma(qkm_c[c0_:c0_ + nc_].rearrange("ch p x -> p ch x"), fom[:, 0:nc_].rearrange("p ch m t -> p ch (m t)"), reads=[fom_r])
            dma(kT[:, t0:t0 + n], fo[:, FM_K, 0:n], reads=[fo_r])
            dma(usT[:, :, t0:t0 + n].rearrange("c p t -> p c t"), fo[:, FM_US:FM_US + 2, 0:n], reads=[fo_r])
            for sb in range(n // 128):
                b1, b1r = banks[1 + (sb % 2) * 2], bres[1 + (sb % 2) * 2]
                b2, b2r = banks[2 + (sb % 2) * 2], bres[2 + (sb % 2) * 2]
                for (bk, bk_r, c0, c1) in ((b1, b1r, 0, 512), (b2, b2r, 512, NTM)):
                    for kc in range(8):
                        op("pe", lambda e, bk=bk, kc=kc, c0=c0, c1=c1, sb=sb: e.matmul(
                            bk[:, 0:c1 - c0], lhsT=hT[:, kc, sb * 128:(sb + 1) * 128], rhs=wtm[:, kc, c0:c1],
                            start=(kc == 0), stop=(kc == 7)), [hT_r, wtm_r], [bk_r])
                hv = lambda a, nh: a.rearrange("p (h d) -> p h d", h=nh)
                op("act", lambda e: e.copy(out=to_va[:, :, 0:64], in_=hv(b1[:, 0:128], 2)), [b1r], [to_va_r])
                op("act", lambda e: e.copy(out=to_km, in_=b1[:, 128:384]), [b1r], [to_km_r])
                op("act", lambda e: e.copy(out=to_vm[:, 0:2, 0:64], in_=hv(b1[:, 384:512], 2)), [b1r], [to_vm_r])
                op("dve", lambda e: e.tensor_copy(out=to_vm[:, 2:4, 0:64], in_=hv(b2[:, 0:128], 2)), [b2r], [to_vm_r])
                op("dve", lambda e: e.tensor_copy(out=to_f, in_=b2[:, 128:400]), [b2r], [to_fr])
                tt = t0 + sb * 128
                if os.environ.get("R4SKIP") != "tm":
                    dma(vaug_tok[tt:tt + 128, :], to_va.rearrange("p a b -> p (a b)"), reads=[to_va_r])
                    dma(km_tok[tt:tt + 128, :], to_km, reads=[to_km_r])
                    dma(vmaug_tok[tt:tt + 128, :], to_vm.rearrange("p a b -> p (a b)"), reads=[to_vm_r])
                    dma(gmom_tok[tt:tt + 128, :], to_f, reads=[to_fr])

    def phase_attn(l, with_ctx):
        esk, esk_r = tile((8,), F32)
        dma(esk, sink[l].partition_broadcast(128), writes=[esk_r])
        op("act", lambda e: e.activation(out=esk, in_=esk, func=AF.Exp), [esk_r], [esk_r])
        kc_, kc_r = tile((CTX,), BF16)
        dma(kc_, kT[:, 0:CTX], writes=[kc_r])
        vc_, vc_r = tile((2, 2, 66), BF16)
        op("pool", lambda e: e.memset(vc_, 1.0), [], [vc_r])
        for j in range(2):
            dma(vc_[:, j].rearrange("p g d -> p (g d)"), vaug_tok[j * 128:(j + 1) * 128, :], writes=[vc_r])
        NS = 3
        qs = [tile((4, 128), BF16) for _ in range(NS)]
        ks = [tile((384,), BF16) for _ in range(NS)]
        vs = [tile((3, 2, 66), BF16) for _ in range(NS)]
        for (v_, v_r) in vs:
            op("pool", lambda e, v_=v_: e.memset(v_, 1.0), [], [v_r])
        pts = [tile((512,), BF16) for _ in range(10)]
        ya, ya_r = tile((512,), BF16)
        yat, yat_r = tile((4, 128), BF16)
        den, den_r = tile((4,), F32)
        nlat = T // 128
        blocks = [("lat", j) for j in range(nlat)]
        if with_ctx:
            blocks += [("ctx", 0), ("ctx", 1)]
        pi = 0
        for bi, (kind, j) in enumerate(blocks):
            q_, q_r = qs[bi % NS]
            k_, k_r = ks[bi % NS]
            v_, v_r = vs[bi % NS]
            tq = CTX + j * 128 if kind == "lat" else j * 128
            dma(q_.rearrange("p a b -> p (a b)"), qa_c[tq // 128], writes=[q_r])
            keys = []
            if kind == "lat":
                lo, hi = max(0, j - 1), min(nlat - 1, j + 1)
                nk = hi - lo + 1
                dma(k_[:, 0:nk * 128], kT[:, CTX + lo * 128:CTX + (hi + 1) * 128], writes=[k_r])
                for i_ in range(nk):
                    dma(v_[:, i_].rearrange("p g d -> p (g d)"), vaug_tok[CTX + (lo + i_) * 128:CTX + (lo + i_ + 1) * 128, :], writes=[v_r])
                for jj in range(lo, hi + 1):
                    m = None if jj == j else ("ge" if jj < j else "le")
                    keys.append((k_[:, (jj - lo) * 128:(jj - lo + 1) * 128], k_r, v_[:, jj - lo], v_r, m))
            for cj in range(2):
                keys.append((kc_[:, cj * 128:(cj + 1) * 128], kc_r, vc_[:, cj], vc_r, None))
            for g in range(2):
                ob, ob_r = banks[4 + g], bres[4 + g]
                plist = []
                for ki, (ka, ka_r, va, va_r, m) in enumerate(keys):
                    sbk, sbk_r = banks[pi % 4], bres[pi % 4]
                    pt, pt_r = pts[g * 5 + ki]
                    pi += 1
                    op("pe", lambda e, sbk=sbk, ka=ka, g=g, q_=q_: e.matmul(
                        sbk[:, 0:512], lhsT=ka[g * 64:(g + 1) * 64, :], rhs=q_[g * 64:(g + 1) * 64, :, :],
                        start=True, stop=True), [ka_r, q_r], [sbk_r])
                    op("act", lambda e, pt=pt, sbk=sbk: e.activation(out=pt, in_=sbk[:, 0:512], func=AF.Exp, scale=HD ** -0.5),
                       [sbk_r], [pt_r])
                    if m is not None:
                        mk, mk_r = (mgeb, mgeb_r) if m == "ge" else (mleb, mleb_r)
                        mb_ = mk.unsqueeze(1).to_broadcast([128, 4, 128])
                        ptv = pt.rearrange("p (a b) -> p a b", a=4)
                        op("dve", lambda e, ptv=ptv, mb_=mb_: e.tensor_tensor(out=ptv, in0=ptv, in1=mb_, op=ALU.mult),
                           [pt_r, mk_r], [pt_r])
                    plist.append((pt, pt_r, va, va_r))
                for hh in range(4):
                    for ki, (pt, pt_r, va, va_r) in enumerate(plist):
                        op("pe", lambda e, ob=ob, hh=hh, pt=pt, va=va, g=g, ki=ki, nk=len(plist): e.matmul(
                            ob[:, hh * 65:(hh + 1) * 65], lhsT=pt[:, hh * 128:(hh + 1) * 128], rhs=va[:, g, 0:65],
                            start=(ki == 0), stop=(ki == nk - 1)), [pt_r, va_r], [ob_r])
                obv = ob[:, 0:260].rearrange("p (a b) -> p a b", a=4)
                op("dve", lambda e, obv=obv, g=g: e.tensor_tensor(out=den, in0=obv[:, :, 64], in1=esk[:, g * 4:(g + 1) * 4], op=ALU.add),
                   [ob_r, esk_r], [den_r])
                op("dve", lambda e: e.reciprocal(out=den, in_=den), [den_r], [den_r])
                yav = ya[:, g * 256:(g + 1) * 256].rearrange("p (a b) -> p a b", a=4)
                db = den.unsqueeze(2).to_broadcast([128, 4, 64])
                op("dve", lambda e, yav=yav, obv=obv, db=db: e.tensor_tensor(out=yav, in0=obv[:, :, 0:64], in1=db, op=ALU.mult),
                   [ob_r, den_r], [ya_r])
            tb, tb_r = banks[6], bres[6]
            tbv = tb[:, 0:256].bitcast(BF16)
            for c in range(4):
                op("pe", lambda e, c=c, tbv=tbv: e.transpose(tbv[:, c * 128:(c + 1) * 128], ya[:, c * 128:(c + 1) * 128], identb),
                   [ya_r, identb_r], [tb_r])
            op("act", lambda e, tbv=tbv: e.copy(out=yat, in_=tbv.rearrange("p (a b) -> p a b", a=4)), [tb_r], [yat_r])
            dma(yaT[:, :, tq:tq + 128].rearrange("c p t -> p c t"), yat, reads=[yat_r])

    def phase_mlstm(l, with_ctx):
        gbt, gbt_r = tile((16,), F32)
        dma(gbt, ml_gb[l].partition_broadcast(128), writes=[gbt_r])
        ngt, ngt_r = tile((256,), F32)
        dma(ngt, ml_ng[l].partition_broadcast(128), writes=[ngt_r])
        ones64b, ones64b_r = tile((64,), F32)
        op("pool", lambda e: e.memset(ones64b, 1.0), [], [ones64b_r])
        nch = NT // 128
        NS = 6
        g272s = [tile((272,), F32) for _ in range(NS)]
        gts = [(g_[:, 256:272], g_r) for (g_, g_r) in g272s]
        oms = [(g_[:, 0:256], g_r) for (g_, g_r) in g272s]
        qks = [tile((4, 128), BF16) for _ in range(NS)]
        qTs = [(t_[:, 0:2, :], t_r) for (t_, t_r) in qks]
        kTs = [(t_[:, 2:4, :], t_r) for (t_, t_r) in qks]
        kts = [tile((256,), BF16) for _ in range(NS)]
        vts = [tile((4, 66), BF16) for _ in range(NS)]
        for (v_, v_r) in vts:
            op("pool", lambda e, v_=v_: e.memset(v_, 1.0), [], [v_r])
        sps = [tile((4,), F32) for _ in range(NS)]
        avs = [tile((4,), F32) for _ in range(NS)]
        rvs = [tile((4,), F32) for _ in range(NS)]
        dcs = [tile((2,), F32) for _ in range(NS)]
        vas = [tile((4, 65), BF16) for _ in range(NS)]
        dts = [tile((4, 128), BF16) for _ in range(NS)]
        hns = [tile((4, 65), F32) for _ in range(NS)]
        dns = [tile((4,), F32) for _ in range(NS)]
        hfs = [tile((256,), F32) for _ in range(NS)]
        for d in range(MLDIRS):
            if d == 1:
                barrier()
            Cst, Cst_r = tile((2, 65), F32)
            Cbf, Cbf_r = tile((2, 65), BF16)
            op("pool", lambda e, Cst=Cst: e.memset(Cst, 0.0), [], [Cst_r])
            op("pool", lambda e, Cbf=Cbf: e.memset(Cbf, 0.0), [], [Cbf_r])
            order = list(range(nch)) if d == 0 else [1, 0] + list(range(nch - 1, 1, -1))
            tri, tri_r = (mle, mle_r) if d == 0 else (mge, mge_r)
            mkb, mkb_r = (mle8, mle8_r) if d == 0 else (mge8, mge8_r)
            ic0, fc0 = (0, 4) if d == 0 else (8, 12)
            gb_res = [Res(), Res()]
            sc_res = [[Res(), Res()], [Res(), Res()]]

            def stA(si, c):
                s_ = si % NS
                t0 = c * 128
                gt, gt_r = gts[s_]
                dma(g272s[s_][0], gmom_tok[t0:t0 + 128, :], writes=[gt_r])
                q_, q_r = qTs[s_]
                k_, k_r = kTs[s_]
                dma(qks[s_][0].rearrange("p a b -> p (a b)"), qkm_c[c], writes=[q_r])
                kt, kt_r = kts[s_]
                dma(kt, km_tok[t0:t0 + 128, :], writes=[kt_r])
                vt, vt_r = vts[s_]
                dma(vt.rearrange("p a b -> p (a b)"), vmaug_tok[t0:t0 + 128, :], writes=[vt_r])
                op("dve", lambda e: e.tensor_tensor(out=gt, in0=gt, in1=gbt, op=ALU.add), [gt_r, gbt_r], [gt_r])
                sp, sp_r = sps[s_]
                op("act", lambda e: e.activation(out=sp, in_=gt[:, fc0:fc0 + 4], func=AF.Exp, scale=-1.0), [gt_r], [sp_r])
                op("act", lambda e: e.activation(out=sp, in_=sp, func=AF.Ln, bias=1.0, scale=1.0), [sp_r], [sp_r])
                for par in range(2):
                    sb_ = banks[par * 2 + si % 2]
                    for a in range(2):
                        hp = par * 64
                        op("pe", lambda e: e.matmul(
                            sb_[:, a * 128:(a + 1) * 128], lhsT=k_[hp:hp + 64, a, :], rhs=q_[hp:hp + 64, a, :],
                            start=True, stop=True), [k_r, q_r], [bres[par * 2 + si % 2]])

            def stB(si, c):
                s_ = si % NS
                sp, sp_r = sps[s_]
                gb_ = banks[si % 2]
                g0 = 256
                op("pe", lambda e: e.matmul(gb_[:, g0:g0 + 4], lhsT=tri, rhs=sp, start=True, stop=True), [sp_r, tri_r], [bres[si % 2]])
                op("pe", lambda e: e.matmul(gb_[:, g0 + 8:g0 + 12], lhsT=ones_f, rhs=sp, start=True, stop=True),
                   [sp_r, ones_r], [bres[si % 2]])
                dt, dt_r = dts[s_]
                dtv = dt.rearrange("p (a two) t -> p a two t", two=2)
                mb_ = mkb.unsqueeze(1).to_broadcast([128, 2, 128])
                for par in range(2):
                    sbv = banks[par * 2 + si % 2][:, 0:256].rearrange("p (a b) -> p a b", a=2)
                    op("dve", lambda e: e.tensor_tensor(out=dtv[:, :, par, :], in0=sbv, in1=mb_, op=ALU.mult),
                       [bres[par * 2 + si % 2], mkb_r], [dt_r])

            def stC(si, c):
                s_ = si % NS
                gt, gt_r = gts[s_]
                gb_ = banks[si % 2]
                g0 = 256
                gb_r = bres[si % 2]
                av, av_r = avs[s_]
                rv, rv_r = rvs[s_]
                dc, dc_r = dcs[s_]
                op("dve", lambda e: e.tensor_tensor(out=av, in0=gb_[:, g0:g0 + 4], in1=gt[:, ic0:ic0 + 4], op=ALU.add),
                   [gb_r, gt_r], [av_r])
                op("act", lambda e: e.activation(out=av, in_=av, func=AF.Exp), [av_r], [av_r])
                op("act", lambda e: e.activation(out=rv, in_=gb_[:, g0:g0 + 4], func=AF.Exp, scale=-1.0), [gb_r], [rv_r])
                totv = gb_[:, g0 + 8:g0 + 12].rearrange("p (a two) -> p a two", two=2)
                op("act", lambda e: e.activation(out=dc[0:64, :], in_=totv[0:64, :, 0], func=AF.Exp, scale=-1.0), [gb_r], [dc_r])
                op("act", lambda e: e.activation(out=dc[64:128, :], in_=totv[64:128, :, 1], func=AF.Exp, scale=-1.0), [gb_r], [dc_r])

            def stD(si, c):
                s_ = si % NS
                av, av_r = avs[s_]
                vt, vt_r = vts[s_]
                kt, kt_r = kts[s_]
                va, va_r = vas[s_]
                ab = av.unsqueeze(2).to_broadcast([128, 4, 65])
                op("dve", lambda e: e.tensor_tensor(out=va, in0=vt[:, :, 0:65], in1=ab, op=ALU.mult), [vt_r, av_r], [va_r])
                kvb, kvb_r = banks[4 + si % 2], bres[4 + si % 2]
                for h in range(4):
                    op("pe", lambda e: e.matmul(
                        kvb[:, h * 65:(h + 1) * 65], lhsT=kt[:, (h // 2) * 128:(h // 2 + 1) * 128], rhs=va[:, h, :], start=True, stop=True),
                       [kt_r, va_r], [kvb_r])

            def seq(si, c):
                s_ = si % NS
                t0 = c * 128
                q_, q_r = qTs[s_]
                k_, k_r = kTs[s_]
                kt, kt_r = kts[s_]
                va, va_r = vas[s_]
                dt, dt_r = dts[s_]
                rv, rv_r = rvs[s_]
                dc, dc_r = dcs[s_]
                kvb, kvb_r = banks[4 + si % 2], bres[4 + si % 2]
                hn, hn_r = hns[s_]
                hnv4 = hn.rearrange("p (a two) b -> p a two b", two=2)
                rv4 = rv.rearrange("p (a two) -> p a two", two=2)
                for par in range(2):
                    ob, ob_r = banks[6 + par], bres[6 + par]
                    hp = par * 64
                    for a in range(2):
                        h = 2 * a + par
                        op("pe", lambda e, ob=ob, a=a, h=h, dt=dt, va=va: e.matmul(
                            ob[:, a * 65:(a + 1) * 65], lhsT=dt[:, h, :], rhs=va[:, h, :], start=True, stop=False), [dt_r, va_r], [ob_r])
                        op("pe", lambda e, ob=ob, a=a, hp=hp, q_=q_, Cbf=Cbf: e.matmul(
                            ob[:, a * 65:(a + 1) * 65], lhsT=q_[hp:hp + 64, a, :], rhs=Cbf[hp:hp + 64, a, :],
                            start=False, stop=True), [q_r, Cbf_r], [ob_r])
                    obv = ob[:, 0:130].rearrange("p (a b) -> p a b", a=2)
                    rb = rv4[:, :, par].unsqueeze(2).to_broadcast([128, 2, 65])
                    op("dve", lambda e, hnv4=hnv4, obv=obv, rb=rb, par=par: e.tensor_tensor(out=hnv4[:, :, par, :], in0=obv, in1=rb, op=ALU.mult),
                       [ob_r, rv_r], [hn_r])
                kvv = kvb[:, 0:260].rearrange("p (a two b) -> p a two b", a=2, two=2)
                for hf_ in range(2):
                    ps_ = slice(hf_ * 64, hf_ * 64 + 64)
                    op("dve", lambda e, Cst=Cst, kvv=kvv, ps_=ps_, hf_=hf_: e.scalar_tensor_tensor(
                        out=Cst[ps_, :, :], in0=kvv[ps_, :, hf_, :], scalar=0.125, in1=Cst[ps_, :, :], op0=ALU.mult, op1=ALU.add),
                       [kvb_r, Cst_r], [Cst_r])
                dcb = dc.unsqueeze(2).to_broadcast([128, 2, 65])
                op("dve", lambda e, Cst=Cst, dcb=dcb: e.tensor_tensor(out=Cst, in0=Cst, in1=dcb, op=ALU.mult), [Cst_r, dc_r], [Cst_r])
                op("act", lambda e, Cst=Cst, Cbf=Cbf: e.copy(out=Cbf, in_=Cst), [Cst_r], [Cbf_r])
                dn, dn_r = dns[s_]
                op("act", lambda e, dn=dn, hn=hn: e.activation(out=dn, in_=hn[:, :, 64], func=AF.Abs), [hn_r], [dn_r])
                op("dve", lambda e, dn=dn: e.tensor_scalar_max(out=dn, in0=dn, scalar1=1.0), [dn_r], [dn_r])
                op("dve", lambda e, dn=dn: e.reciprocal(out=dn, in_=dn), [dn_r], [dn_r])
                hf, hf_r = hfs[s_]
                hfv = hf.rearrange("p (a b) -> p a b", a=4)
                dnb = dn.unsqueeze(2).to_broadcast([128, 4, 64])
                if d == 0:
                    op("dve", lambda e, hfv=hfv, hn=hn, dnb=dnb: e.tensor_tensor(out=hfv, in0=hn[:, :, 0:64], in1=dnb, op=ALU.mult),
                       [hn_r, dn_r], [hf_r])
                    dma(hF[t0:t0 + 128, :], hf, reads=[hf_r])
                    return
                if c < 2 and not with_ctx:
                    return
                dma(hf, hF[t0:t0 + 128, :], writes=[hf_r])
                om, om_r = oms[s_]
                hnv = hn[:, :, 0:64]
                op("dve", lambda e, hnv=hnv, dnb=dnb: e.tensor_tensor(out=hnv, in0=hnv, in1=dnb, op=ALU.mult), [hn_r, dn_r], [hn_r])
                op("dve", lambda e, hfv=hfv, hnv=hnv: e.tensor_tensor(out=hfv, in0=hfv, in1=hnv, op=ALU.add), [hf_r, hn_r], [hf_r])
                op("pool", lambda e, hnv=hnv, hfv=hfv: e.tensor_tensor(out=hnv, in0=hfv, in1=hfv, op=ALU.mult), [hf_r], [hn_r])
                op("dve", lambda e, dn=dn, hnv=hnv: e.tensor_reduce(out=dn, in_=hnv, axis=AX.X, op=ALU.add), [hn_r], [dn_r])
                op("act", lambda e, dn=dn: e.activation(out=dn, in_=dn, func=AF.Sqrt, bias=EPS, scale=1.0 / 64), [dn_r], [dn_r])
                op("dve", lambda e, dn=dn: e.reciprocal(out=dn, in_=dn), [dn_r], [dn_r])
                op("act", lambda e, om=om: e.activation(out=om, in_=om, func=AF.Sigmoid), [om_r], [om_r])
                op("pool", lambda e, om=om: e.tensor_tensor(out=om, in0=om, in1=ngt, op=ALU.mult), [om_r, ngt_r], [om_r])
                op("dve", lambda e, hfv=hfv, dnb=dnb: e.tensor_tensor(out=hfv, in0=hfv, in1=dnb, op=ALU.mult), [hf_r, dn_r], [hf_r])
                ymb = kt
                op("dve", lambda e, ymb=ymb, hf=hf, om=om: e.tensor_tensor(out=ymb, in0=hf, in1=om, op=ALU.mult), [hf_r, om_r], [kt_r])
                tb, tb_r = banks[6], bres[6]
                tbv = tb[:, 256:384].bitcast(BF16)
                for cc in range(2):
                    op("pe", lambda e, cc=cc, tbv=tbv, ymb=ymb: e.transpose(tbv[:, cc * 128:(cc + 1) * 128], ymb[:, cc * 128:(cc + 1) * 128], identb),
                       [kt_r, identb_r], [tb_r])
                yo = k_
                op("act", lambda e, yo=yo, tbv=tbv: e.copy(out=yo, in_=tbv.rearrange("p (a b) -> p a b", a=2)), [tb_r], [k_r])
                dma(ymT[:, :, t0:t0 + 128].rearrange("c p t -> p c t"), yo, reads=[k_r])

            stages = [stA, stB, stC, stD, seq]
            n_ = len(order)
            for it in range(n_ + len(stages) - 1):
                for k, st_ in enumerate(stages):
                    si = it - k
                    if 0 <= si < n_:
                        st_(si, order[si])

    def phase_C(l, xsrc, with_ctx):
        wgb, wgb_r = tile((8, 3072), BF16)
        wba, wba_r = tile((4, 1024), BF16)
        wbs, wbs_r = tile((2, 1024), BF16)
        wbm, wbm_r = tile((2, 1024), BF16)
        wo, wo_r = tile((8, 1024), BF16)
        stg = [tile((8, 512), F32)]
        si = 0
        def wload(dst, dst_r, src, nk, ncols):
            nonlocal si
            for c0 in range(0, ncols, 512):
                st_, st_r = stg[0]
                cast_load(dst[:, :, c0:c0 + 512], dst_r, src[:, c0:c0 + 512].rearrange("(kc p) n -> p kc n", p=128), None,
                          st_[:, 0:nk, :], st_r, eng=("pool" if si % 2 == 0 else "dve"))
                si += 1
        wload(wgb, wgb_r, w_gb[l], 8, 3072)
        wload(wba, wba_r, wb_a[l], 4, 1024)
        wload(wbs, wbs_r, wb_s[l], 2, 1024)
        wload(wbm, wbm_r, wb_m[l], 2, 1024)
        wload(wo, wo_r, w_out[l], 8, 1024)
        xt, xt_r = tile((8, 512), F32)
        sq, _ = tile((8, 512), F32)
        sq_r = [Res() for _ in range(8)]
        rs, rs_r = tile((512,), F32)
        hT, hT_r = tile((8, 512), BF16)
        yat, yat_r = tile((4, 512), BF16)
        yst, yst_r = tile((2, 512), BF16)
        ymt, ymt_r = tile((2, 512), BF16)
        sig, _ = tile((3, 512), F32)
        sig_r = [Res() for _ in range(3)]
        t1, t1_r = tile((512,), F32)
        t2, t2_r = tile((512,), F32)
        yT, yT_r = tile((8, 512), BF16)
        hTs = [(hT, hT_r), tile((8, 512), BF16)]
        xts = [(xt, xt_r), stg[0]]
        tiles_ = token_tiles(with_ctx)

        def ln_C(ti):
            t0, n = tiles_[ti]
            x_, x_r = xts[ti % 2]
            dma(x_[:, :, 0:n], xsrc[:, :, t0:t0 + n].rearrange("c p t -> p c t"), writes=[x_r])
            norm_mod(x_, x_r, n, hTs[ti % 2][0], hTs[ti % 2][1], gs1, gs1_r, 0, 1 if t0 < CTX else 0, sq, sq_r, rs, rs_r,
                     banks[0], bres[0], l)

        ln_C(0)
        for ti, (t0, n) in enumerate(tiles_):
            if ti + 1 < len(tiles_):
                ln_C(ti + 1)
            which = 1 if t0 < CTX else 0
            xt, xt_r = xts[ti % 2]
            hT, hT_r = hTs[ti % 2]
            dma(yat[:, :, 0:n], yaT[:, :, t0:t0 + n].rearrange("c p t -> p c t"), writes=[yat_r])
            dma(yst[:, :, 0:n], ysT[:, :, t0:t0 + n].rearrange("c p t -> p c t"), writes=[yst_r])
            dma(ymt[:, :, 0:n], ymT[:, :, t0:t0 + n].rearrange("c p t -> p c t"), writes=[ymt_r])
            for fc in range(8):
                for b in range(3):
                    bk, bk_r = banks[1 + b], bres[1 + b]
                    for kc in range(8):
                        op("pe", lambda e, bk=bk, kc=kc, b=b, fc=fc: e.matmul(
                            bk[:, 0:n], lhsT=wgb[:, kc, b * 1024 + fc * 128:b * 1024 + (fc + 1) * 128], rhs=hT[:, kc, 0:n],
                            start=(kc == 0), stop=(kc == 7)), [wgb_r, hT_r], [bk_r])
                    op("act", lambda e, bk=bk, b=b: e.activation(out=sig[:, b, 0:n], in_=bk[:, 0:n], func=AF.Sigmoid), [bk_r], [sig_r[b]])
                for b, (w_, w_r, y_, y_r, nk) in enumerate(((wba, wba_r, yat, yat_r, 4), (wbs, wbs_r, yst, yst_r, 2), (wbm, wbm_r, ymt, ymt_r, 2))):
                    bk, bk_r = banks[4 + b], bres[4 + b]
                    for kc in range(nk):
                        op("pe", lambda e, bk=bk, kc=kc, w_=w_, y_=y_, fc=fc, nk=nk: e.matmul(
                            bk[:, 0:n], lhsT=w_[:, kc, fc * 128:(fc + 1) * 128], rhs=y_[:, kc, 0:n],
                            start=(kc == 0), stop=(kc == nk - 1)), [w_r, y_r], [bk_r])
                op("dve", lambda e: e.tensor_tensor(out=t1[:, 0:n], in0=banks[4][:, 0:n], in1=sig[:, 0, 0:n], op=ALU.mult), [bres[4], sig_r[0]], [t1_r])
                op("dve", lambda e: e.tensor_tensor(out=t2[:, 0:n], in0=banks[5][:, 0:n], in1=sig[:, 1, 0:n], op=ALU.mult), [bres[5], sig_r[1]], [t2_r])
                op("pool", lambda e: e.tensor_tensor(out=t1[:, 0:n], in0=t1[:, 0:n], in1=t2[:, 0:n], op=ALU.add), [t1_r, t2_r], [t1_r])
                op("dve", lambda e: e.tensor_tensor(out=t2[:, 0:n], in0=banks[6][:, 0:n], in1=sig[:, 2, 0:n], op=ALU.mult), [bres[6], sig_r[2]], [t2_r])
                op("pool", lambda e, fc=fc: e.tensor_tensor(out=yT[:, fc, 0:n], in0=t1[:, 0:n], in1=t2[:, 0:n], op=ALU.add), [t1_r, t2_r], [yT_r])
            for fo in range(8):
                bk, bk_r = banks[1 + fo % 3], bres[1 + fo % 3]
                for kc in range(8):
                    op("pe", lambda e, bk=bk, kc=kc, fo=fo: e.matmul(
                        bk[:, 0:n], lhsT=wo[:, kc, fo * 128:(fo + 1) * 128], rhs=yT[:, kc, 0:n], start=(kc == 0), stop=(kc == 7)),
                       [wo_r, yT_r], [bk_r])
                op("dve", lambda e, bk=bk, fo=fo: e.scalar_tensor_tensor(
                    out=xt[:, fo, 0:n], in0=bk[:, 0:n], scalar=modv[:, l, 16 + fo, which:which + 1], in1=xt[:, fo, 0:n],
                    op0=ALU.mult, op1=ALU.add), [bk_r, modv_r, xt_r], [xt_r])
            dma(xsrc[:, :, t0:t0 + n].rearrange("c p t -> p c t"), xt[:, :, 0:n], reads=[xt_r])

    def phase_D(l, xsrc, xdst, with_ctx):
        wu, wu_r = tile((8, 2 * D_FF), BF16)
        wd, wd_r = tile((22, 1024), BF16)
        stg, stg_r = tile((8, 256), F32)
        for c0 in range(0, 2 * D_FF, 256):
            cast_load(wu[:, :, c0:c0 + 256], wu_r, w_up[l, :, c0:c0 + 256].rearrange("(kc p) n -> p kc n", p=128), None,
                      stg, stg_r, eng=("pool" if (c0 // 256) % 2 == 0 else "dve"))
        for k0 in range(0, 22, 2):
            for c0 in range(0, 1024, 1024):
                cast_load(wd[:, k0:k0 + 2, :], wd_r, w_dn[l, k0 * 128:(k0 + 2) * 128, :].rearrange("(kc p) n -> p kc n", p=128), None,
                          stg.rearrange("p a b -> p (a b)")[:, 0:2048].rearrange("p (a b) -> p a b", a=2), stg_r,
                          eng=("pool" if (k0 // 2) % 2 == 0 else "dve"))
        cwt, cwt_r = tile((3, 44), F32)
        cbt, cbt_r = tile((44,), F32)
        dma(cwt, cw[l], writes=[cwt_r])
        dma(cbt, cb[l], writes=[cbt_r])
        xt, xt_r = tile((8, 258), F32)
        sq, _ = tile((8, 258), F32)
        sq_r = [Res() for _ in range(8)]
        rs, rs_r = tile((258,), F32)
        hT, hT_r = tile((8, 258), BF16)
        gb_, gb_r = tile((22, 256), BF16)
        tas = [tile((256,), F32) for _ in range(2)]
        tvs = [tile((256,), F32) for _ in range(2)]
        sas = [tile((256,), F32) for _ in range(2)]
        tl = []
        if with_ctx:
            tl.append((0, 0, CTX))
        for t0 in range(CTX, NT, 256):
            tl.append((t0, CTX, NT))
        hTs = [(hT, hT_r), tile((8, 258), BF16)]
        xts = [(xt, xt_r), tile((8, 258), F32)]

        def ln_D(ti):
            t0, seg0, seg1 = tl[ti]
            x_, x_r = xts[ti % 2]
            lo, hi = t0 - 1, t0 + 257
            zc = []
            clo, chi = max(lo, seg0), min(hi, seg1)
            if lo < seg0:
                op("pool", lambda e: e.memset(x_[:, :, 0:1], 1.0), [], [x_r])
                zc.append(0)
            if hi > seg1:
                op("pool", lambda e: e.memset(x_[:, :, 257:258], 1.0), [], [x_r])
                zc.append(257)
            dma(x_[:, :, clo - lo:chi - lo], xsrc[:, :, clo:chi].rearrange("c p t -> p c t"), writes=[x_r])
            norm_mod(x_, x_r, 258, hTs[ti % 2][0], hTs[ti % 2][1], gs2, gs2_r, 3, 1 if t0 < CTX else 0, sq, sq_r, rs, rs_r,
                     banks[0], bres[0], l, zero_cols=zc)

        ln_D(0)
        for ti, (t0, seg0, seg1) in enumerate(tl):
            if ti + 1 < len(tl):
                ln_D(ti + 1)
            which = 1 if t0 < CTX else 0
            xt, xt_r = xts[ti % 2]
            hT, hT_r = hTs[ti % 2]
            for j in range(22):
                ta, ta_r = tas[j % 2]
                tv, tv_r = tvs[j % 2]
                sa, sa_r = sas[j % 2]
                for (ch, bk, bk_r, tt, tt_r) in ((j, banks[1 + (j % 3) * 2], bres[1 + (j % 3) * 2], ta, ta_r),
                                                 (22 + j, banks[2 + (j % 3) * 2], bres[2 + (j % 3) * 2], tv, tv_r)):
                    for kc in range(8):
                        op("pe", lambda e, bk=bk, kc=kc, ch=ch: e.matmul(
                            bk[:, 0:258], lhsT=wu[:, kc, ch * 128:(ch + 1) * 128], rhs=hT[:, kc, :], start=(kc == 0), stop=(kc == 7)),
                           [wu_r, hT_r], [bk_r])
                    op("act", lambda e, bk=bk, ch=ch, tt=tt: e.activation(out=tt, in_=bk[:, 0:256], func=AF.Identity,
                                                                          bias=cbt[:, ch:ch + 1], scale=cwt[:, 0, ch:ch + 1]),
                       [bk_r, cbt_r, cwt_r], [tt_r])
                    op("dve", lambda e, bk=bk, ch=ch, tt=tt: e.scalar_tensor_tensor(
                        out=tt, in0=bk[:, 1:257], scalar=cwt[:, 1, ch:ch + 1], in1=tt, op0=ALU.mult, op1=ALU.add), [bk_r, cwt_r, tt_r], [tt_r])
                    op("dve", lambda e, bk=bk, ch=ch, tt=tt: e.scalar_tensor_tensor(
                        out=tt, in0=bk[:, 2:258], scalar=cwt[:, 2, ch:ch + 1], in1=tt, op0=ALU.mult, op1=ALU.add), [bk_r, cwt_r, tt_r], [tt_r])
                op("act", lambda e, sa=sa, ta=ta: e.activation(out=sa, in_=ta, func=AF.Silu), [ta_r], [sa_r])
                op("pool", lambda e, j=j, sa=sa, tv=tv: e.tensor_tensor(out=gb_[:, j, :], in0=sa, in1=tv, op=ALU.mult), [sa_r, tv_r], [gb_r])
            for fo in range(8):
                bk, bk_r = banks[7 if fo % 2 == 0 else 0], bres[7 if fo % 2 == 0 else 0]
                for kc in range(22):
                    op("pe", lambda e, bk=bk, kc=kc, fo=fo: e.matmul(
                        bk[:, 0:256], lhsT=wd[:, kc, fo * 128:(fo + 1) * 128], rhs=gb_[:, kc, :], start=(kc == 0), stop=(kc == 21)),
                       [wd_r, gb_r], [bk_r])
                op("dve", lambda e, bk=bk, fo=fo: e.scalar_tensor_tensor(
                    out=xt[:, fo, 1:257], in0=bk[:, 0:256], scalar=modv[:, l, 40 + fo, which:which + 1], in1=xt[:, fo, 1:257],
                    op0=ALU.mult, op1=ALU.add), [bk_r, modv_r, xt_r], [xt_r])
            dma(xdst[:, :, t0:t0 + 256].rearrange("c p t -> p c t"), xt[:, :, 1:257], reads=[xt_r])

    def phase_E(xsrc):
        xt, xt_r = tile((8, 512), F32)
        sq, _ = tile((8, 512), F32)
        sq_r = [Res() for _ in range(8)]
        rs, rs_r = tile((512,), F32)
        ots = [tile((1024,), F32) for _ in range(2)]
        fins = []
        bi = 0
        for (t0, n) in token_tiles(False):
            dma(xt[:, :, 0:n], xsrc[:, :, t0:t0 + n].rearrange("c p t -> p c t"), writes=[xt_r])
            for c in range(8):
                if c % 2 == 0:
                    op("act", lambda e, c=c: e.activation(out=sq[:, c, 0:n], in_=xt[:, c, 0:n], func=AF.Square), [xt_r], [sq_r[c]])
                else:
                    op("pool", lambda e, c=c: e.tensor_tensor(out=sq[:, c, 0:n], in0=xt[:, c, 0:n], in1=xt[:, c, 0:n], op=ALU.mult), [xt_r], [sq_r[c]])
            for c in range(8):
                op("pe", lambda e, c=c: e.matmul(banks[0][:, 0:n], lhsT=ones_f, rhs=sq[:, c, 0:n], start=(c == 0), stop=(c == 7)),
                   [sq_r[c], ones_r], [bres[0]])
            op("act", lambda e: e.activation(out=rs[:, 0:n], in_=banks[0][:, 0:n], func=AF.Sqrt, bias=EPS, scale=1.0 / D), [bres[0]], [rs_r])
            op("dve", lambda e: e.reciprocal(out=rs[:, 0:n], in_=rs[:, 0:n]), [rs_r], [rs_r])
            for c in range(8):
                op("dve", lambda e, c=c: e.scalar_tensor_tensor(out=sq[:, c, 0:n], in0=xt[:, c, 0:n], scalar=fnt[:, c:c + 1], in1=rs[:, 0:n],
                                                               op0=ALU.mult, op1=ALU.mult), [xt_r, rs_r, fnt_r], [sq_r[c]])
            for sb in range(n // 128):
                ot, ot_r = ots[bi % 2]
                for half in range(2):
                    bk, bk_r = banks[1 + (bi * 2 + half) % 4], bres[1 + (bi * 2 + half) % 4]
                    for j in range(4):
                        c = half * 4 + j
                        op("pe", lambda e, bk=bk, j=j, c=c, sb=sb: e.transpose(bk[:, j * 128:(j + 1) * 128], sq[:, c, sb * 128:(sb + 1) * 128], ident),
                           [sq_r[c], ident_r], [bk_r])
                    if half == 0:
                        op("act", lambda e, bk=bk, ot=ot: e.copy(out=ot[:, 0:512], in_=bk[:, 0:512]), [bk_r], [ot_r])
                    else:
                        op("dve", lambda e, bk=bk, ot=ot: e.tensor_copy(out=ot[:, 512:1024], in_=bk[:, 0:512]), [bk_r], [ot_r])
                bi += 1
                tt = t0 - CTX + sb * 128
                fins.append(dma(y_out[tt:tt + 128, :], ot, reads=[ot_r]))
        return fins

    def phase_s5(l, with_ctx):
        NB, NCH, G = NT // 8, NT // 128, 16
        INV2PI = 1.0 / TWO_PI
        cols, cols_r = tile((4,), F32)
        dma(cols, s5_cols, writes=[cols_r])
        mphi, mphi_r = tile((2, 136), F32)
        dma(mphi.rearrange("p a b -> p (a b)"), s5_mphi.partition_broadcast(128), writes=[mphi_r])
        mpsi, mpsi_r = tile((2, 136), F32)
        dma(mpsi.rearrange("p a b -> p (a b)"), s5_mpsi.partition_broadcast(128), writes=[mpsi_r])
        mphs, mphs_r = tile((2, 16), F32)
        dma(mphs.rearrange("p a b -> p (a b)"), s5_mphs.partition_broadcast(128), writes=[mphs_r])
        at, at_r = tile((2, 16, 2), F32)
        dma(at.rearrange("p a b c -> p (a b c)"), s5_a[l], writes=[at_r])
        ldt, ldt_r = tile((2, 16), F32)
        dma(ldt.rearrange("p a b -> p (a b)"), s5_logdt[l].partition_broadcast(128), writes=[ldt_r])
        bt, bt_r = tile((16, 16, 2), F32)
        dma(bt.rearrange("p a b c -> p (a b c)"), s5_b[l], writes=[bt_r])
        ct, ct_r = tile((16, 16, 2), F32)
        dma(ct.rearrange("p a b c -> p (a b c)"), s5_c[l], writes=[ct_r])
        dcol, dcol_r = tile((16,), F32)
        dma(dcol, s5_dcol[l], writes=[dcol_r])
        m0, m0_r = tile((2, 128), F32)
        dma(m0, s5_m0.rearrange("d k m -> k d m"), writes=[m0_r])
        swp, swp_r = tile((128,), F32)
        dma(swp, s5_swap, writes=[swp_r])
        gst, gst_r = tile((2, 256), F32)
        gw, gw_r = tile((2, 256), BF16)
        cast_load(gw, gw_r, s5_glu_w[l].rearrange("(kc p) n -> p kc n", p=128), None, gst, gst_r, eng="dve")
        glb, glb_r = tile((2,), F32)
        dma(glb, s5_glu_b[l], writes=[glb_r])
        sel, sel_r = tile((64, 128), BF16)
        U, _ = tile((G, NB), BF16)
        NBG = (NCH + 31) // 32
        bgs = [(b * 32, min(32, NCH - b * 32)) for b in range(NBG)]
        U_r = [[Res() for _ in bgs] for _ in range(G)]

        small = lambda: tile((2, 16), F32)
        dts, dts_r = small()
        op("act", lambda e: e.activation(out=dts, in_=ldt, func=AF.Exp), [ldt_r], [dts_r])
        dre, dre_r = small()
        dim, dim_r = small()
        op("dve", lambda e: e.tensor_tensor(out=dre, in0=dts, in1=at[:, :, :, 0], op=ALU.mult), [dts_r, at_r], [dre_r])
        op("dve", lambda e: e.tensor_tensor(out=dim, in0=dts, in1=at[:, :, :, 1], op=ALU.mult), [dts_r, at_r], [dim_r])
        tI, tI_r = tile((2, 16), I32)
        tF, tF_r = small()

        def sin_red(dst, dst_r, ang, ang_r, tI, tI_r, tF, tF_r):
            op("dve", lambda e: e.tensor_scalar(out=tI, in0=ang, scalar1=INV2PI, scalar2=None, op0=ALU.mult), [ang_r], [tI_r])
            op("dve", lambda e: e.tensor_copy(out=tF, in_=tI), [tI_r], [tF_r])
            op("dve", lambda e: e.scalar_tensor_tensor(out=tF, in0=tF, scalar=-TWO_PI, in1=ang, op0=ALU.mult, op1=ALU.add),
               [tF_r, ang_r], [tF_r])
            op("act", lambda e: e.activation(out=dst, in_=tF, func=AF.Sin), [tF_r], [dst_r])

        def lam_pow(kpow):
            a1, a1_r = small()
            a2, a2_r = small()
            mg, mg_r = small()
            sr, sr_r = small()
            ci_, ci_r = small()
            op("dve", lambda e: e.tensor_scalar(out=a1, in0=dim, scalar1=float(kpow), scalar2=None, op0=ALU.mult), [dim_r], [a1_r])
            op("dve", lambda e: e.tensor_scalar(out=a2, in0=dim, scalar1=float(kpow), scalar2=math.pi / 2, op0=ALU.mult, op1=ALU.add),
               [dim_r], [a2_r])
            sin_red(sr, sr_r, a1, a1_r, tI, tI_r, tF, tF_r)
            sin_red(ci_, ci_r, a2, a2_r, tI, tI_r, tF, tF_r)
            op("act", lambda e: e.activation(out=mg, in_=dre, func=AF.Exp, scale=float(kpow)), [dre_r], [mg_r])
            op("dve", lambda e: e.tensor_tensor(out=sr, in0=sr, in1=mg, op=ALU.mult), [sr_r, mg_r], [sr_r])
            op("dve", lambda e: e.tensor_tensor(out=ci_, in0=ci_, in1=mg, op=ALU.mult), [ci_r, mg_r], [ci_r])
            return (ci_, ci_r), (sr, sr_r)

        (lr, lr_r), (li, li_r) = lam_pow(1)
        (l128r, l128r_r), (l128i, l128i_r) = lam_pow(128)
        ar, ai = at[:, :, :, 0], at[:, :, :, 1]
        den, den_r = small()
        t1, t1_r = small()
        t2, t2_r = small()
        cr, cr_r = small()
        ci, ci_r = small()
        op("dve", lambda e: e.tensor_tensor(out=den, in0=ar, in1=ar, op=ALU.mult), [at_r], [den_r])
        op("dve", lambda e: e.tensor_tensor(out=t1, in0=ai, in1=ai, op=ALU.mult), [at_r], [t1_r])
        op("dve", lambda e: e.tensor_tensor(out=den, in0=den, in1=t1, op=ALU.add), [den_r, t1_r], [den_r])
        op("dve", lambda e: e.reciprocal(out=den, in_=den), [den_r], [den_r])
        op("dve", lambda e: e.tensor_scalar_add(out=lr, in0=lr, scalar1=-1.0), [lr_r], [lr_r])
        op("dve", lambda e: e.tensor_tensor(out=t1, in0=lr, in1=ar, op=ALU.mult), [lr_r, at_r], [t1_r])
        op("dve", lambda e: e.tensor_tensor(out=t2, in0=li, in1=ai, op=ALU.mult), [li_r, at_r], [t2_r])
        op("dve", lambda e: e.tensor_tensor(out=t1, in0=t1, in1=t2, op=ALU.add), [t1_r, t2_r], [t1_r])
        op("dve", lambda e: e.tensor_tensor(out=cr, in0=t1, in1=den, op=ALU.mult), [t1_r, den_r], [cr_r])
        op("dve", lambda e: e.tensor_tensor(out=t1, in0=li, in1=ar, op=ALU.mult), [li_r, at_r], [t1_r])
        op("dve", lambda e: e.tensor_tensor(out=t2, in0=lr, in1=ai, op=ALU.mult), [lr_r, at_r], [t2_r])
        op("dve", lambda e: e.tensor_tensor(out=t1, in0=t1, in1=t2, op=ALU.subtract), [t1_r, t2_r], [t1_r])
        op("dve", lambda e: e.tensor_tensor(out=ci, in0=t1, in1=den, op=ALU.mult), [t1_r, den_r], [ci_r])
        br, bi = bt[:, :, :, 0], bt[:, :, :, 1]
        BA, BA_r = tile((2, 16, 16), F32)
        BB, BB_r = tile((2, 16, 16), F32)
        w1, w1_r = tile((16, 16), F32)
        w2, w2_r = tile((16, 16), F32)
        for d in range(2):
            crb = cr[:, d, :].unsqueeze(2).to_broadcast([128, 16, 16])
            cib = ci[:, d, :].unsqueeze(2).to_broadcast([128, 16, 16])
            op("dve", lambda e: e.tensor_tensor(out=w1, in0=br, in1=crb, op=ALU.mult), [bt_r, cr_r], [w1_r])
            op("dve", lambda e: e.tensor_tensor(out=w2, in0=bi, in1=cib, op=ALU.mult), [bt_r, ci_r], [w2_r])
            op("dve", lambda e: e.tensor_tensor(out=BA[:, d], in0=w1, in1=w2, op=ALU.subtract), [w1_r, w2_r], [BA_r])
            op("dve", lambda e: e.tensor_tensor(out=w1, in0=bi, in1=crb, op=ALU.mult), [bt_r, cr_r], [w1_r])
            op("dve", lambda e: e.tensor_tensor(out=w2, in0=br, in1=cib, op=ALU.mult), [bt_r, ci_r], [w2_r])
            op("dve", lambda e: e.tensor_tensor(out=w1, in0=w1, in1=w2, op=ALU.add), [w1_r, w2_r], [w1_r])
            op("dve", lambda e: e.tensor_scalar(out=BB[:, d], in0=w1, scalar1=cols[:, 2:3], scalar2=None, op0=ALU.mult), [w1_r, cols_r], [BB_r])
        CA, CA_r = tile((16, 16), F32)
        CB, CB_r = tile((16, 16), F32)
        op("dve", lambda e: e.tensor_scalar(out=CA, in0=ct[:, :, :, 0], scalar1=cols[:, 3:4], scalar2=None, op0=ALU.mult), [ct_r, cols_r], [CA_r])
        op("dve", lambda e: e.tensor_scalar(out=CB, in0=ct[:, :, :, 1], scalar1=-1.0, scalar2=None, op0=ALU.mult), [ct_r], [CB_r])
        ais, ais_r = small()
        op("dve", lambda e: e.tensor_scalar(out=ais, in0=l128i, scalar1=cols[:, 3:4], scalar2=None, op0=ALU.mult), [l128i_r, cols_r], [ais_r])

        HG = 8
        pa, pa_r = tile((HG, 136), F32)
        pI, pI_r = tile((HG, 136), I32)
        pF, pF_r = tile((HG, 136), F32)
        pm, pm_r = tile((HG, 136), F32)

        def pw_tables(d, g0, mt, mt_r, n, outA, outA_r, outB, outB_r):
            mtb = mt.unsqueeze(1).to_broadcast([128, HG, n])
            dimb = dim[:, d, g0:g0 + HG].unsqueeze(2).to_broadcast([128, HG, n])
            dreb = dre[:, d, g0:g0 + HG].unsqueeze(2).to_broadcast([128, HG, n])
            pa_, pI_, pF_, pm_ = pa[:, :, 0:n], pI[:, :, 0:n], pF[:, :, 0:n], pm[:, :, 0:n]
            op("dve", lambda e: e.tensor_tensor(out=pm_, in0=mtb, in1=dreb, op=ALU.mult), [mt_r, dre_r], [pm_r])
            op("act", lambda e: e.activation(out=pm_, in_=pm_, func=AF.Exp), [pm_r], [pm_r])
            for (o_, o_r, phc) in ((outA, outA_r, 0), (outB, outB_r, 1)):
                op("dve", lambda e: e.tensor_tensor(out=pa_, in0=mtb, in1=dimb, op=ALU.mult), [mt_r, dim_r], [pa_r])
                op("dve", lambda e: e.tensor_scalar(out=pa_, in0=pa_, scalar1=cols[:, phc:phc + 1], scalar2=None, op0=ALU.add), [pa_r, cols_r], [pa_r])
                sin_red(o_, o_r, pa_, pa_r, pI_, pI_r, pF_, pF_r)
                op("pool", lambda e: e.tensor_tensor(out=o_, in0=o_, in1=pm_, op=ALU.mult), [o_r, pm_r], [o_r])

        oa, oa_r = tile((136, 16), F32)
        aw, aw_r = oa.rearrange("p a b -> p (a b)")[:, 0:G * 128].rearrange("p (a b) -> p a b", a=G), oa_r
        ob_, ob_r = tile((136, 16), F32)

        def outer(dst, dst_r, PA, PA_r, PB, PB_r, gi, XA, XA_r, XB, XB_r, n):
            pab = PA[:, gi, :].unsqueeze(2).to_broadcast([128, n, 16])
            pbb = PB[:, gi, :].unsqueeze(2).to_broadcast([128, n, 16])
            xab = XA.unsqueeze(1).to_broadcast([128, n, 16])
            xbb = XB.unsqueeze(1).to_broadcast([128, n, 16])
            op("dve", lambda e: e.tensor_tensor(out=oa[:, 0:n, :], in0=pab, in1=xab, op=ALU.mult), [PA_r, XA_r], [oa_r])
            op("pool", lambda e: e.tensor_tensor(out=ob_[:, 0:n, :], in0=pbb, in1=xbb, op=ALU.mult), [PB_r, XB_r], [ob_r])
            op("dve", lambda e: e.tensor_tensor(out=dst, in0=oa[:, 0:n, :], in1=ob_[:, 0:n, :], op=ALU.add), [oa_r, ob_r], [dst_r])

        dma(sel, s5_sel, writes=[sel_r])
        ust, ust_r = tile((2, 4096), BF16)
        cgs = [(c0, min(512, NB - c0)) for c0 in range(0, NB, 512)]
        bg_of_col = lambda c0: c0 // 512
        for (c0, nb) in cgs:
            dma(ust[:, :, 0:nb * 8], usT[:, :, c0 * 8:(c0 + nb) * 8].rearrange("c p t -> p c t"), writes=[ust_r])
            for g in range(G):
                hf, gl = g // 8, g % 8
                bk, bk_r = banks[g % 2], bres[g % 2]
                uv = ust[:, hf, 0:nb * 8].rearrange("p (tb s) -> p tb s", s=8)
                for s8 in range(8):
                    op("pe", lambda e: e.matmul(bk[:, 0:nb], lhsT=sel[:, gl * 8 + s8, :], rhs=uv[:, :, s8], start=(s8 == 0), stop=(s8 == 7)),
                       [sel_r, ust_r], [bk_r])
                if g % 2 == 0:
                    op("act", lambda e: e.copy(out=U[:, g, c0:c0 + nb], in_=bk[:, 0:nb]), [bk_r], [U_r[g][bg_of_col(c0)]])
                else:
                    op("dve", lambda e: e.tensor_copy(out=U[:, g, c0:c0 + nb], in_=bk[:, 0:nb]), [bk_r], [U_r[g][bg_of_col(c0)]])
        dma(sel, s5_selT, writes=[sel_r])

        Eall, Eall_r = tile((2, G, NCH), F32)
        Zst, Zst_r = tile((G, NCH), F32)
        Zin, Zin_r = tile((2, G, NCH), BF16)
        op("pool", lambda e: e.memset(Zin, 0.0), [], [Zin_r])
        A128, A128_r = tile((G, 128), F32)
        PhA, PhA_r = tile((HG, 136), F32)
        PhB, PhB_r = tile((HG, 136), F32)
        Phi, Phi_r = tile((136, 16), BF16)
        Wt, Wt_r = tile((16, 128), BF16)
        for d in range(2):
            for g0 in range(0, G, HG):
                pw_tables(d, g0, mphi[:, d, :], mphi_r, 136, PhA, PhA_r, PhB, PhB_r)
                for gi in range(HG):
                    g = g0 + gi
                    outer(Phi, Phi_r, PhA, PhA_r, PhB, PhB_r, gi, BA[:, d, g], BA_r, BB[:, d, g], BB_r, 136)
                    woff = 0 if d == 0 else 8
                    for hb in range(2):
                        bk, bk_r = banks[1 + hb], bres[1 + hb]
                        bkv = bk[:, 0:512].bitcast(BF16)
                        for jj in range(8):
                            j = hb * 8 + jj
                            src = Phi[:, woff + 8 * j:woff + 8 * j + 8, :].rearrange("p a b -> p (a b)")
                            op("pe", lambda e: e.transpose(bkv[:, jj * 128:(jj + 1) * 128], src, identb), [Phi_r, identb_r], [bk_r])
                        dstw = Wt[:, hb * 8:(hb + 1) * 8, :]
                        srcw = bkv.rearrange("p (a b) -> p a b", a=8)
                        if hb == 0:
                            op("act", lambda e: e.copy(out=dstw, in_=srcw), [bk_r], [Wt_r])
                        else:
                            op("dve", lambda e: e.tensor_copy(out=dstw, in_=srcw), [bk_r], [Wt_r])
                    eb, eb_r = banks[3], bres[3]
                    ucv = U[:, g, :].rearrange("p (ch j) -> p ch j", j=16)
                    for j in range(16):
                        op("pe", lambda e: e.matmul(eb[:, 0:NCH], lhsT=Wt[:, j, :], rhs=ucv[:, :, j], start=(j == 0), stop=(j == 15)),
                           [Wt_r] + U_r[g], [eb_r])
                    op("act", lambda e: e.copy(out=Eall[:, d, g, :], in_=eb[:, 0:NCH]), [eb_r], [Eall_r])
            idb = ident.unsqueeze(1).to_broadcast([128, G, 128])
            swb = swp.unsqueeze(1).to_broadcast([128, G, 128])
            arb = l128r[:, d, :].unsqueeze(2).to_broadcast([128, G, 128])
            aib = ais[:, d, :].unsqueeze(2).to_broadcast([128, G, 128])
            op("dve", lambda e: e.tensor_tensor(out=A128, in0=idb, in1=arb, op=ALU.mult), [ident_r, l128r_r], [A128_r])
            op("pool", lambda e: e.tensor_tensor(out=aw, in0=swb, in1=aib, op=ALU.mult), [swp_r, ais_r], [aw_r])
            op("dve", lambda e: e.tensor_tensor(out=A128, in0=A128, in1=aw, op=ALU.add), [A128_r, aw_r], [A128_r])
            order = list(range(NCH)) if d == 0 else [1, 0] + list(range(NCH - 1, 1, -1))
            prev = None
            zb, zb_r = banks[4], bres[4]
            for ch in order:
                if prev is None:
                    op("dve", lambda e: e.tensor_copy(out=Zst[:, :, ch], in_=Eall[:, d, :, ch]), [Eall_r], [Zst_r])
                else:
                    for g in range(G):
                        op("pe", lambda e: e.matmul(zb[:, g:g + 1], lhsT=A128[:, g, :], rhs=Zst[:, g, prev:prev + 1], start=True, stop=True),
                           [A128_r, Zst_r], [zb_r])
                    op("dve", lambda e: e.tensor_tensor(out=Zst[:, :, ch], in0=zb[:, 0:G], in1=Eall[:, d, :, ch], op=ALU.add),
                       [zb_r, Eall_r], [Zst_r])
                    op("act", lambda e: e.copy(out=Zin[:, d, :, ch], in_=Zst[:, :, prev]), [Zst_r], [Zin_r])
                prev = ch

        PsA = [(PhA, PhA_r), tile((HG, 136), F32)]
        PsB = [(PhB, PhB_r), tile((HG, 136), F32)]
        PsmA = [tile((HG, 16), F32) for _ in range(2)]
        PsmB = [tile((HG, 16), F32) for _ in range(2)]
        Rt = [(Phi, Phi_r), tile((136, 16), BF16)]
        Lt = [tile((16, 16), BF16) for _ in range(2)]
        Tt = [(Wt, Wt_r), tile((16, 128), BF16)]
        yv, yv_r = tile((512,), F32)
        y2, y2_r = tile((512,), F32)
        y3, y3_r = tile((512,), F32)
        for g0 in range(0, G, HG):
            for d in range(2):
                pw_tables(d, g0, mpsi[:, d, :], mpsi_r, 136, PsA[d][0], PsA[d][1], PsB[d][0], PsB[d][1])
                pw_tables(d, g0, mphs[:, d, :], mphs_r, 16, PsmA[d][0], PsmA[d][1], PsmB[d][0], PsmB[d][1])
            for gi in range(HG):
                g = g0 + gi
                for d in range(2):
                    R, R_r = Rt[d]
                    L, L_r = Lt[d]
                    Tt_, Tt_r = Tt[d]
                    outer(R, R_r, PsA[d][0], PsA[d][1], PsB[d][0], PsB[d][1], gi, CA[:, g], CA_r, CB[:, g], CB_r, 136)
                    outer(L, L_r, PsmA[d][0], PsmA[d][1], PsmB[d][0], PsmB[d][1], gi, BA[:, d, g], BA_r, BB[:, d, g], BB_r, 16)
                    for q4 in range(4):
                        tb_, tb_r = banks[4], bres[4]
                        for dd in range(4):
                            Dl = q4 * 4 + dd
                            lh = L[:, 0:8, :] if Dl == 0 else L[:, 8:16, :]
                            if d == 0:
                                r0 = 0 if Dl == 0 else 8 * (Dl - 1) + 1
                            else:
                                r0 = 0 if Dl == 0 else 8 * (Dl - 1)
                            op("pe", lambda e: e.matmul(tb_[:, dd * 128:(dd + 1) * 128], lhsT=lh.rearrange("p a b -> p (a b)"),
                                                        rhs=R[:, r0:r0 + 8, :].rearrange("p a b -> p (a b)"), start=True, stop=True),
                               [L_r, R_r], [tb_r])
                        if q4 == 0:
                            op("dve", lambda e: e.tensor_tensor(out=y3[:, 0:128], in0=tb_[:, 0:128], in1=m0[:, d, :], op=ALU.mult), [tb_r, m0_r], [y3_r])
                            if d == 0:
                                op("dve", lambda e: e.scalar_tensor_tensor(out=Tt_[:, 0, :], in0=ident, scalar=dcol[:, g:g + 1], in1=y3[:, 0:128],
                                                                          op0=ALU.mult, op1=ALU.add), [y3_r, ident_r, dcol_r], [Tt_r])
                            else:
                                op("dve", lambda e: e.tensor_copy(out=Tt_[:, 0, :], in_=y3[:, 0:128]), [y3_r], [Tt_r])
                            op("act", lambda e: e.copy(out=Tt_[:, 1:4, :], in_=tb_[:, 128:512].rearrange("p (a b) -> p a b", a=3)), [tb_r], [Tt_r])
                        else:
                            op("act", lambda e: e.copy(out=Tt_[:, q4 * 4:q4 * 4 + 4, :], in_=tb_[:, 0:512].rearrange("p (a b) -> p a b", a=4)), [tb_r], [Tt_r])
                for bgi, (ch0, nch) in enumerate(bgs):
                    yb, yb_r = banks[5 + (g * NBG + bgi) % 2], bres[5 + (g * NBG + bgi) % 2]
                    ncol = nch * 16
                    ybv = yb[:, 0:ncol].rearrange("p (ch j) -> p ch j", j=16)
                    ucv = U[:, g, ch0 * 16:ch0 * 16 + ncol].rearrange("p (ch j) -> p ch j", j=16)
                    first = True
                    for d in range(2):
                        R, R_r = Rt[d]
                        Tt_, Tt_r = Tt[d]
                        for Dl in range(16):
                            if d == 0:
                                o_, r_ = ybv[:, :, Dl:16], ucv[:, :, 0:16 - Dl]
                            else:
                                o_, r_ = ybv[:, :, 0:16 - Dl], ucv[:, :, Dl:16]
                            op("pe", lambda e: e.matmul(o_, lhsT=Tt_[:, Dl, :], rhs=r_, start=first, stop=False), [Tt_r, U_r[g][bgi]], [yb_r])
                            first = False
                        for j in range(16):
                            r0 = 8 * j + 1 if d == 0 else 8 * (15 - j)
                            last = (d == 1 and j == 15)
                            op("pe", lambda e: e.matmul(ybv[:, :, j], lhsT=R[:, r0:r0 + 8, :].rearrange("p a b -> p (a b)"),
                                                        rhs=Zin[:, d, g, ch0:ch0 + nch], start=False, stop=last), [R_r, Zin_r], [yb_r])
                    n_ = ncol
                    op("act", lambda e: e.copy(out=yv[:, 0:n_], in_=yb[:, 0:n_]), [yb_r], [yv_r])
                    op("pool", lambda e: e.tensor_tensor(out=y2[:, 0:n_], in0=yv[:, 0:n_], in1=yv[:, 0:n_], op=ALU.mult), [yv_r], [y2_r])
                    op("dve", lambda e: e.tensor_scalar(out=y2[:, 0:n_], in0=y2[:, 0:n_], scalar1=0.044715, scalar2=1.0, op0=ALU.mult, op1=ALU.add),
                       [y2_r], [y2_r])
                    op("pool", lambda e: e.tensor_tensor(out=y2[:, 0:n_], in0=y2[:, 0:n_], in1=yv[:, 0:n_], op=ALU.mult), [y2_r, yv_r], [y2_r])
                    op("act", lambda e: e.activation(out=y2[:, 0:n_], in_=y2[:, 0:n_], func=AF.Sigmoid, scale=1.5957691216057308), [y2_r], [y2_r])
                    op("dve", lambda e: e.tensor_tensor(out=U[:, g, ch0 * 16:ch0 * 16 + n_], in0=yv[:, 0:n_], in1=y2[:, 0:n_], op=ALU.mult),
                       [yv_r, y2_r], [U_r[g][bgi]])

        yfm, yfm_r = ust, ust_r
        yo, yo_r = tile((2, 512), BF16)
        sg, sg_r = tile((512,), F32)
        for (c0, nb) in cgs:
            bgi = bg_of_col(c0)
            for hf in range(2):
                yfv = yfm[:, hf, 0:nb * 8].rearrange("p (tb s) -> p tb s", s=8)
                for t8 in range(8):
                    bk, bk_r = banks[t8 % 2], bres[t8 % 2]
                    for gl in range(8):
                        g = hf * 8 + gl
                        op("pe", lambda e: e.matmul(bk[:, 0:nb], lhsT=sel[:, gl * 8 + t8, :], rhs=U[:, g, c0:c0 + nb], start=(gl == 0), stop=(gl == 7)),
                           [sel_r, U_r[g][bgi]], [bk_r])
                    if t8 % 2 == 0:
                        op("act", lambda e: e.copy(out=yfv[:, :, t8], in_=bk[:, 0:nb]), [bk_r], [yfm_r])
                    else:
                        op("dve", lambda e: e.tensor_copy(out=yfv[:, :, t8], in_=bk[:, 0:nb]), [bk_r], [yfm_r])
            ntok = nb * 8
            for tt in range(0, ntok, 512):
                n_ = min(512, ntok - tt)
                tglob = c0 * 8 + tt
                for fc in range(2):
                    bk, bk_r = banks[7], bres[7]
                    for kc in range(2):
                        op("pe", lambda e: e.matmul(bk[:, 0:n_], lhsT=gw[:, kc, fc * 128:(fc + 1) * 128], rhs=yfm[:, kc, tt:tt + n_],
                                                    start=(kc == 0), stop=(kc == 1)), [gw_r, yfm_r], [bk_r])
                    op("act", lambda e: e.activation(out=sg[:, 0:n_], in_=bk[:, 0:n_], func=AF.Sigmoid, bias=glb[:, fc:fc + 1], scale=1.0),
                       [bk_r, glb_r], [sg_r])
                    op("dve", lambda e: e.tensor_tensor(out=yo[:, fc, 0:n_], in0=yfm[:, fc, tt:tt + n_], in1=sg[:, 0:n_], op=ALU.mult),
                       [yfm_r, sg_r], [yo_r])
                dma(ysT[:, :, tglob:tglob + n_].rearrange("c p t -> p c t"), yo[:, :, 0:n_], reads=[yo_r])

    def phase_s5_stub(l, with_ctx):
        z, z_r = tile((2, 512), BF16)
        op("pool", lambda e: e.memset(z, 0.0), [], [z_r])
        for (t0, n) in token_tiles(True):
            dma(ysT[:, :, t0:t0 + n].rearrange("c p t -> p c t"), z[:, :, 0:n], reads=[z_r])

    fins = []
    steps = [("mod", lambda: phase_mod()), ("tin", lambda: phase_transpose_in())]
    cur = 0
    for l in range(depth):
        with_ctx = l < depth - 1
        steps.append(("A%d" % l, lambda l=l, cur=cur: phase_A(l, xT[cur])))
        steps.append(("attn%d" % l, lambda l=l, w=with_ctx: phase_attn(l, w)))
        steps.append(("ml%d" % l, lambda l=l, w=with_ctx: phase_mlstm(l, w)))
        steps.append(("s5%d" % l, lambda l=l, w=with_ctx: (phase_s5 if HAVE_S5 else phase_s5_stub)(l, w)))
        steps.append(("C%d" % l, lambda l=l, cur=cur, w=with_ctx: phase_C(l, xT[cur], w)))
        steps.append(("D%d" % l, lambda l=l, cur=cur, w=with_ctx: phase_D(l, xT[cur], xT[1 - cur], w)))
        cur = 1 - cur
    steps.append(("E", lambda cur=cur: fins.extend(phase_E(xT[cur]))))
    for i, (nm, fn) in enumerate(steps):
        if upto is not None and nm == upto:
            break
        if i > 0:
            new_phase()
        fn()
    stats = S.emit(final_ops=fins)
    es.close()
    return nc, dbg_outs, stats


HAVE_S5 = True
import os
MLCUT = int(os.environ.get('MLCUT', '0'))
MLDIRS = int(os.environ.get('MLDIRS', '2'))


def make_in_maps(inp, T, depth=2, ncores=8):
    fm, tm, gb = _col_layout()
    cst = _consts(T)
    f = lambda a: np.ascontiguousarray(a, dtype=np.float32)
    shared = {}
    shared["mod_w"] = f(inp["mod_w"])
    shared["mod_b"] = f(inp["mod_b"].reshape(depth, 48, 128).transpose(0, 2, 1))
    shared["norm1_g"] = f(inp["norm1_g"].reshape(depth, 8, 128).transpose(0, 2, 1))
    shared["norm2_g"] = f(inp["norm2_g"].reshape(depth, 8, 128).transpose(0, 2, 1))
    shared["final_norm_g"] = f(inp["final_norm_g"].reshape(8, 128).T)
    shared["w_fm"] = f(inp["w_in"][:, :, fm])
    shared["w_tm"] = f(inp["w_in"][:, :, tm])
    shared["w_gb"] = f(inp["w_in"][:, :, gb])
    for k in ("w_branch_attn", "w_branch_s5", "w_branch_ml", "w_out", "ffn_w_up", "ffn_w_down"):
        shared[k] = f(inp[k])
    shared["ffn_conv_w"] = f(inp["ffn_conv_w"].reshape(depth, 3, 44, 128).transpose(0, 3, 1, 2))
    shared["ffn_conv_b"] = f(inp["ffn_conv_b"].reshape(depth, 44, 128).transpose(0, 2, 1))
    shared["attn_sink"] = f(inp["attn_sink"].reshape(depth, 1, 8))
    gbias = np.stack([np.concatenate([inp["ml_igate_b"][l, 0], inp["ml_fgate_b"][l, 0], inp["ml_igate_b"][l, 1], inp["ml_fgate_b"][l, 1]])
                      for l in range(depth)])
    shared["ml_gate_b"] = f(gbias.reshape(depth, 1, 16))
    shared["ml_norm_g"] = f(inp["ml_norm_g"].reshape(depth, 1, 256))
    a_st = np.stack([inp["s5_a_re"], inp["s5_a_im"]], axis=-1)
    a_st = a_st.transpose(0, 3, 1, 2, 4)
    shared["s5_a"] = f(np.concatenate([a_st, a_st], axis=1).reshape(depth, 128, 64))
    shared["s5_logdt"] = f(inp["s5_log_dt"].reshape(depth, 1, 32))
    b_st = np.stack([inp["s5_b_re"], inp["s5_b_im"]], axis=-1).transpose(0, 2, 1, 3, 4)
    shared["s5_b"] = f(np.concatenate([b_st, b_st], axis=1).reshape(depth, 128, 512))
    c_st = np.stack([inp["s5_c_re"], inp["s5_c_im"]], axis=-1).transpose(0, 3, 1, 2, 4)
    shared["s5_c"] = f(np.concatenate([c_st, c_st], axis=1).reshape(depth, 128, 512))
    dd = inp["s5_d"].reshape(depth, 16, 16).transpose(0, 2, 1)
    shared["s5_dcol"] = f(np.tile(dd, (1, 8, 1)))
    shared["s5_glu_w"] = f(inp["s5_glu_w"])
    shared["s5_glu_b"] = f(inp["s5_glu_b"].reshape(depth, 2, 128).transpose(0, 2, 1))
    shared.update(cst)
    maps = []
    for b in range(ncores):
        m = dict(shared)
        m["x"] = f(inp["x"][b, :T])
        m["ctx"] = f(inp["ctx"][b])
        cv = np.stack([inp["c"][b], inp["c_ctx"]], axis=-1)
        m["cvec"] = f(cv.reshape(8, 128, 2).transpose(1, 0, 2))
        maps.append(m)
    return maps


_CACHE = {}


def kernel(**inputs):
    inp = {k: np.asarray(v) for k, v in inputs.items()}
    B, T = inp["x"].shape[0], inp["x"].shape[1]
    if T not in _CACHE:
        _CACHE[T] = build(T)
    nc = _CACHE[T][0]
    maps = make_in_maps(inp, T, ncores=B)
    res = run_bass_kernel_spmd(nc, maps, core_ids=list(range(B)))
    return np.stack([r["y"] for r in res.results], axis=0).astype(np.float32)
```
